# Optimizing a Trainium2 kernel written in Bass

```python
import math
import jax, jax.numpy as jnp
from jax import lax
import numpy as np

D_MODEL = 1024
BATCH = 8
SEQ = 4096
DEPTH = 2

S5_GROUP = 16
S5_WIDTH = D_MODEL // 4
S5_GROUPS = S5_WIDTH // S5_GROUP
S5_STATE = 64
S5_DT_MIN = 1e-3
S5_DT_MAX = 1e-1
LRU_WIDTH = D_MODEL // 2
LRU_BLOCK = 64
LRU_BLOCKS = LRU_WIDTH // LRU_BLOCK
LRU_CONV = 4
LRU_C = 8.0
GLA_HEADS = 4
GLA_DK = 64
GLA_DV = 64
GLA_QK = GLA_HEADS * GLA_DK
GLA_WIDTH = GLA_HEADS * GLA_DV
GLA_RANK = 16
GLA_TAU = 16.0
GLA_CHUNK = 64
N_BRANCH = 3
N_EXPERTS = 16
EXPERT_FF = 2 * D_MODEL
CAPACITY_FACTOR = 2
EPS = 1e-6

OFF_S5 = 0
OFF_LRU = OFF_S5 + S5_WIDTH
OFF_Q = OFF_LRU + LRU_WIDTH
OFF_K = OFF_Q + GLA_QK
OFF_V = OFF_K + GLA_QK
OFF_OG = OFF_V + GLA_WIDTH
OFF_ALR = OFF_OG + GLA_WIDTH
OFF_GATE = OFF_ALR + 2 * GLA_RANK
IN_WIDTH = OFF_GATE + N_BRANCH * D_MODEL

kernel_name = "hybrid_bidir_s5_rglru_gla_ec"


def rms_norm(x, g):
    x32 = x.astype(jnp.float32)
    y = x32 * lax.rsqrt(jnp.mean(x32 * x32, axis=-1, keepdims=True) + EPS)
    return (y * g.astype(jnp.float32)).astype(x.dtype)


def _lin_combine(e_i, e_j):
    a_i, b_i = e_i
    a_j, b_j = e_j
    return a_j * a_i, a_j * b_i + b_j


def _cplx_combine(e_i, e_j):
    ar_i, ai_i, br_i, bi_i = e_i
    ar_j, ai_j, br_j, bi_j = e_j
    return (ar_j * ar_i - ai_j * ai_i,
            ar_j * ai_i + ai_j * ar_i,
            ar_j * br_i - ai_j * bi_i + br_j,
            ar_j * bi_i + ai_j * br_i + bi_j)


def s5_direction(ug, lam_re, lam_im, log_dt, b_re, b_im, c_re, c_im, reverse):
    seq = ug.shape[1]
    lr = jnp.minimum(lam_re, -1e-4)
    dt = jnp.exp(log_dt)[:, None]
    mag = jnp.exp(lr * dt)
    ang = lam_im * dt
    a_re = mag * jnp.cos(ang)
    a_im = mag * jnp.sin(ang)
    den = lr * lr + lam_im * lam_im
    k_re = (((a_re - 1) * lr + a_im * lam_im) / den)[..., None]
    k_im = ((a_im * lr - (a_re - 1) * lam_im) / den)[..., None]
    bb_re = k_re * b_re - k_im * b_im
    bb_im = k_re * b_im + k_im * b_re
    bu_re = jnp.einsum('bsgh,gph->bsgp', ug, bb_re)
    bu_im = jnp.einsum('bsgh,gph->bsgp', ug, bb_im)
    shp = (1, seq) + a_re.shape
    ar = jnp.broadcast_to(a_re, shp)
    ai = jnp.broadcast_to(a_im, shp)
    _, _, x_re, x_im = lax.associative_scan(
        _cplx_combine, (ar, ai, bu_re, bu_im), reverse=reverse, axis=1)
    return (jnp.einsum('bsgp,ghp->bsgh', x_re, c_re)
            - jnp.einsum('bsgp,ghp->bsgh', x_im, c_im))


def s5_mixer(u, lam_re, lam_im, log_dt, b_re, b_im, c_re, c_im, d, w_glu):
    bsz, seq, _ = u.shape
    ug = u.reshape(bsz, seq, S5_GROUPS, S5_GROUP)
    y = d.reshape(S5_GROUPS, S5_GROUP) * ug
    for di in range(2):
        y = y + s5_direction(ug, lam_re[di], lam_im[di], log_dt[di], b_re[di], b_im[di],
                             c_re[di], c_im[di], reverse=(di == 1))
    y = y.reshape(bsz, seq, S5_WIDTH)
    z = jax.nn.gelu(y)
    return z * jax.nn.sigmoid(z @ w_glu)


def rg_lru_direction(xc, w_a, b_a, w_x, b_x, lam, reverse):
    bsz, seq, _ = xc.shape
    xr = xc.reshape(bsz, seq, LRU_BLOCKS, LRU_BLOCK)
    r = jax.nn.sigmoid(jnp.einsum('bsnc,ncd->bsnd', xr, w_a).reshape(bsz, seq, LRU_WIDTH) + b_a)
    i = jax.nn.sigmoid(jnp.einsum('bsnc,ncd->bsnd', xr, w_x).reshape(bsz, seq, LRU_WIDTH) + b_x)
    log_a = -LRU_C * r * jax.nn.softplus(-lam)
    a = jnp.exp(log_a)
    mult = jnp.sqrt(jnp.maximum(-jnp.expm1(2 * log_a), 0))
    _, h = lax.associative_scan(_lin_combine, (a, mult * (i * xc)), reverse=reverse, axis=1)
    return h


def rg_lru_mixer(xb, conv_w, conv_b, w_a, b_a, w_x, b_x, lam):
    pad = (LRU_CONV // 2, LRU_CONV - 1 - LRU_CONV // 2)
    xc = lax.conv_general_dilated(
        xb, conv_w[:, None, :], window_strides=(1,), padding=[pad],
        dimension_numbers=('NWC', 'WIO', 'NWC'), feature_group_count=LRU_WIDTH) + conv_b
    h = rg_lru_direction(xc, w_a[0], b_a[0], w_x[0], b_x[0], lam[0], reverse=False)
    return h + rg_lru_direction(xc, w_a[1], b_a[1], w_x[1], b_x[1], lam[1], reverse=True)


def gla_direction(q, k, v, g, strict):
    bsz, seq, nh, _ = q.shape
    n_chunk = seq // GLA_CHUNK

    def chunks(t):
        return t.reshape(bsz, n_chunk, GLA_CHUNK, nh, t.shape[-1]).transpose(0, 3, 1, 2, 4)

    q, k, v, g = chunks(q), chunks(k), chunks(v), chunks(g)
    bcum = jnp.cumsum(g.astype(jnp.float32), axis=3)
    b_last = bcum[:, :, :, -1:, :]
    q_dec = q * jnp.exp(bcum)
    k_dec = k * jnp.exp(-bcum)
    mask = jnp.tril(jnp.ones((GLA_CHUNK, GLA_CHUNK), dtype=bool), -1 if strict else 0)
    scores = jnp.where(mask, jnp.einsum('bhnid,bhnjd->bhnij', q_dec, k_dec), 0.0)
    o_intra = jnp.einsum('bhnij,bhnjv->bhniv', scores, v)
    chunk_kv = jnp.einsum('bhnjd,bhnjv->bhndv', k * jnp.exp(b_last - bcum), v)
    decay = jnp.exp(b_last[:, :, :, 0, :])

    def step(state, inp):
        dec, kv = inp
        return dec[..., None] * state + kv, state

    init = jnp.zeros((bsz, nh, GLA_DK, GLA_DV), chunk_kv.dtype)
    _, prev = lax.scan(step, init, (jnp.moveaxis(decay, 2, 0), jnp.moveaxis(chunk_kv, 2, 0)))
    prev = jnp.moveaxis(prev, 0, 2)
    o = o_intra + jnp.einsum('bhnid,bhndv->bhniv', q_dec, prev)
    return o.transpose(0, 2, 3, 1, 4).reshape(bsz, seq, nh, GLA_DV).astype(v.dtype)


def gla_mixer(q, k, v, og, alr, w_alpha, b_alpha, norm_g):
    bsz, seq, _ = q.shape
    shp = (bsz, seq, GLA_HEADS, GLA_DK)
    q = q.reshape(shp) * (GLA_DK ** -0.5)
    k = k.reshape(shp)
    v = v.reshape(bsz, seq, GLA_HEADS, GLA_DV)
    g_f = jax.nn.log_sigmoid(alr[..., :GLA_RANK] @ w_alpha[0] + b_alpha[0]).reshape(shp) / GLA_TAU
    g_b = jax.nn.log_sigmoid(alr[..., GLA_RANK:] @ w_alpha[1] + b_alpha[1]).reshape(shp) / GLA_TAU
    o_f = gla_direction(q, k, v, g_f, strict=False)
    fl = lambda t: jnp.flip(t, axis=1)
    o_b = fl(gla_direction(fl(q), fl(k), fl(v), fl(g_b), strict=True))
    o = rms_norm(o_f + o_b, norm_g)
    return o.reshape(bsz, seq, GLA_WIDTH) * jax.nn.silu(og)


def ec_ffn(hn, w_router, w_gate, w_up, w_down):
    bsz, seq, d = hn.shape
    cap = CAPACITY_FACTOR * seq // N_EXPERTS
    probs = jax.nn.softmax((hn @ w_router).astype(jnp.float32), axis=-1)
    gate, idx = lax.top_k(jnp.swapaxes(probs, 1, 2), cap)
    xe = jax.vmap(lambda t, i: t[i])(hn, idx)
    hid = jax.nn.silu(jnp.einsum('becd,edf->becf', xe, w_gate)) * jnp.einsum('becd,edf->becf', xe, w_up)
    ye = jnp.einsum('becf,efd->becd', hid, w_down) * gate.astype(hn.dtype)[..., None]
    return jax.vmap(lambda i, y: jnp.zeros((seq, d), y.dtype).at[i.reshape(-1)].add(y.reshape(-1, d)))(idx, ye)


def setup_inputs(seed: int = 0) -> dict:
    key = jax.random.key(seed)
    ks = iter(jax.random.split(key, 40))

    def nrm(shape, scale):
        return scale * jax.random.normal(next(ks), shape, jnp.float32)

    L = DEPTH
    G, P, H = S5_GROUPS, S5_STATE, S5_GROUP
    n = jnp.arange(P, dtype=jnp.float32)
    log_dt = jax.random.uniform(next(ks), (L, 2, G), jnp.float32,
                                math.log(S5_DT_MIN), math.log(S5_DT_MAX))
    a_c = jax.random.uniform(next(ks), (L, 2, LRU_WIDTH), jnp.float32, 0.9, 0.999)
    a = a_c ** (1.0 / LRU_C)
    return dict(
        x=nrm((BATCH, SEQ, D_MODEL), 1.0),
        mix_norm_g=1.0 + nrm((L, D_MODEL), 0.02),
        w_in=nrm((L, D_MODEL, IN_WIDTH), D_MODEL ** -0.5),
        s5_lam_re=-0.5 + nrm((L, 2, G, P), 0.01),
        s5_lam_im=math.pi * n + nrm((L, 2, G, P), 0.01),
        s5_log_dt=log_dt,
        s5_b_re=nrm((L, 2, G, P, H), (2 * H) ** -0.5),
        s5_b_im=nrm((L, 2, G, P, H), (2 * H) ** -0.5),
        s5_c_re=nrm((L, 2, G, H, P), (2 * P) ** -0.5),
        s5_c_im=nrm((L, 2, G, H, P), (2 * P) ** -0.5),
        s5_d=nrm((L, S5_WIDTH), 1.0),
        s5_w_glu=nrm((L, S5_WIDTH, S5_WIDTH), S5_WIDTH ** -0.5),
        lru_conv_w=nrm((L, LRU_CONV, LRU_WIDTH), LRU_CONV ** -0.5),
        lru_conv_b=nrm((L, LRU_WIDTH), 0.01),
        lru_w_a=nrm((L, 2, LRU_BLOCKS, LRU_BLOCK, LRU_BLOCK), LRU_BLOCK ** -0.5),
        lru_b_a=nrm((L, 2, LRU_WIDTH), 0.01),
        lru_w_x=nrm((L, 2, LRU_BLOCKS, LRU_BLOCK, LRU_BLOCK), LRU_BLOCK ** -0.5),
        lru_b_x=nrm((L, 2, LRU_WIDTH), 0.01),
        lru_lam=jnp.log(a) - jnp.log1p(-a),
        gla_w_alpha=nrm((L, 2, GLA_RANK, GLA_QK), GLA_RANK ** -0.5),
        gla_b_alpha=nrm((L, 2, GLA_QK), 0.1),
        gla_norm_g=1.0 + nrm((L, GLA_DV), 0.02),
        w_up_s5=nrm((L, S5_WIDTH, D_MODEL), S5_WIDTH ** -0.5),
        w_up_lru=nrm((L, LRU_WIDTH, D_MODEL), LRU_WIDTH ** -0.5),
        w_up_gla=nrm((L, GLA_WIDTH, D_MODEL), GLA_WIDTH ** -0.5),
        w_mix_out=nrm((L, D_MODEL, D_MODEL), D_MODEL ** -0.5),
        ffn_norm_g=1.0 + nrm((L, D_MODEL), 0.02),
        w_router=nrm((L, D_MODEL, N_EXPERTS), D_MODEL ** -0.5),
        w_exp_gate=nrm((L, N_EXPERTS, D_MODEL, EXPERT_FF), D_MODEL ** -0.5),
        w_exp_up=nrm((L, N_EXPERTS, D_MODEL, EXPERT_FF), D_MODEL ** -0.5),
        w_exp_down=nrm((L, N_EXPERTS, EXPERT_FF, D_MODEL), EXPERT_FF ** -0.5),
        final_norm_g=1.0 + nrm((D_MODEL,), 0.02),
    )


def reference(x, mix_norm_g, w_in, s5_lam_re, s5_lam_im, s5_log_dt, s5_b_re, s5_b_im,
              s5_c_re, s5_c_im, s5_d, s5_w_glu, lru_conv_w, lru_conv_b, lru_w_a, lru_b_a,
              lru_w_x, lru_b_x, lru_lam, gla_w_alpha, gla_b_alpha, gla_norm_g,
              w_up_s5, w_up_lru, w_up_gla, w_mix_out, ffn_norm_g, w_router,
              w_exp_gate, w_exp_up, w_exp_down, final_norm_g):
    bsz, seq, _ = x.shape
    h = x
    for l in range(DEPTH):
        xn = rms_norm(h, mix_norm_g[l])
        p = xn @ w_in[l]
        y_s5 = s5_mixer(p[..., OFF_S5:OFF_LRU], s5_lam_re[l], s5_lam_im[l], s5_log_dt[l],
                        s5_b_re[l], s5_b_im[l], s5_c_re[l], s5_c_im[l], s5_d[l], s5_w_glu[l])
        y_lru = rg_lru_mixer(p[..., OFF_LRU:OFF_Q], lru_conv_w[l], lru_conv_b[l], lru_w_a[l],
                             lru_b_a[l], lru_w_x[l], lru_b_x[l], lru_lam[l])
        y_gla = gla_mixer(p[..., OFF_Q:OFF_K], p[..., OFF_K:OFF_V], p[..., OFF_V:OFF_OG],
                          p[..., OFF_OG:OFF_ALR], p[..., OFF_ALR:OFF_GATE],
                          gla_w_alpha[l], gla_b_alpha[l], gla_norm_g[l])
        gates = jax.nn.sigmoid(p[..., OFF_GATE:].reshape(bsz, seq, N_BRANCH, D_MODEL))
        merged = (gates[:, :, 0] * (y_s5 @ w_up_s5[l])
                  + gates[:, :, 1] * (y_lru @ w_up_lru[l])
                  + gates[:, :, 2] * (y_gla @ w_up_gla[l]))
        h = h + merged @ w_mix_out[l]
        h = h + ec_ffn(rms_norm(h, ffn_norm_g[l]), w_router[l], w_exp_gate[l],
                       w_exp_up[l], w_exp_down[l])
    return rms_norm(h, final_norm_g)
```

```python
import math
from contextlib import ExitStack
import numpy as np
import concourse.bass as bass
import concourse.mybir as mybir
from concourse.bass_utils import run_bass_kernel_spmd

F32 = mybir.dt.float32
BF16 = mybir.dt.bfloat16
I32 = mybir.dt.int32
U32 = mybir.dt.uint32
AF = mybir.ActivationFunctionType
ALU = mybir.AluOpType
AX = mybir.AxisListType

D = 1024
KC = 8
INW = 4896
NE = 16
FF = 2048
EPS = 1e-6
SAME_ENGINE_SYNC = True


class KB:
    NS = 12

    def __init__(self, nc):
        self.nc = nc
        self.eng = {'pe': nc.tensor, 'act': nc.scalar, 'dve': nc.vector, 'pool': nc.gpsimd, 'sp': nc.sync}
        self.csem = {}
        self.ccnt = {}
        for e in ['pe', 'act', 'dve', 'pool']:
            self.csem[e] = nc.semaphore('c_' + e).__enter__()
            self.ccnt[e] = 0
        self.dsem = {}
        self.dval = {}
        self.dnext = {}
        for q in ['sp', 'act', 'pool']:
            self.dsem[q] = [nc.semaphore('d_%s%d' % (q, i)).__enter__() for i in range(self.NS)]
            self.dval[q] = [0] * self.NS
            self.dnext[q] = 0
        self.semname = {}
        self.semowner = {}
        for e, s in self.csem.items():
            self.semname[id(s)] = 'c_' + e
            self.semowner['c_' + e] = e
        self.semobj = {}
        for e, s in self.csem.items():
            self.semobj['c_' + e] = s
        for q in self.dsem:
            for i, s in enumerate(self.dsem[q]):
                self.semobj['d_%s%d' % (q, i)] = s
        self.waited = {e: {} for e in self.eng}
        self.lastw = {}
        self.readers = {}
        self.ninstr = 0

    def _wait(self, e, tok):
        name, val = tok
        if val <= 0:
            return
        if self.waited[e].get(name, 0) >= val:
            return
        owner = self.semowner.get(name)
        if owner == e and (e == 'pe' or not SAME_ENGINE_SYNC):
            return
        self.eng[e].wait_ge(self.semobj[name], val)
        self.waited[e][name] = val
        self.ninstr += 1

    def _deps(self, r, w):
        toks = {}

        def add(t):
            if t is None:
                return
            if toks.get(t[0], 0) < t[1]:
                toks[t[0]] = t[1]
        for k in r:
            add(self.lastw.get(k))
        for k in w:
            add(self.lastw.get(k))
            for n, v in self.readers.get(k, {}).items():
                add((n, v))
        return list(toks.items())

    def _register(self, tok, r, w):
        for k in w:
            self.lastw[k] = tok
            self.readers[k] = {}
        for k in r:
            d = self.readers.setdefault(k, {})
            if d.get(tok[0], 0) < tok[1]:
                d[tok[0]] = tok[1]

    def op(self, e, fn, r=(), w=()):
        psk = [k for k in r if isinstance(k, tuple) and k[0] == 'ps']
        toks = dict(self._deps(r, w))
        me = 'c_' + e
        for k in psk:
            for n, v in self.readers.get(k, {}).items():
                if n != me and toks.get(n, 0) < v:
                    toks[n] = v
        for t in toks.items():
            self._wait(e, t)
        ins = fn(self.eng[e])
        self.ccnt[e] += 1
        ins.then_inc(self.csem[e], 1)
        tok = ('c_' + e, self.ccnt[e])
        self._register(tok, r, w)
        self.ninstr += 1
        return tok

    def dma(self, q, out, in_, r=(), w=(), **kw):
        slot = self.dnext[q] % self.NS
        self.dnext[q] += 1
        name = 'd_%s%d' % (q, slot)
        self._wait(q, (name, self.dval[q][slot]))
        for t in self._deps(r, w):
            self._wait(q, t)
        ins = self.eng[q].dma_start(out=out, in_=in_, **kw)
        ins.then_inc(self.dsem[q][slot], 16)
        self.dval[q][slot] += 16
        tok = (name, self.dval[q][slot])
        self._register(tok, r, w)
        self.ninstr += 1
        return tok

    def idma(self, fn, r=(), w=()):
        q = 'pool'
        slot = self.dnext[q] % self.NS
        self.dnext[q] += 1
        name = 'd_%s%d' % (q, slot)
        self._wait(q, (name, self.dval[q][slot]))
        for t in self._deps(r, w):
            self._wait(q, t)
        ins = fn(self.eng[q])
        ins.then_inc(self.dsem[q][slot], 16)
        self.dval[q][slot] += 16
        tok = (name, self.dval[q][slot])
        self._register(tok, r, w)
        self.ninstr += 1
        return tok

    def barrier(self):
        toks = [('c_' + e, self.ccnt[e]) for e in self.ccnt]
        for q in self.dsem:
            for i in range(self.NS):
                toks.append(('d_%s%d' % (q, i), self.dval[q][i]))
        for e in self.eng:
            for t in toks:
                self._wait(e, t)
        self.lastw = {}
        self.readers = {}


class Ctx:
    pass


_NMC = [0]


def _nm(name):
    _NMC[0] += 1
    return '%s_%d' % (name, _NMC[0])


def _bc_mid(ap2d, n):
    return ap2d.unsqueeze(1).broadcast_to([ap2d.shape[0], n, ap2d.shape[1]])


def _bc_last(ap2d, n):
    return ap2d.unsqueeze(2).broadcast_to([ap2d.shape[0], ap2d.shape[1], n])


def _pcol(v, nch):
    return np.ascontiguousarray(np.asarray(v, np.float32).reshape(nch, 128).T)


def prep_layer(inp, l):
    f = lambda k: np.asarray(inp[k], np.float32)
    o = {}
    o['w_in'] = np.ascontiguousarray(f('w_in')[l])
    o['mix_g'] = _pcol(f('mix_norm_g')[l], 8)
    o['ffn_g'] = _pcol(f('ffn_norm_g')[l], 8)
    cw = f('lru_conv_w')[l]
    o['lru_cw'] = np.ascontiguousarray(cw.reshape(4, 4, 128).transpose(2, 1, 0))
    o['lru_cb'] = _pcol(f('lru_conv_b')[l], 4)
    for nm in ['a', 'x']:
        w = f('lru_w_' + nm)[l]
        bd = np.zeros((128, 2, 4, 128), np.float32)
        for di in range(2):
            for ct in range(4):
                bd[0:64, di, ct, 0:64] = w[di, 2 * ct]
                bd[64:128, di, ct, 64:128] = w[di, 2 * ct + 1]
        o['lru_w' + nm] = bd
        b = f('lru_b_' + nm)[l]
        o['lru_b' + nm] = np.ascontiguousarray(b.reshape(2, 4, 128).transpose(2, 0, 1))
    o['lru_lam'] = np.ascontiguousarray(f('lru_lam')[l].reshape(2, 4, 128).transpose(2, 0, 1))
    prep_gla(inp, l, o)
    prep_s5(inp, l, o)
    prep_rest(inp, l, o)
    return o


def build(T, nlayers, layer_shapes, dbg=()):
    nc = bass.Bass("TRN2", target_bir_lowering=False)
    kb = KB(nc)
    NTB = T // 512
    NT = T // 128
    c = Ctx()
    c.nc, c.kb, c.T, c.NTB, c.NT = nc, kb, T, NTB, NT

    def dram_in(name, shape, dt=F32):
        return nc.dram_tensor(name, list(shape), dt, kind="ExternalInput").ap()

    def dram_out(name, shape, dt=F32):
        return nc.dram_tensor(name, list(shape), dt, kind="ExternalOutput").ap()

    def dram_tmp(name, shape, dt=F32):
        return nc.dram_tensor(name, list(shape), dt, kind="Internal").ap()

    c.xT = dram_in('xT', [D, T])
    c.consts = dram_in('consts', [128, NCONST, 128])
    c.iota = dram_in('iota', [128, 513])
    c.fin_g = dram_in('fin_g', [128, 8])
    c.lw = []
    for l in range(nlayers):
        c.lw.append({k: dram_in('l%d_%s' % (l, k), shp) for k, shp in layer_shapes.items()})
    c.outT = dram_out('outT', [D, T])
    c.hT = dram_tmp('hT', [D, T])
    c.pmixT = dram_tmp('pmixT', [768, T])
    c.gT = dram_tmp('gT', [3072, T], BF16)
    c.ptm = dram_tmp('ptm', [T, 1056])
    c.alrT = dram_tmp('alrT', [32, T], BF16)
    c.yT = dram_tmp('yT', [1024, T], BF16)
    c.ffn_tm = dram_tmp('ffn_tm', [T, D])
    c.hn_tm = dram_tmp('hn_tm', [T, D], BF16)
    c.dbg = {}
    c.lvl = 99
    for name, shp in dbg:
        if name.startswith('lvl'):
            c.lvl = int(name[3:])
            continue
        c.dbg[name] = dram_out('dbg_' + name, shp)

    c.ps = [nc.psum_tensor('ps%d' % i, [128, 512], F32).__enter__() for i in range(8)]

    c.ones_bf = nc.sbuf_tensor(_nm('ones_bf'), [128, 128], BF16).__enter__()
    kb.op('dve', lambda e: e.memset(c.ones_bf[:], 1.0), w=['ones_bf'])

    load_consts(c)
    for l in range(nlayers):
        stage_norm_proj(c, l, src=(c.xT if l == 0 else c.hT), add_tm=(l > 0))
        kb.barrier()
        if 'skip_lru' not in c.dbg:
            stage_lru(c, l)
        kb.barrier()
        if 'skip_gla' not in c.dbg:
            stage_gla(c, l)
        kb.barrier()
        if 'skip_s5' not in c.dbg:
            stage_s5(c, l)
        kb.barrier()
        if 'stop_mixers' in c.dbg:
            continue
        with nc.sbuf_tensor(_nm('hnT'), [128, KC, T], BF16) as hnT:
            stage_merge(c, l, hnT)
            kb.barrier()
            if 'skip_ffn' not in c.dbg:
                stage_ffn(c, l, hnT)
            kb.barrier()
    if 'stop_mixers' not in c.dbg:
        stage_final(c)
    kb.barrier()
    return nc, c


def stage_norm_proj(c, l, src, add_tm=False):
    nc, kb, T, NTB, NT = c.nc, c.kb, c.T, c.NTB, c.NT
    W = c.lw[l]
    with ExitStack() as es:
        xn = es.enter_context(nc.sbuf_tensor(_nm('xn'), [128, KC, T], BF16))
        hblk = es.enter_context(nc.sbuf_tensor(_nm('hblk'), [128, 2, KC, 512], F32))
        sq = es.enter_context(nc.sbuf_tensor(_nm('sq'), [128, 2, KC, 512], BF16))
        rstd = es.enter_context(nc.sbuf_tensor(_nm('rstd'), [128, 2, 512], F32))
        g_mix = es.enter_context(nc.sbuf_tensor(_nm('g_mix'), [128, KC], F32))
        wA = es.enter_context(nc.sbuf_tensor(_nm('wA'), [128, KC, 768], BF16))
        wB = es.enter_context(nc.sbuf_tensor(_nm('wB'), [128, KC, 1056], BF16))
        wG = es.enter_context(nc.sbuf_tensor(_nm('wG'), [128, 2, KC, 512], BF16))
        stf = es.enter_context(nc.sbuf_tensor(_nm('stf'), [128, 4, 512], F32))
        stg = es.enter_context(nc.sbuf_tensor(_nm('stg'), [128, 4, 512], BF16))
        sttm = es.enter_context(nc.sbuf_tensor(_nm('sttm'), [128, 2, 1056], F32))
        stalr = es.enter_context(nc.sbuf_tensor(_nm('stalr'), [32, T], BF16))
        ftile = es.enter_context(nc.sbuf_tensor(_nm('ftile'), [128, 2, 1024], F32))
        kb.dma('sp', g_mix[:], W['mix_g'][:, :], w=['g_mix'])
        w_in_v = W['w_in'].rearrange("(k p) c -> p k c", p=128)
        kb.dma('pool', wA[:], w_in_v[:, :, 0:768], w=['wA'])
        kb.dma('pool', wB[:], w_in_v[:, :, 768:1824], w=['wB'])
        srcv = src.rearrange("(k p) t -> p k t", p=128)
        for tb in range(NTB):
            b = tb % 2
            ts = slice(tb * 512, (tb + 1) * 512)
            kb.dma('sp', hblk[:, b], srcv[:, :, ts], w=[('hblk', b)])
            if add_tm:
                emit_add_tm(c, hblk[:, b], ('hblk', b), tb, ftile, (2, 3))
                kb.dma('sp', srcv[:, :, ts], hblk[:, b], r=[('hblk', b)], w=[('hTw', tb)])
            kb.op('act', lambda e: e.activation(out=sq[:, b], in_=hblk[:, b], func=AF.Square),
                  r=[('hblk', b)], w=[('sq', b)])
            pb = c.ps[b]
            for k in range(KC):
                kb.op('pe', lambda e, k=k: e.matmul(pb[:], lhsT=c.ones_bf[:], rhs=sq[:, b, k, :],
                                                    start=(k == 0), stop=(k == KC - 1)),
                      r=[('sq', b), 'ones_bf'], w=[('ps', b)])
            kb.op('act', lambda e: e.activation(out=rstd[:, b], in_=pb[:], func=AF.Sqrt,
                                                scale=1.0 / D, bias=EPS),
                  r=[('ps', b)], w=[('rstd', b)])
            kb.op('dve', lambda e: e.reciprocal(out=rstd[:, b], in_=rstd[:, b]),
                  r=[('rstd', b)], w=[('rstd', b)])
            for k in range(KC):
                kb.op('dve', lambda e, k=k: e.scalar_tensor_tensor(
                    out=xn[:, k, ts], in0=hblk[:, b, k, :], scalar=g_mix[:, k:k + 1], in1=rstd[:, b],
                    op0=ALU.mult, op1=ALU.mult),
                    r=[('hblk', b), ('rstd', b), 'g_mix'], w=[('xn', tb)])
        nps = 0
        nst = 0
        for cc in range(6):
            for tb in range(NTB):
                ts = slice(tb * 512, (tb + 1) * 512)
                pi = 2 + (nps % 4)
                nps += 1
                sb = nst % 4
                nst += 1
                pb = c.ps[pi]
                for k in range(KC):
                    kb.op('pe', lambda e, k=k, pb=pb: e.matmul(
                        pb[:], lhsT=wA[:, k, cc * 128:(cc + 1) * 128], rhs=xn[:, k, ts],
                        start=(k == 0), stop=(k == KC - 1)),
                        r=['wA', ('xn', tb)], w=[('ps', pi)])
                if tb % 2 == 0:
                    kb.op('act', lambda e, pb=pb: e.copy(out=stf[:, sb, :], in_=pb[:]),
                          r=[('ps', pi)], w=[('stf', sb)])
                else:
                    kb.op('dve', lambda e, pb=pb: e.tensor_copy(out=stf[:, sb, :], in_=pb[:]),
                          r=[('ps', pi)], w=[('stf', sb)])
                kb.dma('sp', c.pmixT[cc * 128:(cc + 1) * 128, ts], stf[:, sb, :], r=[('stf', sb)], w=[('pmixT', cc, tb)])
        for tb in range(NTB):
            ts = slice(tb * 512, (tb + 1) * 512)
            pi = 2 + (nps % 4)
            nps += 1
            pb = c.ps[pi]
            for k in range(KC):
                kb.op('pe', lambda e, k=k, pb=pb: e.matmul(
                    pb[0:32, :], lhsT=wB[:, k, 1024:1056], rhs=xn[:, k, ts],
                    start=(k == 0), stop=(k == KC - 1)),
                    r=['wB', ('xn', tb)], w=[('ps', pi)])
            kb.op('act', lambda e, pb=pb: e.copy(out=stalr[:, ts], in_=pb[0:32, :]),
                  r=[('ps', pi)], w=[('stalr', tb)])
        kb.dma('sp', c.alrT[:, :], stalr[:, :], r=[('stalr', tb) for tb in range(NTB)], w=['alrT'])
        for tt in range(NT):
            sb = tt % 2
            tb = tt // 4
            tsl = slice(tt * 128, (tt + 1) * 128)
            for gi, (c0, c1) in enumerate([(0, 512), (512, 1024), (1024, 1056)]):
                pi = 2 + (nps % 4)
                nps += 1
                pb = c.ps[pi]
                for k in range(KC):
                    kb.op('pe', lambda e, k=k, pb=pb: e.matmul(
                        pb[:, 0:c1 - c0], lhsT=xn[:, k, tsl], rhs=wB[:, k, c0:c1],
                        start=(k == 0), stop=(k == KC - 1)),
                        r=['wB', ('xn', tb)], w=[('ps', pi)])
                if gi == 0:
                    kb.op('act', lambda e, pb=pb: e.copy(out=sttm[:, sb, c0:c1], in_=pb[:, 0:c1 - c0]),
                          r=[('ps', pi)], w=[('sttm', sb, gi)])
                else:
                    kb.op('dve', lambda e, pb=pb: e.tensor_copy(out=sttm[:, sb, c0:c1], in_=pb[:, 0:c1 - c0]),
                          r=[('ps', pi)], w=[('sttm', sb, gi)])
            kb.dma('sp', c.ptm[tsl, :], sttm[:, sb, :], r=[('sttm', sb, gi) for gi in range(3)],
                   w=[('ptm', tt)])
        for cg in range(6):
            wb_ = cg % 2
            kb.dma('pool', wG[:, wb_], w_in_v[:, :, 1824 + cg * 512:1824 + (cg + 1) * 512], w=[('wG', wb_)])
            for c4 in range(4):
                cc = cg * 4 + c4
                for tb in range(NTB):
                    ts = slice(tb * 512, (tb + 1) * 512)
                    pi = 2 + (nps % 4)
                    nps += 1
                    pb = c.ps[pi]
                    for k in range(KC):
                        kb.op('pe', lambda e, k=k, pb=pb: e.matmul(
                            pb[:], lhsT=wG[:, wb_, k, c4 * 128:(c4 + 1) * 128], rhs=xn[:, k, ts],
                            start=(k == 0), stop=(k == KC - 1)),
                            r=[('wG', wb_), ('xn', tb)], w=[('ps', pi)])
                    sb = nst % 4
                    nst += 1
                    kb.op('act', lambda e, pb=pb: e.activation(out=stg[:, sb, :], in_=pb[:], func=AF.Sigmoid),
                          r=[('ps', pi)], w=[('stg', sb)])
                    kb.dma('sp', c.gT[cc * 128:(cc + 1) * 128, ts], stg[:, sb, :], r=[('stg', sb)], w=[('gT', cc, tb)])
        if 'pmixT' in c.dbg:
            kb.barrier()
            kb.dma('sp', c.dbg['pmixT'][:, :], c.pmixT[:, :], w=['dbg_pmixT'])
        kb.barrier()


def stage_lru(c, l):
    nc, kb, T, NTB, NT = c.nc, c.kb, c.T, c.NTB, c.NT
    W = c.lw[l]
    with ExitStack() as es:
        xbp = es.enter_context(nc.sbuf_tensor(_nm('xbp'), [128, T + 4], F32))
        xc = es.enter_context(nc.sbuf_tensor(_nm('xc'), [128, T], F32))
        xcb = es.enter_context(nc.sbuf_tensor(_nm('xcb'), [128, T], BF16))
        bA = es.enter_context(nc.sbuf_tensor(_nm('bA'), [128, T], F32))
        bB = es.enter_context(nc.sbuf_tensor(_nm('bB'), [128, T], F32))
        bC = es.enter_context(nc.sbuf_tensor(_nm('bC'), [128, T], F32))
        hf = es.enter_context(nc.sbuf_tensor(_nm('hf'), [128, T], F32))
        ybf = es.enter_context(nc.sbuf_tensor(_nm('ybf'), [128, 2, T], BF16))
        l_cw = es.enter_context(nc.sbuf_tensor(_nm('l_cw'), [128, 4, 4], F32))
        l_cb = es.enter_context(nc.sbuf_tensor(_nm('l_cb'), [128, 4], F32))
        l_wa = es.enter_context(nc.sbuf_tensor(_nm('l_wa'), [128, 2, 4, 128], BF16))
        l_wx = es.enter_context(nc.sbuf_tensor(_nm('l_wx'), [128, 2, 4, 128], BF16))
        l_ba = es.enter_context(nc.sbuf_tensor(_nm('l_ba'), [128, 8], F32))
        l_bx = es.enter_context(nc.sbuf_tensor(_nm('l_bx'), [128, 8], F32))
        l_lam = es.enter_context(nc.sbuf_tensor(_nm('l_lam'), [128, 8], F32))
        l_c = es.enter_context(nc.sbuf_tensor(_nm('l_c'), [128, 8], F32))
        l_c2 = es.enter_context(nc.sbuf_tensor(_nm('l_c2'), [128, 8], F32))
        l_t = es.enter_context(nc.sbuf_tensor(_nm('l_t'), [128, 6, 8], F32))
        kb.dma('sp', l_cw[:], W['lru_cw'][:, :, :], w=['l_cw'])
        kb.dma('sp', l_cb[:], W['lru_cb'][:, :], w=['l_cb'])
        kb.dma('pool', l_wa[:], W['lru_wa'][:, :, :, :], w=['l_wa'])
        kb.dma('pool', l_wx[:], W['lru_wx'][:, :, :, :], w=['l_wx'])
        kb.dma('sp', l_ba[:], W['lru_ba'].rearrange("p a b -> p (a b)"), w=['l_ba'])
        kb.dma('sp', l_bx[:], W['lru_bx'].rearrange("p a b -> p (a b)"), w=['l_bx'])
        kb.dma('sp', l_lam[:], W['lru_lam'].rearrange("p a b -> p (a b)"), w=['l_lam'])
        P = 'l_par'
        tz, tw, tw2, tacc, tm, tabs = [l_t[:, i, :] for i in range(6)]
        dv = lambda fn, r=(P,), w=(P,): kb.op('dve', fn, r=list(r) + ['l_lam'], w=list(w))
        dv(lambda e: e.scalar_tensor_tensor(out=tabs, in0=l_lam[:], scalar=-1.0, in1=l_lam[:], op0=ALU.mult, op1=ALU.max))
        kb.op('act', lambda e: e.activation(out=tz, in_=tabs, func=AF.Exp, scale=-1.0), r=[P], w=[P])
        dv(lambda e: e.tensor_scalar(out=tw, in0=tz, scalar1=2.0, scalar2=None, op0=ALU.add))
        dv(lambda e: e.reciprocal(out=tw, in_=tw))
        dv(lambda e: e.tensor_tensor(out=tw, in0=tw, in1=tz, op=ALU.mult))
        dv(lambda e: e.tensor_tensor(out=tw2, in0=tw, in1=tw, op=ALU.mult))
        dv(lambda e: e.memset(tacc, 1.0 / 13.0))
        for kk in [11, 9, 7, 5, 3, 1]:
            dv(lambda e: e.tensor_tensor(out=tacc, in0=tacc, in1=tw2, op=ALU.mult))
            dv(lambda e, kk=kk: e.tensor_scalar(out=tacc, in0=tacc, scalar1=1.0 / kk, scalar2=None, op0=ALU.add))
        dv(lambda e: e.tensor_tensor(out=tacc, in0=tacc, in1=tw, op=ALU.mult))
        dv(lambda e: e.tensor_scalar(out=tm, in0=l_lam[:], scalar1=-1.0, scalar2=0.0, op0=ALU.mult, op1=ALU.max))
        dv(lambda e: e.scalar_tensor_tensor(out=tacc, in0=tacc, scalar=2.0, in1=tm, op0=ALU.mult, op1=ALU.add))
        dv(lambda e: e.tensor_scalar(out=l_c[:], in0=tacc, scalar1=-8.0, scalar2=None, op0=ALU.mult), w=[P, 'l_c'])
        dv(lambda e: e.tensor_scalar(out=l_c2[:], in0=tacc, scalar1=-16.0, scalar2=None, op0=ALU.mult), w=[P, 'l_c'])
        kb.op('dve', lambda e: e.memset(xbp[:, 0:2], 0.0), w=['xbp_pad'])
        kb.op('dve', lambda e: e.memset(xbp[:, T + 2:T + 4], 0.0), w=['xbp_pad'])
        nps = 0
        for ct in range(4):
            kb.dma('sp', xbp[:, 2:T + 2], c.pmixT[256 + ct * 128:256 + (ct + 1) * 128, :], w=['xbp'])
            kb.op('dve', lambda e: e.tensor_scalar(out=xc[:], in0=xbp[:, 0:T], scalar1=l_cw[:, ct, 0:1],
                                                   scalar2=l_cb[:, ct:ct + 1], op0=ALU.mult, op1=ALU.add),
                  r=['xbp', 'xbp_pad', 'l_cw', 'l_cb'], w=['xc'])
            for j in range(1, 4):
                kb.op('dve', lambda e, j=j: e.scalar_tensor_tensor(
                    out=xc[:], in0=xbp[:, j:j + T], scalar=l_cw[:, ct, j:j + 1], in1=xc[:],
                    op0=ALU.mult, op1=ALU.add), r=['xbp', 'xbp_pad', 'l_cw', 'xc'], w=['xc'])
            kb.op('act', lambda e: e.copy(out=xcb[:], in_=xc[:]), r=['xc'], w=['xcb'])
            for di in range(2):
                for (wt, bt, dst, key) in [(l_wa, l_ba, bA, 'bA'), (l_wx, l_bx, bB, 'bB')]:
                    for tb in range(NTB):
                        ts = slice(tb * 512, (tb + 1) * 512)
                        pi = nps % 4
                        nps += 1
                        pb = c.ps[pi]
                        kb.op('pe', lambda e, pb=pb, wt=wt: e.matmul(pb[:], lhsT=wt[:, di, ct, :], rhs=xcb[:, ts],
                                                                     start=True, stop=True),
                              r=['xcb', 'l_wa', 'l_wx'], w=[('ps', pi)])
                        kb.op('act', lambda e, pb=pb, bt=bt, dst=dst: e.activation(
                            out=dst[:, ts], in_=pb[:], func=AF.Sigmoid,
                            bias=bt[:, di * 4 + ct:di * 4 + ct + 1]),
                            r=[('ps', pi), 'l_ba', 'l_bx'], w=[key])
                cs = l_c[:, di * 4 + ct:di * 4 + ct + 1]
                c2s = l_c2[:, di * 4 + ct:di * 4 + ct + 1]
                kb.op('act', lambda e: e.activation(out=bC[:], in_=bA[:], func=AF.Exp, scale=cs),
                      r=['bA', 'l_c'], w=['bC'])
                kb.op('act', lambda e: e.activation(out=bA[:], in_=bA[:], func=AF.Exp, scale=c2s),
                      r=['bA', 'l_c'], w=['bA'])
                kb.op('act', lambda e: e.activation(out=bA[:], in_=bA[:], func=AF.Sqrt, scale=-1.0, bias=1.0),
                      r=['bA'], w=['bA'])
                kb.op('dve', lambda e: e.tensor_tensor(out=bB[:], in0=bB[:], in1=bA[:], op=ALU.mult),
                      r=['bA', 'bB'], w=['bB'])
                kb.op('dve', lambda e: e.tensor_tensor(out=bB[:], in0=bB[:], in1=xc[:], op=ALU.mult),
                      r=['xc', 'bB'], w=['bB'])
                if di == 0:
                    kb.op('dve', lambda e: e.tensor_tensor_scan(out=hf[:], data0=bC[:], data1=bB[:], initial=0.0,
                                                                op0=ALU.mult, op1=ALU.add),
                          r=['bC', 'bB'], w=['hf'])
                else:
                    rev = lambda t: bass.AP(t[:].tensor, t[:, T - 1:T].offset, [[t[:].ap[0][0], 128], [-1, T]])
                    kb.op('dve', lambda e: e.tensor_tensor_scan(out=rev(bA), data0=rev(bC), data1=rev(bB),
                                                                initial=0.0, op0=ALU.mult, op1=ALU.add),
                          r=['bC', 'bB', 'bA'], w=['bA'])
                    sb = ct % 2
                    kb.op('dve', lambda e: e.tensor_tensor(out=ybf[:, sb, :], in0=hf[:], in1=bA[:], op=ALU.add),
                          r=['hf', 'bA'], w=[('ybf', sb)])
                    kb.dma('sp', c.yT[256 + ct * 128:256 + (ct + 1) * 128, :], ybf[:, sb, :],
                           r=[('ybf', sb)], w=[('yT', 2 + ct)])
        if 'ylru' in c.dbg:
            kb.barrier()
            with nc.sbuf_tensor(_nm('dbgt'), [128, T], F32) as dbgt:
                for ct in range(4):
                    kb.dma('pool', dbgt[:], c.yT[256 + ct * 128:256 + (ct + 1) * 128, :], w=['dbgt'])
                    kb.dma('sp', c.dbg['ylru'][ct * 128:(ct + 1) * 128, :], dbgt[:], r=['dbgt'], w=[('dbgo', ct)])
                kb.barrier()
        kb.barrier()


LAYER_SHAPES = None


def layer_shapes_from(prep):
    return {k: v.shape for k, v in prep.items()}


NCONST = 10


def make_consts():
    s = np.arange(128)[:, None]
    t = np.arange(128)[None, :]
    cst = np.zeros((128, NCONST, 128), np.float32)
    cst[:, 0] = (s == t)
    cst[:, 1] = (s <= t) * (-1.0 / 16)
    cst[:, 2] = (s >= t) * (-1.0 / 16)
    cst[:, 3] = -1.0 / 16
    cst[:, 4] = (s <= t)
    cst[:, 5] = (s > t)
    cst[:, 6] = (s < t)
    cst[:, 7] = 1.0
    cst[:, 8] = (s <= t)
    cst[:, 9] = (s >= t)
    return cst


def prep_gla(inp, l, o):
    f = lambda k: np.asarray(inp[k], np.float32)
    wal = np.zeros((33, 512), np.float32)
    wal[0:16, 0:256] = f('gla_w_alpha')[l, 0]
    wal[16:32, 256:512] = f('gla_w_alpha')[l, 1]
    wal[32, 0:256] = f('gla_b_alpha')[l, 0]
    wal[32, 256:512] = f('gla_b_alpha')[l, 1]
    o['gla_wal'] = wal
    o['gla_ng'] = np.ascontiguousarray(np.broadcast_to(f('gla_norm_g')[l][None, :], (128, 64)))


def load_consts(c):
    nc, kb = c.nc, c.kb
    c.cst = nc.sbuf_tensor(_nm('cst'), [128, NCONST, 128], F32).__enter__()
    c.cstb = nc.sbuf_tensor(_nm('cstb'), [128, NCONST, 128], BF16).__enter__()
    kb.dma('sp', c.cst[:], c.consts[:, :, :], w=['cst'])
    kb.dma('pool', c.cstb[:], c.consts[:, :, :], w=['cst'])


def stage_gla(c, l):
    nc, kb, T, NTB, NT = c.nc, c.kb, c.T, c.NTB, c.NT
    W = c.lw[l]
    cst, cstb = c.cst, c.cstb
    ident_b = cstb[:, 0, :]
    with ExitStack() as es:
        A = lambda name, shp, dt: es.enter_context(nc.sbuf_tensor(_nm(name), shp, dt))
        ptile = A('g_pt', [128, 2, 1056], F32)
        alrx = A('g_alrx', [64, T], BF16)
        wal = A('g_wal', [64, 512], BF16)
        spt = A('g_sp', [128, 512], F32)
        E1 = A('g_E1', [128, 512], F32)
        E2 = A('g_E2', [128, 512], F32)
        decrow = A('g_decrow', [128, 512], F32)
        qd = A('g_qd', [128, 2, 256], BF16)
        kd = A('g_kd', [128, 2, 256], BF16)
        kk = A('g_kk', [128, 2, 256], BF16)
        vb = A('g_vb', [128, 256], BF16)
        qdT = A('g_qdT', [128, 4, T], BF16)
        kdT = A('g_kdT', [128, 4, 128], BF16)
        scm = A('g_scm', [128, 2, 4, 128], BF16)
        kv = A('g_kv', [128, NT, 4, 64], F32)
        dec = A('g_dec', [128, NT, 4], F32)
        oacc = A('g_oacc', [128, NT, 256], F32)
        sog = A('g_sog', [128, NT, 256], BF16)
        S = A('g_S', [128, 4, 64], F32)
        Sb = A('g_Sb', [128, 4, 64], BF16)
        ng = A('g_ng', [128, 64], F32)
        ygT = A('g_ygT', [128, 2, T], BF16)
        ss = A('g_ss', [128, NT * 4], F32)
        ps = c.ps
        kb.op('dve', lambda e: e.memset(alrx[32:64, :], 1.0), w=['alrx1'])
        kb.dma('sp', alrx[0:32, :], c.alrT[:, :], w=['alrx'])
        kb.op('dve', lambda e: e.memset(wal[:], 0.0), w=['wal'])
        kb.dma('pool', wal[0:33, :], W['gla_wal'][:, :], r=[], w=['wal'])
        kb.dma('sp', ng[:], W['gla_ng'][:, :], w=['ng'])
        for n in range(NT):
            b = n % 2
            nsl = slice(n * 128, (n + 1) * 128)
            kb.dma('sp', ptile[:, b, :], c.ptm[nsl, :], w=[('pt', b)])
            q_ap = ptile[:, b, 0:256]
            k_ap = ptile[:, b, 256:512]
            v_ap = ptile[:, b, 512:768]
            og_ap = ptile[:, b, 768:1024]
            kb.op('pe', lambda e: e.matmul(ps[0][:], lhsT=alrx[0:33, nsl], rhs=wal[0:33, :], start=True, stop=True),
                  r=['alrx', 'alrx1', 'wal'], w=[('ps', 0)])
            kb.op('act', lambda e: e.activation(out=spt[:], in_=ps[0][:], func=AF.Exp, scale=-1.0),
                  r=[('ps', 0)], w=['spt'])
            kb.op('act', lambda e: e.activation(out=spt[:], in_=spt[:], func=AF.Ln, bias=1.0),
                  r=['spt'], w=['spt'])
            if 'gla_stop0' in c.dbg:
                continue
            kb.op('pe', lambda e: e.matmul(ps[1][:, 0:256], lhsT=cst[:, 1, :], rhs=spt[:, 0:256], start=True, stop=True),
                  r=['spt', 'cst'], w=[('ps', 1)])
            kb.op('pe', lambda e: e.matmul(ps[1][:, 256:512], lhsT=cst[:, 2, :], rhs=spt[:, 256:512], start=True, stop=True),
                  r=['spt', 'cst'], w=[('ps', 1)])
            kb.op('pe', lambda e: e.matmul(ps[2][:], lhsT=cst[:, 3, :], rhs=spt[:], start=True, stop=True),
                  r=['spt', 'cst'], w=[('ps', 2)])
            if c.lvl < 1:
                continue
            for i4 in range(4):
                kb.op('pe', lambda e, i4=i4: e.matmul(ps[6][:, 256 + 2 * i4:258 + 2 * i4], lhsT=spt[:, i4 * 128:(i4 + 1) * 128],
                                                     rhs=cst[:, 3, 0:2], start=True, stop=True),
                      r=['spt', 'cst'], w=[('ps', 6)])
            kb.op('act', lambda e: e.activation(out=dec[:, n, :], in_=ps[6][:, 256:264:2], func=AF.Exp),
                  r=[('ps', 6)], w=['dec'])
            if c.lvl < 2:
                continue
            kb.op('act', lambda e: e.activation(out=E1[:], in_=ps[1][:], func=AF.Exp), r=[('ps', 1)], w=['E1'])
            kb.op('act', lambda e: e.activation(out=E2[:], in_=ps[1][:], func=AF.Exp, scale=-1.0), r=[('ps', 1)], w=['E2'])
            kb.op('act', lambda e: e.activation(out=decrow[:], in_=ps[2][:], func=AF.Exp), r=[('ps', 2)], w=['decrow'])
            if c.lvl < 3:
                continue
            v2 = lambda ap: ap.rearrange("p (a b) -> p a b", a=2)
            kb.op('dve', lambda e: e.scalar_tensor_tensor(out=qd[:], in0=_bc_mid(q_ap, 2), scalar=0.125, in1=v2(E1[:]),
                                                          op0=ALU.mult, op1=ALU.mult),
                  r=[('pt', b), 'E1'], w=['qd'])
            kb.op('dve', lambda e: e.tensor_tensor(out=kd[:], in0=_bc_mid(k_ap, 2), in1=v2(E2[:]), op=ALU.mult),
                  r=[('pt', b), 'E2'], w=['kd'])
            kb.op('dve', lambda e: e.tensor_tensor(out=E2[:], in0=E2[:], in1=decrow[:], op=ALU.mult),
                  r=['E2', 'decrow'], w=['E2'])
            kb.op('dve', lambda e: e.tensor_tensor(out=kk[:], in0=_bc_mid(k_ap, 2), in1=v2(E2[:]), op=ALU.mult),
                  r=[('pt', b), 'E2'], w=['kk'])
            if c.lvl < 4:
                continue
            kb.op('act', lambda e: e.copy(out=vb[:], in_=v_ap), r=[('pt', b)], w=['vb'])
            kb.op('act', lambda e: e.activation(out=decrow[:, 0:256], in_=og_ap, func=AF.Sigmoid), r=[('pt', b), 'E2'], w=['decrow'])
            kb.op('dve', lambda e: e.tensor_tensor(out=sog[:, n, :], in0=og_ap, in1=decrow[:, 0:256], op=ALU.mult), r=[('pt', b), 'decrow'], w=['sog'])
            if c.lvl < 5:
                continue
            psE = ps[3][:].bitcast(BF16)
            for i8 in range(8):
                src = (qd if i8 < 4 else kd)
                di, ct = (i8 % 4) // 2, i8 % 2
                kb.op('pe', lambda e, src=src, di=di, ct=ct, i8=i8: e.transpose(
                    out=psE[:, i8 * 128:(i8 + 1) * 128], in_=src[:, di, ct * 128:(ct + 1) * 128], identity=ident_b),
                    r=['qd', 'kd', 'cst'], w=[('ps', 3)])
            if 'noevac' in c.dbg:
                continue
            kb.op('act', lambda e: e.copy(out=qdT[:, :, nsl], in_=psE[:, 0:512].rearrange("p (a b) -> p a b", a=4)),
                  r=[('ps', 3)], w=[('qdT', n)])
            if 'evac_act_only' in c.dbg:
                continue
            kb.op('act', lambda e: e.copy(out=kdT[:], in_=psE[:, 512:1024].rearrange("p (a b) -> p a b", a=4)),
                  r=[('ps', 3)], w=['kdT'])
            if c.lvl < 6:
                continue
            for di in range(2):
                for h in range(4):
                    ct, r0 = h // 2, (h % 2) * 64
                    cb = di * 2 + h // 2
                    kb.op('pe', lambda e, di=di, h=h, ct=ct, r0=r0, cb=cb: e.matmul(
                        ps[4 + h % 2][:, cb * 128:(cb + 1) * 128], lhsT=kdT[r0:r0 + 64, di * 2 + ct, :],
                        rhs=qdT[r0:r0 + 64, di * 2 + ct, nsl], start=True, stop=True),
                        r=['kdT', ('qdT', n)], w=[('ps', 4 + h % 2)])
            mask4 = cst[:, 4:6, :].unsqueeze(2).broadcast_to([128, 2, 2, 128])
            for rg in range(2):
                kb.op('dve', lambda e, rg=rg: e.tensor_tensor(
                    out=scm[:, :, rg::2, :], in0=ps[4 + rg][:].rearrange("p (a b c) -> p a b c", a=2, b=2),
                    in1=mask4, op=ALU.mult),
                    r=[('ps', 4 + rg), 'cst'], w=[('scm', 0), ('scm', 1)])
            if c.lvl < 7:
                continue
            for h in range(4):
                for di in range(2):
                    kb.op('pe', lambda e, di=di, h=h: e.matmul(
                        ps[6][:, h * 64:(h + 1) * 64], lhsT=scm[:, di, h, :], rhs=vb[:, h * 64:(h + 1) * 64],
                        start=(di == 0), stop=(di == 1)),
                        r=[('scm', 0), ('scm', 1), 'vb'], w=[('ps', 6)])
            kb.op('act', lambda e: e.copy(out=oacc[:, n, :], in_=ps[6][:, 0:256]), r=[('ps', 6)], w=[('oacc', n)])
            if c.lvl < 8:
                continue
            for di in range(2):
                for hp in range(2):
                    i4 = di * 2 + hp
                    kb.op('pe', lambda e, di=di, hp=hp, i4=i4: e.matmul(
                        ps[7][:, i4 * 128:(i4 + 1) * 128], lhsT=kk[:, di, hp * 128:(hp + 1) * 128],
                        rhs=vb[:, hp * 128:(hp + 1) * 128], start=True, stop=True),
                        r=['kk', 'vb'], w=[('ps', 7)])
            p7 = ps[7][:].rearrange("p (a b) -> p a b", a=4)
            kb.op('act', lambda e: e.copy(out=kv[0:64, n, :, :], in_=p7[0:64, :, 0:64]), r=[('ps', 7)], w=[('kv', n, 0)])
            kb.op('dve', lambda e: e.tensor_copy(out=kv[64:128, n, :, :], in_=p7[64:128, :, 64:128]),
                  r=[('ps', 7)], w=[('kv', n, 1)])
        if 'gla_stop1' in c.dbg:
            kb.barrier()
            return
        kb.op('dve', lambda e: e.memset(S[:], 0.0), w=['S0', 'S1'])
        kb.op('dve', lambda e: e.memset(Sb[:], 0.0), w=['Sb0', 'Sb1'])
        for idx in range(NT):
            for di in range(2):
                n = idx if di == 0 else NT - 1 - idx
                nsl = slice(n * 128, (n + 1) * 128)
                if idx > 0:
                    for h in range(4):
                        hp, r0 = h // 2, (h % 2) * 64
                        bk = di * 2 + h % 2
                        kb.op('pe', lambda e, h=h, hp=hp, r0=r0, bk=bk: e.matmul(
                            ps[bk][:, hp * 64:(hp + 1) * 64], lhsT=qdT[r0:r0 + 64, di * 2 + hp, nsl],
                            rhs=Sb[r0:r0 + 64, di * 2 + hp, :], start=True, stop=True),
                            r=[('qdT', n), 'Sb%d' % di], w=[('ps', bk)])
                    for rg in range(2):
                        bk = di * 2 + rg
                        ov = oacc[:, n, :].rearrange("p (a b c) -> p a b c", a=2, b=2)[:, :, rg, :]
                        kb.op('dve', lambda e, bk=bk, ov=ov: e.tensor_tensor(
                            out=ov, in0=ov, in1=ps[bk][:, 0:128].rearrange("p (a c) -> p a c", a=2), op=ALU.add),
                            r=[('ps', bk), ('oacc', n)], w=[('oacc', n)])
                if idx < NT - 1:
                    for hp in range(2):
                        i4 = di * 2 + hp
                        kb.op('dve', lambda e, i4=i4: e.scalar_tensor_tensor(
                            out=S[:, i4, :], in0=S[:, i4, :], scalar=dec[:, n, i4:i4 + 1], in1=kv[:, n, i4, :],
                            op0=ALU.mult, op1=ALU.add),
                            r=['S%d' % di, 'dec', ('kv', n, 0), ('kv', n, 1)], w=['S%d' % di])
                    kb.op('act', lambda e: e.copy(out=Sb[:, di * 2:di * 2 + 2, :], in_=S[:, di * 2:di * 2 + 2, :]),
                          r=['S%d' % di], w=['Sb%d' % di])
        allo = [('oacc', n) for n in range(NT)]
        sqv = kv[:].rearrange("p n a b -> p (n a b)")
        oflat = oacc[:].rearrange("p n c -> p (n c)")
        kb.op('act', lambda e: e.activation(out=sqv, in_=oflat, func=AF.Square), r=allo + [('kv', n, j) for n in range(NT) for j in range(2)],
              w=['sqv'])
        kb.op('dve', lambda e: e.tensor_reduce(out=ss[:], in_=sqv.rearrange("p (a b) -> p a b", b=64), axis=AX.X, op=ALU.add),
              r=['sqv'], w=['ss'])
        kb.op('act', lambda e: e.activation(out=ss[:], in_=ss[:], func=AF.Sqrt, scale=1.0 / 64, bias=EPS), r=['ss'], w=['ss'])
        kb.op('dve', lambda e: e.reciprocal(out=ss[:], in_=ss[:]), r=['ss'], w=['ss'])
        o3 = oflat.rearrange("p (a b) -> p a b", b=64)
        kb.op('dve', lambda e: e.tensor_tensor(out=o3, in0=o3, in1=_bc_last(ss[:], 64), op=ALU.mult), r=allo + ['ss'], w=allo)
        kb.op('dve', lambda e: e.tensor_tensor(out=o3, in0=o3, in1=_bc_mid(ng[:], NT * 4), op=ALU.mult), r=allo + ['ng'], w=allo)
        sflat = sog[:].rearrange("p n c -> p (n c)")
        kb.op('dve', lambda e: e.tensor_tensor(out=sflat, in0=oflat, in1=sflat, op=ALU.mult), r=allo + ['sog'], w=['sog'])
        for n4 in range(NT // 4):
            pb = ps[2 + n4 % 2]
            pbb = pb[:].bitcast(BF16)
            for j in range(4):
                n = n4 * 4 + j
                for ct in range(2):
                    kb.op('pe', lambda e, n=n, ct=ct, j=j: e.transpose(
                        out=pbb[:, (ct * 4 + j) * 128:(ct * 4 + j + 1) * 128], in_=sog[:, n, ct * 128:(ct + 1) * 128],
                        identity=ident_b), r=['sog', 'cst'], w=[('ps', 2 + n4 % 2)])
            kb.op('act',
                  (lambda e: e.copy(out=ygT[:, :, n4 * 512:(n4 + 1) * 512], in_=pbb.rearrange("p (a b) -> p a b", a=2))),
                  r=[('ps', 2 + n4 % 2)], w=[('ygT', n4)])
        for ct in range(2):
            kb.dma('sp', c.yT[768 + ct * 128:768 + (ct + 1) * 128, :], ygT[:, ct, :],
                   r=[('ygT', n4) for n4 in range(NT // 4)], w=[('yT', 6 + ct)])
        if 'ygla' in c.dbg:
            kb.barrier()
            with nc.sbuf_tensor(_nm('dbgt2'), [128, T], F32) as dbgt:
                for ct in range(2):
                    kb.dma('pool', dbgt[:], c.yT[768 + ct * 128:768 + (ct + 1) * 128, :], w=['dbgt'])
                    kb.dma('sp', c.dbg['ygla'][ct * 128:(ct + 1) * 128, :], dbgt[:], r=['dbgt'], w=[('dbgo', ct)])
                kb.barrier()
        kb.barrier()


TWO_PI = 2.0 * math.pi
CW1 = 6.28125
CW2 = TWO_PI - CW1


def prep_s5(inp, l, o):
    f = lambda k: np.asarray(inp[k], np.float32)
    lr, li, ldt = f('s5_lam_re')[l], f('s5_lam_im')[l], f('s5_log_dt')[l]
    par = np.zeros((2, 3, 1024), np.float32)
    par[:, 0] = lr.reshape(2, 1024)
    par[:, 1] = li.reshape(2, 1024)
    par[:, 2] = np.repeat(ldt, 64, axis=1)
    o['s5_par_tm'] = np.ascontiguousarray(np.broadcast_to(par[None], (128, 2, 3, 1024)))
    o['s5_par_sm'] = np.ascontiguousarray(par.reshape(2, 3, 8, 128).transpose(3, 0, 1, 2))
    bre, bim = f('s5_b_re')[l], f('s5_b_im')[l]
    bT = np.zeros((128, 2, 2, 2, 512), np.float32)
    cre, cim = f('s5_c_re')[l], f('s5_c_im')[l]
    cT = np.zeros((128, 2, 8, 2, 128), np.float32)
    for di in range(2):
        for g in range(16):
            k, gl = g // 8, g % 8
            bT[gl * 16:(gl + 1) * 16, di, k, 0, gl * 64:(gl + 1) * 64] = bre[di, g].T
            bT[gl * 16:(gl + 1) * 16, di, k, 1, gl * 64:(gl + 1) * 64] = bim[di, g].T
            st, g2 = g // 2, g % 2
            cT[g2 * 64:(g2 + 1) * 64, di, st, 0, gl * 16:(gl + 1) * 16] = cre[di, g].T
            cT[g2 * 64:(g2 + 1) * 64, di, st, 1, gl * 16:(gl + 1) * 16] = cim[di, g].T
    o['s5_bT'] = bT
    o['s5_cT'] = cT
    o['s5_d'] = _pcol(f('s5_d')[l], 2)
    o['s5_wglu'] = np.ascontiguousarray(f('s5_w_glu')[l].reshape(2, 128, 256).transpose(1, 0, 2))
    kc = np.zeros((128, 2 + 256), np.float32)
    s = np.arange(128, dtype=np.float32)
    kc[:, 0] = s + 1
    kc[:, 1] = 128 - s
    kc[:, 2:130] = (s + 1)[None, :]
    kc[:, 130:258] = (128 - s)[None, :]
    o['s5_kc'] = kc


def _trig(kb, ang, n_t, ni_t, cos_t, sin_t, key):
    D_ = lambda fn, r, w: kb.op('dve', fn, r=r, w=w)
    D_(lambda e: e.tensor_scalar(out=n_t, in0=ang, scalar1=1.0 / TWO_PI, scalar2=None, op0=ALU.mult), [key], [key + 'n'])
    D_(lambda e: e.tensor_copy(out=ni_t, in_=n_t), [key + 'n'], [key + 'ni'])
    D_(lambda e: e.tensor_copy(out=n_t, in_=ni_t), [key + 'ni'], [key + 'n'])
    for (dst, shift) in [(sin_t, 0.0), (cos_t, math.pi / 2)]:
        D_(lambda e: e.scalar_tensor_tensor(out=dst, in0=n_t, scalar=-CW1, in1=ang, op0=ALU.mult, op1=ALU.add),
           [key, key + 'n'], [key + 'o'])
        D_(lambda e: e.scalar_tensor_tensor(out=dst, in0=n_t, scalar=-CW2, in1=dst, op0=ALU.mult, op1=ALU.add),
           [key + 'n', key + 'o'], [key + 'o'])
        if shift != 0.0:
            D_(lambda e: e.tensor_scalar(out=dst, in0=dst, scalar1=shift, scalar2=None, op0=ALU.add), [key + 'o'], [key + 'o'])
        for _ in range(2):
            D_(lambda e: e.tensor_scalar(out=ni_t.bitcast(F32), in0=dst, scalar1=math.pi, scalar2=-TWO_PI, op0=ALU.is_gt, op1=ALU.mult),
               [key + 'o'], [key + 'ni'])
            D_(lambda e: e.tensor_tensor(out=dst, in0=dst, in1=ni_t.bitcast(F32), op=ALU.add), [key + 'o', key + 'ni'], [key + 'o'])
        for _ in range(2):
            D_(lambda e: e.tensor_scalar(out=ni_t.bitcast(F32), in0=dst, scalar1=-math.pi, scalar2=TWO_PI, op0=ALU.is_lt, op1=ALU.mult),
               [key + 'o'], [key + 'ni'])
            D_(lambda e: e.tensor_tensor(out=dst, in0=dst, in1=ni_t.bitcast(F32), op=ALU.add), [key + 'o', key + 'ni'], [key + 'o'])
        D_(lambda e: e.tensor_scalar(out=dst, in0=dst, scalar1=math.pi, scalar2=-math.pi, op0=ALU.min, op1=ALU.max), [key + 'o'], [key + 'o'])
        kb.op('act', lambda e: e.activation(out=dst, in_=dst, func=AF.Sin), r=[key + 'o'], w=[key + 'o'])


def stage_s5(c, l):
    nc, kb, T, NTB, NT = c.nc, c.kb, c.T, c.NTB, c.NT
    W = c.lw[l]
    cst, cstb = c.cst, c.cstb
    ps = c.ps
    with ExitStack() as es:
        A = lambda name, shp, dt: es.enter_context(nc.sbuf_tensor(_nm(name), shp, dt))
        yacc = A('s_yacc', [128, 2, T], F32)
        uTb = A('s_uTb', [128, 2, T], BF16)
        Nre = A('s_Nre', [128, 2, 1024], F32)
        Nim = A('s_Nim', [128, 2, 1024], F32)
        Tre = A('s_Tre', [128, 2, 8, 128], F32)
        Tim = A('s_Tim', [128, 2, 8, 128], F32)
        BbT = A('s_BbT', [128, 2, 2, 2, 512], BF16)
        CT = A('s_CT', [128, 2, 8, 2, 128], BF16)
        sd = A('s_d', [128, 2], F32)
        kc = A('s_kc', [128, 258], F32)
        nkc = A('s_nkc', [128, 2], F32)
        psm = A('s_psm', [128, 2, 3, 8], F32)
        x1sm = A('s_x1sm', [128, 2, 8], F32)
        thsm = A('s_thsm', [128, 2, 8], F32)
        dtsm = A('s_dtsm', [128, 2, 8], F32)
        kb.dma('sp', sd[:], W['s5_d'][:, :], w=['sd'])
        kb.dma('sp', kc[:], W['s5_kc'][:, :], w=['kc'])
        kb.dma('sp', psm[:], W['s5_par_sm'][:, :, :, :], w=['psm'])
        kb.dma('pool', CT[:], W['s5_cT'][:, :, :, :, :], w=['CT'])
        kb.op('dve', lambda e: e.tensor_scalar(out=CT[:, :, :, 1, :], in0=CT[:, :, :, 1, :], scalar1=-1.0, scalar2=None, op0=ALU.mult),
              r=['CT'], w=['CT'])
        kb.op('dve', lambda e: e.tensor_scalar(out=nkc[:], in0=kc[:, 0:2], scalar1=-1.0, scalar2=None, op0=ALU.mult), r=['kc'], w=['nkc'])
        for k in range(2):
            kb.dma('sp', yacc[:, k, :], c.pmixT[k * 128:(k + 1) * 128, :], w=[('yacc', k)])
            kb.op('act', lambda e, k=k: e.copy(out=uTb[:, k, :], in_=yacc[:, k, :]), r=[('yacc', k)], w=[('uTb', k)])
            kb.op('dve', lambda e, k=k: e.tensor_scalar(out=yacc[:, k, :], in0=yacc[:, k, :], scalar1=sd[:, k:k + 1], scalar2=None,
                                                        op0=ALU.mult), r=[('yacc', k), 'sd', ('uTb', k)], w=[('yacc', k)])
        with ExitStack() as es2:
            B_ = lambda name, shp, dt: es2.enter_context(nc.sbuf_tensor(_nm(name), shp, dt))
            ptm = B_('s_ptm', [128, 3, 1024], F32)
            x1 = B_('s_x1', [128, 1024], F32)
            th = B_('s_th', [128, 1024], F32)
            t_n = B_('s_tn', [128, 2048], F32)
            t_ni = B_('s_tni', [128, 2048], I32)
            t_c = B_('s_tc', [128, 2048], F32)
            t_s = B_('s_ts', [128, 2048], F32)
            t_a = B_('s_ta', [128, 2048], F32)
            kR = B_('s_kR', [128, 1024], F32)
            kI = B_('s_kI', [128, 1024], F32)
            BT = B_('s_BT', [128, 2, 2, 512], F32)
            tb1 = B_('s_tb1', [128, 1024], F32)
            tb2 = B_('s_tb2', [128, 1024], F32)
            Dv = lambda fn, r, w: kb.op('dve', fn, r=r, w=w)
            for di in range(2):
                kb.dma('sp', ptm[:], W['s5_par_tm'][:, di, :, :], w=['ptm'])
                kb.dma('sp', BT[:], W['s5_bT'][:, di, :, :, :], w=['BT'])
                lr_, li_, ldt_ = ptm[:, 0, :], ptm[:, 1, :], ptm[:, 2, :]
                Dv(lambda e: e.tensor_scalar(out=lr_, in0=lr_, scalar1=-1e-4, scalar2=None, op0=ALU.min), ['ptm'], ['ptm'])
                kb.op('act', lambda e: e.activation(out=ldt_, in_=ldt_, func=AF.Exp), r=['ptm'], w=['ptm'])
                Dv(lambda e: e.tensor_tensor(out=x1[:], in0=lr_, in1=ldt_, op=ALU.mult), ['ptm'], ['x1'])
                Dv(lambda e: e.tensor_tensor(out=th[:], in0=li_, in1=ldt_, op=ALU.mult), ['ptm'], ['th'])
                Dv(lambda e: e.tensor_copy(out=t_a[:, 0:1024], in_=th[:]), ['th'], ['tg'])
                _trig(kb, t_a[:, 0:1024], t_n[:, 0:1024], t_ni[:, 0:1024], t_c[:, 0:1024], t_s[:, 0:1024], 'tg')
                kb.op('act', lambda e: e.activation(out=tb1[:], in_=x1[:], func=AF.Exp), r=['x1'], w=['tb1'])
                aR, aI = t_c[:, 0:1024], t_s[:, 0:1024]
                Dv(lambda e: e.tensor_tensor(out=aR, in0=aR, in1=tb1[:], op=ALU.mult), ['tgo', 'tb1'], ['tgo'])
                Dv(lambda e: e.tensor_tensor(out=aI, in0=aI, in1=tb1[:], op=ALU.mult), ['tgo', 'tb1'], ['tgo'])
                Dv(lambda e: e.tensor_tensor(out=tb1[:], in0=lr_, in1=lr_, op=ALU.mult), ['ptm', 'tgo'], ['tb1'])
                Dv(lambda e: e.tensor_tensor(out=tb2[:], in0=li_, in1=li_, op=ALU.mult), ['ptm'], ['tb2'])
                Dv(lambda e: e.tensor_tensor(out=tb1[:], in0=tb1[:], in1=tb2[:], op=ALU.add), ['tb1', 'tb2'], ['tb1'])
                Dv(lambda e: e.reciprocal(out=tb1[:], in_=tb1[:]), ['tb1'], ['tb1'])
                Dv(lambda e: e.tensor_scalar(out=aR, in0=aR, scalar1=-1.0, scalar2=None, op0=ALU.add), ['tgo'], ['tgo'])
                Dv(lambda e: e.tensor_tensor(out=kR[:], in0=aR, in1=lr_, op=ALU.mult), ['tgo', 'ptm'], ['kR'])
                Dv(lambda e: e.tensor_tensor(out=tb2[:], in0=aI, in1=li_, op=ALU.mult), ['tgo', 'ptm'], ['tb2'])
                Dv(lambda e: e.tensor_tensor(out=kR[:], in0=kR[:], in1=tb2[:], op=ALU.add), ['kR', 'tb2'], ['kR'])
                Dv(lambda e: e.tensor_tensor(out=kR[:], in0=kR[:], in1=tb1[:], op=ALU.mult), ['kR', 'tb1'], ['kR'])
                Dv(lambda e: e.tensor_tensor(out=kI[:], in0=aI, in1=lr_, op=ALU.mult), ['tgo', 'ptm'], ['kI'])
                Dv(lambda e: e.tensor_tensor(out=tb2[:], in0=aR, in1=li_, op=ALU.mult), ['tgo', 'ptm', 'kR'], ['tb2'])
                Dv(lambda e: e.tensor_tensor(out=kI[:], in0=kI[:], in1=tb2[:], op=ALU.subtract), ['kI', 'tb2'], ['kI'])
                Dv(lambda e: e.tensor_tensor(out=kI[:], in0=kI[:], in1=tb1[:], op=ALU.mult), ['kI', 'tb1'], ['kI'])
                kRv = kR[:].rearrange("p (k s) -> p k s", k=2)
                kIv = kI[:].rearrange("p (k s) -> p k s", k=2)
                t1v = tb1[:].rearrange("p (k s) -> p k s", k=2)
                t2v = tb2[:].rearrange("p (k s) -> p k s", k=2)
                Dv(lambda e: e.tensor_tensor(out=t1v, in0=kRv, in1=BT[:, :, 0, :], op=ALU.mult), ['kR', 'BT', 'kI'], ['tb1'])
                Dv(lambda e: e.tensor_tensor(out=t2v, in0=kIv, in1=BT[:, :, 1, :], op=ALU.mult), ['kI', 'BT'], ['tb2'])
                Dv(lambda e: e.tensor_tensor(out=BbT[:, di, :, 0, :], in0=t1v, in1=t2v, op=ALU.subtract), ['tb1', 'tb2'], ['BbT'])
                Dv(lambda e: e.tensor_tensor(out=t1v, in0=kRv, in1=BT[:, :, 1, :], op=ALU.mult), ['kR', 'BT', 'BbT'], ['tb1'])
                Dv(lambda e: e.tensor_tensor(out=t2v, in0=kIv, in1=BT[:, :, 0, :], op=ALU.mult), ['kI', 'BT', 'BbT'], ['tb2'])
                Dv(lambda e: e.tensor_tensor(out=BbT[:, di, :, 1, :], in0=t1v, in1=t2v, op=ALU.add), ['tb1', 'tb2'], ['BbT'])
                Dv(lambda e: e.tensor_scalar(out=t_a[:, 0:1024], in0=th[:], scalar1=kc[:, di:di + 1], scalar2=None, op0=ALU.mult),
                   ['th', 'kc', 'tgo', 'tg'], ['tg'])
                _trig(kb, t_a[:, 0:1024], t_n[:, 0:1024], t_ni[:, 0:1024], t_c[:, 0:1024], t_s[:, 0:1024], 'tg')
                kb.op('act', lambda e: e.activation(out=tb1[:], in_=x1[:], func=AF.Exp, scale=nkc[:, di:di + 1]),
                      r=['x1', 'nkc', 'BbT'], w=['tb1'])
                Dv(lambda e: e.tensor_tensor(out=Nre[:, di, :], in0=tb1[:], in1=t_c[:, 0:1024], op=ALU.mult), ['tb1', 'tgo'], ['Nre'])
                Dv(lambda e: e.scalar_tensor_tensor(out=Nim[:, di, :], in0=tb1[:], scalar=-1.0, in1=t_s[:, 0:1024],
                                                    op0=ALU.mult, op1=ALU.mult), ['tb1', 'tgo'], ['Nim'])
            Dv(lambda e: e.tensor_scalar(out=psm[:, :, 0, :], in0=psm[:, :, 0, :], scalar1=-1e-4, scalar2=None, op0=ALU.min), ['psm'], ['psm'])
            kb.op('act', lambda e: e.activation(out=dtsm[:], in_=psm[:, :, 2, :], func=AF.Exp), r=['psm'], w=['dtsm'])
            Dv(lambda e: e.tensor_tensor(out=x1sm[:], in0=psm[:, :, 0, :], in1=dtsm[:], op=ALU.mult), ['psm', 'dtsm'], ['x1sm'])
            Dv(lambda e: e.tensor_tensor(out=thsm[:], in0=psm[:, :, 1, :], in1=dtsm[:], op=ALU.mult), ['psm', 'dtsm'], ['thsm'])
            magT = tb1[:].rearrange("p (a b) -> p a b", a=8)
            for di in range(2):
                krow = kc[:, 2 + di * 128:2 + (di + 1) * 128]
                for st in range(8):
                    Dv(lambda e, st=st: e.tensor_scalar(out=t_a[:, (di * 8 + st) * 128:(di * 8 + st + 1) * 128], in0=krow,
                                                        scalar1=thsm[:, di, st:st + 1], scalar2=None, op0=ALU.mult),
                       ['kc', 'thsm', 'tg', 'tgo'], ['tg'])
            _trig(kb, t_a[:], t_n[:], t_ni[:], t_c[:], t_s[:], 'tg')
            for di in range(2):
                krow = kc[:, 2 + di * 128:2 + (di + 1) * 128]
                for st in range(8):
                    kb.op('act', lambda e, st=st: e.activation(out=magT[:, st, :], in_=krow, func=AF.Exp, scale=x1sm[:, di, st:st + 1]),
                          r=['kc', 'x1sm', 'Nre', 'Nim'], w=['tb1'])
                Dv(lambda e: e.tensor_tensor(out=Tre[:, di], in0=magT, in1=t_c[:, di * 1024:(di + 1) * 1024].rearrange("p (a b) -> p a b", a=8),
                                             op=ALU.mult), ['tb1', 'tgo'], ['Tre'])
                Dv(lambda e: e.tensor_tensor(out=Tim[:, di], in0=magT, in1=t_s[:, di * 1024:(di + 1) * 1024].rearrange("p (a b) -> p a b", a=8),
                                             op=ALU.mult), ['tb1', 'tgo'], ['Tim'])
            kb.barrier()
        with ExitStack() as es3:
            B_ = lambda name, shp, dt: es3.enter_context(nc.sbuf_tensor(_nm(name), shp, dt))
            tt = B_('s_tt', [128, 2, 4, 512], F32)
            Wc = B_('s_W', [128, 2, 2, 2, 2, 512], BF16)
            mm = B_('s_mm', [128, 2, 4, 4, 128], F32)
            xs = B_('s_xs', [128, 2, 2, 8, 128], F32)
            xb = B_('s_xb', [128, 2, 2, 8, 128], BF16)

            def chunk_of(idx, di):
                return idx if di == 0 else NT - 1 - idx

            def step_A(idx, di, k):
                n = chunk_of(idx, di)
                nsl = slice(n * 128, (n + 1) * 128)
                base = di * 4
                par = idx % 2
                ksl = slice(k * 512, (k + 1) * 512)
                for ri in range(2):
                    kb.op('pe', lambda e, ri=ri: e.matmul(ps[base + ri][:], lhsT=uTb[:, k, nsl], rhs=BbT[:, di, k, ri, :],
                                                          start=True, stop=True),
                          r=[('uTb', k), 'BbT'], w=[('ps', base + ri)])
                pr, pi_ = ps[base], ps[base + 1]
                for slot, (pp, tab, bkk, tkey) in enumerate([(pr, Nre, base, 'Nre'), (pi_, Nim, base + 1, 'Nim'),
                                                            (pi_, Nre, base + 1, 'Nre'), (pr, Nim, base, 'Nim')]):
                    kb.op('dve', lambda e, slot=slot, pp=pp, tab=tab: e.tensor_tensor(
                        out=tt[:, di, slot, :], in0=pp[:], in1=tab[:, di, ksl], op=ALU.mult),
                        r=[('ps', bkk), tkey], w=[('tt', di, slot)])
                kb.op('pool', lambda e: e.tensor_tensor(out=Wc[:, par, di, k, 0, :], in0=tt[:, di, 0, :], in1=tt[:, di, 1, :], op=ALU.subtract),
                      r=[('tt', di, 0), ('tt', di, 1)], w=[('W', par, di, k, 0)])
                kb.op('pool', lambda e: e.tensor_tensor(out=Wc[:, par, di, k, 1, :], in0=tt[:, di, 2, :], in1=tt[:, di, 3, :], op=ALU.add),
                      r=[('tt', di, 2), ('tt', di, 3)], w=[('W', par, di, k, 1)])

            def step_B(idx, di, k):
                base = di * 4
                par = idx % 2
                tri = cstb[:, 8 + di, :]
                ccol = 127 if di == 0 else 0
                for ri in range(2):
                    for s4 in range(4):
                        kb.op('pe', lambda e, ri=ri, s4=s4: e.matmul(
                            ps[base + 2 + ri][:, s4 * 128:(s4 + 1) * 128], lhsT=Wc[:, par, di, k, ri, s4 * 128:(s4 + 1) * 128], rhs=tri,
                            start=True, stop=True), r=[('W', par, di, k, ri), 'cst'], w=[('ps', base + 2 + ri)])
                for s4 in range(4):
                    st = k * 4 + s4
                    pre = ps[base + 2][:, s4 * 128:(s4 + 1) * 128]
                    pim = ps[base + 3][:, s4 * 128:(s4 + 1) * 128]
                    if idx == 0:
                        cr, ci = 0.0, 0.0
                    else:
                        cr = xs[:, di, 0, st, ccol:ccol + 1]
                        ci = xs[:, di, 1, st, ccol:ccol + 1]
                    rk = [('ps', base + 2), ('ps', base + 3), 'Tre', 'Tim', ('xs', di, k)]
                    for slot, (pp, cc_, tab) in enumerate([(pre, cr, Tre), (pim, ci, Tim), (pim, ci, Tre), (pre, cr, Tim)]):
                        kb.op('dve', lambda e, slot=slot, pp=pp, cc_=cc_, tab=tab, s4=s4, st=st: e.scalar_tensor_tensor(
                            out=mm[:, di, slot, s4, :], in0=pp, scalar=cc_, in1=tab[:, di, st, :], op0=ALU.add, op1=ALU.mult),
                            r=rk, w=[('mm', di, slot, s4)])
                kb.op('pool', lambda e: e.tensor_tensor(out=xs[:, di, 0, k * 4:(k + 1) * 4, :], in0=mm[:, di, 0], in1=mm[:, di, 1], op=ALU.subtract),
                      r=[('mm', di, 0, s4) for s4 in range(4)] + [('mm', di, 1, s4) for s4 in range(4)], w=[('xs', di, k)])
                kb.op('pool', lambda e: e.tensor_tensor(out=xs[:, di, 1, k * 4:(k + 1) * 4, :], in0=mm[:, di, 2], in1=mm[:, di, 3], op=ALU.add),
                      r=[('mm', di, 2, s4) for s4 in range(4)] + [('mm', di, 3, s4) for s4 in range(4)], w=[('xs', di, k)])
                kb.op('act', lambda e: e.copy(out=xb[:, di, :, k * 4:(k + 1) * 4, :], in_=xs[:, di, :, k * 4:(k + 1) * 4, :]),
                      r=[('xs', di, k)], w=[('xb', di, k)])

            def step_C(idx, di):
                n = chunk_of(idx, di)
                nsl = slice(n * 128, (n + 1) * 128)
                base = di * 4
                for k in range(2):
                    cnt = 0
                    for st in range(k * 4, k * 4 + 4):
                        for ri in range(2):
                            kb.op('pe', lambda e, k=k, st=st, ri=ri, cnt=cnt: e.matmul(
                                ps[base][:, k * 128:(k + 1) * 128], lhsT=CT[:, di, st, ri, :], rhs=xb[:, di, ri, st, :],
                                start=(cnt == 0), stop=(cnt == 7)), r=['CT', ('xb', di, k)], w=[('ps', base)])
                            cnt += 1
                kb.op('dve', lambda e: e.tensor_tensor(out=yacc[:, :, nsl], in0=yacc[:, :, nsl],
                                                       in1=ps[base][:, 0:256].rearrange("p (a b) -> p a b", a=2), op=ALU.add),
                      r=[('ps', base), ('yacc', n)], w=[('yacc', n)])

            for k in range(2):
                for di in range(2):
                    step_A(0, di, k)
            for idx in range(NT):
                if idx + 1 < NT:
                    for k in range(2):
                        for di in range(2):
                            step_A(idx + 1, di, k)
                for k in range(2):
                    for di in range(2):
                        step_B(idx, di, k)
                for di in range(2):
                    step_C(idx, di)
            kb.barrier()
        with ExitStack() as es3:
            B_ = lambda name, shp, dt: es3.enter_context(nc.sbuf_tensor(_nm(name), shp, dt))
            z = B_('s_z', [128, 2, T], F32)
            yst = B_('s_yst', [128, 2, 512], BF16)
            yf = yacc[:].rearrange("p k t -> p (k t)")
            zf = z[:].rearrange("p k t -> p (k t)")
            GC = 0.7978845608028654
            kb.op('act', lambda e: e.activation(out=zf, in_=yf, func=AF.Square), r=[('yacc', 0), ('yacc', 1)] + [('yacc', n) for n in range(NT)], w=['z'])
            kb.op('dve', lambda e: e.tensor_scalar(out=zf, in0=zf, scalar1=0.044715, scalar2=1.0, op0=ALU.mult, op1=ALU.add), r=['z'], w=['z'])
            kb.op('dve', lambda e: e.tensor_tensor(out=zf, in0=zf, in1=yf, op=ALU.mult), r=['z', ('yacc', 0), ('yacc', 1)], w=['z'])
            kb.op('act', lambda e: e.activation(out=zf, in_=zf, func=AF.Sigmoid, scale=2.0 * GC), r=['z'], w=['z'])
            kb.op('dve', lambda e: e.tensor_tensor(out=zf, in0=zf, in1=yf, op=ALU.mult), r=['z', ('yacc', 0), ('yacc', 1)], w=['z'])
            kb.op('act', lambda e: e.copy(out=uTb[:].rearrange("p k t -> p (k t)"), in_=zf), r=['z'], w=[('uTb', 0), ('uTb', 1)])
            wgl = B_('s_wgl', [128, 2, 256], BF16)
            kb.dma('pool', wgl[:], W['s5_wglu'][:, :, :], w=['wgl'])
            nps = 0
            for ko in range(2):
                for tb in range(NTB):
                    ts = slice(tb * 512, (tb + 1) * 512)
                    bk = 1 + nps % 4
                    nps += 1
                    for ki in range(2):
                        kb.op('pe', lambda e, ki=ki, bk=bk: e.matmul(ps[bk][:], lhsT=wgl[:, ki, ko * 128:(ko + 1) * 128], rhs=uTb[:, ki, ts],
                                                              start=(ki == 0), stop=(ki == 1)),
                              r=['wgl', ('uTb', 0), ('uTb', 1)], w=[('ps', bk)])
                    kb.op('act', lambda e, bk=bk: e.activation(out=yacc[:, ko, ts], in_=ps[bk][:], func=AF.Sigmoid),
                          r=[('ps', bk)], w=[('yg', ko, tb)])
                    yb_ = nps % 2
                    kb.op('dve', lambda e, yb_=yb_: e.tensor_tensor(out=yst[:, yb_, :], in0=yacc[:, ko, ts], in1=z[:, ko, ts],
                                                           op=ALU.mult), r=[('yg', ko, tb), 'z'], w=[('yst', yb_)])
                    kb.dma('sp', c.yT[ko * 128:(ko + 1) * 128, ts], yst[:, yb_, :], r=[('yst', yb_)], w=[('yT', ko, tb)])
        if 'ys5' in c.dbg:
            kb.barrier()
            with nc.sbuf_tensor(_nm('dbgt3'), [128, T], F32) as dbgt:
                for ct in range(2):
                    kb.dma('pool', dbgt[:], c.yT[ct * 128:(ct + 1) * 128, :], w=['dbgt'])
                    kb.dma('sp', c.dbg['ys5'][ct * 128:(ct + 1) * 128, :], dbgt[:], r=['dbgt'], w=[('dbgo', ct)])
                kb.barrier()
        kb.barrier()


def prep_rest(inp, l, o):
    f = lambda k: np.asarray(inp[k], np.float32)
    o['w_up'] = np.ascontiguousarray(np.concatenate([f('w_up_s5')[l], f('w_up_lru')[l], f('w_up_gla')[l]], axis=0))
    o['w_mix'] = np.ascontiguousarray(f('w_mix_out')[l])
    o['w_router'] = np.ascontiguousarray(f('w_router')[l].reshape(8, 128, 16).transpose(1, 0, 2))
    o['w_eg'] = np.ascontiguousarray(f('w_exp_gate')[l])
    o['w_eu'] = np.ascontiguousarray(f('w_exp_up')[l])
    o['w_ed'] = np.ascontiguousarray(f('w_exp_down')[l])


def make_iota():
    io = np.zeros((128, 513), np.float32)
    io[:, 0] = np.arange(128)
    io[:, 1:] = np.arange(512)[None, :]
    return io


def emit_rmsnorm(c, hb, hkey, gcol, gkey, out_of_k, outkeys, bank, sq, sqkey, rstd, rkey):
    kb = c.kb
    kb.op('act', lambda e: e.activation(out=sq, in_=hb, func=AF.Square), r=[hkey], w=[sqkey])
    pb = c.ps[bank]
    for k in range(KC):
        kb.op('pe', lambda e, k=k: e.matmul(pb[:], lhsT=c.ones_bf[:], rhs=sq[:, k, :], start=(k == 0), stop=(k == KC - 1)),
              r=[sqkey, 'ones_bf'], w=[('ps', bank)])
    kb.op('act', lambda e: e.activation(out=rstd, in_=pb[:], func=AF.Sqrt, scale=1.0 / D, bias=EPS), r=[('ps', bank)], w=[rkey])
    kb.op('dve', lambda e: e.reciprocal(out=rstd, in_=rstd), r=[rkey], w=[rkey])
    for k in range(KC):
        kb.op('dve', lambda e, k=k: e.scalar_tensor_tensor(out=out_of_k(k), in0=hb[:, k, :], scalar=gcol[:, k:k + 1], in1=rstd,
                                                           op0=ALU.mult, op1=ALU.mult),
              r=[hkey, rkey, gkey], w=outkeys)


def emit_add_tm(c, hb, hkey, tb, ftile, banks):
    kb = c.kb
    ident = c.cst[:, 0, :]
    for t4 in range(4):
        tt = tb * 4 + t4
        fb = t4 % 2
        kb.dma('sp', ftile[:, fb, :], c.ffn_tm[tt * 128:(tt + 1) * 128, :], w=[('ftile', fb)])
        for half in range(2):
            bk = banks[half]
            for k4 in range(4):
                k = half * 4 + k4
                kb.op('pe', lambda e, k=k, k4=k4, bk=bk: e.transpose(out=c.ps[bk][:, k4 * 128:(k4 + 1) * 128],
                                                                    in_=ftile[:, fb, k * 128:(k + 1) * 128], identity=ident),
                      r=[('ftile', fb), 'cst'], w=[('ps', bk)])
            hv = hb[:, half * 4:(half + 1) * 4, t4 * 128:(t4 + 1) * 128]
            kb.op('dve', lambda e, hv=hv, bk=bk: e.tensor_tensor(out=hv, in0=hv, in1=c.ps[bk][:].rearrange("p (a b) -> p a b", a=4), op=ALU.add),
                  r=[('ps', bk), hkey], w=[hkey])


def stage_merge(c, l, hnT):
    nc, kb, T, NTB, NT = c.nc, c.kb, c.T, c.NTB, c.NT
    W = c.lw[l]
    ps = c.ps
    res_src = c.xT if l == 0 else c.hT
    with ExitStack() as es:
        A = lambda name, shp, dt: es.enter_context(nc.sbuf_tensor(_nm(name), shp, dt))
        wup = A('m_wup', [128, 8, 1024], BF16)
        wmix = A('m_wmix', [128, 8, 1024], BF16)
        yblk = A('m_yblk', [128, 2, 8, 512], BF16)
        goc = A('m_goc', [128, 2, 3, 512], BF16)
        mrg = A('m_mrg', [128, 8, 512], BF16)
        hblk = A('m_hblk', [128, 2, 8, 512], F32)
        t1 = A('m_t1', [128, 2, 3, 512], F32)
        sq = A('m_sq', [128, 8, 512], BF16)
        rstd = A('m_rstd', [128, 512], F32)
        gff = A('m_gff', [128, 8], F32)
        kb.dma('pool', wup[:], W['w_up'].rearrange("(k p) d -> p k d", p=128), w=['wup'])
        kb.dma('pool', wmix[:], W['w_mix'].rearrange("(k p) d -> p k d", p=128), w=['wmix'])
        kb.dma('sp', gff[:], W['ffn_g'][:, :], w=['gff'])
        yTv = c.yT.rearrange("(k p) t -> p k t", p=128)
        gTv = c.gT.rearrange("(b o p) t -> p b o t", p=128, b=3)
        resv = res_src.rearrange("(k p) t -> p k t", p=128)
        hTv = c.hT.rearrange("(k p) t -> p k t", p=128)
        branch_k = [(0, 2), (2, 6), (6, 8)]
        ng = 0
        for tb in range(NTB):
            b = tb % 2
            ts = slice(tb * 512, (tb + 1) * 512)
            kb.dma('sp', yblk[:, b], yTv[:, :, ts], w=[('yblk', b)])
            kb.dma('sp', hblk[:, b], resv[:, :, ts], w=[('hblk', b)])
            for oc in range(8):
                gb = ng % 2
                ng += 1
                kb.dma('sp', goc[:, gb], gTv[:, :, oc, ts], w=[('goc', gb)])
                for br in range(3):
                    bk = gb * 3 + br
                    k0, k1 = branch_k[br]
                    for k in range(k0, k1):
                        kb.op('pe', lambda e, k=k, bk=bk, k0=k0, k1=k1: e.matmul(
                            ps[bk][:], lhsT=wup[:, k, oc * 128:(oc + 1) * 128], rhs=yblk[:, b, k, :],
                            start=(k == k0), stop=(k == k1 - 1)), r=['wup', ('yblk', b)], w=[('ps', bk)])
                tb_ = oc % 2
                kb.op('dve', lambda e: e.tensor_tensor(out=t1[:, tb_, 0, :], in0=ps[gb * 3][:], in1=goc[:, gb, 0, :], op=ALU.mult),
                      r=[('ps', gb * 3), ('goc', gb)], w=[('t1', tb_, 0)])
                kb.op('dve', lambda e: e.tensor_tensor(out=t1[:, tb_, 1, :], in0=ps[gb * 3 + 1][:], in1=goc[:, gb, 1, :], op=ALU.mult),
                      r=[('ps', gb * 3 + 1), ('goc', gb)], w=[('t1', tb_, 1)])
                kb.op('dve', lambda e: e.tensor_tensor(out=t1[:, tb_, 2, :], in0=ps[gb * 3 + 2][:], in1=goc[:, gb, 2, :], op=ALU.mult),
                      r=[('ps', gb * 3 + 2), ('goc', gb)], w=[('t1', tb_, 2)])
                kb.op('pool', lambda e: e.tensor_tensor(out=t1[:, tb_, 0, :], in0=t1[:, tb_, 0, :], in1=t1[:, tb_, 1, :], op=ALU.add),
                      r=[('t1', tb_, 0), ('t1', tb_, 1)], w=[('t1', tb_, 0)])
                kb.op('pool', lambda e: e.tensor_tensor(out=mrg[:, oc, :], in0=t1[:, tb_, 0, :], in1=t1[:, tb_, 2, :], op=ALU.add),
                      r=[('t1', tb_, 0), ('t1', tb_, 2)], w=[('mrg', oc)])
            for oc in range(8):
                bk = 6 + oc % 2
                for k in range(8):
                    kb.op('pe', lambda e, k=k, bk=bk: e.matmul(ps[bk][:], lhsT=wmix[:, k, oc * 128:(oc + 1) * 128], rhs=mrg[:, k, :],
                                                          start=(k == 0), stop=(k == 7)),
                          r=['wmix'] + [('mrg', kk) for kk in range(8)], w=[('ps', bk)])
                kb.op('dve', lambda e, bk=bk: e.tensor_tensor(out=hblk[:, b, oc, :], in0=hblk[:, b, oc, :], in1=ps[bk][:], op=ALU.add),
                      r=[('ps', bk), ('hblk', b)], w=[('hblk', b)])
            kb.dma('sp', hTv[:, :, ts], hblk[:, b], r=[('hblk', b)], w=[('hT', tb)])
            emit_rmsnorm(c, hblk[:, b], ('hblk', b), gff, 'gff', lambda k: hnT[:, k, ts], [('hnT', tb)], 6, sq[:], 'sq', rstd[:], 'rstd')
        if 'hmid' in c.dbg:
            kb.barrier()
            with nc.sbuf_tensor(_nm('dbgt4'), [128, T], F32) as dbgt:
                for ct in range(8):
                    kb.dma('sp', dbgt[:], c.hT[ct * 128:(ct + 1) * 128, :], w=['dbgt'])
                    kb.dma('sp', c.dbg['hmid'][ct * 128:(ct + 1) * 128, :], dbgt[:], r=['dbgt'], w=[('dbgo', ct)])
                kb.barrier()
        kb.barrier()


def stage_ffn(c, l, hnT):
    nc, kb, T, NTB, NT = c.nc, c.kb, c.T, c.NTB, c.NT
    W = c.lw[l]
    ps = c.ps
    cst, cstb = c.cst, c.cstb
    CAP = 2 * T // NE
    CT_ = CAP // 128
    NJ = NT * NE
    with ExitStack() as es:
        A = lambda name, shp, dt: es.enter_context(nc.sbuf_tensor(_nm(name), shp, dt))
        seli = A('f_seli', [128, NE, CT_], I32)
        selg = A('f_selg', [128, NE, CT_], F32)
        with ExitStack() as es2:
            B_ = lambda name, shp, dt: es2.enter_context(nc.sbuf_tensor(_nm(name), shp, dt))
            wr = B_('f_wr', [128, 8, 16], BF16)
            iota = B_('f_iota', [128, 513], F32)
            probs = B_('f_probs', [128, NT, NE], F32)
            mask = B_('f_mask', [128, NT, NE], F32)
            maskb = B_('f_maskb', [128, NT, NE], BF16)
            gw = B_('f_gw', [128, NT, NE], F32)
            pos = B_('f_pos', [128, NT, NE], F32)
            base = B_('f_base', [128, NT, NE], F32)
            csum = B_('f_csum', [128, NT, NE], F32)
            cmpb = B_('f_cmpb', [128, NT, NE], BF16)
            red = B_('f_red', [128, NT], F32)
            lo = B_('f_lo', [128, NE], F32)
            mid = B_('f_mid', [128, NE], F32)
            cnt = B_('f_cnt', [128, NE], F32)
            R = B_('f_R', [128, NT, NE, 4], BF16)
            ghi_f = B_('f_ghif', [128, NT, NE], F32)
            OH = B_('f_OH', [128, 2, CAP], BF16)
            selsb = B_('f_selsb', [128, NE, CT_, 4], F32)
            stg = B_('f_stg', [128, 2, 1024], BF16)
            zt = B_('f_zt', [128, 1024], F32)
            kb.dma('pool', wr[:], W['w_router'][:, :, :], w=['wr'])
            kb.dma('sp', iota[:], c.iota[:, :], w=['iota'])
            kb.op('dve', lambda e: e.memset(zt[:], 0.0), w=['zt'])
            for tt in range(NT):
                kb.dma('sp', c.ffn_tm[tt * 128:(tt + 1) * 128, :], zt[:], r=['zt'], w=[('ffn_tm', tt)])
            for tt in range(NT):
                sb = tt % 2
                psE = ps[sb][:].bitcast(BF16)
                for k in range(8):
                    kb.op('pe', lambda e, k=k, psE=psE: e.transpose(out=psE[:, k * 128:(k + 1) * 128], in_=hnT[:, k, tt * 128:(tt + 1) * 128],
                                                                   identity=cstb[:, 0, :]),
                          r=[('hnT', tt // 4), 'cst'], w=[('ps', sb)])
                kb.op('act', lambda e, psE=psE: e.copy(out=stg[:, sb, :], in_=psE), r=[('ps', sb)], w=[('stg', sb)])
                kb.dma('sp', c.hn_tm[tt * 128:(tt + 1) * 128, :], stg[:, sb, :], r=[('stg', sb)], w=[('hn_tm', tt)])
            for tt in range(NT):
                for k in range(8):
                    kb.op('pe', lambda e, k=k: e.matmul(ps[2][:, tt * NE:(tt + 1) * NE], lhsT=hnT[:, k, tt * 128:(tt + 1) * 128], rhs=wr[:, k, :],
                                                        start=(k == 0), stop=(k == 7)), r=[('hnT', tt // 4), 'wr'], w=[('ps', 2)])
            lg = ps[2][:, 0:NJ].rearrange("p (j e) -> p j e", e=NE)
            Dv = lambda fn, r, w: kb.op('dve', fn, r=r, w=w)
            Dv(lambda e: e.tensor_reduce(out=red[:], in_=lg, axis=AX.X, op=ALU.max), [('ps', 2)], ['red'])
            Dv(lambda e: e.tensor_tensor(out=probs[:], in0=lg, in1=_bc_last(red[:], NE), op=ALU.subtract), [('ps', 2), 'red'], ['probs'])
            kb.op('act', lambda e: e.activation(out=probs[:], in_=probs[:], func=AF.Exp), r=['probs'], w=['probs'])
            Dv(lambda e: e.tensor_reduce(out=red[:], in_=probs[:], axis=AX.X, op=ALU.add), ['probs'], ['red'])
            Dv(lambda e: e.reciprocal(out=red[:], in_=red[:]), ['red'], ['red'])
            Dv(lambda e: e.tensor_tensor(out=probs[:], in0=probs[:], in1=_bc_last(red[:], NE), op=ALU.mult), ['probs', 'red'], ['probs'])
            Dv(lambda e: e.memset(lo[:], 0.0), [], ['lo'])
            pflat = probs[:].rearrange("p j e -> p (j e)")
            for it in range(1, 33):
                wstep = 2.0 ** (-it)
                Dv(lambda e: e.tensor_scalar(out=mid[:], in0=lo[:], scalar1=wstep, scalar2=None, op0=ALU.add), ['lo'], ['mid'])
                Dv(lambda e: e.tensor_tensor(out=cmpb[:], in0=probs[:], in1=_bc_mid(mid[:], NT), op=ALU.is_ge), ['probs', 'mid'], ['cmpb'])
                kb.op('pe', lambda e: e.matmul(ps[3][:, 0:NJ], lhsT=c.ones_bf[:], rhs=cmpb[:].rearrange("p j e -> p (j e)"), start=True, stop=True),
                      r=['cmpb', 'ones_bf'], w=[('ps', 3)])
                Dv(lambda e: e.tensor_reduce(out=cnt[:], in_=ps[3][:, 0:NJ].rearrange("p (j e) -> p e j", e=NE), axis=AX.X, op=ALU.add),
                   [('ps', 3)], ['cnt'])
                Dv(lambda e: e.tensor_scalar(out=cnt[:], in0=cnt[:], scalar1=float(CAP) - 0.5, scalar2=wstep, op0=ALU.is_ge, op1=ALU.mult),
                   ['cnt'], ['cnt'])
                Dv(lambda e: e.tensor_tensor(out=lo[:], in0=lo[:], in1=cnt[:], op=ALU.add), ['lo', 'cnt'], ['lo'])
            Dv(lambda e: e.tensor_tensor(out=mask[:], in0=probs[:], in1=_bc_mid(lo[:], NT), op=ALU.is_ge), ['probs', 'lo'], ['mask'])
            Dv(lambda e: e.tensor_copy(out=maskb[:], in_=mask[:]), ['mask'], ['maskb'])
            Dv(lambda e: e.tensor_tensor(out=gw[:], in0=probs[:], in1=mask[:], op=ALU.mult), ['probs', 'mask'], ['gw'])
            mbf = maskb[:].rearrange("p j e -> p (j e)")
            kb.op('pe', lambda e: e.matmul(ps[3][:, 0:NJ], lhsT=c.ones_bf[:], rhs=mbf, start=True, stop=True), r=['maskb', 'ones_bf'], w=[('ps', 3)])
            Dv(lambda e: e.tensor_copy(out=csum[:].rearrange("p j e -> p (j e)"), in_=ps[3][:, 0:NJ]), [('ps', 3)], ['csum'])
            Dv(lambda e: e.memset(base[:, 0, :], 0.0), [], ['base'])
            for j in range(1, NT):
                Dv(lambda e, j=j: e.tensor_tensor(out=base[:, j, :], in0=base[:, j - 1, :], in1=csum[:, j - 1, :], op=ALU.add), ['base', 'csum'], ['base'])
            for j in range(NT):
                kb.op('pe', lambda e, j=j: e.matmul(ps[3][:, j * NE:(j + 1) * NE], lhsT=cstb[:, 6, :], rhs=maskb[:, j, :], start=True, stop=True),
                      r=['maskb', 'cst'], w=[('ps', 3)])
            Dv(lambda e: e.tensor_tensor(out=pos[:].rearrange("p j e -> p (j e)"), in0=base[:].rearrange("p j e -> p (j e)"), in1=ps[3][:, 0:NJ],
                                         op=ALU.add), [('ps', 3), 'base'], ['pos'])
            pidx = bass.AP(iota[:].tensor, iota[:, 0:1].offset, [[iota[:].ap[0][0], 128], [0, NT], [0, NE]])
            Dv(lambda e: e.tensor_copy(out=R[:, :, :, 0], in_=pidx), ['iota'], ['R'])
            for j in range(NT):
                Dv(lambda e, j=j: e.memset(R[:, j, :, 1], float(j)), [], ['R'])
            Dv(lambda e: e.tensor_copy(out=R[:, :, :, 2], in_=gw[:]), ['gw'], ['R'])
            Dv(lambda e: e.tensor_copy(out=ghi_f[:], in_=R[:, :, :, 2]), ['R'], ['ghi_f'])
            Dv(lambda e: e.tensor_tensor(out=R[:, :, :, 3], in0=gw[:], in1=ghi_f[:], op=ALU.subtract), ['gw', 'ghi_f'], ['R'])
            noh = 0
            for e_ in range(NE):
                for j in range(NT):
                    ob = noh % 2
                    noh += 1
                    Dv(lambda e, j=j, ob=ob: e.tensor_scalar(out=OH[:, ob, :], in0=iota[:, 1:1 + CAP], scalar1=pos[:, j, e_:e_ + 1],
                                                             scalar2=mask[:, j, e_:e_ + 1], op0=ALU.is_equal, op1=ALU.mult),
                       ['iota', 'pos', 'mask'], [('OH', ob)])
                    for ct in range(CT_):
                        bk = 4 + ct
                        kb.op('pe', lambda e, j=j, ob=ob, ct=ct, bk=bk: e.matmul(ps[bk][:, 0:4], lhsT=OH[:, ob, ct * 128:(ct + 1) * 128],
                                                                              rhs=R[:, j, e_, :], start=(j == 0), stop=(j == NT - 1)),
                              r=[('OH', ob), 'R'], w=[('ps', bk)])
                for ct in range(CT_):
                    kb.op('act', lambda e, ct=ct: e.copy(out=selsb[:, e_, ct, :], in_=ps[4 + ct][:, 0:4]), r=[('ps', 4 + ct)], w=['selsb'])
            Dv(lambda e: e.scalar_tensor_tensor(out=selg[:], in0=selsb[:, :, :, 1], scalar=128.0, in1=selsb[:, :, :, 0], op0=ALU.mult, op1=ALU.add),
               ['selsb'], ['selg'])
            Dv(lambda e: e.tensor_copy(out=seli[:], in_=selg[:]), ['selg'], ['seli'])
            Dv(lambda e: e.tensor_tensor(out=selg[:], in0=selsb[:, :, :, 2], in1=selsb[:, :, :, 3], op=ALU.add), ['selsb', 'seli'], ['selg'])
            kb.barrier()
        with ExitStack() as es3:
            B_ = lambda name, shp, dt: es3.enter_context(nc.sbuf_tensor(_nm(name), shp, dt))
            xe = B_('f_xe', [128, 2, CT_, 1024], BF16)
            xeT = B_('f_xeT', [128, 8, CAP], BF16)
            wg = B_('f_wg', [128, 2, 8, 512], BF16)
            wu = B_('f_wu', [128, 2, 8, 512], BF16)
            wd = B_('f_wd', [128, 16, 1024], BF16)
            hidT = B_('f_hidT', [128, 16, CAP], BF16)
            sg = B_('f_sg', [128, 2, CAP], F32)
            ye = B_('f_ye', [128, CT_, 1024], F32)
            deferred = []
            nwb = 0
            nab = 0
            nye = 0
            def emit_gather(ee):
                for ct in range(CT_):
                    kb.idma(lambda g, ct=ct: g.indirect_dma_start(
                        out=xe[:, ee % 2, ct, :], out_offset=None, in_=c.hn_tm[:, :],
                        in_offset=bass.IndirectOffsetOnAxis(ap=seli[:, ee, ct:ct + 1], axis=0)),
                        r=['seli'] + [('hn_tm', tt) for tt in range(NT)], w=[('xe', ee % 2, ct)])
            emit_gather(0)
            for e_ in range(NE):
                if e_ + 1 < NE:
                    emit_gather(e_ + 1)
                for ct in range(CT_):
                    psE = ps[6][:].bitcast(BF16)
                    for k in range(8):
                        kb.op('pe', lambda e, k=k, ct=ct: e.transpose(out=psE[:, k * 128:(k + 1) * 128], in_=xe[:, e_ % 2, ct, k * 128:(k + 1) * 128],
                                                                      identity=cstb[:, 0, :]), r=[('xe', e_ % 2, ct), 'cst'], w=[('ps', 6)])
                    kb.op('act', lambda e, ct=ct: e.copy(out=xeT[:, :, ct * 128:(ct + 1) * 128], in_=psE.rearrange("p (a b) -> p a b", a=8)),
                          r=[('ps', 6)], w=['xeT'])
                for fg in range(4):
                    wb_ = nwb % 2
                    nwb += 1
                    fs = slice(fg * 512, (fg + 1) * 512)
                    kb.dma('pool', wg[:, wb_], W['w_eg'][e_].rearrange("(k p) f -> p k f", p=128)[:, :, fs], w=[('wg', wb_)])
                    kb.dma('pool', wu[:, wb_], W['w_eu'][e_].rearrange("(k p) f -> p k f", p=128)[:, :, fs], w=[('wu', wb_)])
                    kb.dma('pool', wd[:, fg * 4:(fg + 1) * 4, :], W['w_ed'][e_][fs, :].rearrange("(c p) d -> p c d", p=128), w=[('wd', fg)])
                    if fg == 0:
                        for fn_ in deferred:
                            fn_()
                        deferred = []
                    for fc in range(4):
                        fch = fg * 4 + fc
                        ab = nab % 2
                        nab += 1
                        pa, pb_ = ps[ab * 2], ps[ab * 2 + 1]
                        for (wt, pp, wkey, bk) in [(wg, pa, 'wg', ab * 2), (wu, pb_, 'wu', ab * 2 + 1)]:
                            for k in range(8):
                                kb.op('pe', lambda e, k=k, wt=wt, pp=pp: e.matmul(pp[:, 0:CAP], lhsT=wt[:, wb_, k, fc * 128:(fc + 1) * 128],
                                                                              rhs=xeT[:, k, :], start=(k == 0), stop=(k == 7)),
                                      r=[(wkey, wb_), 'xeT'], w=[('ps', bk)])
                        kb.op('act', lambda e, pa=pa: e.activation(out=sg[:, ab, :], in_=pa[:, 0:CAP], func=AF.Sigmoid), r=[('ps', ab * 2)], w=[('sg', ab)])
                        kb.op('dve', lambda e, pa=pa: e.tensor_tensor(out=sg[:, ab, :], in0=pa[:, 0:CAP], in1=sg[:, ab, :], op=ALU.mult),
                              r=[('ps', ab * 2), ('sg', ab)], w=[('sg', ab)])
                        kb.op('dve', lambda e, pb_=pb_: e.tensor_tensor(out=hidT[:, fch, :], in0=pb_[:, 0:CAP], in1=sg[:, ab, :], op=ALU.mult),
                              r=[('ps', ab * 2 + 1), ('sg', ab)], w=[('hidT', fch)])
                for ct in range(CT_):
                    yb = ct
                    for half in range(2):
                        bk = 4 + half
                        for fch in range(16):
                            kb.op('pe', lambda e, fch=fch, half=half, bk=bk, ct=ct: e.matmul(
                                ps[bk][:], lhsT=hidT[:, fch, ct * 128:(ct + 1) * 128], rhs=wd[:, fch, half * 512:(half + 1) * 512],
                                start=(fch == 0), stop=(fch == 15)), r=[('hidT', fch), ('wd', fch // 4)], w=[('ps', bk)])
                        kb.op('dve', lambda e, half=half, bk=bk, ct=ct: e.tensor_scalar(
                            out=ye[:, yb, half * 512:(half + 1) * 512], in0=ps[bk][:], scalar1=selg[:, e_, ct:ct + 1], scalar2=None, op0=ALU.mult),
                            r=[('ps', bk), 'selg'], w=[('ye', yb, half)])

                    def scat(ct=ct, yb=yb, e_=e_):
                        kb.idma(lambda g: g.indirect_dma_start(
                            out=c.ffn_tm[:, :], out_offset=bass.IndirectOffsetOnAxis(ap=seli[:, e_, ct:ct + 1], axis=0),
                            in_=ye[:, yb, :], in_offset=None, compute_op=ALU.add),
                            r=['seli', ('ye', yb, 0), ('ye', yb, 1)] + [('scat', e_ - 1, c2) for c2 in range(CT_)], w=[('scat', e_, ct)])
                    deferred.append(scat)
            for fn_ in deferred:
                fn_()
            kb.barrier()
        kb.barrier()


def stage_final(c):
    nc, kb, T, NTB, NT = c.nc, c.kb, c.T, c.NTB, c.NT
    with ExitStack() as es:
        A = lambda name, shp, dt: es.enter_context(nc.sbuf_tensor(_nm(name), shp, dt))
        hblk = A('z_hblk', [128, 2, 8, 512], F32)
        oblk = A('z_oblk', [128, 2, 8, 512], F32)
        ftile = A('z_ftile', [128, 2, 1024], F32)
        sq = A('z_sq', [128, 8, 512], BF16)
        rstd = A('z_rstd', [128, 512], F32)
        gfin = A('z_gfin', [128, 8], F32)
        kb.dma('sp', gfin[:], c.fin_g[:, :], w=['gfin'])
        hTv = c.hT.rearrange("(k p) t -> p k t", p=128)
        oTv = c.outT.rearrange("(k p) t -> p k t", p=128)
        for tb in range(NTB):
            b = tb % 2
            ts = slice(tb * 512, (tb + 1) * 512)
            kb.dma('sp', hblk[:, b], hTv[:, :, ts], w=[('hblk', b)])
            if 'skip_ffn' not in c.dbg:
                emit_add_tm(c, hblk[:, b], ('hblk', b), tb, ftile, (0, 1))
            emit_rmsnorm(c, hblk[:, b], ('hblk', b), gfin, 'gfin', lambda k: oblk[:, b, k, :], [('oblk', b)], 2, sq[:], 'sq', rstd[:], 'rstd')
            kb.dma('sp', oTv[:, :, ts], oblk[:, b], r=[('oblk', b)], w=[('outT', tb)])
        kb.barrier()


_CACHE = {}


def kernel(**inputs):
    x = np.asarray(inputs['x'], np.float32)
    B, T, _ = x.shape
    L = np.asarray(inputs['w_in']).shape[0]
    preps = [prep_layer(inputs, l) for l in range(L)]
    shapes = layer_shapes_from(preps[0])
    key = (T, L)
    if key not in _CACHE:
        _CACHE[key] = build(T, L, shapes)
    nc, c = _CACHE[key]
    common = {'consts': make_consts(), 'iota': make_iota(), 'fin_g': _pcol(np.asarray(inputs['final_norm_g'], np.float32), 8)}
    for l in range(L):
        for k, v in preps[l].items():
            common['l%d_%s' % (l, k)] = v
    in_maps = []
    for b in range(B):
        m = dict(common)
        m['xT'] = np.ascontiguousarray(x[b].T)
        in_maps.append(m)
    res = run_bass_kernel_spmd(nc, in_maps, core_ids=list(range(B)))
    out = np.stack([np.ascontiguousarray(res.results[b]['outT'].T) for b in range(B)], axis=0)
    return out.astype(np.float32)
```

```python
import math
from contextlib import ExitStack
import numpy as np
import concourse.bass as bass
import concourse.mybir as mybir
from concourse.bass_utils import run_bass_kernel_spmd

F32 = mybir.dt.float32
BF16 = mybir.dt.bfloat16
I32 = mybir.dt.int32
U32 = mybir.dt.uint32
AF = mybir.ActivationFunctionType
ALU = mybir.AluOpType
AX = mybir.AxisListType

D = 1024
KC = 8
INW = 4896
NE = 16
FF = 2048
EPS = 1e-6
SAME_ENGINE_SYNC = True


class KB:
    NS = 6

    def __init__(self, nc):
        self.nc = nc
        self.eng = {'pe': nc.tensor, 'act': nc.scalar, 'dve': nc.vector, 'pool': nc.gpsimd, 'sp': nc.sync}
        self.csem = {}
        self.ccnt = {}
        for e in ['pe', 'act', 'dve', 'pool']:
            self.csem[e] = nc.semaphore('c_' + e).__enter__()
            self.ccnt[e] = 0
        self.dsem = {}
        self.dval = {}
        self.dnext = {}
        for q in ['sp', 'act', 'pool']:
            self.dsem[q] = [nc.semaphore('d_%s%d' % (q, i)).__enter__() for i in range(self.NS)]
            self.dval[q] = [0] * self.NS
            self.dnext[q] = 0
        self.semname = {}
        self.semowner = {}
        for e, s in self.csem.items():
            self.semname[id(s)] = 'c_' + e
            self.semowner['c_' + e] = e
        self.semobj = {}
        for e, s in self.csem.items():
            self.semobj['c_' + e] = s
        for q in self.dsem:
            for i, s in enumerate(self.dsem[q]):
                self.semobj['d_%s%d' % (q, i)] = s
        self.waited = {e: {} for e in self.eng}
        self.lastw = {}
        self.readers = {}
        self.ninstr = 0

    def _wait(self, e, tok):
        name, val = tok
        if val <= 0:
            return
        if self.waited[e].get(name, 0) >= val:
            return
        owner = self.semowner.get(name)
        if owner == e and (e == 'pe' or not SAME_ENGINE_SYNC):
            return
        self.eng[e].wait_ge(self.semobj[name], val)
        self.waited[e][name] = val
        self.ninstr += 1

    def _deps(self, r, w):
        toks = {}

        def add(t):
            if t is None:
                return
            if toks.get(t[0], 0) < t[1]:
                toks[t[0]] = t[1]
        for k in r:
            add(self.lastw.get(k))
        for k in w:
            add(self.lastw.get(k))
            for n, v in self.readers.get(k, {}).items():
                add((n, v))
        return list(toks.items())

    def _register(self, tok, r, w):
        for k in w:
            self.lastw[k] = tok
            self.readers[k] = {}
        for k in r:
            d = self.readers.setdefault(k, {})
            if d.get(tok[0], 0) < tok[1]:
                d[tok[0]] = tok[1]

    def op(self, e, fn, r=(), w=()):
        psk = [k for k in r if isinstance(k, tuple) and k[0] == 'ps']
        toks = dict(self._deps(r, w))
        me = 'c_' + e
        for k in psk:
            for n, v in self.readers.get(k, {}).items():
                if n != me and toks.get(n, 0) < v:
                    toks[n] = v
        for t in toks.items():
            self._wait(e, t)
        ins = fn(self.eng[e])
        self.ccnt[e] += 1
        ins.then_inc(self.csem[e], 1)
        tok = ('c_' + e, self.ccnt[e])
        self._register(tok, r, w)
        self.ninstr += 1
        return tok

    def dma(self, q, out, in_, r=(), w=(), **kw):
        slot = self.dnext[q] % self.NS
        self.dnext[q] += 1
        name = 'd_%s%d' % (q, slot)
        self._wait(q, (name, self.dval[q][slot]))
        for t in self._deps(r, w):
            self._wait(q, t)
        ins = self.eng[q].dma_start(out=out, in_=in_, **kw)
        ins.then_inc(self.dsem[q][slot], 16)
        self.dval[q][slot] += 16
        tok = (name, self.dval[q][slot])
        self._register(tok, r, w)
        self.ninstr += 1
        return tok

    def idma(self, fn, r=(), w=()):
        q = 'pool'
        slot = self.dnext[q] % self.NS
        self.dnext[q] += 1
        name = 'd_%s%d' % (q, slot)
        self._wait(q, (name, self.dval[q][slot]))
        for t in self._deps(r, w):
            self._wait(q, t)
        ins = fn(self.eng[q])
        ins.then_inc(self.dsem[q][slot], 16)
        self.dval[q][slot] += 16
        tok = (name, self.dval[q][slot])
        self._register(tok, r, w)
        self.ninstr += 1
        return tok

    def barrier(self):
        toks = [('c_' + e, self.ccnt[e]) for e in self.ccnt]
        for q in self.dsem:
            for i in range(self.NS):
                toks.append(('d_%s%d' % (q, i), self.dval[q][i]))
        for e in self.eng:
            for t in toks:
                self._wait(e, t)
        self.lastw = {}
        self.readers = {}


class Ctx:
    pass


_NMC = [0]


def _nm(name):
    _NMC[0] += 1
    return '%s_%d' % (name, _NMC[0])


def _bc_mid(ap2d, n):
    return ap2d.unsqueeze(1).broadcast_to([ap2d.shape[0], n, ap2d.shape[1]])


def _bc_last(ap2d, n):
    return ap2d.unsqueeze(2).broadcast_to([ap2d.shape[0], ap2d.shape[1], n])


def _pcol(v, nch):
    return np.ascontiguousarray(np.asarray(v, np.float32).reshape(nch, 128).T)


def prep_layer(inp, l):
    f = lambda k: np.asarray(inp[k], np.float32)
    o = {}
    o['w_in'] = np.ascontiguousarray(f('w_in')[l])
    o['mix_g'] = _pcol(f('mix_norm_g')[l], 8)
    o['ffn_g'] = _pcol(f('ffn_norm_g')[l], 8)
    cw = f('lru_conv_w')[l]
    o['lru_cw'] = np.ascontiguousarray(cw.reshape(4, 4, 128).transpose(2, 1, 0))
    o['lru_cb'] = _pcol(f('lru_conv_b')[l], 4)
    for nm in ['a', 'x']:
        w = f('lru_w_' + nm)[l]
        bd = np.zeros((128, 2, 4, 128), np.float32)
        for di in range(2):
            for ct in range(4):
                bd[0:64, di, ct, 0:64] = w[di, 2 * ct]
                bd[64:128, di, ct, 64:128] = w[di, 2 * ct + 1]
        o['lru_w' + nm] = bd
        b = f('lru_b_' + nm)[l]
        o['lru_b' + nm] = np.ascontiguousarray(b.reshape(2, 4, 128).transpose(2, 0, 1))
    o['lru_lam'] = np.ascontiguousarray(f('lru_lam')[l].reshape(2, 4, 128).transpose(2, 0, 1))
    prep_gla(inp, l, o)
    prep_s5(inp, l, o)
    prep_rest(inp, l, o)
    return o


def build(T, nlayers, layer_shapes, dbg=()):
    nc = bass.Bass("TRN2", target_bir_lowering=False)
    kb = KB(nc)
    NTB = T // 512
    NT = T // 128
    c = Ctx()
    c.nc, c.kb, c.T, c.NTB, c.NT = nc, kb, T, NTB, NT

    def dram_in(name, shape, dt=F32):
        return nc.dram_tensor(name, list(shape), dt, kind="ExternalInput").ap()

    def dram_out(name, shape, dt=F32):
        return nc.dram_tensor(name, list(shape), dt, kind="ExternalOutput").ap()

    def dram_tmp(name, shape, dt=F32):
        return nc.dram_tensor(name, list(shape), dt, kind="Internal").ap()

    c.xT = dram_in('xT', [D, T])
    c.consts = dram_in('consts', [128, NCONST, 128])
    c.iota = dram_in('iota', [128, 513])
    c.fin_g = dram_in('fin_g', [128, 8])
    c.lw = []
    for l in range(nlayers):
        c.lw.append({k: dram_in('l%d_%s' % (l, k), shp) for k, shp in layer_shapes.items()})
    c.outT = dram_out('outT', [D, T])
    c.hT = dram_tmp('hT', [D, T])
    c.pmixT = dram_tmp('pmixT', [768, T])
    c.gT = dram_tmp('gT', [3072, T], BF16)
    c.ptm = dram_tmp('ptm', [T, 1056])
    c.alrT = dram_tmp('alrT', [32, T], BF16)
    c.yT = dram_tmp('yT', [1024, T], BF16)
    c.ffn_tm = dram_tmp('ffn_tm', [T, D])
    c.hn_tm = dram_tmp('hn_tm', [T, D], BF16)
    c.dbg = {}
    c.lvl = 99
    for name, shp in dbg:
        if name.startswith('lvl'):
            c.lvl = int(name[3:])
            continue
        c.dbg[name] = dram_out('dbg_' + name, shp)

    c.ps = [nc.psum_tensor('ps%d' % i, [128, 512], F32).__enter__() for i in range(8)]

    c.ones_bf = nc.sbuf_tensor(_nm('ones_bf'), [128, 128], BF16).__enter__()
    kb.op('dve', lambda e: e.memset(c.ones_bf[:], 1.0), w=['ones_bf'])

    load_consts(c)
    for l in range(nlayers):
        stage_norm_proj(c, l, src=(c.xT if l == 0 else c.hT), add_tm=(l > 0))
        kb.barrier()
        if 'skip_lru' not in c.dbg:
            stage_lru(c, l)
        kb.barrier()
        if 'skip_gla' not in c.dbg:
            stage_gla(c, l)
        kb.barrier()
        if 'skip_s5' not in c.dbg:
            stage_s5(c, l)
        kb.barrier()
        if 'stop_mixers' in c.dbg:
            continue
        CAPT = (2 * T // NE) // 128
        with ExitStack() as esl:
            seli = esl.enter_context(nc.sbuf_tensor(_nm('f_seli'), [128, NE, CAPT], I32))
            selg = esl.enter_context(nc.sbuf_tensor(_nm('f_selg'), [128, NE, CAPT], F32))
            with nc.sbuf_tensor(_nm('hnT'), [128, KC, T], BF16) as hnT:
                stage_merge(c, l, hnT)
                kb.barrier()
                if 'skip_ffn' not in c.dbg:
                    stage_ffn(c, l, hnT, seli, selg, 0)
                kb.barrier()
            if 'skip_ffn' not in c.dbg:
                stage_ffn(c, l, None, seli, selg, 1)
            kb.barrier()
    if 'stop_mixers' not in c.dbg:
        stage_final(c)
    kb.barrier()
    return nc, c


def stage_norm_proj(c, l, src, add_tm=False):
    nc, kb, T, NTB, NT = c.nc, c.kb, c.T, c.NTB, c.NT
    W = c.lw[l]
    with ExitStack() as es:
        xn = es.enter_context(nc.sbuf_tensor(_nm('xn'), [128, KC, T], BF16))
        hblk = es.enter_context(nc.sbuf_tensor(_nm('hblk'), [128, 2, KC, 512], F32))
        sq = es.enter_context(nc.sbuf_tensor(_nm('sq'), [128, 2, KC, 512], BF16))
        rstd = es.enter_context(nc.sbuf_tensor(_nm('rstd'), [128, 2, 512], F32))
        g_mix = es.enter_context(nc.sbuf_tensor(_nm('g_mix'), [128, KC], F32))
        wA = es.enter_context(nc.sbuf_tensor(_nm('wA'), [128, KC, 768], BF16))
        wB = es.enter_context(nc.sbuf_tensor(_nm('wB'), [128, KC, 1056], BF16))
        wG = es.enter_context(nc.sbuf_tensor(_nm('wG'), [128, 2, KC, 512], BF16))
        stf = es.enter_context(nc.sbuf_tensor(_nm('stf'), [128, 4, 512], F32))
        stg = es.enter_context(nc.sbuf_tensor(_nm('stg'), [128, 4, 512], BF16))
        sttm = es.enter_context(nc.sbuf_tensor(_nm('sttm'), [128, 2, 1056], F32))
        stalr = es.enter_context(nc.sbuf_tensor(_nm('stalr'), [32, T], BF16))
        ftile = es.enter_context(nc.sbuf_tensor(_nm('ftile'), [128, 2, 1024], F32))
        kb.dma('sp', g_mix[:], W['mix_g'][:, :], w=['g_mix'])
        w_in_v = W['w_in'].rearrange("(k p) c -> p k c", p=128)
        kb.dma('pool', wA[:], w_in_v[:, :, 0:768], w=['wA'])
        kb.dma('pool', wB[:], w_in_v[:, :, 768:1824], w=['wB'])
        srcv = src.rearrange("(k p) t -> p k t", p=128)
        for tb in range(NTB):
            b = tb % 2
            ts = slice(tb * 512, (tb + 1) * 512)
            kb.dma('sp', hblk[:, b], srcv[:, :, ts], w=[('hblk', b)])
            if add_tm:
                emit_add_tm(c, hblk[:, b], ('hblk', b), tb, ftile, (2, 3))
                kb.dma('sp', srcv[:, :, ts], hblk[:, b], r=[('hblk', b)], w=[('hTw', tb)])
            kb.op('act', lambda e: e.activation(out=sq[:, b], in_=hblk[:, b], func=AF.Square),
                  r=[('hblk', b)], w=[('sq', b)])
            pb = c.ps[b]
            for k in range(KC):
                kb.op('pe', lambda e, k=k: e.matmul(pb[:], lhsT=c.ones_bf[:], rhs=sq[:, b, k, :],
                                                    start=(k == 0), stop=(k == KC - 1)),
                      r=[('sq', b), 'ones_bf'], w=[('ps', b)])
            kb.op('act', lambda e: e.activation(out=rstd[:, b], in_=pb[:], func=AF.Sqrt,
                                                scale=1.0 / D, bias=EPS),
                  r=[('ps', b)], w=[('rstd', b)])
            kb.op('dve', lambda e: e.reciprocal(out=rstd[:, b], in_=rstd[:, b]),
                  r=[('rstd', b)], w=[('rstd', b)])
            for k in range(KC):
                kb.op('dve', lambda e, k=k: e.scalar_tensor_tensor(
                    out=xn[:, k, ts], in0=hblk[:, b, k, :], scalar=g_mix[:, k:k + 1], in1=rstd[:, b],
                    op0=ALU.mult, op1=ALU.mult),
                    r=[('hblk', b), ('rstd', b), 'g_mix'], w=[('xn', tb)])
        nps = 0
        nst = 0
        for cc in range(6):
            for tb in range(NTB):
                ts = slice(tb * 512, (tb + 1) * 512)
                pi = 2 + (nps % 4)
                nps += 1
                sb = nst % 4
                nst += 1
                pb = c.ps[pi]
                for k in range(KC):
                    kb.op('pe', lambda e, k=k, pb=pb: e.matmul(
                        pb[:], lhsT=wA[:, k, cc * 128:(cc + 1) * 128], rhs=xn[:, k, ts],
                        start=(k == 0), stop=(k == KC - 1)),
                        r=['wA', ('xn', tb)], w=[('ps', pi)])
                if tb % 2 == 0:
                    kb.op('act', lambda e, pb=pb: e.copy(out=stf[:, sb, :], in_=pb[:]),
                          r=[('ps', pi)], w=[('stf', sb)])
                else:
                    kb.op('dve', lambda e, pb=pb: e.tensor_copy(out=stf[:, sb, :], in_=pb[:]),
                          r=[('ps', pi)], w=[('stf', sb)])
                kb.dma('sp', c.pmixT[cc * 128:(cc + 1) * 128, ts], stf[:, sb, :], r=[('stf', sb)], w=[('pmixT', cc, tb)])
        for tb in range(NTB):
            ts = slice(tb * 512, (tb + 1) * 512)
            pi = 2 + (nps % 4)
            nps += 1
            pb = c.ps[pi]
            for k in range(KC):
                kb.op('pe', lambda e, k=k, pb=pb: e.matmul(
                    pb[0:32, :], lhsT=wB[:, k, 1024:1056], rhs=xn[:, k, ts],
                    start=(k == 0), stop=(k == KC - 1)),
                    r=['wB', ('xn', tb)], w=[('ps', pi)])
            kb.op('act', lambda e, pb=pb: e.copy(out=stalr[:, ts], in_=pb[0:32, :]),
                  r=[('ps', pi)], w=[('stalr', tb)])
        kb.dma('sp', c.alrT[:, :], stalr[:, :], r=[('stalr', tb) for tb in range(NTB)], w=['alrT'])
        for tt in range(NT):
            sb = tt % 2
            tb = tt // 4
            tsl = slice(tt * 128, (tt + 1) * 128)
            for gi, (c0, c1) in enumerate([(0, 512), (512, 1024), (1024, 1056)]):
                pi = 2 + (nps % 4)
                nps += 1
                pb = c.ps[pi]
                for k in range(KC):
                    kb.op('pe', lambda e, k=k, pb=pb: e.matmul(
                        pb[:, 0:c1 - c0], lhsT=xn[:, k, tsl], rhs=wB[:, k, c0:c1],
                        start=(k == 0), stop=(k == KC - 1)),
                        r=['wB', ('xn', tb)], w=[('ps', pi)])
                if gi == 0:
                    kb.op('act', lambda e, pb=pb: e.copy(out=sttm[:, sb, c0:c1], in_=pb[:, 0:c1 - c0]),
                          r=[('ps', pi)], w=[('sttm', sb, gi)])
                else:
                    kb.op('dve', lambda e, pb=pb: e.tensor_copy(out=sttm[:, sb, c0:c1], in_=pb[:, 0:c1 - c0]),
                          r=[('ps', pi)], w=[('sttm', sb, gi)])
            kb.dma('sp', c.ptm[tsl, :], sttm[:, sb, :], r=[('sttm', sb, gi) for gi in range(3)],
                   w=[('ptm', tt)])
        for cg in range(6):
            wb_ = cg % 2
            kb.dma('pool', wG[:, wb_], w_in_v[:, :, 1824 + cg * 512:1824 + (cg + 1) * 512], w=[('wG', wb_)])
            for c4 in range(4):
                cc = cg * 4 + c4
                for tb in range(NTB):
                    ts = slice(tb * 512, (tb + 1) * 512)
                    pi = 2 + (nps % 4)
                    nps += 1
                    pb = c.ps[pi]
                    for k in range(KC):
                        kb.op('pe', lambda e, k=k, pb=pb: e.matmul(
                            pb[:], lhsT=wG[:, wb_, k, c4 * 128:(c4 + 1) * 128], rhs=xn[:, k, ts],
                            start=(k == 0), stop=(k == KC - 1)),
                            r=[('wG', wb_), ('xn', tb)], w=[('ps', pi)])
                    sb = nst % 4
                    nst += 1
                    kb.op('act', lambda e, pb=pb: e.activation(out=stg[:, sb, :], in_=pb[:], func=AF.Sigmoid),
                          r=[('ps', pi)], w=[('stg', sb)])
                    kb.dma('sp', c.gT[cc * 128:(cc + 1) * 128, ts], stg[:, sb, :], r=[('stg', sb)], w=[('gT', cc, tb)])
        if 'pmixT' in c.dbg:
            kb.barrier()
            kb.dma('sp', c.dbg['pmixT'][:, :], c.pmixT[:, :], w=['dbg_pmixT'])
        kb.barrier()


def stage_lru(c, l):
    nc, kb, T, NTB, NT = c.nc, c.kb, c.T, c.NTB, c.NT
    W = c.lw[l]
    with ExitStack() as es:
        xbp = es.enter_context(nc.sbuf_tensor(_nm('xbp'), [128, T + 4], F32))
        xc = es.enter_context(nc.sbuf_tensor(_nm('xc'), [128, T], F32))
        xcb = es.enter_context(nc.sbuf_tensor(_nm('xcb'), [128, T], BF16))
        bA = es.enter_context(nc.sbuf_tensor(_nm('bA'), [128, T], F32))
        bB = es.enter_context(nc.sbuf_tensor(_nm('bB'), [128, T], F32))
        bC = es.enter_context(nc.sbuf_tensor(_nm('bC'), [128, T], F32))
        hf = es.enter_context(nc.sbuf_tensor(_nm('hf'), [128, T], F32))
        ybf = es.enter_context(nc.sbuf_tensor(_nm('ybf'), [128, 2, T], BF16))
        l_cw = es.enter_context(nc.sbuf_tensor(_nm('l_cw'), [128, 4, 4], F32))
        l_cb = es.enter_context(nc.sbuf_tensor(_nm('l_cb'), [128, 4], F32))
        l_wa = es.enter_context(nc.sbuf_tensor(_nm('l_wa'), [128, 2, 4, 128], BF16))
        l_wx = es.enter_context(nc.sbuf_tensor(_nm('l_wx'), [128, 2, 4, 128], BF16))
        l_ba = es.enter_context(nc.sbuf_tensor(_nm('l_ba'), [128, 8], F32))
        l_bx = es.enter_context(nc.sbuf_tensor(_nm('l_bx'), [128, 8], F32))
        l_lam = es.enter_context(nc.sbuf_tensor(_nm('l_lam'), [128, 8], F32))
        l_c = es.enter_context(nc.sbuf_tensor(_nm('l_c'), [128, 8], F32))
        l_c2 = es.enter_context(nc.sbuf_tensor(_nm('l_c2'), [128, 8], F32))
        l_t = es.enter_context(nc.sbuf_tensor(_nm('l_t'), [128, 6, 8], F32))
        kb.dma('sp', l_cw[:], W['lru_cw'][:, :, :], w=['l_cw'])
        kb.dma('sp', l_cb[:], W['lru_cb'][:, :], w=['l_cb'])
        kb.dma('pool', l_wa[:], W['lru_wa'][:, :, :, :], w=['l_wa'])
        kb.dma('pool', l_wx[:], W['lru_wx'][:, :, :, :], w=['l_wx'])
        kb.dma('sp', l_ba[:], W['lru_ba'].rearrange("p a b -> p (a b)"), w=['l_ba'])
        kb.dma('sp', l_bx[:], W['lru_bx'].rearrange("p a b -> p (a b)"), w=['l_bx'])
        kb.dma('sp', l_lam[:], W['lru_lam'].rearrange("p a b -> p (a b)"), w=['l_lam'])
        P = 'l_par'
        tz, tw, tw2, tacc, tm, tabs = [l_t[:, i, :] for i in range(6)]
        dv = lambda fn, r=(P,), w=(P,): kb.op('dve', fn, r=list(r) + ['l_lam'], w=list(w))
        dv(lambda e: e.scalar_tensor_tensor(out=tabs, in0=l_lam[:], scalar=-1.0, in1=l_lam[:], op0=ALU.mult, op1=ALU.max))
        kb.op('act', lambda e: e.activation(out=tz, in_=tabs, func=AF.Exp, scale=-1.0), r=[P], w=[P])
        dv(lambda e: e.tensor_scalar(out=tw, in0=tz, scalar1=2.0, scalar2=None, op0=ALU.add))
        dv(lambda e: e.reciprocal(out=tw, in_=tw))
        dv(lambda e: e.tensor_tensor(out=tw, in0=tw, in1=tz, op=ALU.mult))
        dv(lambda e: e.tensor_tensor(out=tw2, in0=tw, in1=tw, op=ALU.mult))
        dv(lambda e: e.memset(tacc, 1.0 / 13.0))
        for kk in [11, 9, 7, 5, 3, 1]:
            dv(lambda e: e.tensor_tensor(out=tacc, in0=tacc, in1=tw2, op=ALU.mult))
            dv(lambda e, kk=kk: e.tensor_scalar(out=tacc, in0=tacc, scalar1=1.0 / kk, scalar2=None, op0=ALU.add))
        dv(lambda e: e.tensor_tensor(out=tacc, in0=tacc, in1=tw, op=ALU.mult))
        dv(lambda e: e.tensor_scalar(out=tm, in0=l_lam[:], scalar1=-1.0, scalar2=0.0, op0=ALU.mult, op1=ALU.max))
        dv(lambda e: e.scalar_tensor_tensor(out=tacc, in0=tacc, scalar=2.0, in1=tm, op0=ALU.mult, op1=ALU.add))
        dv(lambda e: e.tensor_scalar(out=l_c[:], in0=tacc, scalar1=-8.0, scalar2=None, op0=ALU.mult), w=[P, 'l_c'])
        dv(lambda e: e.tensor_scalar(out=l_c2[:], in0=tacc, scalar1=-16.0, scalar2=None, op0=ALU.mult), w=[P, 'l_c'])
        kb.op('dve', lambda e: e.memset(xbp[:, 0:2], 0.0), w=['xbp_pad'])
        kb.op('dve', lambda e: e.memset(xbp[:, T + 2:T + 4], 0.0), w=['xbp_pad'])
        nps = 0
        for ct in range(4):
            kb.dma('sp', xbp[:, 2:T + 2], c.pmixT[256 + ct * 128:256 + (ct + 1) * 128, :], w=['xbp'])
            kb.op('dve', lambda e: e.tensor_scalar(out=xc[:], in0=xbp[:, 0:T], scalar1=l_cw[:, ct, 0:1],
                                                   scalar2=l_cb[:, ct:ct + 1], op0=ALU.mult, op1=ALU.add),
                  r=['xbp', 'xbp_pad', 'l_cw', 'l_cb'], w=['xc'])
            for j in range(1, 4):
                kb.op('dve', lambda e, j=j: e.scalar_tensor_tensor(
                    out=xc[:], in0=xbp[:, j:j + T], scalar=l_cw[:, ct, j:j + 1], in1=xc[:],
                    op0=ALU.mult, op1=ALU.add), r=['xbp', 'xbp_pad', 'l_cw', 'xc'], w=['xc'])
            kb.op('act', lambda e: e.copy(out=xcb[:], in_=xc[:]), r=['xc'], w=['xcb'])
            for di in range(2):
                for (wt, bt, dst, key) in [(l_wa, l_ba, bA, 'bA'), (l_wx, l_bx, bB, 'bB')]:
                    for tb in range(NTB):
                        ts = slice(tb * 512, (tb + 1) * 512)
                        pi = nps % 4
                        nps += 1
                        pb = c.ps[pi]
                        kb.op('pe', lambda e, pb=pb, wt=wt: e.matmul(pb[:], lhsT=wt[:, di, ct, :], rhs=xcb[:, ts],
                                                                     start=True, stop=True),
                              r=['xcb', 'l_wa', 'l_wx'], w=[('ps', pi)])
                        kb.op('act', lambda e, pb=pb, bt=bt, dst=dst: e.activation(
                            out=dst[:, ts], in_=pb[:], func=AF.Sigmoid,
                            bias=bt[:, di * 4 + ct:di * 4 + ct + 1]),
                            r=[('ps', pi), 'l_ba', 'l_bx'], w=[key])
                cs = l_c[:, di * 4 + ct:di * 4 + ct + 1]
                c2s = l_c2[:, di * 4 + ct:di * 4 + ct + 1]
                kb.op('act', lambda e: e.activation(out=bC[:], in_=bA[:], func=AF.Exp, scale=cs),
                      r=['bA', 'l_c'], w=['bC'])
                kb.op('act', lambda e: e.activation(out=bA[:], in_=bA[:], func=AF.Exp, scale=c2s),
                      r=['bA', 'l_c'], w=['bA'])
                kb.op('act', lambda e: e.activation(out=bA[:], in_=bA[:], func=AF.Sqrt, scale=-1.0, bias=1.0),
                      r=['bA'], w=['bA'])
                kb.op('dve', lambda e: e.tensor_tensor(out=bB[:], in0=bB[:], in1=bA[:], op=ALU.mult),
                      r=['bA', 'bB'], w=['bB'])
                kb.op('dve', lambda e: e.tensor_tensor(out=bB[:], in0=bB[:], in1=xc[:], op=ALU.mult),
                      r=['xc', 'bB'], w=['bB'])
                if di == 0:
                    kb.op('dve', lambda e: e.tensor_tensor_scan(out=hf[:], data0=bC[:], data1=bB[:], initial=0.0,
                                                                op0=ALU.mult, op1=ALU.add),
                          r=['bC', 'bB'], w=['hf'])
                else:
                    rev = lambda t: bass.AP(t[:].tensor, t[:, T - 1:T].offset, [[t[:].ap[0][0], 128], [-1, T]])
                    kb.op('dve', lambda e: e.tensor_tensor_scan(out=rev(bA), data0=rev(bC), data1=rev(bB),
                                                                initial=0.0, op0=ALU.mult, op1=ALU.add),
                          r=['bC', 'bB', 'bA'], w=['bA'])
                    sb = ct % 2
                    kb.op('dve', lambda e: e.tensor_tensor(out=ybf[:, sb, :], in0=hf[:], in1=bA[:], op=ALU.add),
                          r=['hf', 'bA'], w=[('ybf', sb)])
                    kb.dma('sp', c.yT[256 + ct * 128:256 + (ct + 1) * 128, :], ybf[:, sb, :],
                           r=[('ybf', sb)], w=[('yT', 2 + ct)])
        if 'ylru' in c.dbg:
            kb.barrier()
            with nc.sbuf_tensor(_nm('dbgt'), [128, T], F32) as dbgt:
                for ct in range(4):
                    kb.dma('pool', dbgt[:], c.yT[256 + ct * 128:256 + (ct + 1) * 128, :], w=['dbgt'])
                    kb.dma('sp', c.dbg['ylru'][ct * 128:(ct + 1) * 128, :], dbgt[:], r=['dbgt'], w=[('dbgo', ct)])
                kb.barrier()
        kb.barrier()


LAYER_SHAPES = None


def layer_shapes_from(prep):
    return {k: v.shape for k, v in prep.items()}


NCONST = 10


def make_consts():
    s = np.arange(128)[:, None]
    t = np.arange(128)[None, :]
    cst = np.zeros((128, NCONST, 128), np.float32)
    cst[:, 0] = (s == t)
    cst[:, 1] = (s <= t) * (-1.0 / 16)
    cst[:, 2] = (s >= t) * (-1.0 / 16)
    cst[:, 3] = -1.0 / 16
    cst[:, 4] = (s <= t)
    cst[:, 5] = (s > t)
    cst[:, 6] = (s < t)
    cst[:, 7] = 1.0
    cst[:, 8] = (s <= t)
    cst[:, 9] = (s >= t)
    return cst


def prep_gla(inp, l, o):
    f = lambda k: np.asarray(inp[k], np.float32)
    wal = np.zeros((33, 512), np.float32)
    wal[0:16, 0:256] = f('gla_w_alpha')[l, 0]
    wal[16:32, 256:512] = f('gla_w_alpha')[l, 1]
    wal[32, 0:256] = f('gla_b_alpha')[l, 0]
    wal[32, 256:512] = f('gla_b_alpha')[l, 1]
    o['gla_wal'] = wal
    o['gla_ng'] = np.ascontiguousarray(np.broadcast_to(f('gla_norm_g')[l][None, :], (128, 64)))


def load_consts(c):
    nc, kb = c.nc, c.kb
    c.cst = nc.sbuf_tensor(_nm('cst'), [128, NCONST, 128], F32).__enter__()
    c.cstb = nc.sbuf_tensor(_nm('cstb'), [128, NCONST, 128], BF16).__enter__()
    kb.dma('sp', c.cst[:], c.consts[:, :, :], w=['cst'])
    kb.dma('pool', c.cstb[:], c.consts[:, :, :], w=['cst'])


def stage_gla(c, l):
    nc, kb, T, NTB, NT = c.nc, c.kb, c.T, c.NTB, c.NT
    W = c.lw[l]
    cst, cstb = c.cst, c.cstb
    ident_b = cstb[:, 0, :]
    with ExitStack() as es:
        A = lambda name, shp, dt: es.enter_context(nc.sbuf_tensor(_nm(name), shp, dt))
        ptile = A('g_pt', [128, 2, 1056], F32)
        alrx = A('g_alrx', [64, T], BF16)
        wal = A('g_wal', [64, 512], BF16)
        spt = A('g_sp', [128, 512], F32)
        E1 = A('g_E1', [128, 512], F32)
        E2 = A('g_E2', [128, 512], F32)
        decrow = A('g_decrow', [128, 512], F32)
        qd = A('g_qd', [128, 2, 256], BF16)
        kd = A('g_kd', [128, 2, 256], BF16)
        kk = A('g_kk', [128, 2, 256], BF16)
        vb = A('g_vb', [128, 256], BF16)
        qdT = A('g_qdT', [128, 4, T], BF16)
        kdT = A('g_kdT', [128, 4, 128], BF16)
        scm = A('g_scm', [128, 2, 4, 128], BF16)
        kv = A('g_kv', [128, NT, 4, 64], F32)
        dec = A('g_dec', [128, NT, 4], F32)
        oacc = A('g_oacc', [128, NT, 256], F32)
        sog = A('g_sog', [128, NT, 256], BF16)
        S = A('g_S', [128, 4, 64], F32)
        Sb = A('g_Sb', [128, 4, 64], BF16)
        ng = A('g_ng', [128, 64], F32)
        ygT = A('g_ygT', [128, 2, T], BF16)
        ss = A('g_ss', [128, NT * 4], F32)
        ps = c.ps
        kb.op('dve', lambda e: e.memset(alrx[32:64, :], 1.0), w=['alrx1'])
        kb.dma('sp', alrx[0:32, :], c.alrT[:, :], w=['alrx'])
        kb.op('dve', lambda e: e.memset(wal[:], 0.0), w=['wal'])
        kb.dma('pool', wal[0:33, :], W['gla_wal'][:, :], r=[], w=['wal'])
        kb.dma('sp', ng[:], W['gla_ng'][:, :], w=['ng'])
        for n in range(NT):
            b = n % 2
            nsl = slice(n * 128, (n + 1) * 128)
            kb.dma('sp', ptile[:, b, :], c.ptm[nsl, :], w=[('pt', b)])
            q_ap = ptile[:, b, 0:256]
            k_ap = ptile[:, b, 256:512]
            v_ap = ptile[:, b, 512:768]
            og_ap = ptile[:, b, 768:1024]
            kb.op('pe', lambda e: e.matmul(ps[0][:], lhsT=alrx[0:33, nsl], rhs=wal[0:33, :], start=True, stop=True),
                  r=['alrx', 'alrx1', 'wal'], w=[('ps', 0)])
            kb.op('act', lambda e: e.activation(out=spt[:], in_=ps[0][:], func=AF.Exp, scale=-1.0),
                  r=[('ps', 0)], w=['spt'])
            kb.op('act', lambda e: e.activation(out=spt[:], in_=spt[:], func=AF.Ln, bias=1.0),
                  r=['spt'], w=['spt'])
            if 'gla_stop0' in c.dbg:
                continue
            kb.op('pe', lambda e: e.matmul(ps[1][:, 0:256], lhsT=cst[:, 1, :], rhs=spt[:, 0:256], start=True, stop=True),
                  r=['spt', 'cst'], w=[('ps', 1)])
            kb.op('pe', lambda e: e.matmul(ps[1][:, 256:512], lhsT=cst[:, 2, :], rhs=spt[:, 256:512], start=True, stop=True),
                  r=['spt', 'cst'], w=[('ps', 1)])
            kb.op('pe', lambda e: e.matmul(ps[2][:], lhsT=cst[:, 3, :], rhs=spt[:], start=True, stop=True),
                  r=['spt', 'cst'], w=[('ps', 2)])
            if c.lvl < 1:
                continue
            for i4 in range(4):
                kb.op('pe', lambda e, i4=i4: e.matmul(ps[6][:, 256 + 2 * i4:258 + 2 * i4], lhsT=spt[:, i4 * 128:(i4 + 1) * 128],
                                                     rhs=cst[:, 3, 0:2], start=True, stop=True),
                      r=['spt', 'cst'], w=[('ps', 6)])
            kb.op('act', lambda e: e.activation(out=dec[:, n, :], in_=ps[6][:, 256:264:2], func=AF.Exp),
                  r=[('ps', 6)], w=['dec'])
            if c.lvl < 2:
                continue
            kb.op('act', lambda e: e.activation(out=E1[:], in_=ps[1][:], func=AF.Exp), r=[('ps', 1)], w=['E1'])
            kb.op('act', lambda e: e.activation(out=E2[:], in_=ps[1][:], func=AF.Exp, scale=-1.0), r=[('ps', 1)], w=['E2'])
            kb.op('act', lambda e: e.activation(out=decrow[:], in_=ps[2][:], func=AF.Exp), r=[('ps', 2)], w=['decrow'])
            if c.lvl < 3:
                continue
            v2 = lambda ap: ap.rearrange("p (a b) -> p a b", a=2)
            kb.op('dve', lambda e: e.scalar_tensor_tensor(out=qd[:], in0=_bc_mid(q_ap, 2), scalar=0.125, in1=v2(E1[:]),
                                                          op0=ALU.mult, op1=ALU.mult),
                  r=[('pt', b), 'E1'], w=['qd'])
            kb.op('dve', lambda e: e.tensor_tensor(out=kd[:], in0=_bc_mid(k_ap, 2), in1=v2(E2[:]), op=ALU.mult),
                  r=[('pt', b), 'E2'], w=['kd'])
            kb.op('dve', lambda e: e.tensor_tensor(out=E2[:], in0=E2[:], in1=decrow[:], op=ALU.mult),
                  r=['E2', 'decrow'], w=['E2'])
            kb.op('dve', lambda e: e.tensor_tensor(out=kk[:], in0=_bc_mid(k_ap, 2), in1=v2(E2[:]), op=ALU.mult),
                  r=[('pt', b), 'E2'], w=['kk'])
            if c.lvl < 4:
                continue
            kb.op('act', lambda e: e.copy(out=vb[:], in_=v_ap), r=[('pt', b)], w=['vb'])
            kb.op('act', lambda e: e.activation(out=decrow[:, 0:256], in_=og_ap, func=AF.Sigmoid), r=[('pt', b), 'E2'], w=['decrow'])
            kb.op('dve', lambda e: e.tensor_tensor(out=sog[:, n, :], in0=og_ap, in1=decrow[:, 0:256], op=ALU.mult), r=[('pt', b), 'decrow'], w=['sog'])
            if c.lvl < 5:
                continue
            psE = ps[3][:].bitcast(BF16)
            for i8 in range(8):
                src = (qd if i8 < 4 else kd)
                di, ct = (i8 % 4) // 2, i8 % 2
                kb.op('pe', lambda e, src=src, di=di, ct=ct, i8=i8: e.transpose(
                    out=psE[:, i8 * 128:(i8 + 1) * 128], in_=src[:, di, ct * 128:(ct + 1) * 128], identity=ident_b),
                    r=['qd', 'kd', 'cst'], w=[('ps', 3)])
            if 'noevac' in c.dbg:
                continue
            kb.op('act', lambda e: e.copy(out=qdT[:, :, nsl], in_=psE[:, 0:512].rearrange("p (a b) -> p a b", a=4)),
                  r=[('ps', 3)], w=[('qdT', n)])
            if 'evac_act_only' in c.dbg:
                continue
            kb.op('act', lambda e: e.copy(out=kdT[:], in_=psE[:, 512:1024].rearrange("p (a b) -> p a b", a=4)),
                  r=[('ps', 3)], w=['kdT'])
            if c.lvl < 6:
                continue
            for di in range(2):
                for h in range(4):
                    ct, r0 = h // 2, (h % 2) * 64
                    cb = di * 2 + h // 2
                    kb.op('pe', lambda e, di=di, h=h, ct=ct, r0=r0, cb=cb: e.matmul(
                        ps[4 + h % 2][:, cb * 128:(cb + 1) * 128], lhsT=kdT[r0:r0 + 64, di * 2 + ct, :],
                        rhs=qdT[r0:r0 + 64, di * 2 + ct, nsl], start=True, stop=True),
                        r=['kdT', ('qdT', n)], w=[('ps', 4 + h % 2)])
            mask4 = cst[:, 4:6, :].unsqueeze(2).broadcast_to([128, 2, 2, 128])
            for rg in range(2):
                kb.op('dve', lambda e, rg=rg: e.tensor_tensor(
                    out=scm[:, :, rg::2, :], in0=ps[4 + rg][:].rearrange("p (a b c) -> p a b c", a=2, b=2),
                    in1=mask4, op=ALU.mult),
                    r=[('ps', 4 + rg), 'cst'], w=[('scm', 0), ('scm', 1)])
            if c.lvl < 7:
                continue
            for h in range(4):
                for di in range(2):
                    kb.op('pe', lambda e, di=di, h=h: e.matmul(
                        ps[6][:, h * 64:(h + 1) * 64], lhsT=scm[:, di, h, :], rhs=vb[:, h * 64:(h + 1) * 64],
                        start=(di == 0), stop=(di == 1)),
                        r=[('scm', 0), ('scm', 1), 'vb'], w=[('ps', 6)])
            kb.op('act', lambda e: e.copy(out=oacc[:, n, :], in_=ps[6][:, 0:256]), r=[('ps', 6)], w=[('oacc', n)])
            if c.lvl < 8:
                continue
            for di in range(2):
                for hp in range(2):
                    i4 = di * 2 + hp
                    kb.op('pe', lambda e, di=di, hp=hp, i4=i4: e.matmul(
                        ps[7][:, i4 * 128:(i4 + 1) * 128], lhsT=kk[:, di, hp * 128:(hp + 1) * 128],
                        rhs=vb[:, hp * 128:(hp + 1) * 128], start=True, stop=True),
                        r=['kk', 'vb'], w=[('ps', 7)])
            p7 = ps[7][:].rearrange("p (a b) -> p a b", a=4)
            kb.op('act', lambda e: e.copy(out=kv[0:64, n, :, :], in_=p7[0:64, :, 0:64]), r=[('ps', 7)], w=[('kv', n, 0)])
            kb.op('dve', lambda e: e.tensor_copy(out=kv[64:128, n, :, :], in_=p7[64:128, :, 64:128]),
                  r=[('ps', 7)], w=[('kv', n, 1)])
        if 'gla_stop1' in c.dbg:
            kb.barrier()
            return
        kb.op('dve', lambda e: e.memset(S[:], 0.0), w=['S0', 'S1'])
        kb.op('dve', lambda e: e.memset(Sb[:], 0.0), w=['Sb0', 'Sb1'])
        for idx in range(NT):
            for di in range(2):
                n = idx if di == 0 else NT - 1 - idx
                nsl = slice(n * 128, (n + 1) * 128)
                if idx > 0:
                    for h in range(4):
                        hp, r0 = h // 2, (h % 2) * 64
                        bk = di * 2 + h % 2
                        kb.op('pe', lambda e, h=h, hp=hp, r0=r0, bk=bk: e.matmul(
                            ps[bk][:, hp * 64:(hp + 1) * 64], lhsT=qdT[r0:r0 + 64, di * 2 + hp, nsl],
                            rhs=Sb[r0:r0 + 64, di * 2 + hp, :], start=True, stop=True),
                            r=[('qdT', n), 'Sb%d' % di], w=[('ps', bk)])
                    for rg in range(2):
                        bk = di * 2 + rg
                        ov = oacc[:, n, :].rearrange("p (a b c) -> p a b c", a=2, b=2)[:, :, rg, :]
                        kb.op('dve', lambda e, bk=bk, ov=ov: e.tensor_tensor(
                            out=ov, in0=ov, in1=ps[bk][:, 0:128].rearrange("p (a c) -> p a c", a=2), op=ALU.add),
                            r=[('ps', bk), ('oacc', n)], w=[('oacc', n)])
                if idx < NT - 1:
                    for hp in range(2):
                        i4 = di * 2 + hp
                        kb.op('dve', lambda e, i4=i4: e.scalar_tensor_tensor(
                            out=S[:, i4, :], in0=S[:, i4, :], scalar=dec[:, n, i4:i4 + 1], in1=kv[:, n, i4, :],
                            op0=ALU.mult, op1=ALU.add),
                            r=['S%d' % di, 'dec', ('kv', n, 0), ('kv', n, 1)], w=['S%d' % di])
                    kb.op('act', lambda e: e.copy(out=Sb[:, di * 2:di * 2 + 2, :], in_=S[:, di * 2:di * 2 + 2, :]),
                          r=['S%d' % di], w=['Sb%d' % di])
        allo = [('oacc', n) for n in range(NT)]
        sqv = kv[:].rearrange("p n a b -> p (n a b)")
        oflat = oacc[:].rearrange("p n c -> p (n c)")
        kb.op('act', lambda e: e.activation(out=sqv, in_=oflat, func=AF.Square), r=allo + [('kv', n, j) for n in range(NT) for j in range(2)],
              w=['sqv'])
        kb.op('dve', lambda e: e.tensor_reduce(out=ss[:], in_=sqv.rearrange("p (a b) -> p a b", b=64), axis=AX.X, op=ALU.add),
              r=['sqv'], w=['ss'])
        kb.op('act', lambda e: e.activation(out=ss[:], in_=ss[:], func=AF.Sqrt, scale=1.0 / 64, bias=EPS), r=['ss'], w=['ss'])
        kb.op('dve', lambda e: e.reciprocal(out=ss[:], in_=ss[:]), r=['ss'], w=['ss'])
        o3 = oflat.rearrange("p (a b) -> p a b", b=64)
        kb.op('dve', lambda e: e.tensor_tensor(out=o3, in0=o3, in1=_bc_last(ss[:], 64), op=ALU.mult), r=allo + ['ss'], w=allo)
        kb.op('dve', lambda e: e.tensor_tensor(out=o3, in0=o3, in1=_bc_mid(ng[:], NT * 4), op=ALU.mult), r=allo + ['ng'], w=allo)
        sflat = sog[:].rearrange("p n c -> p (n c)")
        kb.op('dve', lambda e: e.tensor_tensor(out=sflat, in0=oflat, in1=sflat, op=ALU.mult), r=allo + ['sog'], w=['sog'])
        for n4 in range(NT // 4):
            pb = ps[2 + n4 % 2]
            pbb = pb[:].bitcast(BF16)
            for j in range(4):
                n = n4 * 4 + j
                for ct in range(2):
                    kb.op('pe', lambda e, n=n, ct=ct, j=j: e.transpose(
                        out=pbb[:, (ct * 4 + j) * 128:(ct * 4 + j + 1) * 128], in_=sog[:, n, ct * 128:(ct + 1) * 128],
                        identity=ident_b), r=['sog', 'cst'], w=[('ps', 2 + n4 % 2)])
            kb.op('act',
                  (lambda e: e.copy(out=ygT[:, :, n4 * 512:(n4 + 1) * 512], in_=pbb.rearrange("p (a b) -> p a b", a=2))),
                  r=[('ps', 2 + n4 % 2)], w=[('ygT', n4)])
        for ct in range(2):
            kb.dma('sp', c.yT[768 + ct * 128:768 + (ct + 1) * 128, :], ygT[:, ct, :],
                   r=[('ygT', n4) for n4 in range(NT // 4)], w=[('yT', 6 + ct)])
        if 'ygla' in c.dbg:
            kb.barrier()
            with nc.sbuf_tensor(_nm('dbgt2'), [128, T], F32) as dbgt:
                for ct in range(2):
                    kb.dma('pool', dbgt[:], c.yT[768 + ct * 128:768 + (ct + 1) * 128, :], w=['dbgt'])
                    kb.dma('sp', c.dbg['ygla'][ct * 128:(ct + 1) * 128, :], dbgt[:], r=['dbgt'], w=[('dbgo', ct)])
                kb.barrier()
        kb.barrier()


TWO_PI = 2.0 * math.pi
CW1 = 6.28125
CW2 = TWO_PI - CW1


def prep_s5(inp, l, o):
    f = lambda k: np.asarray(inp[k], np.float32)
    lr, li, ldt = f('s5_lam_re')[l], f('s5_lam_im')[l], f('s5_log_dt')[l]
    par = np.zeros((2, 3, 1024), np.float32)
    par[:, 0] = lr.reshape(2, 1024)
    par[:, 1] = li.reshape(2, 1024)
    par[:, 2] = np.repeat(ldt, 64, axis=1)
    o['s5_par_tm'] = np.ascontiguousarray(np.broadcast_to(par[None], (128, 2, 3, 1024)))
    o['s5_par_sm'] = np.ascontiguousarray(par.reshape(2, 3, 8, 128).transpose(3, 0, 1, 2))
    bre, bim = f('s5_b_re')[l], f('s5_b_im')[l]
    bT = np.zeros((128, 2, 2, 2, 512), np.float32)
    cre, cim = f('s5_c_re')[l], f('s5_c_im')[l]
    cT = np.zeros((128, 2, 8, 2, 128), np.float32)
    for di in range(2):
        for g in range(16):
            k, gl = g // 8, g % 8
            bT[gl * 16:(gl + 1) * 16, di, k, 0, gl * 64:(gl + 1) * 64] = bre[di, g].T
            bT[gl * 16:(gl + 1) * 16, di, k, 1, gl * 64:(gl + 1) * 64] = bim[di, g].T
            st, g2 = g // 2, g % 2
            cT[g2 * 64:(g2 + 1) * 64, di, st, 0, gl * 16:(gl + 1) * 16] = cre[di, g].T
            cT[g2 * 64:(g2 + 1) * 64, di, st, 1, gl * 16:(gl + 1) * 16] = cim[di, g].T
    o['s5_bT'] = bT
    o['s5_cT'] = cT
    o['s5_d'] = _pcol(f('s5_d')[l], 2)
    o['s5_wglu'] = np.ascontiguousarray(f('s5_w_glu')[l].reshape(2, 128, 256).transpose(1, 0, 2))
    kc = np.zeros((128, 2 + 256), np.float32)
    s = np.arange(128, dtype=np.float32)
    kc[:, 0] = s + 1
    kc[:, 1] = 128 - s
    kc[:, 2:130] = (s + 1)[None, :]
    kc[:, 130:258] = (128 - s)[None, :]
    o['s5_kc'] = kc


def _trig(kb, ang, n_t, ni_t, cos_t, sin_t, key):
    D_ = lambda fn, r, w: kb.op('dve', fn, r=r, w=w)
    D_(lambda e: e.tensor_scalar(out=n_t, in0=ang, scalar1=1.0 / TWO_PI, scalar2=None, op0=ALU.mult), [key], [key + 'n'])
    D_(lambda e: e.tensor_copy(out=ni_t, in_=n_t), [key + 'n'], [key + 'ni'])
    D_(lambda e: e.tensor_copy(out=n_t, in_=ni_t), [key + 'ni'], [key + 'n'])
    for (dst, shift) in [(sin_t, 0.0), (cos_t, math.pi / 2)]:
        D_(lambda e: e.scalar_tensor_tensor(out=dst, in0=n_t, scalar=-CW1, in1=ang, op0=ALU.mult, op1=ALU.add),
           [key, key + 'n'], [key + 'o'])
        D_(lambda e: e.scalar_tensor_tensor(out=dst, in0=n_t, scalar=-CW2, in1=dst, op0=ALU.mult, op1=ALU.add),
           [key + 'n', key + 'o'], [key + 'o'])
        if shift != 0.0:
            D_(lambda e: e.tensor_scalar(out=dst, in0=dst, scalar1=shift, scalar2=None, op0=ALU.add), [key + 'o'], [key + 'o'])
        for _ in range(2):
            D_(lambda e: e.tensor_scalar(out=ni_t.bitcast(F32), in0=dst, scalar1=math.pi, scalar2=-TWO_PI, op0=ALU.is_gt, op1=ALU.mult),
               [key + 'o'], [key + 'ni'])
            D_(lambda e: e.tensor_tensor(out=dst, in0=dst, in1=ni_t.bitcast(F32), op=ALU.add), [key + 'o', key + 'ni'], [key + 'o'])
        for _ in range(2):
            D_(lambda e: e.tensor_scalar(out=ni_t.bitcast(F32), in0=dst, scalar1=-math.pi, scalar2=TWO_PI, op0=ALU.is_lt, op1=ALU.mult),
               [key + 'o'], [key + 'ni'])
            D_(lambda e: e.tensor_tensor(out=dst, in0=dst, in1=ni_t.bitcast(F32), op=ALU.add), [key + 'o', key + 'ni'], [key + 'o'])
        D_(lambda e: e.tensor_scalar(out=dst, in0=dst, scalar1=math.pi, scalar2=-math.pi, op0=ALU.min, op1=ALU.max), [key + 'o'], [key + 'o'])
        kb.op('act', lambda e: e.activation(out=dst, in_=dst, func=AF.Sin), r=[key + 'o'], w=[key + 'o'])


def stage_s5(c, l):
    nc, kb, T, NTB, NT = c.nc, c.kb, c.T, c.NTB, c.NT
    W = c.lw[l]
    cst, cstb = c.cst, c.cstb
    ps = c.ps
    with ExitStack() as es:
        A = lambda name, shp, dt: es.enter_context(nc.sbuf_tensor(_nm(name), shp, dt))
        yacc = A('s_yacc', [128, 2, T], F32)
        uTb = A('s_uTb', [128, 2, T], BF16)
        Nre = A('s_Nre', [128, 2, 1024], F32)
        Nim = A('s_Nim', [128, 2, 1024], F32)
        Tre = A('s_Tre', [128, 2, 8, 128], F32)
        Tim = A('s_Tim', [128, 2, 8, 128], F32)
        BbT = A('s_BbT', [128, 2, 2, 2, 512], BF16)
        CT = A('s_CT', [128, 2, 8, 2, 128], BF16)
        sd = A('s_d', [128, 2], F32)
        kc = A('s_kc', [128, 258], F32)
        nkc = A('s_nkc', [128, 2], F32)
        psm = A('s_psm', [128, 2, 3, 8], F32)
        x1sm = A('s_x1sm', [128, 2, 8], F32)
        thsm = A('s_thsm', [128, 2, 8], F32)
        dtsm = A('s_dtsm', [128, 2, 8], F32)
        kb.dma('sp', sd[:], W['s5_d'][:, :], w=['sd'])
        kb.dma('sp', kc[:], W['s5_kc'][:, :], w=['kc'])
        kb.dma('sp', psm[:], W['s5_par_sm'][:, :, :, :], w=['psm'])
        kb.dma('pool', CT[:], W['s5_cT'][:, :, :, :, :], w=['CT'])
        kb.op('dve', lambda e: e.tensor_scalar(out=CT[:, :, :, 1, :], in0=CT[:, :, :, 1, :], scalar1=-1.0, scalar2=None, op0=ALU.mult),
              r=['CT'], w=['CT'])
        kb.op('dve', lambda e: e.tensor_scalar(out=nkc[:], in0=kc[:, 0:2], scalar1=-1.0, scalar2=None, op0=ALU.mult), r=['kc'], w=['nkc'])
        for k in range(2):
            kb.dma('sp', yacc[:, k, :], c.pmixT[k * 128:(k + 1) * 128, :], w=[('yacc', k)])
            kb.op('act', lambda e, k=k: e.copy(out=uTb[:, k, :], in_=yacc[:, k, :]), r=[('yacc', k)], w=[('uTb', k)])
            kb.op('dve', lambda e, k=k: e.tensor_scalar(out=yacc[:, k, :], in0=yacc[:, k, :], scalar1=sd[:, k:k + 1], scalar2=None,
                                                        op0=ALU.mult), r=[('yacc', k), 'sd', ('uTb', k)], w=[('yacc', k)])
        with ExitStack() as es2:
            B_ = lambda name, shp, dt: es2.enter_context(nc.sbuf_tensor(_nm(name), shp, dt))
            ptm = B_('s_ptm', [128, 3, 1024], F32)
            x1 = B_('s_x1', [128, 1024], F32)
            th = B_('s_th', [128, 1024], F32)
            t_n = B_('s_tn', [128, 2048], F32)
            t_ni = B_('s_tni', [128, 2048], I32)
            t_c = B_('s_tc', [128, 2048], F32)
            t_s = B_('s_ts', [128, 2048], F32)
            t_a = B_('s_ta', [128, 2048], F32)
            kR = B_('s_kR', [128, 1024], F32)
            kI = B_('s_kI', [128, 1024], F32)
            BT = B_('s_BT', [128, 2, 2, 512], F32)
            tb1 = B_('s_tb1', [128, 1024], F32)
            tb2 = B_('s_tb2', [128, 1024], F32)
            Dv = lambda fn, r, w: kb.op('dve', fn, r=r, w=w)
            for di in range(2):
                kb.dma('sp', ptm[:], W['s5_par_tm'][:, di, :, :], w=['ptm'])
                kb.dma('sp', BT[:], W['s5_bT'][:, di, :, :, :], w=['BT'])
                lr_, li_, ldt_ = ptm[:, 0, :], ptm[:, 1, :], ptm[:, 2, :]
                Dv(lambda e: e.tensor_scalar(out=lr_, in0=lr_, scalar1=-1e-4, scalar2=None, op0=ALU.min), ['ptm'], ['ptm'])
                kb.op('act', lambda e: e.activation(out=ldt_, in_=ldt_, func=AF.Exp), r=['ptm'], w=['ptm'])
                Dv(lambda e: e.tensor_tensor(out=x1[:], in0=lr_, in1=ldt_, op=ALU.mult), ['ptm'], ['x1'])
                Dv(lambda e: e.tensor_tensor(out=th[:], in0=li_, in1=ldt_, op=ALU.mult), ['ptm'], ['th'])
                Dv(lambda e: e.tensor_copy(out=t_a[:, 0:1024], in_=th[:]), ['th'], ['tg'])
                _trig(kb, t_a[:, 0:1024], t_n[:, 0:1024], t_ni[:, 0:1024], t_c[:, 0:1024], t_s[:, 0:1024], 'tg')
                kb.op('act', lambda e: e.activation(out=tb1[:], in_=x1[:], func=AF.Exp), r=['x1'], w=['tb1'])
                aR, aI = t_c[:, 0:1024], t_s[:, 0:1024]
                Dv(lambda e: e.tensor_tensor(out=aR, in0=aR, in1=tb1[:], op=ALU.mult), ['tgo', 'tb1'], ['tgo'])
                Dv(lambda e: e.tensor_tensor(out=aI, in0=aI, in1=tb1[:], op=ALU.mult), ['tgo', 'tb1'], ['tgo'])
                Dv(lambda e: e.tensor_tensor(out=tb1[:], in0=lr_, in1=lr_, op=ALU.mult), ['ptm', 'tgo'], ['tb1'])
                Dv(lambda e: e.tensor_tensor(out=tb2[:], in0=li_, in1=li_, op=ALU.mult), ['ptm'], ['tb2'])
                Dv(lambda e: e.tensor_tensor(out=tb1[:], in0=tb1[:], in1=tb2[:], op=ALU.add), ['tb1', 'tb2'], ['tb1'])
                Dv(lambda e: e.reciprocal(out=tb1[:], in_=tb1[:]), ['tb1'], ['tb1'])
                Dv(lambda e: e.tensor_scalar(out=aR, in0=aR, scalar1=-1.0, scalar2=None, op0=ALU.add), ['tgo'], ['tgo'])
                Dv(lambda e: e.tensor_tensor(out=kR[:], in0=aR, in1=lr_, op=ALU.mult), ['tgo', 'ptm'], ['kR'])
                Dv(lambda e: e.tensor_tensor(out=tb2[:], in0=aI, in1=li_, op=ALU.mult), ['tgo', 'ptm'], ['tb2'])
                Dv(lambda e: e.tensor_tensor(out=kR[:], in0=kR[:], in1=tb2[:], op=ALU.add), ['kR', 'tb2'], ['kR'])
                Dv(lambda e: e.tensor_tensor(out=kR[:], in0=kR[:], in1=tb1[:], op=ALU.mult), ['kR', 'tb1'], ['kR'])
                Dv(lambda e: e.tensor_tensor(out=kI[:], in0=aI, in1=lr_, op=ALU.mult), ['tgo', 'ptm'], ['kI'])
                Dv(lambda e: e.tensor_tensor(out=tb2[:], in0=aR, in1=li_, op=ALU.mult), ['tgo', 'ptm', 'kR'], ['tb2'])
                Dv(lambda e: e.tensor_tensor(out=kI[:], in0=kI[:], in1=tb2[:], op=ALU.subtract), ['kI', 'tb2'], ['kI'])
                Dv(lambda e: e.tensor_tensor(out=kI[:], in0=kI[:], in1=tb1[:], op=ALU.mult), ['kI', 'tb1'], ['kI'])
                kRv = kR[:].rearrange("p (k s) -> p k s", k=2)
                kIv = kI[:].rearrange("p (k s) -> p k s", k=2)
                t1v = tb1[:].rearrange("p (k s) -> p k s", k=2)
                t2v = tb2[:].rearrange("p (k s) -> p k s", k=2)
                Dv(lambda e: e.tensor_tensor(out=t1v, in0=kRv, in1=BT[:, :, 0, :], op=ALU.mult), ['kR', 'BT', 'kI'], ['tb1'])
                Dv(lambda e: e.tensor_tensor(out=t2v, in0=kIv, in1=BT[:, :, 1, :], op=ALU.mult), ['kI', 'BT'], ['tb2'])
                Dv(lambda e: e.tensor_tensor(out=BbT[:, di, :, 0, :], in0=t1v, in1=t2v, op=ALU.subtract), ['tb1', 'tb2'], ['BbT'])
                Dv(lambda e: e.tensor_tensor(out=t1v, in0=kRv, in1=BT[:, :, 1, :], op=ALU.mult), ['kR', 'BT', 'BbT'], ['tb1'])
                Dv(lambda e: e.tensor_tensor(out=t2v, in0=kIv, in1=BT[:, :, 0, :], op=ALU.mult), ['kI', 'BT', 'BbT'], ['tb2'])
                Dv(lambda e: e.tensor_tensor(out=BbT[:, di, :, 1, :], in0=t1v, in1=t2v, op=ALU.add), ['tb1', 'tb2'], ['BbT'])
                Dv(lambda e: e.tensor_scalar(out=t_a[:, 0:1024], in0=th[:], scalar1=kc[:, di:di + 1], scalar2=None, op0=ALU.mult),
                   ['th', 'kc', 'tgo', 'tg'], ['tg'])
                _trig(kb, t_a[:, 0:1024], t_n[:, 0:1024], t_ni[:, 0:1024], t_c[:, 0:1024], t_s[:, 0:1024], 'tg')
                kb.op('act', lambda e: e.activation(out=tb1[:], in_=x1[:], func=AF.Exp, scale=nkc[:, di:di + 1]),
                      r=['x1', 'nkc', 'BbT'], w=['tb1'])
                Dv(lambda e: e.tensor_tensor(out=Nre[:, di, :], in0=tb1[:], in1=t_c[:, 0:1024], op=ALU.mult), ['tb1', 'tgo'], ['Nre'])
                Dv(lambda e: e.scalar_tensor_tensor(out=Nim[:, di, :], in0=tb1[:], scalar=-1.0, in1=t_s[:, 0:1024],
                                                    op0=ALU.mult, op1=ALU.mult), ['tb1', 'tgo'], ['Nim'])
            Dv(lambda e: e.tensor_scalar(out=psm[:, :, 0, :], in0=psm[:, :, 0, :], scalar1=-1e-4, scalar2=None, op0=ALU.min), ['psm'], ['psm'])
            kb.op('act', lambda e: e.activation(out=dtsm[:], in_=psm[:, :, 2, :], func=AF.Exp), r=['psm'], w=['dtsm'])
            Dv(lambda e: e.tensor_tensor(out=x1sm[:], in0=psm[:, :, 0, :], in1=dtsm[:], op=ALU.mult), ['psm', 'dtsm'], ['x1sm'])
            Dv(lambda e: e.tensor_tensor(out=thsm[:], in0=psm[:, :, 1, :], in1=dtsm[:], op=ALU.mult), ['psm', 'dtsm'], ['thsm'])
            magT = tb1[:].rearrange("p (a b) -> p a b", a=8)
            for di in range(2):
                krow = kc[:, 2 + di * 128:2 + (di + 1) * 128]
                for st in range(8):
                    Dv(lambda e, st=st: e.tensor_scalar(out=t_a[:, (di * 8 + st) * 128:(di * 8 + st + 1) * 128], in0=krow,
                                                        scalar1=thsm[:, di, st:st + 1], scalar2=None, op0=ALU.mult),
                       ['kc', 'thsm', 'tg', 'tgo'], ['tg'])
            _trig(kb, t_a[:], t_n[:], t_ni[:], t_c[:], t_s[:], 'tg')
            for di in range(2):
                krow = kc[:, 2 + di * 128:2 + (di + 1) * 128]
                for st in range(8):
                    kb.op('act', lambda e, st=st: e.activation(out=magT[:, st, :], in_=krow, func=AF.Exp, scale=x1sm[:, di, st:st + 1]),
                          r=['kc', 'x1sm', 'Nre', 'Nim'], w=['tb1'])
                Dv(lambda e: e.tensor_tensor(out=Tre[:, di], in0=magT, in1=t_c[:, di * 1024:(di + 1) * 1024].rearrange("p (a b) -> p a b", a=8),
                                             op=ALU.mult), ['tb1', 'tgo'], ['Tre'])
                Dv(lambda e: e.tensor_tensor(out=Tim[:, di], in0=magT, in1=t_s[:, di * 1024:(di + 1) * 1024].rearrange("p (a b) -> p a b", a=8),
                                             op=ALU.mult), ['tb1', 'tgo'], ['Tim'])
            kb.barrier()
        with ExitStack() as es3:
            B_ = lambda name, shp, dt: es3.enter_context(nc.sbuf_tensor(_nm(name), shp, dt))
            tt = B_('s_tt', [128, 2, 4, 512], F32)
            Wc = B_('s_W', [128, 2, 2, 2, 2, 512], BF16)
            mm = B_('s_mm', [128, 2, 4, 4, 128], F32)
            xs = B_('s_xs', [128, 2, 2, 8, 128], F32)
            xb = B_('s_xb', [128, 2, 2, 8, 128], BF16)

            def chunk_of(idx, di):
                return idx if di == 0 else NT - 1 - idx

            def step_A(idx, di, k):
                n = chunk_of(idx, di)
                nsl = slice(n * 128, (n + 1) * 128)
                base = di * 4
                par = idx % 2
                ksl = slice(k * 512, (k + 1) * 512)
                for ri in range(2):
                    kb.op('pe', lambda e, ri=ri: e.matmul(ps[base + ri][:], lhsT=uTb[:, k, nsl], rhs=BbT[:, di, k, ri, :],
                                                          start=True, stop=True),
                          r=[('uTb', k), 'BbT'], w=[('ps', base + ri)])
                pr, pi_ = ps[base], ps[base + 1]
                for slot, (pp, tab, bkk, tkey) in enumerate([(pr, Nre, base, 'Nre'), (pi_, Nim, base + 1, 'Nim'),
                                                            (pi_, Nre, base + 1, 'Nre'), (pr, Nim, base, 'Nim')]):
                    kb.op('dve', lambda e, slot=slot, pp=pp, tab=tab: e.tensor_tensor(
                        out=tt[:, di, slot, :], in0=pp[:], in1=tab[:, di, ksl], op=ALU.mult),
                        r=[('ps', bkk), tkey], w=[('tt', di, slot)])
                kb.op('pool', lambda e: e.tensor_tensor(out=Wc[:, par, di, k, 0, :], in0=tt[:, di, 0, :], in1=tt[:, di, 1, :], op=ALU.subtract),
                      r=[('tt', di, 0), ('tt', di, 1)], w=[('W', par, di, k, 0)])
                kb.op('pool', lambda e: e.tensor_tensor(out=Wc[:, par, di, k, 1, :], in0=tt[:, di, 2, :], in1=tt[:, di, 3, :], op=ALU.add),
                      r=[('tt', di, 2), ('tt', di, 3)], w=[('W', par, di, k, 1)])

            def step_B(idx, di, k):
                base = di * 4
                par = idx % 2
                tri = cstb[:, 8 + di, :]
                ccol = 127 if di == 0 else 0
                for ri in range(2):
                    for s4 in range(4):
                        kb.op('pe', lambda e, ri=ri, s4=s4: e.matmul(
                            ps[base + 2 + ri][:, s4 * 128:(s4 + 1) * 128], lhsT=Wc[:, par, di, k, ri, s4 * 128:(s4 + 1) * 128], rhs=tri,
                            start=True, stop=True), r=[('W', par, di, k, ri), 'cst'], w=[('ps', base + 2 + ri)])
                for s4 in range(4):
                    st = k * 4 + s4
                    pre = ps[base + 2][:, s4 * 128:(s4 + 1) * 128]
                    pim = ps[base + 3][:, s4 * 128:(s4 + 1) * 128]
                    if idx == 0:
                        cr, ci = 0.0, 0.0
                    else:
                        cr = xs[:, di, 0, st, ccol:ccol + 1]
                        ci = xs[:, di, 1, st, ccol:ccol + 1]
                    rk = [('ps', base + 2), ('ps', base + 3), 'Tre', 'Tim', ('xs', di, k)]
                    for slot, (pp, cc_, tab) in enumerate([(pre, cr, Tre), (pim, ci, Tim), (pim, ci, Tre), (pre, cr, Tim)]):
                        kb.op('dve', lambda e, slot=slot, pp=pp, cc_=cc_, tab=tab, s4=s4, st=st: e.scalar_tensor_tensor(
                            out=mm[:, di, slot, s4, :], in0=pp, scalar=cc_, in1=tab[:, di, st, :], op0=ALU.add, op1=ALU.mult),
                            r=rk, w=[('mm', di, slot, s4)])
                kb.op('pool', lambda e: e.tensor_tensor(out=xs[:, di, 0, k * 4:(k + 1) * 4, :], in0=mm[:, di, 0], in1=mm[:, di, 1], op=ALU.subtract),
                      r=[('mm', di, 0, s4) for s4 in range(4)] + [('mm', di, 1, s4) for s4 in range(4)], w=[('xs', di, k)])
                kb.op('pool', lambda e: e.tensor_tensor(out=xs[:, di, 1, k * 4:(k + 1) * 4, :], in0=mm[:, di, 2], in1=mm[:, di, 3], op=ALU.add),
                      r=[('mm', di, 2, s4) for s4 in range(4)] + [('mm', di, 3, s4) for s4 in range(4)], w=[('xs', di, k)])
                kb.op('act', lambda e: e.copy(out=xb[:, di, :, k * 4:(k + 1) * 4, :], in_=xs[:, di, :, k * 4:(k + 1) * 4, :]),
                      r=[('xs', di, k)], w=[('xb', di, k)])

            def step_C(idx, di):
                n = chunk_of(idx, di)
                nsl = slice(n * 128, (n + 1) * 128)
                base = di * 4
                for k in range(2):
                    cnt = 0
                    for st in range(k * 4, k * 4 + 4):
                        for ri in range(2):
                            kb.op('pe', lambda e, k=k, st=st, ri=ri, cnt=cnt: e.matmul(
                                ps[base][:, k * 128:(k + 1) * 128], lhsT=CT[:, di, st, ri, :], rhs=xb[:, di, ri, st, :],
                                start=(cnt == 0), stop=(cnt == 7)), r=['CT', ('xb', di, k)], w=[('ps', base)])
                            cnt += 1
                kb.op('dve', lambda e: e.tensor_tensor(out=yacc[:, :, nsl], in0=yacc[:, :, nsl],
                                                       in1=ps[base][:, 0:256].rearrange("p (a b) -> p a b", a=2), op=ALU.add),
                      r=[('ps', base), ('yacc', n)], w=[('yacc', n)])

            for k in range(2):
                for di in range(2):
                    step_A(0, di, k)
            for idx in range(NT):
                if idx + 1 < NT:
                    for k in range(2):
                        for di in range(2):
                            step_A(idx + 1, di, k)
                for k in range(2):
                    for di in range(2):
                        step_B(idx, di, k)
                for di in range(2):
                    step_C(idx, di)
            kb.barrier()
        with ExitStack() as es3:
            B_ = lambda name, shp, dt: es3.enter_context(nc.sbuf_tensor(_nm(name), shp, dt))
            z = B_('s_z', [128, 2, T], F32)
            yst = B_('s_yst', [128, 2, 512], BF16)
            yf = yacc[:].rearrange("p k t -> p (k t)")
            zf = z[:].rearrange("p k t -> p (k t)")
            GC = 0.7978845608028654
            kb.op('act', lambda e: e.activation(out=zf, in_=yf, func=AF.Square), r=[('yacc', 0), ('yacc', 1)] + [('yacc', n) for n in range(NT)], w=['z'])
            kb.op('dve', lambda e: e.tensor_scalar(out=zf, in0=zf, scalar1=0.044715, scalar2=1.0, op0=ALU.mult, op1=ALU.add), r=['z'], w=['z'])
            kb.op('dve', lambda e: e.tensor_tensor(out=zf, in0=zf, in1=yf, op=ALU.mult), r=['z', ('yacc', 0), ('yacc', 1)], w=['z'])
            kb.op('act', lambda e: e.activation(out=zf, in_=zf, func=AF.Sigmoid, scale=2.0 * GC), r=['z'], w=['z'])
            kb.op('dve', lambda e: e.tensor_tensor(out=zf, in0=zf, in1=yf, op=ALU.mult), r=['z', ('yacc', 0), ('yacc', 1)], w=['z'])
            kb.op('act', lambda e: e.copy(out=uTb[:].rearrange("p k t -> p (k t)"), in_=zf), r=['z'], w=[('uTb', 0), ('uTb', 1)])
            wgl = B_('s_wgl', [128, 2, 256], BF16)
            kb.dma('pool', wgl[:], W['s5_wglu'][:, :, :], w=['wgl'])
            nps = 0
            for ko in range(2):
                for tb in range(NTB):
                    ts = slice(tb * 512, (tb + 1) * 512)
                    bk = 1 + nps % 4
                    nps += 1
                    for ki in range(2):
                        kb.op('pe', lambda e, ki=ki, bk=bk: e.matmul(ps[bk][:], lhsT=wgl[:, ki, ko * 128:(ko + 1) * 128], rhs=uTb[:, ki, ts],
                                                              start=(ki == 0), stop=(ki == 1)),
                              r=['wgl', ('uTb', 0), ('uTb', 1)], w=[('ps', bk)])
                    kb.op('act', lambda e, bk=bk: e.activation(out=yacc[:, ko, ts], in_=ps[bk][:], func=AF.Sigmoid),
                          r=[('ps', bk)], w=[('yg', ko, tb)])
                    yb_ = nps % 2
                    kb.op('dve', lambda e, yb_=yb_: e.tensor_tensor(out=yst[:, yb_, :], in0=yacc[:, ko, ts], in1=z[:, ko, ts],
                                                           op=ALU.mult), r=[('yg', ko, tb), 'z'], w=[('yst', yb_)])
                    kb.dma('sp', c.yT[ko * 128:(ko + 1) * 128, ts], yst[:, yb_, :], r=[('yst', yb_)], w=[('yT', ko, tb)])
        if 'ys5' in c.dbg:
            kb.barrier()
            with nc.sbuf_tensor(_nm('dbgt3'), [128, T], F32) as dbgt:
                for ct in range(2):
                    kb.dma('pool', dbgt[:], c.yT[ct * 128:(ct + 1) * 128, :], w=['dbgt'])
                    kb.dma('sp', c.dbg['ys5'][ct * 128:(ct + 1) * 128, :], dbgt[:], r=['dbgt'], w=[('dbgo', ct)])
                kb.barrier()
        kb.barrier()


def prep_rest(inp, l, o):
    f = lambda k: np.asarray(inp[k], np.float32)
    o['w_up'] = np.ascontiguousarray(np.concatenate([f('w_up_s5')[l], f('w_up_lru')[l], f('w_up_gla')[l]], axis=0))
    o['w_mix'] = np.ascontiguousarray(f('w_mix_out')[l])
    o['w_router'] = np.ascontiguousarray(f('w_router')[l].reshape(8, 128, 16).transpose(1, 0, 2))
    o['w_eg'] = np.ascontiguousarray(f('w_exp_gate')[l])
    o['w_eu'] = np.ascontiguousarray(f('w_exp_up')[l])
    o['w_ed'] = np.ascontiguousarray(f('w_exp_down')[l])


def make_iota():
    io = np.zeros((128, 513), np.float32)
    io[:, 0] = np.arange(128)
    io[:, 1:] = np.arange(512)[None, :]
    return io


def emit_rmsnorm(c, hb, hkey, gcol, gkey, out_of_k, outkeys, bank, sq, sqkey, rstd, rkey):
    kb = c.kb
    kb.op('act', lambda e: e.activation(out=sq, in_=hb, func=AF.Square), r=[hkey], w=[sqkey])
    pb = c.ps[bank]
    for k in range(KC):
        kb.op('pe', lambda e, k=k: e.matmul(pb[:], lhsT=c.ones_bf[:], rhs=sq[:, k, :], start=(k == 0), stop=(k == KC - 1)),
              r=[sqkey, 'ones_bf'], w=[('ps', bank)])
    kb.op('act', lambda e: e.activation(out=rstd, in_=pb[:], func=AF.Sqrt, scale=1.0 / D, bias=EPS), r=[('ps', bank)], w=[rkey])
    kb.op('dve', lambda e: e.reciprocal(out=rstd, in_=rstd), r=[rkey], w=[rkey])
    for k in range(KC):
        kb.op('dve', lambda e, k=k: e.scalar_tensor_tensor(out=out_of_k(k), in0=hb[:, k, :], scalar=gcol[:, k:k + 1], in1=rstd,
                                                           op0=ALU.mult, op1=ALU.mult),
              r=[hkey, rkey, gkey], w=outkeys)


def emit_add_tm(c, hb, hkey, tb, ftile, banks):
    kb = c.kb
    ident = c.cst[:, 0, :]
    for t4 in range(4):
        tt = tb * 4 + t4
        fb = t4 % 2
        kb.dma('sp', ftile[:, fb, :], c.ffn_tm[tt * 128:(tt + 1) * 128, :], w=[('ftile', fb)])
        for half in range(2):
            bk = banks[half]
            for k4 in range(4):
                k = half * 4 + k4
                kb.op('pe', lambda e, k=k, k4=k4, bk=bk: e.transpose(out=c.ps[bk][:, k4 * 128:(k4 + 1) * 128],
                                                                    in_=ftile[:, fb, k * 128:(k + 1) * 128], identity=ident),
                      r=[('ftile', fb), 'cst'], w=[('ps', bk)])
            hv = hb[:, half * 4:(half + 1) * 4, t4 * 128:(t4 + 1) * 128]
            kb.op('dve', lambda e, hv=hv, bk=bk: e.tensor_tensor(out=hv, in0=hv, in1=c.ps[bk][:].rearrange("p (a b) -> p a b", a=4), op=ALU.add),
                  r=[('ps', bk), hkey], w=[hkey])


def stage_merge(c, l, hnT):
    nc, kb, T, NTB, NT = c.nc, c.kb, c.T, c.NTB, c.NT
    W = c.lw[l]
    ps = c.ps
    res_src = c.xT if l == 0 else c.hT
    with ExitStack() as es:
        A = lambda name, shp, dt: es.enter_context(nc.sbuf_tensor(_nm(name), shp, dt))
        wup = A('m_wup', [128, 8, 1024], BF16)
        wmix = A('m_wmix', [128, 8, 1024], BF16)
        yblk = A('m_yblk', [128, 2, 8, 512], BF16)
        goc = A('m_goc', [128, 2, 3, 512], BF16)
        mrg = A('m_mrg', [128, 8, 512], BF16)
        hblk = A('m_hblk', [128, 2, 8, 512], F32)
        t1 = A('m_t1', [128, 2, 3, 512], F32)
        sq = A('m_sq', [128, 8, 512], BF16)
        rstd = A('m_rstd', [128, 512], F32)
        gff = A('m_gff', [128, 8], F32)
        kb.dma('pool', wup[:], W['w_up'].rearrange("(k p) d -> p k d", p=128), w=['wup'])
        kb.dma('pool', wmix[:], W['w_mix'].rearrange("(k p) d -> p k d", p=128), w=['wmix'])
        kb.dma('sp', gff[:], W['ffn_g'][:, :], w=['gff'])
        yTv = c.yT.rearrange("(k p) t -> p k t", p=128)
        gTv = c.gT.rearrange("(b o p) t -> p b o t", p=128, b=3)
        resv = res_src.rearrange("(k p) t -> p k t", p=128)
        hTv = c.hT.rearrange("(k p) t -> p k t", p=128)
        branch_k = [(0, 2), (2, 6), (6, 8)]
        ng = 0
        for tb in range(NTB):
            b = tb % 2
            ts = slice(tb * 512, (tb + 1) * 512)
            kb.dma('sp', yblk[:, b], yTv[:, :, ts], w=[('yblk', b)])
            kb.dma('sp', hblk[:, b], resv[:, :, ts], w=[('hblk', b)])
            for oc in range(8):
                gb = ng % 2
                ng += 1
                kb.dma('sp', goc[:, gb], gTv[:, :, oc, ts], w=[('goc', gb)])
                for br in range(3):
                    bk = gb * 3 + br
                    k0, k1 = branch_k[br]
                    for k in range(k0, k1):
                        kb.op('pe', lambda e, k=k, bk=bk, k0=k0, k1=k1: e.matmul(
                            ps[bk][:], lhsT=wup[:, k, oc * 128:(oc + 1) * 128], rhs=yblk[:, b, k, :],
                            start=(k == k0), stop=(k == k1 - 1)), r=['wup', ('yblk', b)], w=[('ps', bk)])
                tb_ = oc % 2
                kb.op('dve', lambda e: e.tensor_tensor(out=t1[:, tb_, 0, :], in0=ps[gb * 3][:], in1=goc[:, gb, 0, :], op=ALU.mult),
                      r=[('ps', gb * 3), ('goc', gb)], w=[('t1', tb_, 0)])
                kb.op('dve', lambda e: e.tensor_tensor(out=t1[:, tb_, 1, :], in0=ps[gb * 3 + 1][:], in1=goc[:, gb, 1, :], op=ALU.mult),
                      r=[('ps', gb * 3 + 1), ('goc', gb)], w=[('t1', tb_, 1)])
                kb.op('dve', lambda e: e.tensor_tensor(out=t1[:, tb_, 2, :], in0=ps[gb * 3 + 2][:], in1=goc[:, gb, 2, :], op=ALU.mult),
                      r=[('ps', gb * 3 + 2), ('goc', gb)], w=[('t1', tb_, 2)])
                kb.op('pool', lambda e: e.tensor_tensor(out=t1[:, tb_, 0, :], in0=t1[:, tb_, 0, :], in1=t1[:, tb_, 1, :], op=ALU.add),
                      r=[('t1', tb_, 0), ('t1', tb_, 1)], w=[('t1', tb_, 0)])
                kb.op('pool', lambda e: e.tensor_tensor(out=mrg[:, oc, :], in0=t1[:, tb_, 0, :], in1=t1[:, tb_, 2, :], op=ALU.add),
                      r=[('t1', tb_, 0), ('t1', tb_, 2)], w=[('mrg', oc)])
            for oc in range(8):
                bk = 6 + oc % 2
                for k in range(8):
                    kb.op('pe', lambda e, k=k, bk=bk: e.matmul(ps[bk][:], lhsT=wmix[:, k, oc * 128:(oc + 1) * 128], rhs=mrg[:, k, :],
                                                          start=(k == 0), stop=(k == 7)),
                          r=['wmix'] + [('mrg', kk) for kk in range(8)], w=[('ps', bk)])
                kb.op('dve', lambda e, bk=bk: e.tensor_tensor(out=hblk[:, b, oc, :], in0=hblk[:, b, oc, :], in1=ps[bk][:], op=ALU.add),
                      r=[('ps', bk), ('hblk', b)], w=[('hblk', b)])
            kb.dma('sp', hTv[:, :, ts], hblk[:, b], r=[('hblk', b)], w=[('hT', tb)])
            emit_rmsnorm(c, hblk[:, b], ('hblk', b), gff, 'gff', lambda k: hnT[:, k, ts], [('hnT', tb)], 6, sq[:], 'sq', rstd[:], 'rstd')
        if 'hmid' in c.dbg:
            kb.barrier()
            with nc.sbuf_tensor(_nm('dbgt4'), [128, T], F32) as dbgt:
                for ct in range(8):
                    kb.dma('sp', dbgt[:], c.hT[ct * 128:(ct + 1) * 128, :], w=['dbgt'])
                    kb.dma('sp', c.dbg['hmid'][ct * 128:(ct + 1) * 128, :], dbgt[:], r=['dbgt'], w=[('dbgo', ct)])
                kb.barrier()
        kb.barrier()


def stage_ffn(c, l, hnT, seli, selg, phase):
    nc, kb, T, NTB, NT = c.nc, c.kb, c.T, c.NTB, c.NT
    W = c.lw[l]
    ps = c.ps
    cst, cstb = c.cst, c.cstb
    CAP = 2 * T // NE
    CT_ = CAP // 128
    NJ = NT * NE
    with ExitStack() as es:
        A = lambda name, shp, dt: es.enter_context(nc.sbuf_tensor(_nm(name), shp, dt))
        for _once in ([0] if phase == 0 else []):
          with ExitStack() as es2:
            B_ = lambda name, shp, dt: es2.enter_context(nc.sbuf_tensor(_nm(name), shp, dt))
            wr = B_('f_wr', [128, 8, 16], BF16)
            iota = B_('f_iota', [128, 513], F32)
            probs = B_('f_probs', [128, NT, NE], F32)
            mask = B_('f_mask', [128, NT, NE], F32)
            maskb = B_('f_maskb', [128, NT, NE], BF16)
            gw = B_('f_gw', [128, NT, NE], F32)
            pos = B_('f_pos', [128, NT, NE], F32)
            base = B_('f_base', [128, NT, NE], F32)
            csum = B_('f_csum', [128, NT, NE], F32)
            cmpb = B_('f_cmpb', [128, NT, NE], BF16)
            red = B_('f_red', [128, NT], F32)
            lo = B_('f_lo', [128, NE], F32)
            mid = B_('f_mid', [128, NE], F32)
            cnt = B_('f_cnt', [128, NE], F32)
            R = B_('f_R', [128, NT, NE, 4], BF16)
            ghi_f = B_('f_ghif', [128, NT, NE], F32)
            OH = B_('f_OH', [128, 2, CAP], BF16)
            selsb = B_('f_selsb', [128, NE, CT_, 4], F32)
            stg = B_('f_stg', [128, 2, 1024], BF16)
            zt = B_('f_zt', [128, 1024], F32)
            kb.dma('pool', wr[:], W['w_router'][:, :, :], w=['wr'])
            kb.dma('sp', iota[:], c.iota[:, :], w=['iota'])
            kb.op('dve', lambda e: e.memset(zt[:], 0.0), w=['zt'])
            for tt in range(NT):
                kb.dma('sp', c.ffn_tm[tt * 128:(tt + 1) * 128, :], zt[:], r=['zt'], w=[('ffn_tm', tt)])
            for tt in range(NT):
                sb = tt % 2
                psE = ps[sb][:].bitcast(BF16)
                for k in range(8):
                    kb.op('pe', lambda e, k=k, psE=psE: e.transpose(out=psE[:, k * 128:(k + 1) * 128], in_=hnT[:, k, tt * 128:(tt + 1) * 128],
                                                                   identity=cstb[:, 0, :]),
                          r=[('hnT', tt // 4), 'cst'], w=[('ps', sb)])
                kb.op('act', lambda e, psE=psE: e.copy(out=stg[:, sb, :], in_=psE), r=[('ps', sb)], w=[('stg', sb)])
                kb.dma('sp', c.hn_tm[tt * 128:(tt + 1) * 128, :], stg[:, sb, :], r=[('stg', sb)], w=[('hn_tm', tt)])
            for tt in range(NT):
                for k in range(8):
                    kb.op('pe', lambda e, k=k: e.matmul(ps[2][:, tt * NE:(tt + 1) * NE], lhsT=hnT[:, k, tt * 128:(tt + 1) * 128], rhs=wr[:, k, :],
                                                        start=(k == 0), stop=(k == 7)), r=[('hnT', tt // 4), 'wr'], w=[('ps', 2)])
            lg = ps[2][:, 0:NJ].rearrange("p (j e) -> p j e", e=NE)
            Dv = lambda fn, r, w: kb.op('dve', fn, r=r, w=w)
            Dv(lambda e: e.tensor_reduce(out=red[:], in_=lg, axis=AX.X, op=ALU.max), [('ps', 2)], ['red'])
            Dv(lambda e: e.tensor_tensor(out=probs[:], in0=lg, in1=_bc_last(red[:], NE), op=ALU.subtract), [('ps', 2), 'red'], ['probs'])
            kb.op('act', lambda e: e.activation(out=probs[:], in_=probs[:], func=AF.Exp), r=['probs'], w=['probs'])
            Dv(lambda e: e.tensor_reduce(out=red[:], in_=probs[:], axis=AX.X, op=ALU.add), ['probs'], ['red'])
            Dv(lambda e: e.reciprocal(out=red[:], in_=red[:]), ['red'], ['red'])
            Dv(lambda e: e.tensor_tensor(out=probs[:], in0=probs[:], in1=_bc_last(red[:], NE), op=ALU.mult), ['probs', 'red'], ['probs'])
            Dv(lambda e: e.memset(lo[:], 0.0), [], ['lo'])
            pflat = probs[:].rearrange("p j e -> p (j e)")
            for it in range(1, 33):
                wstep = 2.0 ** (-it)
                Dv(lambda e: e.tensor_scalar(out=mid[:], in0=lo[:], scalar1=wstep, scalar2=None, op0=ALU.add), ['lo'], ['mid'])
                Dv(lambda e: e.tensor_tensor(out=cmpb[:], in0=probs[:], in1=_bc_mid(mid[:], NT), op=ALU.is_ge), ['probs', 'mid'], ['cmpb'])
                kb.op('pe', lambda e: e.matmul(ps[3][:, 0:NJ], lhsT=c.ones_bf[:], rhs=cmpb[:].rearrange("p j e -> p (j e)"), start=True, stop=True),
                      r=['cmpb', 'ones_bf'], w=[('ps', 3)])
                Dv(lambda e: e.tensor_reduce(out=cnt[:], in_=ps[3][:, 0:NJ].rearrange("p (j e) -> p e j", e=NE), axis=AX.X, op=ALU.add),
                   [('ps', 3)], ['cnt'])
                Dv(lambda e: e.tensor_scalar(out=cnt[:], in0=cnt[:], scalar1=float(CAP) - 0.5, scalar2=wstep, op0=ALU.is_ge, op1=ALU.mult),
                   ['cnt'], ['cnt'])
                Dv(lambda e: e.tensor_tensor(out=lo[:], in0=lo[:], in1=cnt[:], op=ALU.add), ['lo', 'cnt'], ['lo'])
            Dv(lambda e: e.tensor_tensor(out=mask[:], in0=probs[:], in1=_bc_mid(lo[:], NT), op=ALU.is_ge), ['probs', 'lo'], ['mask'])
            Dv(lambda e: e.tensor_copy(out=maskb[:], in_=mask[:]), ['mask'], ['maskb'])
            Dv(lambda e: e.tensor_tensor(out=gw[:], in0=probs[:], in1=mask[:], op=ALU.mult), ['probs', 'mask'], ['gw'])
            mbf = maskb[:].rearrange("p j e -> p (j e)")
            kb.op('pe', lambda e: e.matmul(ps[3][:, 0:NJ], lhsT=c.ones_bf[:], rhs=mbf, start=True, stop=True), r=['maskb', 'ones_bf'], w=[('ps', 3)])
            Dv(lambda e: e.tensor_copy(out=csum[:].rearrange("p j e -> p (j e)"), in_=ps[3][:, 0:NJ]), [('ps', 3)], ['csum'])
            Dv(lambda e: e.memset(base[:, 0, :], 0.0), [], ['base'])
            for j in range(1, NT):
                Dv(lambda e, j=j: e.tensor_tensor(out=base[:, j, :], in0=base[:, j - 1, :], in1=csum[:, j - 1, :], op=ALU.add), ['base', 'csum'], ['base'])
            for j in range(NT):
                kb.op('pe', lambda e, j=j: e.matmul(ps[3][:, j * NE:(j + 1) * NE], lhsT=cstb[:, 6, :], rhs=maskb[:, j, :], start=True, stop=True),
                      r=['maskb', 'cst'], w=[('ps', 3)])
            Dv(lambda e: e.tensor_tensor(out=pos[:].rearrange("p j e -> p (j e)"), in0=base[:].rearrange("p j e -> p (j e)"), in1=ps[3][:, 0:NJ],
                                         op=ALU.add), [('ps', 3), 'base'], ['pos'])
            pidx = bass.AP(iota[:].tensor, iota[:, 0:1].offset, [[iota[:].ap[0][0], 128], [0, NT], [0, NE]])
            Dv(lambda e: e.tensor_copy(out=R[:, :, :, 0], in_=pidx), ['iota'], ['R'])
            for j in range(NT):
                Dv(lambda e, j=j: e.memset(R[:, j, :, 1], float(j)), [], ['R'])
            Dv(lambda e: e.tensor_copy(out=R[:, :, :, 2], in_=gw[:]), ['gw'], ['R'])
            Dv(lambda e: e.tensor_copy(out=ghi_f[:], in_=R[:, :, :, 2]), ['R'], ['ghi_f'])
            Dv(lambda e: e.tensor_tensor(out=R[:, :, :, 3], in0=gw[:], in1=ghi_f[:], op=ALU.subtract), ['gw', 'ghi_f'], ['R'])
            noh = 0
            for e_ in range(NE):
                for j in range(NT):
                    ob = noh % 2
                    noh += 1
                    Dv(lambda e, j=j, ob=ob: e.tensor_scalar(out=OH[:, ob, :], in0=iota[:, 1:1 + CAP], scalar1=pos[:, j, e_:e_ + 1],
                                                             scalar2=mask[:, j, e_:e_ + 1], op0=ALU.is_equal, op1=ALU.mult),
                       ['iota', 'pos', 'mask'], [('OH', ob)])
                    for ct in range(CT_):
                        bk = 4 + ct
                        kb.op('pe', lambda e, j=j, ob=ob, ct=ct, bk=bk: e.matmul(ps[bk][:, 0:4], lhsT=OH[:, ob, ct * 128:(ct + 1) * 128],
                                                                              rhs=R[:, j, e_, :], start=(j == 0), stop=(j == NT - 1)),
                              r=[('OH', ob), 'R'], w=[('ps', bk)])
                for ct in range(CT_):
                    kb.op('act', lambda e, ct=ct: e.copy(out=selsb[:, e_, ct, :], in_=ps[4 + ct][:, 0:4]), r=[('ps', 4 + ct)], w=['selsb'])
            Dv(lambda e: e.scalar_tensor_tensor(out=selg[:], in0=selsb[:, :, :, 1], scalar=128.0, in1=selsb[:, :, :, 0], op0=ALU.mult, op1=ALU.add),
               ['selsb'], ['selg'])
            Dv(lambda e: e.tensor_copy(out=seli[:], in_=selg[:]), ['selg'], ['seli'])
            Dv(lambda e: e.tensor_tensor(out=selg[:], in0=selsb[:, :, :, 2], in1=selsb[:, :, :, 3], op=ALU.add), ['selsb', 'seli'], ['selg'])
            kb.barrier()
        for _once in ([0] if phase == 1 else []):
          with ExitStack() as es3:
            B_ = lambda name, shp, dt: es3.enter_context(nc.sbuf_tensor(_nm(name), shp, dt))
            xe = B_('f_xe', [128, 2, CT_, 1024], BF16)
            xeT = B_('f_xeT', [128, 8, CAP], BF16)
            wg = B_('f_wg', [128, 3, 8, 512], BF16)
            wu = B_('f_wu', [128, 3, 8, 512], BF16)
            wd = B_('f_wd', [128, 2, 16, 1024], BF16)
            hidT = B_('f_hidT', [128, 16, CAP], BF16)
            sg = B_('f_sg', [128, 2, CAP], F32)
            ye = B_('f_ye', [128, CT_, 1024], F32)
            deferred = []
            nwb = 0
            nab = 0
            nye = 0
            def emit_gather(ee):
                for ct in range(CT_):
                    kb.idma(lambda g, ct=ct: g.indirect_dma_start(
                        out=xe[:, ee % 2, ct, :], out_offset=None, in_=c.hn_tm[:, :],
                        in_offset=bass.IndirectOffsetOnAxis(ap=seli[:, ee, ct:ct + 1], axis=0)),
                        r=['seli'] + [('hn_tm', tt) for tt in range(NT)], w=[('xe', ee % 2, ct)])
            emit_gather(0)
            for e_ in range(NE):
                if e_ + 1 < NE:
                    emit_gather(e_ + 1)
                for ct in range(CT_):
                    psE = ps[6][:].bitcast(BF16)
                    for k in range(8):
                        kb.op('pe', lambda e, k=k, ct=ct: e.transpose(out=psE[:, k * 128:(k + 1) * 128], in_=xe[:, e_ % 2, ct, k * 128:(k + 1) * 128],
                                                                      identity=cstb[:, 0, :]), r=[('xe', e_ % 2, ct), 'cst'], w=[('ps', 6)])
                    kb.op('act', lambda e, ct=ct: e.copy(out=xeT[:, :, ct * 128:(ct + 1) * 128], in_=psE.rearrange("p (a b) -> p a b", a=8)),
                          r=[('ps', 6)], w=['xeT'])
                for fg in range(4):
                    wb_ = nwb % 3
                    nwb += 1
                    fs = slice(fg * 512, (fg + 1) * 512)
                    kb.dma('pool', wg[:, wb_], W['w_eg'][e_].rearrange("(k p) f -> p k f", p=128)[:, :, fs], w=[('wg', wb_)])
                    kb.dma('pool', wu[:, wb_], W['w_eu'][e_].rearrange("(k p) f -> p k f", p=128)[:, :, fs], w=[('wu', wb_)])
                    kb.dma('pool', wd[:, e_ % 2, fg * 4:(fg + 1) * 4, :], W['w_ed'][e_][fs, :].rearrange("(c p) d -> p c d", p=128), w=[('wd', e_ % 2, fg)])
                    if fg == 1:
                        for fn_ in deferred:
                            fn_()
                        deferred = []
                    for fc in range(4):
                        fch = fg * 4 + fc
                        ab = nab % 2
                        nab += 1
                        pa, pb_ = ps[ab * 2], ps[ab * 2 + 1]
                        for (wt, pp, wkey, bk) in [(wg, pa, 'wg', ab * 2), (wu, pb_, 'wu', ab * 2 + 1)]:
                            for k in range(8):
                                kb.op('pe', lambda e, k=k, wt=wt, pp=pp: e.matmul(pp[:, 0:CAP], lhsT=wt[:, wb_, k, fc * 128:(fc + 1) * 128],
                                                                              rhs=xeT[:, k, :], start=(k == 0), stop=(k == 7)),
                                      r=[(wkey, wb_), 'xeT'], w=[('ps', bk)])
                        kb.op('act', lambda e, pa=pa: e.activation(out=sg[:, ab, :], in_=pa[:, 0:CAP], func=AF.Sigmoid), r=[('ps', ab * 2)], w=[('sg', ab)])
                        kb.op('dve', lambda e, pa=pa: e.tensor_tensor(out=sg[:, ab, :], in0=pa[:, 0:CAP], in1=sg[:, ab, :], op=ALU.mult),
                              r=[('ps', ab * 2), ('sg', ab)], w=[('sg', ab)])
                        kb.op('dve', lambda e, pb_=pb_: e.tensor_tensor(out=hidT[:, fch, :], in0=pb_[:, 0:CAP], in1=sg[:, ab, :], op=ALU.mult),
                              r=[('ps', ab * 2 + 1), ('sg', ab)], w=[('hidT', fch)])
                for ct in range(CT_):
                    yb = ct
                    for half in range(2):
                        bk = 4 + half
                        for fch in range(16):
                            kb.op('pe', lambda e, fch=fch, half=half, bk=bk, ct=ct: e.matmul(
                                ps[bk][:], lhsT=hidT[:, fch, ct * 128:(ct + 1) * 128], rhs=wd[:, e_ % 2, fch, half * 512:(half + 1) * 512],
                                start=(fch == 0), stop=(fch == 15)), r=[('hidT', fch), ('wd', e_ % 2, fch // 4)], w=[('ps', bk)])
                        kb.op('dve', lambda e, half=half, bk=bk, ct=ct: e.tensor_scalar(
                            out=ye[:, yb, half * 512:(half + 1) * 512], in0=ps[bk][:], scalar1=selg[:, e_, ct:ct + 1], scalar2=None, op0=ALU.mult),
                            r=[('ps', bk), 'selg'], w=[('ye', yb, half)])

                    def scat(ct=ct, yb=yb, e_=e_):
                        kb.idma(lambda g: g.indirect_dma_start(
                            out=c.ffn_tm[:, :], out_offset=bass.IndirectOffsetOnAxis(ap=seli[:, e_, ct:ct + 1], axis=0),
                            in_=ye[:, yb, :], in_offset=None, compute_op=ALU.add),
                            r=['seli', ('ye', yb, 0), ('ye', yb, 1)] + [('scat', e_ - 1, c2) for c2 in range(CT_)], w=[('scat', e_, ct)])
                    deferred.append(scat)
            for fn_ in deferred:
                fn_()
            kb.barrier()
        kb.barrier()


def stage_final(c):
    nc, kb, T, NTB, NT = c.nc, c.kb, c.T, c.NTB, c.NT
    with ExitStack() as es:
        A = lambda name, shp, dt: es.enter_context(nc.sbuf_tensor(_nm(name), shp, dt))
        hblk = A('z_hblk', [128, 2, 8, 512], F32)
        oblk = A('z_oblk', [128, 2, 8, 512], F32)
        ftile = A('z_ftile', [128, 2, 1024], F32)
        sq = A('z_sq', [128, 8, 512], BF16)
        rstd = A('z_rstd', [128, 512], F32)
        gfin = A('z_gfin', [128, 8], F32)
        kb.dma('sp', gfin[:], c.fin_g[:, :], w=['gfin'])
        hTv = c.hT.rearrange("(k p) t -> p k t", p=128)
        oTv = c.outT.rearrange("(k p) t -> p k t", p=128)
        for tb in range(NTB):
            b = tb % 2
            ts = slice(tb * 512, (tb + 1) * 512)
            kb.dma('sp', hblk[:, b], hTv[:, :, ts], w=[('hblk', b)])
            if 'skip_ffn' not in c.dbg:
                emit_add_tm(c, hblk[:, b], ('hblk', b), tb, ftile, (0, 1))
            emit_rmsnorm(c, hblk[:, b], ('hblk', b), gfin, 'gfin', lambda k: oblk[:, b, k, :], [('oblk', b)], 2, sq[:], 'sq', rstd[:], 'rstd')
            kb.dma('sp', oTv[:, :, ts], oblk[:, b], r=[('oblk', b)], w=[('outT', tb)])
        kb.barrier()


_CACHE = {}


def kernel(**inputs):
    x = np.asarray(inputs['x'], np.float32)
    B, T, _ = x.shape
    L = np.asarray(inputs['w_in']).shape[0]
    preps = [prep_layer(inputs, l) for l in range(L)]
    shapes = layer_shapes_from(preps[0])
    key = (T, L)
    if key not in _CACHE:
        _CACHE[key] = build(T, L, shapes)
    nc, c = _CACHE[key]
    common = {'consts': make_consts(), 'iota': make_iota(), 'fin_g': _pcol(np.asarray(inputs['final_norm_g'], np.float32), 8)}
    for l in range(L):
        for k, v in preps[l].items():
            common['l%d_%s' % (l, k)] = v
    in_maps = []
    for b in range(B):
        m = dict(common)
        m['xT'] = np.ascontiguousarray(x[b].T)
        in_maps.append(m)
    res = run_bass_kernel_spmd(nc, in_maps, core_ids=list(range(B)))
    out = np.stack([np.ascontiguousarray(res.results[b]['outT'].T) for b in range(B)], axis=0)
    return out.astype(np.float32)
```

```python
import math
from contextlib import ExitStack
import numpy as np
import concourse.bass as bass
import concourse.mybir as mybir
from concourse.bass_utils import run_bass_kernel_spmd

F32 = mybir.dt.float32
BF16 = mybir.dt.bfloat16
I32 = mybir.dt.int32
U32 = mybir.dt.uint32
AF = mybir.ActivationFunctionType
ALU = mybir.AluOpType
AX = mybir.AxisListType

D = 1024
KC = 8
INW = 4896
NE = 16
FF = 2048
EPS = 1e-6
SAME_ENGINE_SYNC = True


class KB:
    NS = 6

    def __init__(self, nc):
        self.nc = nc
        self.eng = {'pe': nc.tensor, 'act': nc.scalar, 'dve': nc.vector, 'pool': nc.gpsimd, 'sp': nc.sync}
        self.csem = {}
        self.ccnt = {}
        for e in ['pe', 'act', 'dve', 'pool']:
            self.csem[e] = nc.semaphore('c_' + e).__enter__()
            self.ccnt[e] = 0
        self.dsem = {}
        self.dval = {}
        self.dnext = {}
        for q in ['sp', 'act', 'pool']:
            self.dsem[q] = [nc.semaphore('d_%s%d' % (q, i)).__enter__() for i in range(self.NS)]
            self.dval[q] = [0] * self.NS
            self.dnext[q] = 0
        self.semname = {}
        self.semowner = {}
        for e, s in self.csem.items():
            self.semname[id(s)] = 'c_' + e
            self.semowner['c_' + e] = e
        self.semobj = {}
        for e, s in self.csem.items():
            self.semobj['c_' + e] = s
        for q in self.dsem:
            for i, s in enumerate(self.dsem[q]):
                self.semobj['d_%s%d' % (q, i)] = s
        self.waited = {e: {} for e in self.eng}
        self.lastw = {}
        self.readers = {}
        self.ninstr = 0

    def _wait(self, e, tok):
        name, val = tok
        if val <= 0:
            return
        if self.waited[e].get(name, 0) >= val:
            return
        owner = self.semowner.get(name)
        if owner == e and (e == 'pe' or not SAME_ENGINE_SYNC):
            return
        self.eng[e].wait_ge(self.semobj[name], val)
        self.waited[e][name] = val
        self.ninstr += 1

    def _deps(self, r, w):
        toks = {}

        def add(t):
            if t is None:
                return
            if toks.get(t[0], 0) < t[1]:
                toks[t[0]] = t[1]
        for k in r:
            add(self.lastw.get(k))
        for k in w:
            add(self.lastw.get(k))
            for n, v in self.readers.get(k, {}).items():
                add((n, v))
        return list(toks.items())

    def _register(self, tok, r, w):
        for k in w:
            self.lastw[k] = tok
            self.readers[k] = {}
        for k in r:
            d = self.readers.setdefault(k, {})
            if d.get(tok[0], 0) < tok[1]:
                d[tok[0]] = tok[1]

    def op(self, e, fn, r=(), w=()):
        psk = [k for k in r if isinstance(k, tuple) and k[0] == 'ps']
        toks = dict(self._deps(r, w))
        me = 'c_' + e
        for k in psk:
            for n, v in self.readers.get(k, {}).items():
                if n != me and toks.get(n, 0) < v:
                    toks[n] = v
        for t in toks.items():
            self._wait(e, t)
        ins = fn(self.eng[e])
        self.ccnt[e] += 1
        ins.then_inc(self.csem[e], 1)
        tok = ('c_' + e, self.ccnt[e])
        self._register(tok, r, w)
        self.ninstr += 1
        return tok

    def dma(self, q, out, in_, r=(), w=(), **kw):
        slot = self.dnext[q] % self.NS
        self.dnext[q] += 1
        name = 'd_%s%d' % (q, slot)
        self._wait(q, (name, self.dval[q][slot]))
        for t in self._deps(r, w):
            self._wait(q, t)
        ins = self.eng[q].dma_start(out=out, in_=in_, **kw)
        ins.then_inc(self.dsem[q][slot], 16)
        self.dval[q][slot] += 16
        tok = (name, self.dval[q][slot])
        self._register(tok, r, w)
        self.ninstr += 1
        return tok

    def idma(self, fn, r=(), w=()):
        q = 'pool'
        slot = self.dnext[q] % self.NS
        self.dnext[q] += 1
        name = 'd_%s%d' % (q, slot)
        self._wait(q, (name, self.dval[q][slot]))
        for t in self._deps(r, w):
            self._wait(q, t)
        ins = fn(self.eng[q])
        ins.then_inc(self.dsem[q][slot], 16)
        self.dval[q][slot] += 16
        tok = (name, self.dval[q][slot])
        self._register(tok, r, w)
        self.ninstr += 1
        return tok

    def barrier(self):
        toks = [('c_' + e, self.ccnt[e]) for e in self.ccnt]
        for q in self.dsem:
            for i in range(self.NS):
                toks.append(('d_%s%d' % (q, i), self.dval[q][i]))
        for e in self.eng:
            for t in toks:
                self._wait(e, t)
        self.lastw = {}
        self.readers = {}


class Ctx:
    pass


_NMC = [0]


def _nm(name):
    _NMC[0] += 1
    return '%s_%d' % (name, _NMC[0])


def _bc_mid(ap2d, n):
    return ap2d.unsqueeze(1).broadcast_to([ap2d.shape[0], n, ap2d.shape[1]])


def _bc_last(ap2d, n):
    return ap2d.unsqueeze(2).broadcast_to([ap2d.shape[0], ap2d.shape[1], n])


def _pcol(v, nch):
    return np.ascontiguousarray(np.asarray(v, np.float32).reshape(nch, 128).T)


def prep_layer(inp, l):
    f = lambda k: np.asarray(inp[k], np.float32)
    o = {}
    o['w_in'] = np.ascontiguousarray(f('w_in')[l])
    o['mix_g'] = _pcol(f('mix_norm_g')[l], 8)
    o['ffn_g'] = _pcol(f('ffn_norm_g')[l], 8)
    cw = f('lru_conv_w')[l]
    o['lru_cw'] = np.ascontiguousarray(cw.reshape(4, 4, 128).transpose(2, 1, 0))
    o['lru_cb'] = _pcol(f('lru_conv_b')[l], 4)
    cwd = np.zeros((128, 4, 4, 128), np.float32)
    for ct_ in range(4):
        for j_ in range(4):
            cwd[np.arange(128), ct_, j_, np.arange(128)] = cw[j_, ct_ * 128:(ct_ + 1) * 128]
    o['lru_cwd'] = cwd
    for nm in ['a', 'x']:
        w = f('lru_w_' + nm)[l]
        bd = np.zeros((128, 2, 4, 128), np.float32)
        for di in range(2):
            for ct in range(4):
                bd[0:64, di, ct, 0:64] = w[di, 2 * ct]
                bd[64:128, di, ct, 64:128] = w[di, 2 * ct + 1]
        o['lru_w' + nm] = bd
        b = f('lru_b_' + nm)[l]
        o['lru_b' + nm] = np.ascontiguousarray(b.reshape(2, 4, 128).transpose(2, 0, 1))
    o['lru_lam'] = np.ascontiguousarray(f('lru_lam')[l].reshape(2, 4, 128).transpose(2, 0, 1))
    prep_gla(inp, l, o)
    prep_s5(inp, l, o)
    prep_rest(inp, l, o)
    return o


def build(T, nlayers, layer_shapes, dbg=()):
    nc = bass.Bass("TRN2", target_bir_lowering=False)
    kb = KB(nc)
    NTB = T // 512
    NT = T // 128
    c = Ctx()
    c.nc, c.kb, c.T, c.NTB, c.NT = nc, kb, T, NTB, NT

    def dram_in(name, shape, dt=F32):
        return nc.dram_tensor(name, list(shape), dt, kind="ExternalInput").ap()

    def dram_out(name, shape, dt=F32):
        return nc.dram_tensor(name, list(shape), dt, kind="ExternalOutput").ap()

    def dram_tmp(name, shape, dt=F32):
        return nc.dram_tensor(name, list(shape), dt, kind="Internal").ap()

    c.xT = dram_in('xT', [D, T])
    c.consts = dram_in('consts', [128, NCONST, 128])
    c.iota = dram_in('iota', [128, 513])
    c.fin_g = dram_in('fin_g', [128, 8])
    c.lw = []
    for l in range(nlayers):
        c.lw.append({k: dram_in('l%d_%s' % (l, k), shp) for k, shp in layer_shapes.items()})
    c.outT = dram_out('outT', [D, T])
    c.hT = dram_tmp('hT', [D, T])
    c.pmixT = dram_tmp('pmixT', [768, T])
    c.gT = dram_tmp('gT', [3072, T], BF16)
    c.ptm = dram_tmp('ptm', [T, 1056])
    c.alrT = dram_tmp('alrT', [32, T], BF16)
    c.yT = dram_tmp('yT', [1024, T], BF16)
    c.ffn_tm = dram_tmp('ffn_tm', [T, D])
    c.hn_tm = dram_tmp('hn_tm', [T, D], BF16)
    c.dbg = {}
    c.lvl = 99
    for name, shp in dbg:
        if name.startswith('lvl'):
            c.lvl = int(name[3:])
            continue
        c.dbg[name] = dram_out('dbg_' + name, shp)

    c.ps = [nc.psum_tensor('ps%d' % i, [128, 512], F32).__enter__() for i in range(8)]

    c.ones_bf = nc.sbuf_tensor(_nm('ones_bf'), [128, 128], BF16).__enter__()
    kb.op('dve', lambda e: e.memset(c.ones_bf[:], 1.0), w=['ones_bf'])

    load_consts(c)
    for l in range(nlayers):
        stage_norm_proj(c, l, src=(c.xT if l == 0 else c.hT), add_tm=(l > 0))
        kb.barrier()
        if 'skip_lru' not in c.dbg:
            stage_lru(c, l)
        kb.barrier()
        if 'skip_gla' not in c.dbg:
            stage_gla(c, l)
        kb.barrier()
        if 'skip_s5' not in c.dbg:
            stage_s5(c, l)
        kb.barrier()
        if 'stop_mixers' in c.dbg:
            continue
        CAPT = (2 * T // NE) // 128
        with ExitStack() as esl:
            seli = esl.enter_context(nc.sbuf_tensor(_nm('f_seli'), [128, NE, CAPT], I32))
            selg = esl.enter_context(nc.sbuf_tensor(_nm('f_selg'), [128, NE, CAPT], F32))
            with nc.sbuf_tensor(_nm('hnT'), [128, KC, T], BF16) as hnT:
                stage_merge(c, l, hnT)
                kb.barrier()
                if 'skip_ffn' not in c.dbg:
                    stage_ffn(c, l, hnT, seli, selg, 0)
                kb.barrier()
            if 'skip_ffn' not in c.dbg:
                stage_ffn(c, l, None, seli, selg, 1)
            kb.barrier()
    if 'stop_mixers' not in c.dbg:
        stage_final(c)
    kb.barrier()
    return nc, c


def stage_norm_proj(c, l, src, add_tm=False):
    nc, kb, T, NTB, NT = c.nc, c.kb, c.T, c.NTB, c.NT
    W = c.lw[l]
    with ExitStack() as es:
        xn = es.enter_context(nc.sbuf_tensor(_nm('xn'), [128, KC, T], BF16))
        hblk = es.enter_context(nc.sbuf_tensor(_nm('hblk'), [128, 2, KC, 512], F32))
        sq = es.enter_context(nc.sbuf_tensor(_nm('sq'), [128, 2, KC, 512], BF16))
        rstd = es.enter_context(nc.sbuf_tensor(_nm('rstd'), [128, 2, 512], F32))
        g_mix = es.enter_context(nc.sbuf_tensor(_nm('g_mix'), [128, KC], F32))
        wA = es.enter_context(nc.sbuf_tensor(_nm('wA'), [128, KC, 768], BF16))
        wB = es.enter_context(nc.sbuf_tensor(_nm('wB'), [128, KC, 1056], BF16))
        wG = es.enter_context(nc.sbuf_tensor(_nm('wG'), [128, 2, KC, 512], BF16))
        stf = es.enter_context(nc.sbuf_tensor(_nm('stf'), [128, 4, 512], F32))
        stg = es.enter_context(nc.sbuf_tensor(_nm('stg'), [128, 4, 512], BF16))
        sttm = es.enter_context(nc.sbuf_tensor(_nm('sttm'), [128, 2, 1056], F32))
        stalr = es.enter_context(nc.sbuf_tensor(_nm('stalr'), [32, T], BF16))
        ftile = es.enter_context(nc.sbuf_tensor(_nm('ftile'), [128, 2, 1024], F32))
        kb.dma('sp', g_mix[:], W['mix_g'][:, :], w=['g_mix'])
        w_in_v = W['w_in'].rearrange("(k p) c -> p k c", p=128)
        kb.dma('pool', wA[:], w_in_v[:, :, 0:768], w=['wA'])
        kb.dma('pool', wB[:], w_in_v[:, :, 768:1824], w=['wB'])
        srcv = src.rearrange("(k p) t -> p k t", p=128)
        for tb in range(NTB):
            b = tb % 2
            ts = slice(tb * 512, (tb + 1) * 512)
            kb.dma('sp', hblk[:, b], srcv[:, :, ts], w=[('hblk', b)])
            if add_tm:
                emit_add_tm(c, hblk[:, b], ('hblk', b), tb, ftile, (2, 3))
                kb.dma('sp', srcv[:, :, ts], hblk[:, b], r=[('hblk', b)], w=[('hTw', tb)])
            kb.op('act', lambda e: e.activation(out=sq[:, b], in_=hblk[:, b], func=AF.Square),
                  r=[('hblk', b)], w=[('sq', b)])
            pb = c.ps[b]
            for k in range(KC):
                kb.op('pe', lambda e, k=k: e.matmul(pb[:], lhsT=c.ones_bf[:], rhs=sq[:, b, k, :],
                                                    start=(k == 0), stop=(k == KC - 1)),
                      r=[('sq', b), 'ones_bf'], w=[('ps', b)])
            kb.op('act', lambda e: e.activation(out=rstd[:, b], in_=pb[:], func=AF.Sqrt,
                                                scale=1.0 / D, bias=EPS),
                  r=[('ps', b)], w=[('rstd', b)])
            kb.op('dve', lambda e: e.reciprocal(out=rstd[:, b], in_=rstd[:, b]),
                  r=[('rstd', b)], w=[('rstd', b)])
            for k in range(KC):
                kb.op('dve', lambda e, k=k: e.scalar_tensor_tensor(
                    out=xn[:, k, ts], in0=hblk[:, b, k, :], scalar=g_mix[:, k:k + 1], in1=rstd[:, b],
                    op0=ALU.mult, op1=ALU.mult),
                    r=[('hblk', b), ('rstd', b), 'g_mix'], w=[('xn', tb)])
        nps = 0
        nst = 0
        for cc in range(6):
            for tb in range(NTB):
                ts = slice(tb * 512, (tb + 1) * 512)
                pi = 2 + (nps % 4)
                nps += 1
                sb = nst % 4
                nst += 1
                pb = c.ps[pi]
                for k in range(KC):
                    kb.op('pe', lambda e, k=k, pb=pb: e.matmul(
                        pb[:], lhsT=wA[:, k, cc * 128:(cc + 1) * 128], rhs=xn[:, k, ts],
                        start=(k == 0), stop=(k == KC - 1)),
                        r=['wA', ('xn', tb)], w=[('ps', pi)])
                if tb % 2 == 0:
                    kb.op('act', lambda e, pb=pb: e.copy(out=stf[:, sb, :], in_=pb[:]),
                          r=[('ps', pi)], w=[('stf', sb)])
                else:
                    kb.op('dve', lambda e, pb=pb: e.tensor_copy(out=stf[:, sb, :], in_=pb[:]),
                          r=[('ps', pi)], w=[('stf', sb)])
                kb.dma('sp', c.pmixT[cc * 128:(cc + 1) * 128, ts], stf[:, sb, :], r=[('stf', sb)], w=[('pmixT', cc, tb)])
        for tb in range(NTB):
            ts = slice(tb * 512, (tb + 1) * 512)
            pi = 2 + (nps % 4)
            nps += 1
            pb = c.ps[pi]
            for k in range(KC):
                kb.op('pe', lambda e, k=k, pb=pb: e.matmul(
                    pb[0:32, :], lhsT=wB[:, k, 1024:1056], rhs=xn[:, k, ts],
                    start=(k == 0), stop=(k == KC - 1)),
                    r=['wB', ('xn', tb)], w=[('ps', pi)])
            kb.op('act', lambda e, pb=pb: e.copy(out=stalr[:, ts], in_=pb[0:32, :]),
                  r=[('ps', pi)], w=[('stalr', tb)])
        kb.dma('sp', c.alrT[:, :], stalr[:, :], r=[('stalr', tb) for tb in range(NTB)], w=['alrT'])
        for tt in range(NT):
            sb = tt % 2
            tb = tt // 4
            tsl = slice(tt * 128, (tt + 1) * 128)
            for gi, (c0, c1) in enumerate([(0, 512), (512, 1024), (1024, 1056)]):
                pi = 2 + (nps % 4)
                nps += 1
                pb = c.ps[pi]
                for k in range(KC):
                    kb.op('pe', lambda e, k=k, pb=pb: e.matmul(
                        pb[:, 0:c1 - c0], lhsT=xn[:, k, tsl], rhs=wB[:, k, c0:c1],
                        start=(k == 0), stop=(k == KC - 1)),
                        r=['wB', ('xn', tb)], w=[('ps', pi)])
                if gi == 0:
                    kb.op('act', lambda e, pb=pb: e.copy(out=sttm[:, sb, c0:c1], in_=pb[:, 0:c1 - c0]),
                          r=[('ps', pi)], w=[('sttm', sb, gi)])
                else:
                    kb.op('dve', lambda e, pb=pb: e.tensor_copy(out=sttm[:, sb, c0:c1], in_=pb[:, 0:c1 - c0]),
                          r=[('ps', pi)], w=[('sttm', sb, gi)])
            kb.dma('sp', c.ptm[tsl, :], sttm[:, sb, :], r=[('sttm', sb, gi) for gi in range(3)],
                   w=[('ptm', tt)])
        for cg in range(6):
            wb_ = cg % 2
            kb.dma('pool', wG[:, wb_], w_in_v[:, :, 1824 + cg * 512:1824 + (cg + 1) * 512], w=[('wG', wb_)])
            for c4 in range(4):
                cc = cg * 4 + c4
                for tb in range(NTB):
                    ts = slice(tb * 512, (tb + 1) * 512)
                    pi = 2 + (nps % 4)
                    nps += 1
                    pb = c.ps[pi]
                    for k in range(KC):
                        kb.op('pe', lambda e, k=k, pb=pb: e.matmul(
                            pb[:], lhsT=wG[:, wb_, k, c4 * 128:(c4 + 1) * 128], rhs=xn[:, k, ts],
                            start=(k == 0), stop=(k == KC - 1)),
                            r=[('wG', wb_), ('xn', tb)], w=[('ps', pi)])
                    sb = nst % 4
                    nst += 1
                    kb.op('act', lambda e, pb=pb: e.activation(out=stg[:, sb, :], in_=pb[:], func=AF.Sigmoid),
                          r=[('ps', pi)], w=[('stg', sb)])
                    kb.dma('sp', c.gT[cc * 128:(cc + 1) * 128, ts], stg[:, sb, :], r=[('stg', sb)], w=[('gT', cc, tb)])
        if 'pmixT' in c.dbg:
            kb.barrier()
            kb.dma('sp', c.dbg['pmixT'][:, :], c.pmixT[:, :], w=['dbg_pmixT'])
        kb.barrier()


def stage_lru(c, l):
    nc, kb, T, NTB, NT = c.nc, c.kb, c.T, c.NTB, c.NT
    W = c.lw[l]
    with ExitStack() as es:
        xbp = es.enter_context(nc.sbuf_tensor(_nm('xbpb'), [128, T + 4], BF16))
        l_cwd = es.enter_context(nc.sbuf_tensor(_nm('l_cwd'), [128, 4, 4, 128], BF16))
        xc = es.enter_context(nc.sbuf_tensor(_nm('xc'), [128, T], F32))
        xcb = es.enter_context(nc.sbuf_tensor(_nm('xcb'), [128, T], BF16))
        bA = es.enter_context(nc.sbuf_tensor(_nm('bA'), [128, T], F32))
        bB = es.enter_context(nc.sbuf_tensor(_nm('bB'), [128, T], F32))
        bC = es.enter_context(nc.sbuf_tensor(_nm('bC'), [128, T], F32))
        hf = es.enter_context(nc.sbuf_tensor(_nm('hf'), [128, T], F32))
        ybf = es.enter_context(nc.sbuf_tensor(_nm('ybf'), [128, 2, T], BF16))
        l_cw = es.enter_context(nc.sbuf_tensor(_nm('l_cw'), [128, 4, 4], F32))
        l_cb = es.enter_context(nc.sbuf_tensor(_nm('l_cb'), [128, 4], F32))
        l_wa = es.enter_context(nc.sbuf_tensor(_nm('l_wa'), [128, 2, 4, 128], BF16))
        l_wx = es.enter_context(nc.sbuf_tensor(_nm('l_wx'), [128, 2, 4, 128], BF16))
        l_ba = es.enter_context(nc.sbuf_tensor(_nm('l_ba'), [128, 8], F32))
        l_bx = es.enter_context(nc.sbuf_tensor(_nm('l_bx'), [128, 8], F32))
        l_lam = es.enter_context(nc.sbuf_tensor(_nm('l_lam'), [128, 8], F32))
        l_c = es.enter_context(nc.sbuf_tensor(_nm('l_c'), [128, 8], F32))
        l_c2 = es.enter_context(nc.sbuf_tensor(_nm('l_c2'), [128, 8], F32))
        l_t = es.enter_context(nc.sbuf_tensor(_nm('l_t'), [128, 6, 8], F32))
        kb.dma('sp', l_cw[:], W['lru_cw'][:, :, :], w=['l_cw'])
        kb.dma('pool', l_cwd[:], W['lru_cwd'][:, :, :, :], w=['l_cwd'])
        kb.dma('sp', l_cb[:], W['lru_cb'][:, :], w=['l_cb'])
        kb.dma('pool', l_wa[:], W['lru_wa'][:, :, :, :], w=['l_wa'])
        kb.dma('pool', l_wx[:], W['lru_wx'][:, :, :, :], w=['l_wx'])
        kb.dma('sp', l_ba[:], W['lru_ba'].rearrange("p a b -> p (a b)"), w=['l_ba'])
        kb.dma('sp', l_bx[:], W['lru_bx'].rearrange("p a b -> p (a b)"), w=['l_bx'])
        kb.dma('sp', l_lam[:], W['lru_lam'].rearrange("p a b -> p (a b)"), w=['l_lam'])
        P = 'l_par'
        tz, tw, tw2, tacc, tm, tabs = [l_t[:, i, :] for i in range(6)]
        dv = lambda fn, r=(P,), w=(P,): kb.op('dve', fn, r=list(r) + ['l_lam'], w=list(w))
        dv(lambda e: e.scalar_tensor_tensor(out=tabs, in0=l_lam[:], scalar=-1.0, in1=l_lam[:], op0=ALU.mult, op1=ALU.max))
        kb.op('act', lambda e: e.activation(out=tz, in_=tabs, func=AF.Exp, scale=-1.0), r=[P], w=[P])
        dv(lambda e: e.tensor_scalar(out=tw, in0=tz, scalar1=2.0, scalar2=None, op0=ALU.add))
        dv(lambda e: e.reciprocal(out=tw, in_=tw))
        dv(lambda e: e.tensor_tensor(out=tw, in0=tw, in1=tz, op=ALU.mult))
        dv(lambda e: e.tensor_tensor(out=tw2, in0=tw, in1=tw, op=ALU.mult))
        dv(lambda e: e.memset(tacc, 1.0 / 13.0))
        for kk in [11, 9, 7, 5, 3, 1]:
            dv(lambda e: e.tensor_tensor(out=tacc, in0=tacc, in1=tw2, op=ALU.mult))
            dv(lambda e, kk=kk: e.tensor_scalar(out=tacc, in0=tacc, scalar1=1.0 / kk, scalar2=None, op0=ALU.add))
        dv(lambda e: e.tensor_tensor(out=tacc, in0=tacc, in1=tw, op=ALU.mult))
        dv(lambda e: e.tensor_scalar(out=tm, in0=l_lam[:], scalar1=-1.0, scalar2=0.0, op0=ALU.mult, op1=ALU.max))
        dv(lambda e: e.scalar_tensor_tensor(out=tacc, in0=tacc, scalar=2.0, in1=tm, op0=ALU.mult, op1=ALU.add))
        dv(lambda e: e.tensor_scalar(out=l_c[:], in0=tacc, scalar1=-8.0, scalar2=None, op0=ALU.mult), w=[P, 'l_c'])
        dv(lambda e: e.tensor_scalar(out=l_c2[:], in0=tacc, scalar1=-16.0, scalar2=None, op0=ALU.mult), w=[P, 'l_c'])
        kb.op('dve', lambda e: e.memset(xbp[:, 0:2], 0.0), w=['xbp_pad'])
        kb.op('dve', lambda e: e.memset(xbp[:, T + 2:T + 4], 0.0), w=['xbp_pad'])
        nps = 0
        for ct in range(4):
            CW_ = min(2048, T)
            for c0 in range(0, T, CW_):
                kb.dma('pool', xbp[:, 2 + c0:2 + c0 + CW_], c.pmixT[256 + ct * 128:256 + (ct + 1) * 128, c0:c0 + CW_], w=[('xbp', c0)])
            for tb in range(NTB):
                ts = slice(tb * 512, (tb + 1) * 512)
                bk = 4 + tb % 2
                for j in range(4):
                    kb.op('pe', lambda e, j=j, bk=bk: e.matmul(c.ps[bk][:], lhsT=l_cwd[:, ct, j, :], rhs=xbp[:, tb * 512 + j:tb * 512 + j + 512],
                                                             start=(j == 0), stop=(j == 3)),
                          r=[('xbp', c0) for c0 in range(0, T, CW_)] + ['xbp_pad', 'l_cwd'], w=[('ps', bk)])
                kb.op('act', lambda e, bk=bk: e.activation(out=xc[:, ts], in_=c.ps[bk][:], func=AF.Identity, bias=l_cb[:, ct:ct + 1], scale=1.0),
                      r=[('ps', bk), 'l_cb'], w=['xc'])
                kb.op('dve', lambda e, bk=bk: e.tensor_scalar(out=xcb[:, ts], in0=c.ps[bk][:], scalar1=l_cb[:, ct:ct + 1], scalar2=None, op0=ALU.add),
                      r=[('ps', bk), 'l_cb'], w=['xcb'])
            for di in range(2):
                for (wt, bt, dst, key) in [(l_wa, l_ba, bA, 'bA'), (l_wx, l_bx, bB, 'bB')]:
                    for tb in range(NTB):
                        ts = slice(tb * 512, (tb + 1) * 512)
                        pi = nps % 4
                        nps += 1
                        pb = c.ps[pi]
                        kb.op('pe', lambda e, pb=pb, wt=wt: e.matmul(pb[:], lhsT=wt[:, di, ct, :], rhs=xcb[:, ts],
                                                                     start=True, stop=True),
                              r=['xcb', 'l_wa', 'l_wx'], w=[('ps', pi)])
                        kb.op('act', lambda e, pb=pb, bt=bt, dst=dst: e.activation(
                            out=dst[:, ts], in_=pb[:], func=AF.Sigmoid,
                            bias=bt[:, di * 4 + ct:di * 4 + ct + 1]),
                            r=[('ps', pi), 'l_ba', 'l_bx'], w=[key])
                cs = l_c[:, di * 4 + ct:di * 4 + ct + 1]
                c2s = l_c2[:, di * 4 + ct:di * 4 + ct + 1]
                kb.op('act', lambda e: e.activation(out=bC[:], in_=bA[:], func=AF.Exp, scale=cs),
                      r=['bA', 'l_c'], w=['bC'])
                kb.op('act', lambda e: e.activation(out=bA[:], in_=bA[:], func=AF.Exp, scale=c2s),
                      r=['bA', 'l_c'], w=['bA'])
                kb.op('act', lambda e: e.activation(out=bA[:], in_=bA[:], func=AF.Sqrt, scale=-1.0, bias=1.0),
                      r=['bA'], w=['bA'])
                kb.op('dve', lambda e: e.tensor_tensor(out=bB[:], in0=bB[:], in1=bA[:], op=ALU.mult),
                      r=['bA', 'bB'], w=['bB'])
                kb.op('dve', lambda e: e.tensor_tensor(out=bB[:], in0=bB[:], in1=xc[:], op=ALU.mult),
                      r=['xc', 'bB'], w=['bB'])
                if di == 0:
                    kb.op('dve', lambda e: e.tensor_tensor_scan(out=hf[:], data0=bC[:], data1=bB[:], initial=0.0,
                                                                op0=ALU.mult, op1=ALU.add),
                          r=['bC', 'bB'], w=['hf'])
                else:
                    rev = lambda t: bass.AP(t[:].tensor, t[:, T - 1:T].offset, [[t[:].ap[0][0], 128], [-1, T]])
                    kb.op('dve', lambda e: e.tensor_tensor_scan(out=rev(bA), data0=rev(bC), data1=rev(bB),
                                                                initial=0.0, op0=ALU.mult, op1=ALU.add),
                          r=['bC', 'bB', 'bA'], w=['bA'])
                    sb = ct % 2
                    kb.op('dve', lambda e: e.tensor_tensor(out=ybf[:, sb, :], in0=hf[:], in1=bA[:], op=ALU.add),
                          r=['hf', 'bA'], w=[('ybf', sb)])
                    kb.dma('sp', c.yT[256 + ct * 128:256 + (ct + 1) * 128, :], ybf[:, sb, :],
                           r=[('ybf', sb)], w=[('yT', 2 + ct)])
        if 'ylru' in c.dbg:
            kb.barrier()
            with nc.sbuf_tensor(_nm('dbgt'), [128, T], F32) as dbgt:
                for ct in range(4):
                    kb.dma('pool', dbgt[:], c.yT[256 + ct * 128:256 + (ct + 1) * 128, :], w=['dbgt'])
                    kb.dma('sp', c.dbg['ylru'][ct * 128:(ct + 1) * 128, :], dbgt[:], r=['dbgt'], w=[('dbgo', ct)])
                kb.barrier()
        kb.barrier()


LAYER_SHAPES = None


def layer_shapes_from(prep):
    return {k: v.shape for k, v in prep.items()}


NCONST = 10


def make_consts():
    s = np.arange(128)[:, None]
    t = np.arange(128)[None, :]
    cst = np.zeros((128, NCONST, 128), np.float32)
    cst[:, 0] = (s == t)
    cst[:, 1] = (s <= t) * (-1.0 / 16)
    cst[:, 2] = (s >= t) * (-1.0 / 16)
    cst[:, 3] = -1.0 / 16
    cst[:, 4] = (s <= t)
    cst[:, 5] = (s > t)
    cst[:, 6] = (s < t)
    cst[:, 7] = 1.0
    cst[:, 8] = (s <= t)
    cst[:, 9] = (s >= t)
    return cst


def prep_gla(inp, l, o):
    f = lambda k: np.asarray(inp[k], np.float32)
    wal = np.zeros((33, 512), np.float32)
    wal[0:16, 0:256] = f('gla_w_alpha')[l, 0]
    wal[16:32, 256:512] = f('gla_w_alpha')[l, 1]
    wal[32, 0:256] = f('gla_b_alpha')[l, 0]
    wal[32, 256:512] = f('gla_b_alpha')[l, 1]
    o['gla_wal'] = wal
    o['gla_ng'] = np.ascontiguousarray(np.broadcast_to(f('gla_norm_g')[l][None, :], (128, 64)))


def load_consts(c):
    nc, kb = c.nc, c.kb
    c.cst = nc.sbuf_tensor(_nm('cst'), [128, NCONST, 128], F32).__enter__()
    c.cstb = nc.sbuf_tensor(_nm('cstb'), [128, NCONST, 128], BF16).__enter__()
    kb.dma('sp', c.cst[:], c.consts[:, :, :], w=['cst'])
    kb.dma('pool', c.cstb[:], c.consts[:, :, :], w=['cst'])


def stage_gla(c, l):
    nc, kb, T, NTB, NT = c.nc, c.kb, c.T, c.NTB, c.NT
    W = c.lw[l]
    cst, cstb = c.cst, c.cstb
    ident_b = cstb[:, 0, :]
    with ExitStack() as es:
        A = lambda name, shp, dt: es.enter_context(nc.sbuf_tensor(_nm(name), shp, dt))
        ptile = A('g_pt', [128, 2, 1056], F32)
        alrx = A('g_alrx', [64, T], BF16)
        wal = A('g_wal', [64, 512], BF16)
        spt = A('g_sp', [128, 512], F32)
        E1 = A('g_E1', [128, 512], F32)
        E2 = A('g_E2', [128, 512], F32)
        decrow = A('g_decrow', [128, 512], F32)
        qd = A('g_qd', [128, 2, 256], BF16)
        kd = A('g_kd', [128, 2, 256], BF16)
        kk = A('g_kk', [128, 2, 256], BF16)
        vb = A('g_vb', [128, 256], BF16)
        qdT = A('g_qdT', [128, 4, T], BF16)
        kdT = A('g_kdT', [128, 4, 128], BF16)
        scm = A('g_scm', [128, 2, 4, 128], BF16)
        kv = A('g_kv', [128, NT, 4, 64], F32)
        dec = A('g_dec', [128, NT, 4], F32)
        oacc = A('g_oacc', [128, NT, 256], F32)
        sog = A('g_sog', [128, NT, 256], BF16)
        S = A('g_S', [128, 4, 64], F32)
        Sb = A('g_Sb', [128, 4, 64], BF16)
        ng = A('g_ng', [128, 64], F32)
        ygT = A('g_ygT', [128, 2, T], BF16)
        ss = A('g_ss', [128, NT * 4], F32)
        ps = c.ps
        kb.op('dve', lambda e: e.memset(alrx[32:64, :], 1.0), w=['alrx1'])
        kb.dma('sp', alrx[0:32, :], c.alrT[:, :], w=['alrx'])
        kb.op('dve', lambda e: e.memset(wal[:], 0.0), w=['wal'])
        kb.dma('pool', wal[0:33, :], W['gla_wal'][:, :], r=[], w=['wal'])
        kb.dma('sp', ng[:], W['gla_ng'][:, :], w=['ng'])
        for n in range(NT):
            b = n % 2
            nsl = slice(n * 128, (n + 1) * 128)
            kb.dma('sp', ptile[:, b, :], c.ptm[nsl, :], w=[('pt', b)])
            q_ap = ptile[:, b, 0:256]
            k_ap = ptile[:, b, 256:512]
            v_ap = ptile[:, b, 512:768]
            og_ap = ptile[:, b, 768:1024]
            kb.op('pe', lambda e: e.matmul(ps[0][:], lhsT=alrx[0:33, nsl], rhs=wal[0:33, :], start=True, stop=True),
                  r=['alrx', 'alrx1', 'wal'], w=[('ps', 0)])
            kb.op('act', lambda e: e.activation(out=spt[:], in_=ps[0][:], func=AF.Exp, scale=-1.0),
                  r=[('ps', 0)], w=['spt'])
            kb.op('act', lambda e: e.activation(out=spt[:], in_=spt[:], func=AF.Ln, bias=1.0),
                  r=['spt'], w=['spt'])
            if 'gla_stop0' in c.dbg:
                continue
            kb.op('pe', lambda e: e.matmul(ps[1][:, 0:256], lhsT=cst[:, 1, :], rhs=spt[:, 0:256], start=True, stop=True),
                  r=['spt', 'cst'], w=[('ps', 1)])
            kb.op('pe', lambda e: e.matmul(ps[1][:, 256:512], lhsT=cst[:, 2, :], rhs=spt[:, 256:512], start=True, stop=True),
                  r=['spt', 'cst'], w=[('ps', 1)])
            kb.op('pe', lambda e: e.matmul(ps[2][:], lhsT=cst[:, 3, :], rhs=spt[:], start=True, stop=True),
                  r=['spt', 'cst'], w=[('ps', 2)])
            if c.lvl < 1:
                continue
            for i4 in range(4):
                kb.op('pe', lambda e, i4=i4: e.matmul(ps[6][:, 256 + 2 * i4:258 + 2 * i4], lhsT=spt[:, i4 * 128:(i4 + 1) * 128],
                                                     rhs=cst[:, 3, 0:2], start=True, stop=True),
                      r=['spt', 'cst'], w=[('ps', 6)])
            kb.op('act', lambda e: e.activation(out=dec[:, n, :], in_=ps[6][:, 256:264:2], func=AF.Exp),
                  r=[('ps', 6)], w=['dec'])
            if c.lvl < 2:
                continue
            kb.op('act', lambda e: e.activation(out=E1[:], in_=ps[1][:], func=AF.Exp), r=[('ps', 1)], w=['E1'])
            kb.op('act', lambda e: e.activation(out=E2[:], in_=ps[1][:], func=AF.Exp, scale=-1.0), r=[('ps', 1)], w=['E2'])
            kb.op('act', lambda e: e.activation(out=decrow[:], in_=ps[2][:], func=AF.Exp), r=[('ps', 2)], w=['decrow'])
            if c.lvl < 3:
                continue
            v2 = lambda ap: ap.rearrange("p (a b) -> p a b", a=2)
            kb.op('dve', lambda e: e.scalar_tensor_tensor(out=qd[:], in0=_bc_mid(q_ap, 2), scalar=0.125, in1=v2(E1[:]),
                                                          op0=ALU.mult, op1=ALU.mult),
                  r=[('pt', b), 'E1'], w=['qd'])
            kb.op('dve', lambda e: e.tensor_tensor(out=kd[:], in0=_bc_mid(k_ap, 2), in1=v2(E2[:]), op=ALU.mult),
                  r=[('pt', b), 'E2'], w=['kd'])
            kb.op('dve', lambda e: e.tensor_tensor(out=E2[:], in0=E2[:], in1=decrow[:], op=ALU.mult),
                  r=['E2', 'decrow'], w=['E2'])
            kb.op('dve', lambda e: e.tensor_tensor(out=kk[:], in0=_bc_mid(k_ap, 2), in1=v2(E2[:]), op=ALU.mult),
                  r=[('pt', b), 'E2'], w=['kk'])
            if c.lvl < 4:
                continue
            kb.op('act', lambda e: e.copy(out=vb[:], in_=v_ap), r=[('pt', b)], w=['vb'])
            kb.op('act', lambda e: e.activation(out=decrow[:, 0:256], in_=og_ap, func=AF.Sigmoid), r=[('pt', b), 'E2'], w=['decrow'])
            kb.op('dve', lambda e: e.tensor_tensor(out=sog[:, n, :], in0=og_ap, in1=decrow[:, 0:256], op=ALU.mult), r=[('pt', b), 'decrow'], w=['sog'])
            if c.lvl < 5:
                continue
            psE = ps[3][:].bitcast(BF16)
            for i8 in range(8):
                src = (qd if i8 < 4 else kd)
                di, ct = (i8 % 4) // 2, i8 % 2
                kb.op('pe', lambda e, src=src, di=di, ct=ct, i8=i8: e.transpose(
                    out=psE[:, i8 * 128:(i8 + 1) * 128], in_=src[:, di, ct * 128:(ct + 1) * 128], identity=ident_b),
                    r=['qd', 'kd', 'cst'], w=[('ps', 3)])
            if 'noevac' in c.dbg:
                continue
            kb.op('act', lambda e: e.copy(out=qdT[:, :, nsl], in_=psE[:, 0:512].rearrange("p (a b) -> p a b", a=4)),
                  r=[('ps', 3)], w=[('qdT', n)])
            if 'evac_act_only' in c.dbg:
                continue
            kb.op('act', lambda e: e.copy(out=kdT[:], in_=psE[:, 512:1024].rearrange("p (a b) -> p a b", a=4)),
                  r=[('ps', 3)], w=['kdT'])
            if c.lvl < 6:
                continue
            for di in range(2):
                for h in range(4):
                    ct, r0 = h // 2, (h % 2) * 64
                    cb = di * 2 + h // 2
                    kb.op('pe', lambda e, di=di, h=h, ct=ct, r0=r0, cb=cb: e.matmul(
                        ps[4 + h % 2][:, cb * 128:(cb + 1) * 128], lhsT=kdT[r0:r0 + 64, di * 2 + ct, :],
                        rhs=qdT[r0:r0 + 64, di * 2 + ct, nsl], start=True, stop=True),
                        r=['kdT', ('qdT', n)], w=[('ps', 4 + h % 2)])
            mask4 = cst[:, 4:6, :].unsqueeze(2).broadcast_to([128, 2, 2, 128])
            for rg in range(2):
                kb.op('dve', lambda e, rg=rg: e.tensor_tensor(
                    out=scm[:, :, rg::2, :], in0=ps[4 + rg][:].rearrange("p (a b c) -> p a b c", a=2, b=2),
                    in1=mask4, op=ALU.mult),
                    r=[('ps', 4 + rg), 'cst'], w=[('scm', 0), ('scm', 1)])
            if c.lvl < 7:
                continue
            for h in range(4):
                for di in range(2):
                    kb.op('pe', lambda e, di=di, h=h: e.matmul(
                        ps[6][:, h * 64:(h + 1) * 64], lhsT=scm[:, di, h, :], rhs=vb[:, h * 64:(h + 1) * 64],
                        start=(di == 0), stop=(di == 1)),
                        r=[('scm', 0), ('scm', 1), 'vb'], w=[('ps', 6)])
            kb.op('act', lambda e: e.copy(out=oacc[:, n, :], in_=ps[6][:, 0:256]), r=[('ps', 6)], w=[('oacc', n)])
            if c.lvl < 8:
                continue
            for di in range(2):
                for hp in range(2):
                    i4 = di * 2 + hp
                    kb.op('pe', lambda e, di=di, hp=hp, i4=i4: e.matmul(
                        ps[7][:, i4 * 128:(i4 + 1) * 128], lhsT=kk[:, di, hp * 128:(hp + 1) * 128],
                        rhs=vb[:, hp * 128:(hp + 1) * 128], start=True, stop=True),
                        r=['kk', 'vb'], w=[('ps', 7)])
            p7 = ps[7][:].rearrange("p (a b) -> p a b", a=4)
            kb.op('act', lambda e: e.copy(out=kv[0:64, n, :, :], in_=p7[0:64, :, 0:64]), r=[('ps', 7)], w=[('kv', n, 0)])
            kb.op('dve', lambda e: e.tensor_copy(out=kv[64:128, n, :, :], in_=p7[64:128, :, 64:128]),
                  r=[('ps', 7)], w=[('kv', n, 1)])
        if 'gla_stop1' in c.dbg:
            kb.barrier()
            return
        kb.op('dve', lambda e: e.memset(S[:], 0.0), w=['S0', 'S1'])
        kb.op('dve', lambda e: e.memset(Sb[:], 0.0), w=['Sb0', 'Sb1'])
        for idx in range(NT):
            for di in range(2):
                n = idx if di == 0 else NT - 1 - idx
                nsl = slice(n * 128, (n + 1) * 128)
                if idx > 0:
                    for h in range(4):
                        hp, r0 = h // 2, (h % 2) * 64
                        bk = di * 2 + h % 2
                        kb.op('pe', lambda e, h=h, hp=hp, r0=r0, bk=bk: e.matmul(
                            ps[bk][:, hp * 64:(hp + 1) * 64], lhsT=qdT[r0:r0 + 64, di * 2 + hp, nsl],
                            rhs=Sb[r0:r0 + 64, di * 2 + hp, :], start=True, stop=True),
                            r=[('qdT', n), 'Sb%d' % di], w=[('ps', bk)])
                    for rg in range(2):
                        bk = di * 2 + rg
                        ov = oacc[:, n, :].rearrange("p (a b c) -> p a b c", a=2, b=2)[:, :, rg, :]
                        kb.op('dve', lambda e, bk=bk, ov=ov: e.tensor_tensor(
                            out=ov, in0=ov, in1=ps[bk][:, 0:128].rearrange("p (a c) -> p a c", a=2), op=ALU.add),
                            r=[('ps', bk), ('oacc', n)], w=[('oacc', n)])
                if idx < NT - 1:
                    for hp in range(2):
                        i4 = di * 2 + hp
                        kb.op('dve', lambda e, i4=i4: e.scalar_tensor_tensor(
                            out=S[:, i4, :], in0=S[:, i4, :], scalar=dec[:, n, i4:i4 + 1], in1=kv[:, n, i4, :],
                            op0=ALU.mult, op1=ALU.add),
                            r=['S%d' % di, 'dec', ('kv', n, 0), ('kv', n, 1)], w=['S%d' % di])
                    kb.op('act', lambda e: e.copy(out=Sb[:, di * 2:di * 2 + 2, :], in_=S[:, di * 2:di * 2 + 2, :]),
                          r=['S%d' % di], w=['Sb%d' % di])
        allo = [('oacc', n) for n in range(NT)]
        sqv = kv[:].rearrange("p n a b -> p (n a b)")
        oflat = oacc[:].rearrange("p n c -> p (n c)")
        kb.op('act', lambda e: e.activation(out=sqv, in_=oflat, func=AF.Square), r=allo + [('kv', n, j) for n in range(NT) for j in range(2)],
              w=['sqv'])
        kb.op('dve', lambda e: e.tensor_reduce(out=ss[:], in_=sqv.rearrange("p (a b) -> p a b", b=64), axis=AX.X, op=ALU.add),
              r=['sqv'], w=['ss'])
        kb.op('act', lambda e: e.activation(out=ss[:], in_=ss[:], func=AF.Sqrt, scale=1.0 / 64, bias=EPS), r=['ss'], w=['ss'])
        kb.op('dve', lambda e: e.reciprocal(out=ss[:], in_=ss[:]), r=['ss'], w=['ss'])
        o3 = oflat.rearrange("p (a b) -> p a b", b=64)
        kb.op('dve', lambda e: e.tensor_tensor(out=o3, in0=o3, in1=_bc_last(ss[:], 64), op=ALU.mult), r=allo + ['ss'], w=allo)
        kb.op('dve', lambda e: e.tensor_tensor(out=o3, in0=o3, in1=_bc_mid(ng[:], NT * 4), op=ALU.mult), r=allo + ['ng'], w=allo)
        sflat = sog[:].rearrange("p n c -> p (n c)")
        kb.op('dve', lambda e: e.tensor_tensor(out=sflat, in0=oflat, in1=sflat, op=ALU.mult), r=allo + ['sog'], w=['sog'])
        for n4 in range(NT // 4):
            pb = ps[2 + n4 % 2]
            pbb = pb[:].bitcast(BF16)
            for j in range(4):
                n = n4 * 4 + j
                for ct in range(2):
                    kb.op('pe', lambda e, n=n, ct=ct, j=j: e.transpose(
                        out=pbb[:, (ct * 4 + j) * 128:(ct * 4 + j + 1) * 128], in_=sog[:, n, ct * 128:(ct + 1) * 128],
                        identity=ident_b), r=['sog', 'cst'], w=[('ps', 2 + n4 % 2)])
            kb.op('act',
                  (lambda e: e.copy(out=ygT[:, :, n4 * 512:(n4 + 1) * 512], in_=pbb.rearrange("p (a b) -> p a b", a=2))),
                  r=[('ps', 2 + n4 % 2)], w=[('ygT', n4)])
        for ct in range(2):
            kb.dma('sp', c.yT[768 + ct * 128:768 + (ct + 1) * 128, :], ygT[:, ct, :],
                   r=[('ygT', n4) for n4 in range(NT // 4)], w=[('yT', 6 + ct)])
        if 'ygla' in c.dbg:
            kb.barrier()
            with nc.sbuf_tensor(_nm('dbgt2'), [128, T], F32) as dbgt:
                for ct in range(2):
                    kb.dma('pool', dbgt[:], c.yT[768 + ct * 128:768 + (ct + 1) * 128, :], w=['dbgt'])
                    kb.dma('sp', c.dbg['ygla'][ct * 128:(ct + 1) * 128, :], dbgt[:], r=['dbgt'], w=[('dbgo', ct)])
                kb.barrier()
        kb.barrier()


TWO_PI = 2.0 * math.pi
CW1 = 6.28125
CW2 = TWO_PI - CW1


def prep_s5(inp, l, o):
    f = lambda k: np.asarray(inp[k], np.float32)
    lr, li, ldt = f('s5_lam_re')[l], f('s5_lam_im')[l], f('s5_log_dt')[l]
    par = np.zeros((2, 3, 1024), np.float32)
    par[:, 0] = lr.reshape(2, 1024)
    par[:, 1] = li.reshape(2, 1024)
    par[:, 2] = np.repeat(ldt, 64, axis=1)
    o['s5_par_tm'] = np.ascontiguousarray(np.broadcast_to(par[None], (128, 2, 3, 1024)))
    o['s5_par_sm'] = np.ascontiguousarray(par.reshape(2, 3, 8, 128).transpose(3, 0, 1, 2))
    bre, bim = f('s5_b_re')[l], f('s5_b_im')[l]
    bT = np.zeros((128, 2, 2, 2, 512), np.float32)
    cre, cim = f('s5_c_re')[l], f('s5_c_im')[l]
    cT = np.zeros((128, 2, 8, 2, 128), np.float32)
    for di in range(2):
        for g in range(16):
            k, gl = g // 8, g % 8
            bT[gl * 16:(gl + 1) * 16, di, k, 0, gl * 64:(gl + 1) * 64] = bre[di, g].T
            bT[gl * 16:(gl + 1) * 16, di, k, 1, gl * 64:(gl + 1) * 64] = bim[di, g].T
            st, g2 = g // 2, g % 2
            cT[g2 * 64:(g2 + 1) * 64, di, st, 0, gl * 16:(gl + 1) * 16] = cre[di, g].T
            cT[g2 * 64:(g2 + 1) * 64, di, st, 1, gl * 16:(gl + 1) * 16] = cim[di, g].T
    o['s5_bT'] = bT
    o['s5_cT'] = cT
    o['s5_d'] = _pcol(f('s5_d')[l], 2)
    o['s5_wglu'] = np.ascontiguousarray(f('s5_w_glu')[l].reshape(2, 128, 256).transpose(1, 0, 2))
    kc = np.zeros((128, 2 + 256), np.float32)
    s = np.arange(128, dtype=np.float32)
    kc[:, 0] = s + 1
    kc[:, 1] = 128 - s
    kc[:, 2:130] = (s + 1)[None, :]
    kc[:, 130:258] = (128 - s)[None, :]
    o['s5_kc'] = kc


def _trig(kb, ang, n_t, ni_t, cos_t, sin_t, key):
    D_ = lambda fn, r, w: kb.op('dve', fn, r=r, w=w)
    D_(lambda e: e.tensor_scalar(out=n_t, in0=ang, scalar1=1.0 / TWO_PI, scalar2=None, op0=ALU.mult), [key], [key + 'n'])
    D_(lambda e: e.tensor_copy(out=ni_t, in_=n_t), [key + 'n'], [key + 'ni'])
    D_(lambda e: e.tensor_copy(out=n_t, in_=ni_t), [key + 'ni'], [key + 'n'])
    for (dst, shift) in [(sin_t, 0.0), (cos_t, math.pi / 2)]:
        D_(lambda e: e.scalar_tensor_tensor(out=dst, in0=n_t, scalar=-CW1, in1=ang, op0=ALU.mult, op1=ALU.add),
           [key, key + 'n'], [key + 'o'])
        D_(lambda e: e.scalar_tensor_tensor(out=dst, in0=n_t, scalar=-CW2, in1=dst, op0=ALU.mult, op1=ALU.add),
           [key + 'n', key + 'o'], [key + 'o'])
        if shift != 0.0:
            D_(lambda e: e.tensor_scalar(out=dst, in0=dst, scalar1=shift, scalar2=None, op0=ALU.add), [key + 'o'], [key + 'o'])
        for _ in range(2):
            D_(lambda e: e.tensor_scalar(out=ni_t.bitcast(F32), in0=dst, scalar1=math.pi, scalar2=-TWO_PI, op0=ALU.is_gt, op1=ALU.mult),
               [key + 'o'], [key + 'ni'])
            D_(lambda e: e.tensor_tensor(out=dst, in0=dst, in1=ni_t.bitcast(F32), op=ALU.add), [key + 'o', key + 'ni'], [key + 'o'])
        for _ in range(2):
            D_(lambda e: e.tensor_scalar(out=ni_t.bitcast(F32), in0=dst, scalar1=-math.pi, scalar2=TWO_PI, op0=ALU.is_lt, op1=ALU.mult),
               [key + 'o'], [key + 'ni'])
            D_(lambda e: e.tensor_tensor(out=dst, in0=dst, in1=ni_t.bitcast(F32), op=ALU.add), [key + 'o', key + 'ni'], [key + 'o'])
        D_(lambda e: e.tensor_scalar(out=dst, in0=dst, scalar1=math.pi, scalar2=-math.pi, op0=ALU.min, op1=ALU.max), [key + 'o'], [key + 'o'])
        kb.op('act', lambda e: e.activation(out=dst, in_=dst, func=AF.Sin), r=[key + 'o'], w=[key + 'o'])


def stage_s5(c, l):
    nc, kb, T, NTB, NT = c.nc, c.kb, c.T, c.NTB, c.NT
    W = c.lw[l]
    cst, cstb = c.cst, c.cstb
    ps = c.ps
    with ExitStack() as es:
        A = lambda name, shp, dt: es.enter_context(nc.sbuf_tensor(_nm(name), shp, dt))
        yacc = A('s_yacc', [128, 2, T], F32)
        uTb = A('s_uTb', [128, 2, T], BF16)
        Nre = A('s_Nre', [128, 2, 1024], F32)
        Nim = A('s_Nim', [128, 2, 1024], F32)
        Tre = A('s_Tre', [128, 2, 8, 128], F32)
        Tim = A('s_Tim', [128, 2, 8, 128], F32)
        BbT = A('s_BbT', [128, 2, 2, 2, 512], BF16)
        CT = A('s_CT', [128, 2, 8, 2, 128], BF16)
        sd = A('s_d', [128, 2], F32)
        kc = A('s_kc', [128, 258], F32)
        nkc = A('s_nkc', [128, 2], F32)
        psm = A('s_psm', [128, 2, 3, 8], F32)
        x1sm = A('s_x1sm', [128, 2, 8], F32)
        thsm = A('s_thsm', [128, 2, 8], F32)
        dtsm = A('s_dtsm', [128, 2, 8], F32)
        kb.dma('sp', sd[:], W['s5_d'][:, :], w=['sd'])
        kb.dma('sp', kc[:], W['s5_kc'][:, :], w=['kc'])
        kb.dma('sp', psm[:], W['s5_par_sm'][:, :, :, :], w=['psm'])
        kb.dma('pool', CT[:], W['s5_cT'][:, :, :, :, :], w=['CT'])
        kb.op('dve', lambda e: e.tensor_scalar(out=CT[:, :, :, 1, :], in0=CT[:, :, :, 1, :], scalar1=-1.0, scalar2=None, op0=ALU.mult),
              r=['CT'], w=['CT'])
        kb.op('dve', lambda e: e.tensor_scalar(out=nkc[:], in0=kc[:, 0:2], scalar1=-1.0, scalar2=None, op0=ALU.mult), r=['kc'], w=['nkc'])
        for k in range(2):
            kb.dma('sp', yacc[:, k, :], c.pmixT[k * 128:(k + 1) * 128, :], w=[('yacc', k)])
            kb.op('act', lambda e, k=k: e.copy(out=uTb[:, k, :], in_=yacc[:, k, :]), r=[('yacc', k)], w=[('uTb', k)])
            kb.op('dve', lambda e, k=k: e.tensor_scalar(out=yacc[:, k, :], in0=yacc[:, k, :], scalar1=sd[:, k:k + 1], scalar2=None,
                                                        op0=ALU.mult), r=[('yacc', k), 'sd', ('uTb', k)], w=[('yacc', k)])
        with ExitStack() as es2:
            B_ = lambda name, shp, dt: es2.enter_context(nc.sbuf_tensor(_nm(name), shp, dt))
            ptm = B_('s_ptm', [128, 3, 1024], F32)
            x1 = B_('s_x1', [128, 1024], F32)
            th = B_('s_th', [128, 1024], F32)
            t_n = B_('s_tn', [128, 2048], F32)
            t_ni = B_('s_tni', [128, 2048], I32)
            t_c = B_('s_tc', [128, 2048], F32)
            t_s = B_('s_ts', [128, 2048], F32)
            t_a = B_('s_ta', [128, 2048], F32)
            kR = B_('s_kR', [128, 1024], F32)
            kI = B_('s_kI', [128, 1024], F32)
            BT = B_('s_BT', [128, 2, 2, 512], F32)
            tb1 = B_('s_tb1', [128, 1024], F32)
            tb2 = B_('s_tb2', [128, 1024], F32)
            Dv = lambda fn, r, w: kb.op('dve', fn, r=r, w=w)
            for di in range(2):
                kb.dma('sp', ptm[:], W['s5_par_tm'][:, di, :, :], w=['ptm'])
                kb.dma('sp', BT[:], W['s5_bT'][:, di, :, :, :], w=['BT'])
                lr_, li_, ldt_ = ptm[:, 0, :], ptm[:, 1, :], ptm[:, 2, :]
                Dv(lambda e: e.tensor_scalar(out=lr_, in0=lr_, scalar1=-1e-4, scalar2=None, op0=ALU.min), ['ptm'], ['ptm'])
                kb.op('act', lambda e: e.activation(out=ldt_, in_=ldt_, func=AF.Exp), r=['ptm'], w=['ptm'])
                Dv(lambda e: e.tensor_tensor(out=x1[:], in0=lr_, in1=ldt_, op=ALU.mult), ['ptm'], ['x1'])
                Dv(lambda e: e.tensor_tensor(out=th[:], in0=li_, in1=ldt_, op=ALU.mult), ['ptm'], ['th'])
                Dv(lambda e: e.tensor_copy(out=t_a[:, 0:1024], in_=th[:]), ['th'], ['tg'])
                _trig(kb, t_a[:, 0:1024], t_n[:, 0:1024], t_ni[:, 0:1024], t_c[:, 0:1024], t_s[:, 0:1024], 'tg')
                kb.op('act', lambda e: e.activation(out=tb1[:], in_=x1[:], func=AF.Exp), r=['x1'], w=['tb1'])
                aR, aI = t_c[:, 0:1024], t_s[:, 0:1024]
                Dv(lambda e: e.tensor_tensor(out=aR, in0=aR, in1=tb1[:], op=ALU.mult), ['tgo', 'tb1'], ['tgo'])
                Dv(lambda e: e.tensor_tensor(out=aI, in0=aI, in1=tb1[:], op=ALU.mult), ['tgo', 'tb1'], ['tgo'])
                Dv(lambda e: e.tensor_tensor(out=tb1[:], in0=lr_, in1=lr_, op=ALU.mult), ['ptm', 'tgo'], ['tb1'])
                Dv(lambda e: e.tensor_tensor(out=tb2[:], in0=li_, in1=li_, op=ALU.mult), ['ptm'], ['tb2'])
                Dv(lambda e: e.tensor_tensor(out=tb1[:], in0=tb1[:], in1=tb2[:], op=ALU.add), ['tb1', 'tb2'], ['tb1'])
                Dv(lambda e: e.reciprocal(out=tb1[:], in_=tb1[:]), ['tb1'], ['tb1'])
                Dv(lambda e: e.tensor_scalar(out=aR, in0=aR, scalar1=-1.0, scalar2=None, op0=ALU.add), ['tgo'], ['tgo'])
                Dv(lambda e: e.tensor_tensor(out=kR[:], in0=aR, in1=lr_, op=ALU.mult), ['tgo', 'ptm'], ['kR'])
                Dv(lambda e: e.tensor_tensor(out=tb2[:], in0=aI, in1=li_, op=ALU.mult), ['tgo', 'ptm'], ['tb2'])
                Dv(lambda e: e.tensor_tensor(out=kR[:], in0=kR[:], in1=tb2[:], op=ALU.add), ['kR', 'tb2'], ['kR'])
                Dv(lambda e: e.tensor_tensor(out=kR[:], in0=kR[:], in1=tb1[:], op=ALU.mult), ['kR', 'tb1'], ['kR'])
                Dv(lambda e: e.tensor_tensor(out=kI[:], in0=aI, in1=lr_, op=ALU.mult), ['tgo', 'ptm'], ['kI'])
                Dv(lambda e: e.tensor_tensor(out=tb2[:], in0=aR, in1=li_, op=ALU.mult), ['tgo', 'ptm', 'kR'], ['tb2'])
                Dv(lambda e: e.tensor_tensor(out=kI[:], in0=kI[:], in1=tb2[:], op=ALU.subtract), ['kI', 'tb2'], ['kI'])
                Dv(lambda e: e.tensor_tensor(out=kI[:], in0=kI[:], in1=tb1[:], op=ALU.mult), ['kI', 'tb1'], ['kI'])
                kRv = kR[:].rearrange("p (k s) -> p k s", k=2)
                kIv = kI[:].rearrange("p (k s) -> p k s", k=2)
                t1v = tb1[:].rearrange("p (k s) -> p k s", k=2)
                t2v = tb2[:].rearrange("p (k s) -> p k s", k=2)
                Dv(lambda e: e.tensor_tensor(out=t1v, in0=kRv, in1=BT[:, :, 0, :], op=ALU.mult), ['kR', 'BT', 'kI'], ['tb1'])
                Dv(lambda e: e.tensor_tensor(out=t2v, in0=kIv, in1=BT[:, :, 1, :], op=ALU.mult), ['kI', 'BT'], ['tb2'])
                Dv(lambda e: e.tensor_tensor(out=BbT[:, di, :, 0, :], in0=t1v, in1=t2v, op=ALU.subtract), ['tb1', 'tb2'], ['BbT'])
                Dv(lambda e: e.tensor_tensor(out=t1v, in0=kRv, in1=BT[:, :, 1, :], op=ALU.mult), ['kR', 'BT', 'BbT'], ['tb1'])
                Dv(lambda e: e.tensor_tensor(out=t2v, in0=kIv, in1=BT[:, :, 0, :], op=ALU.mult), ['kI', 'BT', 'BbT'], ['tb2'])
                Dv(lambda e: e.tensor_tensor(out=BbT[:, di, :, 1, :], in0=t1v, in1=t2v, op=ALU.add), ['tb1', 'tb2'], ['BbT'])
                Dv(lambda e: e.tensor_scalar(out=t_a[:, 0:1024], in0=th[:], scalar1=kc[:, di:di + 1], scalar2=None, op0=ALU.mult),
                   ['th', 'kc', 'tgo', 'tg'], ['tg'])
                _trig(kb, t_a[:, 0:1024], t_n[:, 0:1024], t_ni[:, 0:1024], t_c[:, 0:1024], t_s[:, 0:1024], 'tg')
                kb.op('act', lambda e: e.activation(out=tb1[:], in_=x1[:], func=AF.Exp, scale=nkc[:, di:di + 1]),
                      r=['x1', 'nkc', 'BbT'], w=['tb1'])
                Dv(lambda e: e.tensor_tensor(out=Nre[:, di, :], in0=tb1[:], in1=t_c[:, 0:1024], op=ALU.mult), ['tb1', 'tgo'], ['Nre'])
                Dv(lambda e: e.scalar_tensor_tensor(out=Nim[:, di, :], in0=tb1[:], scalar=-1.0, in1=t_s[:, 0:1024],
                                                    op0=ALU.mult, op1=ALU.mult), ['tb1', 'tgo'], ['Nim'])
            Dv(lambda e: e.tensor_scalar(out=psm[:, :, 0, :], in0=psm[:, :, 0, :], scalar1=-1e-4, scalar2=None, op0=ALU.min), ['psm'], ['psm'])
            kb.op('act', lambda e: e.activation(out=dtsm[:], in_=psm[:, :, 2, :], func=AF.Exp), r=['psm'], w=['dtsm'])
            Dv(lambda e: e.tensor_tensor(out=x1sm[:], in0=psm[:, :, 0, :], in1=dtsm[:], op=ALU.mult), ['psm', 'dtsm'], ['x1sm'])
            Dv(lambda e: e.tensor_tensor(out=thsm[:], in0=psm[:, :, 1, :], in1=dtsm[:], op=ALU.mult), ['psm', 'dtsm'], ['thsm'])
            magT = tb1[:].rearrange("p (a b) -> p a b", a=8)
            for di in range(2):
                krow = kc[:, 2 + di * 128:2 + (di + 1) * 128]
                for st in range(8):
                    Dv(lambda e, st=st: e.tensor_scalar(out=t_a[:, (di * 8 + st) * 128:(di * 8 + st + 1) * 128], in0=krow,
                                                        scalar1=thsm[:, di, st:st + 1], scalar2=None, op0=ALU.mult),
                       ['kc', 'thsm', 'tg', 'tgo'], ['tg'])
            _trig(kb, t_a[:], t_n[:], t_ni[:], t_c[:], t_s[:], 'tg')
            for di in range(2):
                krow = kc[:, 2 + di * 128:2 + (di + 1) * 128]
                for st in range(8):
                    kb.op('act', lambda e, st=st: e.activation(out=magT[:, st, :], in_=krow, func=AF.Exp, scale=x1sm[:, di, st:st + 1]),
                          r=['kc', 'x1sm', 'Nre', 'Nim'], w=['tb1'])
                Dv(lambda e: e.tensor_tensor(out=Tre[:, di], in0=magT, in1=t_c[:, di * 1024:(di + 1) * 1024].rearrange("p (a b) -> p a b", a=8),
                                             op=ALU.mult), ['tb1', 'tgo'], ['Tre'])
                Dv(lambda e: e.tensor_tensor(out=Tim[:, di], in0=magT, in1=t_s[:, di * 1024:(di + 1) * 1024].rearrange("p (a b) -> p a b", a=8),
                                             op=ALU.mult), ['tb1', 'tgo'], ['Tim'])
            kb.barrier()
        with ExitStack() as es3:
            B_ = lambda name, shp, dt: es3.enter_context(nc.sbuf_tensor(_nm(name), shp, dt))
            tt = B_('s_tt', [128, 2, 4, 512], F32)
            Wc = B_('s_W', [128, 2, 2, 2, 2, 512], BF16)
            mm = B_('s_mm', [128, 2, 4, 4, 128], F32)
            xs = B_('s_xs', [128, 2, 2, 8, 128], F32)
            xb = B_('s_xb', [128, 2, 2, 8, 128], BF16)

            def chunk_of(idx, di):
                return idx if di == 0 else NT - 1 - idx

            def step_A(idx, di, k):
                n = chunk_of(idx, di)
                nsl = slice(n * 128, (n + 1) * 128)
                base = di * 4
                par = idx % 2
                ksl = slice(k * 512, (k + 1) * 512)
                for ri in range(2):
                    kb.op('pe', lambda e, ri=ri: e.matmul(ps[base + ri][:], lhsT=uTb[:, k, nsl], rhs=BbT[:, di, k, ri, :],
                                                          start=True, stop=True),
                          r=[('uTb', k), 'BbT'], w=[('ps', base + ri)])
                pr, pi_ = ps[base], ps[base + 1]
                for slot, (pp, tab, bkk, tkey) in enumerate([(pr, Nre, base, 'Nre'), (pi_, Nim, base + 1, 'Nim'),
                                                            (pi_, Nre, base + 1, 'Nre'), (pr, Nim, base, 'Nim')]):
                    kb.op('dve', lambda e, slot=slot, pp=pp, tab=tab: e.tensor_tensor(
                        out=tt[:, di, slot, :], in0=pp[:], in1=tab[:, di, ksl], op=ALU.mult),
                        r=[('ps', bkk), tkey], w=[('tt', di, slot)])
                kb.op('pool', lambda e: e.tensor_tensor(out=Wc[:, par, di, k, 0, :], in0=tt[:, di, 0, :], in1=tt[:, di, 1, :], op=ALU.subtract),
                      r=[('tt', di, 0), ('tt', di, 1)], w=[('W', par, di, k, 0)])
                kb.op('pool', lambda e: e.tensor_tensor(out=Wc[:, par, di, k, 1, :], in0=tt[:, di, 2, :], in1=tt[:, di, 3, :], op=ALU.add),
                      r=[('tt', di, 2), ('tt', di, 3)], w=[('W', par, di, k, 1)])

            def step_B(idx, di, k):
                base = di * 4
                par = idx % 2
                tri = cstb[:, 8 + di, :]
                ccol = 127 if di == 0 else 0
                for ri in range(2):
                    for s4 in range(4):
                        kb.op('pe', lambda e, ri=ri, s4=s4: e.matmul(
                            ps[base + 2 + ri][:, s4 * 128:(s4 + 1) * 128], lhsT=Wc[:, par, di, k, ri, s4 * 128:(s4 + 1) * 128], rhs=tri,
                            start=True, stop=True), r=[('W', par, di, k, ri), 'cst'], w=[('ps', base + 2 + ri)])
                for s4 in range(4):
                    st = k * 4 + s4
                    pre = ps[base + 2][:, s4 * 128:(s4 + 1) * 128]
                    pim = ps[base + 3][:, s4 * 128:(s4 + 1) * 128]
                    if idx == 0:
                        cr, ci = 0.0, 0.0
                    else:
                        cr = xs[:, di, 0, st, ccol:ccol + 1]
                        ci = xs[:, di, 1, st, ccol:ccol + 1]
                    rk = [('ps', base + 2), ('ps', base + 3), 'Tre', 'Tim', ('xs', di, k)]
                    for slot, (pp, cc_, tab) in enumerate([(pre, cr, Tre), (pim, ci, Tim), (pim, ci, Tre), (pre, cr, Tim)]):
                        kb.op('dve', lambda e, slot=slot, pp=pp, cc_=cc_, tab=tab, s4=s4, st=st: e.scalar_tensor_tensor(
                            out=mm[:, di, slot, s4, :], in0=pp, scalar=cc_, in1=tab[:, di, st, :], op0=ALU.add, op1=ALU.mult),
                            r=rk, w=[('mm', di, slot, s4)])
                kb.op('pool', lambda e: e.tensor_tensor(out=xs[:, di, 0, k * 4:(k + 1) * 4, :], in0=mm[:, di, 0], in1=mm[:, di, 1], op=ALU.subtract),
                      r=[('mm', di, 0, s4) for s4 in range(4)] + [('mm', di, 1, s4) for s4 in range(4)], w=[('xs', di, k)])
                kb.op('pool', lambda e: e.tensor_tensor(out=xs[:, di, 1, k * 4:(k + 1) * 4, :], in0=mm[:, di, 2], in1=mm[:, di, 3], op=ALU.add),
                      r=[('mm', di, 2, s4) for s4 in range(4)] + [('mm', di, 3, s4) for s4 in range(4)], w=[('xs', di, k)])
                kb.op('act', lambda e: e.copy(out=xb[:, di, :, k * 4:(k + 1) * 4, :], in_=xs[:, di, :, k * 4:(k + 1) * 4, :]),
                      r=[('xs', di, k)], w=[('xb', di, k)])

            def step_C(idx, di):
                n = chunk_of(idx, di)
                nsl = slice(n * 128, (n + 1) * 128)
                base = di * 4
                for k in range(2):
                    cnt = 0
                    for st in range(k * 4, k * 4 + 4):
                        for ri in range(2):
                            kb.op('pe', lambda e, k=k, st=st, ri=ri, cnt=cnt: e.matmul(
                                ps[base][:, k * 128:(k + 1) * 128], lhsT=CT[:, di, st, ri, :], rhs=xb[:, di, ri, st, :],
                                start=(cnt == 0), stop=(cnt == 7)), r=['CT', ('xb', di, k)], w=[('ps', base)])
                            cnt += 1
                kb.op('dve', lambda e: e.tensor_tensor(out=yacc[:, :, nsl], in0=yacc[:, :, nsl],
                                                       in1=ps[base][:, 0:256].rearrange("p (a b) -> p a b", a=2), op=ALU.add),
                      r=[('ps', base), ('yacc', n)], w=[('yacc', n)])

            for k in range(2):
                for di in range(2):
                    step_A(0, di, k)
            for idx in range(NT):
                if idx + 1 < NT:
                    for k in range(2):
                        for di in range(2):
                            step_A(idx + 1, di, k)
                for k in range(2):
                    for di in range(2):
                        step_B(idx, di, k)
                for di in range(2):
                    step_C(idx, di)
            kb.barrier()
        with ExitStack() as es3:
            B_ = lambda name, shp, dt: es3.enter_context(nc.sbuf_tensor(_nm(name), shp, dt))
            z = B_('s_z', [128, 2, T], F32)
            yst = B_('s_yst', [128, 2, 512], BF16)
            yf = yacc[:].rearrange("p k t -> p (k t)")
            zf = z[:].rearrange("p k t -> p (k t)")
            GC = 0.7978845608028654
            kb.op('act', lambda e: e.activation(out=zf, in_=yf, func=AF.Square), r=[('yacc', 0), ('yacc', 1)] + [('yacc', n) for n in range(NT)], w=['z'])
            kb.op('dve', lambda e: e.tensor_scalar(out=zf, in0=zf, scalar1=0.044715, scalar2=1.0, op0=ALU.mult, op1=ALU.add), r=['z'], w=['z'])
            kb.op('dve', lambda e: e.tensor_tensor(out=zf, in0=zf, in1=yf, op=ALU.mult), r=['z', ('yacc', 0), ('yacc', 1)], w=['z'])
            kb.op('act', lambda e: e.activation(out=zf, in_=zf, func=AF.Sigmoid, scale=2.0 * GC), r=['z'], w=['z'])
            kb.op('dve', lambda e: e.tensor_tensor(out=zf, in0=zf, in1=yf, op=ALU.mult), r=['z', ('yacc', 0), ('yacc', 1)], w=['z'])
            kb.op('act', lambda e: e.copy(out=uTb[:].rearrange("p k t -> p (k t)"), in_=zf), r=['z'], w=[('uTb', 0), ('uTb', 1)])
            wgl = B_('s_wgl', [128, 2, 256], BF16)
            kb.dma('pool', wgl[:], W['s5_wglu'][:, :, :], w=['wgl'])
            nps = 0
            for ko in range(2):
                for tb in range(NTB):
                    ts = slice(tb * 512, (tb + 1) * 512)
                    bk = 1 + nps % 4
                    nps += 1
                    for ki in range(2):
                        kb.op('pe', lambda e, ki=ki, bk=bk: e.matmul(ps[bk][:], lhsT=wgl[:, ki, ko * 128:(ko + 1) * 128], rhs=uTb[:, ki, ts],
                                                              start=(ki == 0), stop=(ki == 1)),
                              r=['wgl', ('uTb', 0), ('uTb', 1)], w=[('ps', bk)])
                    kb.op('act', lambda e, bk=bk: e.activation(out=yacc[:, ko, ts], in_=ps[bk][:], func=AF.Sigmoid),
                          r=[('ps', bk)], w=[('yg', ko, tb)])
                    yb_ = nps % 2
                    kb.op('dve', lambda e, yb_=yb_: e.tensor_tensor(out=yst[:, yb_, :], in0=yacc[:, ko, ts], in1=z[:, ko, ts],
                                                           op=ALU.mult), r=[('yg', ko, tb), 'z'], w=[('yst', yb_)])
                    kb.dma('sp', c.yT[ko * 128:(ko + 1) * 128, ts], yst[:, yb_, :], r=[('yst', yb_)], w=[('yT', ko, tb)])
        if 'ys5' in c.dbg:
            kb.barrier()
            with nc.sbuf_tensor(_nm('dbgt3'), [128, T], F32) as dbgt:
                for ct in range(2):
                    kb.dma('pool', dbgt[:], c.yT[ct * 128:(ct + 1) * 128, :], w=['dbgt'])
                    kb.dma('sp', c.dbg['ys5'][ct * 128:(ct + 1) * 128, :], dbgt[:], r=['dbgt'], w=[('dbgo', ct)])
                kb.barrier()
        kb.barrier()


def prep_rest(inp, l, o):
    f = lambda k: np.asarray(inp[k], np.float32)
    o['w_up'] = np.ascontiguousarray(np.concatenate([f('w_up_s5')[l], f('w_up_lru')[l], f('w_up_gla')[l]], axis=0))
    o['w_mix'] = np.ascontiguousarray(f('w_mix_out')[l])
    o['w_router'] = np.ascontiguousarray(f('w_router')[l].reshape(8, 128, 16).transpose(1, 0, 2))
    o['w_eg'] = np.ascontiguousarray(f('w_exp_gate')[l])
    o['w_eu'] = np.ascontiguousarray(f('w_exp_up')[l])
    o['w_ed'] = np.ascontiguousarray(f('w_exp_down')[l])


def make_iota():
    io = np.zeros((128, 513), np.float32)
    io[:, 0] = np.arange(128)
    io[:, 1:] = np.arange(512)[None, :]
    return io


def emit_rmsnorm(c, hb, hkey, gcol, gkey, out_of_k, outkeys, bank, sq, sqkey, rstd, rkey):
    kb = c.kb
    kb.op('act', lambda e: e.activation(out=sq, in_=hb, func=AF.Square), r=[hkey], w=[sqkey])
    pb = c.ps[bank]
    for k in range(KC):
        kb.op('pe', lambda e, k=k: e.matmul(pb[:], lhsT=c.ones_bf[:], rhs=sq[:, k, :], start=(k == 0), stop=(k == KC - 1)),
              r=[sqkey, 'ones_bf'], w=[('ps', bank)])
    kb.op('act', lambda e: e.activation(out=rstd, in_=pb[:], func=AF.Sqrt, scale=1.0 / D, bias=EPS), r=[('ps', bank)], w=[rkey])
    kb.op('dve', lambda e: e.reciprocal(out=rstd, in_=rstd), r=[rkey], w=[rkey])
    for k in range(KC):
        kb.op('dve', lambda e, k=k: e.scalar_tensor_tensor(out=out_of_k(k), in0=hb[:, k, :], scalar=gcol[:, k:k + 1], in1=rstd,
                                                           op0=ALU.mult, op1=ALU.mult),
              r=[hkey, rkey, gkey], w=outkeys)


def emit_add_tm(c, hb, hkey, tb, ftile, banks):
    kb = c.kb
    ident = c.cst[:, 0, :]
    for t4 in range(4):
        tt = tb * 4 + t4
        fb = t4 % 2
        kb.dma('sp', ftile[:, fb, :], c.ffn_tm[tt * 128:(tt + 1) * 128, :], w=[('ftile', fb)])
        for half in range(2):
            bk = banks[half]
            for k4 in range(4):
                k = half * 4 + k4
                kb.op('pe', lambda e, k=k, k4=k4, bk=bk: e.transpose(out=c.ps[bk][:, k4 * 128:(k4 + 1) * 128],
                                                                    in_=ftile[:, fb, k * 128:(k + 1) * 128], identity=ident),
                      r=[('ftile', fb), 'cst'], w=[('ps', bk)])
            hv = hb[:, half * 4:(half + 1) * 4, t4 * 128:(t4 + 1) * 128]
            kb.op('dve', lambda e, hv=hv, bk=bk: e.tensor_tensor(out=hv, in0=hv, in1=c.ps[bk][:].rearrange("p (a b) -> p a b", a=4), op=ALU.add),
                  r=[('ps', bk), hkey], w=[hkey])


def stage_merge(c, l, hnT):
    nc, kb, T, NTB, NT = c.nc, c.kb, c.T, c.NTB, c.NT
    W = c.lw[l]
    ps = c.ps
    res_src = c.xT if l == 0 else c.hT
    with ExitStack() as es:
        A = lambda name, shp, dt: es.enter_context(nc.sbuf_tensor(_nm(name), shp, dt))
        wup = A('m_wup', [128, 8, 1024], BF16)
        wmix = A('m_wmix', [128, 8, 1024], BF16)
        yblk = A('m_yblk', [128, 2, 8, 512], BF16)
        goc = A('m_goc', [128, 2, 3, 512], BF16)
        mrg = A('m_mrg', [128, 8, 512], BF16)
        hblk = A('m_hblk', [128, 2, 8, 512], F32)
        t1 = A('m_t1', [128, 2, 3, 512], F32)
        sq = A('m_sq', [128, 8, 512], BF16)
        rstd = A('m_rstd', [128, 512], F32)
        gff = A('m_gff', [128, 8], F32)
        kb.dma('pool', wup[:], W['w_up'].rearrange("(k p) d -> p k d", p=128), w=['wup'])
        kb.dma('pool', wmix[:], W['w_mix'].rearrange("(k p) d -> p k d", p=128), w=['wmix'])
        kb.dma('sp', gff[:], W['ffn_g'][:, :], w=['gff'])
        yTv = c.yT.rearrange("(k p) t -> p k t", p=128)
        gTv = c.gT.rearrange("(b o p) t -> p b o t", p=128, b=3)
        resv = res_src.rearrange("(k p) t -> p k t", p=128)
        hTv = c.hT.rearrange("(k p) t -> p k t", p=128)
        branch_k = [(0, 2), (2, 6), (6, 8)]
        ng = 0
        for tb in range(NTB):
            b = tb % 2
            ts = slice(tb * 512, (tb + 1) * 512)
            kb.dma('sp', yblk[:, b], yTv[:, :, ts], w=[('yblk', b)])
            kb.dma('sp', hblk[:, b], resv[:, :, ts], w=[('hblk', b)])
            for oc in range(8):
                gb = ng % 2
                ng += 1
                kb.dma('sp', goc[:, gb], gTv[:, :, oc, ts], w=[('goc', gb)])
                for br in range(3):
                    bk = gb * 3 + br
                    k0, k1 = branch_k[br]
                    for k in range(k0, k1):
                        kb.op('pe', lambda e, k=k, bk=bk, k0=k0, k1=k1: e.matmul(
                            ps[bk][:], lhsT=wup[:, k, oc * 128:(oc + 1) * 128], rhs=yblk[:, b, k, :],
                            start=(k == k0), stop=(k == k1 - 1)), r=['wup', ('yblk', b)], w=[('ps', bk)])
                tb_ = oc % 2
                kb.op('dve', lambda e: e.tensor_tensor(out=t1[:, tb_, 0, :], in0=ps[gb * 3][:], in1=goc[:, gb, 0, :], op=ALU.mult),
                      r=[('ps', gb * 3), ('goc', gb)], w=[('t1', tb_, 0)])
                kb.op('dve', lambda e: e.tensor_tensor(out=t1[:, tb_, 1, :], in0=ps[gb * 3 + 1][:], in1=goc[:, gb, 1, :], op=ALU.mult),
                      r=[('ps', gb * 3 + 1), ('goc', gb)], w=[('t1', tb_, 1)])
                kb.op('dve', lambda e: e.tensor_tensor(out=t1[:, tb_, 2, :], in0=ps[gb * 3 + 2][:], in1=goc[:, gb, 2, :], op=ALU.mult),
                      r=[('ps', gb * 3 + 2), ('goc', gb)], w=[('t1', tb_, 2)])
                kb.op('pool', lambda e: e.tensor_tensor(out=t1[:, tb_, 0, :], in0=t1[:, tb_, 0, :], in1=t1[:, tb_, 1, :], op=ALU.add),
                      r=[('t1', tb_, 0), ('t1', tb_, 1)], w=[('t1', tb_, 0)])
                kb.op('pool', lambda e: e.tensor_tensor(out=mrg[:, oc, :], in0=t1[:, tb_, 0, :], in1=t1[:, tb_, 2, :], op=ALU.add),
                      r=[('t1', tb_, 0), ('t1', tb_, 2)], w=[('mrg', oc)])
            for oc in range(8):
                bk = 6 + oc % 2
                for k in range(8):
                    kb.op('pe', lambda e, k=k, bk=bk: e.matmul(ps[bk][:], lhsT=wmix[:, k, oc * 128:(oc + 1) * 128], rhs=mrg[:, k, :],
                                                          start=(k == 0), stop=(k == 7)),
                          r=['wmix'] + [('mrg', kk) for kk in range(8)], w=[('ps', bk)])
                kb.op('dve', lambda e, bk=bk: e.tensor_tensor(out=hblk[:, b, oc, :], in0=hblk[:, b, oc, :], in1=ps[bk][:], op=ALU.add),
                      r=[('ps', bk), ('hblk', b)], w=[('hblk', b)])
            kb.dma('sp', hTv[:, :, ts], hblk[:, b], r=[('hblk', b)], w=[('hT', tb)])
            emit_rmsnorm(c, hblk[:, b], ('hblk', b), gff, 'gff', lambda k: hnT[:, k, ts], [('hnT', tb)], 6, sq[:], 'sq', rstd[:], 'rstd')
        if 'hmid' in c.dbg:
            kb.barrier()
            with nc.sbuf_tensor(_nm('dbgt4'), [128, T], F32) as dbgt:
                for ct in range(8):
                    kb.dma('sp', dbgt[:], c.hT[ct * 128:(ct + 1) * 128, :], w=['dbgt'])
                    kb.dma('sp', c.dbg['hmid'][ct * 128:(ct + 1) * 128, :], dbgt[:], r=['dbgt'], w=[('dbgo', ct)])
                kb.barrier()
        kb.barrier()


def stage_ffn(c, l, hnT, seli, selg, phase):
    nc, kb, T, NTB, NT = c.nc, c.kb, c.T, c.NTB, c.NT
    W = c.lw[l]
    ps = c.ps
    cst, cstb = c.cst, c.cstb
    CAP = 2 * T // NE
    CT_ = CAP // 128
    NJ = NT * NE
    with ExitStack() as es:
        A = lambda name, shp, dt: es.enter_context(nc.sbuf_tensor(_nm(name), shp, dt))
        for _once in ([0] if phase == 0 else []):
          with ExitStack() as es2:
            B_ = lambda name, shp, dt: es2.enter_context(nc.sbuf_tensor(_nm(name), shp, dt))
            wr = B_('f_wr', [128, 8, 16], BF16)
            iota = B_('f_iota', [128, 513], F32)
            probs = B_('f_probs', [128, NT, NE], F32)
            mask = B_('f_mask', [128, NT, NE], F32)
            maskb = B_('f_maskb', [128, NT, NE], BF16)
            gw = B_('f_gw', [128, NT, NE], F32)
            pos = B_('f_pos', [128, NT, NE], F32)
            base = B_('f_base', [128, NT, NE], F32)
            csum = B_('f_csum', [128, NT, NE], F32)
            cmpb = B_('f_cmpb', [128, NT, NE], BF16)
            red = B_('f_red', [128, NT], F32)
            lo = B_('f_lo', [128, NE], F32)
            mid = B_('f_mid', [128, NE], F32)
            cnt = B_('f_cnt', [128, NE], F32)
            R = B_('f_R', [128, NT, NE, 4], BF16)
            ghi_f = B_('f_ghif', [128, NT, NE], F32)
            OH = B_('f_OH', [128, 2, CAP], BF16)
            selsb = B_('f_selsb', [128, NE, CT_, 4], F32)
            stg = B_('f_stg', [128, 2, 1024], BF16)
            zt = B_('f_zt', [128, 1024], F32)
            kb.dma('pool', wr[:], W['w_router'][:, :, :], w=['wr'])
            kb.dma('sp', iota[:], c.iota[:, :], w=['iota'])
            kb.op('dve', lambda e: e.memset(zt[:], 0.0), w=['zt'])
            for tt in range(NT):
                kb.dma('sp', c.ffn_tm[tt * 128:(tt + 1) * 128, :], zt[:], r=['zt'], w=[('ffn_tm', tt)])
            for tt in range(NT):
                sb = tt % 2
                psE = ps[sb][:].bitcast(BF16)
                for k in range(8):
                    kb.op('pe', lambda e, k=k, psE=psE: e.transpose(out=psE[:, k * 128:(k + 1) * 128], in_=hnT[:, k, tt * 128:(tt + 1) * 128],
                                                                   identity=cstb[:, 0, :]),
                          r=[('hnT', tt // 4), 'cst'], w=[('ps', sb)])
                kb.op('act', lambda e, psE=psE: e.copy(out=stg[:, sb, :], in_=psE), r=[('ps', sb)], w=[('stg', sb)])
                kb.dma('sp', c.hn_tm[tt * 128:(tt + 1) * 128, :], stg[:, sb, :], r=[('stg', sb)], w=[('hn_tm', tt)])
            for tt in range(NT):
                for k in range(8):
                    kb.op('pe', lambda e, k=k: e.matmul(ps[2][:, tt * NE:(tt + 1) * NE], lhsT=hnT[:, k, tt * 128:(tt + 1) * 128], rhs=wr[:, k, :],
                                                        start=(k == 0), stop=(k == 7)), r=[('hnT', tt // 4), 'wr'], w=[('ps', 2)])
            lg = ps[2][:, 0:NJ].rearrange("p (j e) -> p j e", e=NE)
            Dv = lambda fn, r, w: kb.op('dve', fn, r=r, w=w)
            Dv(lambda e: e.tensor_reduce(out=red[:], in_=lg, axis=AX.X, op=ALU.max), [('ps', 2)], ['red'])
            Dv(lambda e: e.tensor_tensor(out=probs[:], in0=lg, in1=_bc_last(red[:], NE), op=ALU.subtract), [('ps', 2), 'red'], ['probs'])
            kb.op('act', lambda e: e.activation(out=probs[:], in_=probs[:], func=AF.Exp), r=['probs'], w=['probs'])
            Dv(lambda e: e.tensor_reduce(out=red[:], in_=probs[:], axis=AX.X, op=ALU.add), ['probs'], ['red'])
            Dv(lambda e: e.reciprocal(out=red[:], in_=red[:]), ['red'], ['red'])
            Dv(lambda e: e.tensor_tensor(out=probs[:], in0=probs[:], in1=_bc_last(red[:], NE), op=ALU.mult), ['probs', 'red'], ['probs'])
            Dv(lambda e: e.memset(lo[:], 0.0), [], ['lo'])
            pflat = probs[:].rearrange("p j e -> p (j e)")
            for it in range(1, 33):
                wstep = 2.0 ** (-it)
                Dv(lambda e: e.tensor_scalar(out=mid[:], in0=lo[:], scalar1=wstep, scalar2=None, op0=ALU.add), ['lo'], ['mid'])
                Dv(lambda e: e.tensor_tensor(out=cmpb[:], in0=probs[:], in1=_bc_mid(mid[:], NT), op=ALU.is_ge), ['probs', 'mid'], ['cmpb'])
                kb.op('pe', lambda e: e.matmul(ps[3][:, 0:NJ], lhsT=c.ones_bf[:], rhs=cmpb[:].rearrange("p j e -> p (j e)"), start=True, stop=True),
                      r=['cmpb', 'ones_bf'], w=[('ps', 3)])
                Dv(lambda e: e.tensor_reduce(out=cnt[:], in_=ps[3][:, 0:NJ].rearrange("p (j e) -> p e j", e=NE), axis=AX.X, op=ALU.add),
                   [('ps', 3)], ['cnt'])
                Dv(lambda e: e.tensor_scalar(out=cnt[:], in0=cnt[:], scalar1=float(CAP) - 0.5, scalar2=wstep, op0=ALU.is_ge, op1=ALU.mult),
                   ['cnt'], ['cnt'])
                Dv(lambda e: e.tensor_tensor(out=lo[:], in0=lo[:], in1=cnt[:], op=ALU.add), ['lo', 'cnt'], ['lo'])
            Dv(lambda e: e.tensor_tensor(out=mask[:], in0=probs[:], in1=_bc_mid(lo[:], NT), op=ALU.is_ge), ['probs', 'lo'], ['mask'])
            Dv(lambda e: e.tensor_copy(out=maskb[:], in_=mask[:]), ['mask'], ['maskb'])
            Dv(lambda e: e.tensor_tensor(out=gw[:], in0=probs[:], in1=mask[:], op=ALU.mult), ['probs', 'mask'], ['gw'])
            mbf = maskb[:].rearrange("p j e -> p (j e)")
            kb.op('pe', lambda e: e.matmul(ps[3][:, 0:NJ], lhsT=c.ones_bf[:], rhs=mbf, start=True, stop=True), r=['maskb', 'ones_bf'], w=[('ps', 3)])
            Dv(lambda e: e.tensor_copy(out=csum[:].rearrange("p j e -> p (j e)"), in_=ps[3][:, 0:NJ]), [('ps', 3)], ['csum'])
            Dv(lambda e: e.memset(base[:, 0, :], 0.0), [], ['base'])
            for j in range(1, NT):
                Dv(lambda e, j=j: e.tensor_tensor(out=base[:, j, :], in0=base[:, j - 1, :], in1=csum[:, j - 1, :], op=ALU.add), ['base', 'csum'], ['base'])
            for j in range(NT):
                kb.op('pe', lambda e, j=j: e.matmul(ps[3][:, j * NE:(j + 1) * NE], lhsT=cstb[:, 6, :], rhs=maskb[:, j, :], start=True, stop=True),
                      r=['maskb', 'cst'], w=[('ps', 3)])
            Dv(lambda e: e.tensor_tensor(out=pos[:].rearrange("p j e -> p (j e)"), in0=base[:].rearrange("p j e -> p (j e)"), in1=ps[3][:, 0:NJ],
                                         op=ALU.add), [('ps', 3), 'base'], ['pos'])
            pidx = bass.AP(iota[:].tensor, iota[:, 0:1].offset, [[iota[:].ap[0][0], 128], [0, NT], [0, NE]])
            Dv(lambda e: e.tensor_copy(out=R[:, :, :, 0], in_=pidx), ['iota'], ['R'])
            for j in range(NT):
                Dv(lambda e, j=j: e.memset(R[:, j, :, 1], float(j)), [], ['R'])
            Dv(lambda e: e.tensor_copy(out=R[:, :, :, 2], in_=gw[:]), ['gw'], ['R'])
            Dv(lambda e: e.tensor_copy(out=ghi_f[:], in_=R[:, :, :, 2]), ['R'], ['ghi_f'])
            Dv(lambda e: e.tensor_tensor(out=R[:, :, :, 3], in0=gw[:], in1=ghi_f[:], op=ALU.subtract), ['gw', 'ghi_f'], ['R'])
            noh = 0
            for e_ in range(NE):
                for j in range(NT):
                    ob = noh % 2
                    noh += 1
                    Dv(lambda e, j=j, ob=ob: e.tensor_scalar(out=OH[:, ob, :], in0=iota[:, 1:1 + CAP], scalar1=pos[:, j, e_:e_ + 1],
                                                             scalar2=mask[:, j, e_:e_ + 1], op0=ALU.is_equal, op1=ALU.mult),
                       ['iota', 'pos', 'mask'], [('OH', ob)])
                    for ct in range(CT_):
                        bk = 4 + ct
                        kb.op('pe', lambda e, j=j, ob=ob, ct=ct, bk=bk: e.matmul(ps[bk][:, 0:4], lhsT=OH[:, ob, ct * 128:(ct + 1) * 128],
                                                                              rhs=R[:, j, e_, :], start=(j == 0), stop=(j == NT - 1)),
                              r=[('OH', ob), 'R'], w=[('ps', bk)])
                for ct in range(CT_):
                    kb.op('act', lambda e, ct=ct: e.copy(out=selsb[:, e_, ct, :], in_=ps[4 + ct][:, 0:4]), r=[('ps', 4 + ct)], w=['selsb'])
            Dv(lambda e: e.scalar_tensor_tensor(out=selg[:], in0=selsb[:, :, :, 1], scalar=128.0, in1=selsb[:, :, :, 0], op0=ALU.mult, op1=ALU.add),
               ['selsb'], ['selg'])
            Dv(lambda e: e.tensor_copy(out=seli[:], in_=selg[:]), ['selg'], ['seli'])
            Dv(lambda e: e.tensor_tensor(out=selg[:], in0=selsb[:, :, :, 2], in1=selsb[:, :, :, 3], op=ALU.add), ['selsb', 'seli'], ['selg'])
            kb.barrier()
        for _once in ([0] if phase == 1 else []):
          with ExitStack() as es3:
            B_ = lambda name, shp, dt: es3.enter_context(nc.sbuf_tensor(_nm(name), shp, dt))
            xe = B_('f_xe', [128, 2, CT_, 1024], BF16)
            xeT = B_('f_xeT', [128, 8, CAP], BF16)
            wg = B_('f_wg', [128, 3, 8, 512], BF16)
            wu = B_('f_wu', [128, 3, 8, 512], BF16)
            wd = B_('f_wd', [128, 2, 16, 1024], BF16)
            hidT = B_('f_hidT', [128, 16, CAP], BF16)
            sg = B_('f_sg', [128, 2, CAP], F32)
            ye = B_('f_ye', [128, CT_, 1024], F32)
            deferred = []
            nwb = 0
            nab = 0
            nye = 0
            def emit_gather(ee):
                for ct in range(CT_):
                    kb.idma(lambda g, ct=ct: g.indirect_dma_start(
                        out=xe[:, ee % 2, ct, :], out_offset=None, in_=c.hn_tm[:, :],
                        in_offset=bass.IndirectOffsetOnAxis(ap=seli[:, ee, ct:ct + 1], axis=0)),
                        r=['seli'] + [('hn_tm', tt) for tt in range(NT)], w=[('xe', ee % 2, ct)])
            emit_gather(0)
            for e_ in range(NE):
                if e_ + 1 < NE:
                    emit_gather(e_ + 1)
                for ct in range(CT_):
                    psE = ps[6][:].bitcast(BF16)
                    for k in range(8):
                        kb.op('pe', lambda e, k=k, ct=ct: e.transpose(out=psE[:, k * 128:(k + 1) * 128], in_=xe[:, e_ % 2, ct, k * 128:(k + 1) * 128],
                                                                      identity=cstb[:, 0, :]), r=[('xe', e_ % 2, ct), 'cst'], w=[('ps', 6)])
                    kb.op('act', lambda e, ct=ct: e.copy(out=xeT[:, :, ct * 128:(ct + 1) * 128], in_=psE.rearrange("p (a b) -> p a b", a=8)),
                          r=[('ps', 6)], w=['xeT'])
                for fg in range(4):
                    wb_ = nwb % 3
                    nwb += 1
                    fs = slice(fg * 512, (fg + 1) * 512)
                    kb.dma('pool', wg[:, wb_], W['w_eg'][e_].rearrange("(k p) f -> p k f", p=128)[:, :, fs], w=[('wg', wb_)])
                    kb.dma('pool', wu[:, wb_], W['w_eu'][e_].rearrange("(k p) f -> p k f", p=128)[:, :, fs], w=[('wu', wb_)])
                    kb.dma('pool', wd[:, e_ % 2, fg * 4:(fg + 1) * 4, :], W['w_ed'][e_][fs, :].rearrange("(c p) d -> p c d", p=128), w=[('wd', e_ % 2, fg)])
                    if fg == 1:
                        for fn_ in deferred:
                            fn_()
                        deferred = []
                    for fc in range(4):
                        fch = fg * 4 + fc
                        ab = nab % 2
                        nab += 1
                        pa, pb_ = ps[ab * 2], ps[ab * 2 + 1]
                        for (wt, pp, wkey, bk) in [(wg, pa, 'wg', ab * 2), (wu, pb_, 'wu', ab * 2 + 1)]:
                            for k in range(8):
                                kb.op('pe', lambda e, k=k, wt=wt, pp=pp: e.matmul(pp[:, 0:CAP], lhsT=wt[:, wb_, k, fc * 128:(fc + 1) * 128],
                                                                              rhs=xeT[:, k, :], start=(k == 0), stop=(k == 7)),
                                      r=[(wkey, wb_), 'xeT'], w=[('ps', bk)])
                        kb.op('act', lambda e, pa=pa: e.activation(out=sg[:, ab, :], in_=pa[:, 0:CAP], func=AF.Sigmoid), r=[('ps', ab * 2)], w=[('sg', ab)])
                        kb.op('dve', lambda e, pa=pa: e.tensor_tensor(out=sg[:, ab, :], in0=pa[:, 0:CAP], in1=sg[:, ab, :], op=ALU.mult),
                              r=[('ps', ab * 2), ('sg', ab)], w=[('sg', ab)])
                        kb.op('dve', lambda e, pb_=pb_: e.tensor_tensor(out=hidT[:, fch, :], in0=pb_[:, 0:CAP], in1=sg[:, ab, :], op=ALU.mult),
                              r=[('ps', ab * 2 + 1), ('sg', ab)], w=[('hidT', fch)])
                for ct in range(CT_):
                    yb = ct
                    for half in range(2):
                        bk = 4 + half
                        for fch in range(16):
                            kb.op('pe', lambda e, fch=fch, half=half, bk=bk, ct=ct: e.matmul(
                                ps[bk][:], lhsT=hidT[:, fch, ct * 128:(ct + 1) * 128], rhs=wd[:, e_ % 2, fch, half * 512:(half + 1) * 512],
                                start=(fch == 0), stop=(fch == 15)), r=[('hidT', fch), ('wd', e_ % 2, fch // 4)], w=[('ps', bk)])
                        kb.op('dve', lambda e, half=half, bk=bk, ct=ct: e.tensor_scalar(
                            out=ye[:, yb, half * 512:(half + 1) * 512], in0=ps[bk][:], scalar1=selg[:, e_, ct:ct + 1], scalar2=None, op0=ALU.mult),
                            r=[('ps', bk), 'selg'], w=[('ye', yb, half)])

                    def scat(ct=ct, yb=yb, e_=e_):
                        kb.idma(lambda g: g.indirect_dma_start(
                            out=c.ffn_tm[:, :], out_offset=bass.IndirectOffsetOnAxis(ap=seli[:, e_, ct:ct + 1], axis=0),
                            in_=ye[:, yb, :], in_offset=None, compute_op=ALU.add),
                            r=['seli', ('ye', yb, 0), ('ye', yb, 1)] + [('scat', e_ - 1, c2) for c2 in range(CT_)], w=[('scat', e_, ct)])
                    deferred.append(scat)
            for fn_ in deferred:
                fn_()
            kb.barrier()
        kb.barrier()


def stage_final(c):
    nc, kb, T, NTB, NT = c.nc, c.kb, c.T, c.NTB, c.NT
    with ExitStack() as es:
        A = lambda name, shp, dt: es.enter_context(nc.sbuf_tensor(_nm(name), shp, dt))
        hblk = A('z_hblk', [128, 2, 8, 512], F32)
        oblk = A('z_oblk', [128, 2, 8, 512], F32)
        ftile = A('z_ftile', [128, 2, 1024], F32)
        sq = A('z_sq', [128, 8, 512], BF16)
        rstd = A('z_rstd', [128, 512], F32)
        gfin = A('z_gfin', [128, 8], F32)
        kb.dma('sp', gfin[:], c.fin_g[:, :], w=['gfin'])
        hTv = c.hT.rearrange("(k p) t -> p k t", p=128)
        oTv = c.outT.rearrange("(k p) t -> p k t", p=128)
        for tb in range(NTB):
            b = tb % 2
            ts = slice(tb * 512, (tb + 1) * 512)
            kb.dma('sp', hblk[:, b], hTv[:, :, ts], w=[('hblk', b)])
            if 'skip_ffn' not in c.dbg:
                emit_add_tm(c, hblk[:, b], ('hblk', b), tb, ftile, (0, 1))
            emit_rmsnorm(c, hblk[:, b], ('hblk', b), gfin, 'gfin', lambda k: oblk[:, b, k, :], [('oblk', b)], 2, sq[:], 'sq', rstd[:], 'rstd')
            kb.dma('sp', oTv[:, :, ts], oblk[:, b], r=[('oblk', b)], w=[('outT', tb)])
        kb.barrier()


_CACHE = {}


def kernel(**inputs):
    x = np.asarray(inputs['x'], np.float32)
    B, T, _ = x.shape
    L = np.asarray(inputs['w_in']).shape[0]
    preps = [prep_layer(inputs, l) for l in range(L)]
    shapes = layer_shapes_from(preps[0])
    key = (T, L)
    if key not in _CACHE:
        _CACHE[key] = build(T, L, shapes)
    nc, c = _CACHE[key]
    common = {'consts': make_consts(), 'iota': make_iota(), 'fin_g': _pcol(np.asarray(inputs['final_norm_g'], np.float32), 8)}
    for l in range(L):
        for k, v in preps[l].items():
            common['l%d_%s' % (l, k)] = v
    in_maps = []
    for b in range(B):
        m = dict(common)
        m['xT'] = np.ascontiguousarray(x[b].T)
        in_maps.append(m)
    res = run_bass_kernel_spmd(nc, in_maps, core_ids=list(range(B)))
    out = np.stack([np.ascontiguousarray(res.results[b]['outT'].T) for b in range(B)], axis=0)
    return out.astype(np.float32)
```

```python
import math
from contextlib import ExitStack
import numpy as np
import concourse.bass as bass
import concourse.mybir as mybir
from concourse.bass_utils import run_bass_kernel_spmd

F32 = mybir.dt.float32
BF16 = mybir.dt.bfloat16
I32 = mybir.dt.int32
U32 = mybir.dt.uint32
AF = mybir.ActivationFunctionType
ALU = mybir.AluOpType
AX = mybir.AxisListType

D = 1024
KC = 8
INW = 4896
NE = 16
FF = 2048
EPS = 1e-6
SAME_ENGINE_SYNC = True


class KB:
    NS = 6

    def __init__(self, nc):
        self.nc = nc
        self.eng = {'pe': nc.tensor, 'act': nc.scalar, 'dve': nc.vector, 'pool': nc.gpsimd, 'sp': nc.sync}
        self.csem = {}
        self.ccnt = {}
        for e in ['pe', 'act', 'dve', 'pool']:
            self.csem[e] = nc.semaphore('c_' + e).__enter__()
            self.ccnt[e] = 0
        self.dsem = {}
        self.dval = {}
        self.dnext = {}
        for q in ['sp', 'act', 'pool']:
            self.dsem[q] = [nc.semaphore('d_%s%d' % (q, i)).__enter__() for i in range(self.NS)]
            self.dval[q] = [0] * self.NS
            self.dnext[q] = 0
        self.semname = {}
        self.semowner = {}
        for e, s in self.csem.items():
            self.semname[id(s)] = 'c_' + e
            self.semowner['c_' + e] = e
        self.semobj = {}
        for e, s in self.csem.items():
            self.semobj['c_' + e] = s
        for q in self.dsem:
            for i, s in enumerate(self.dsem[q]):
                self.semobj['d_%s%d' % (q, i)] = s
        self.waited = {e: {} for e in self.eng}
        self.lastw = {}
        self.readers = {}
        self.ninstr = 0

    def _wait(self, e, tok):
        name, val = tok
        if val <= 0:
            return
        if self.waited[e].get(name, 0) >= val:
            return
        owner = self.semowner.get(name)
        if owner == e and (e == 'pe' or not SAME_ENGINE_SYNC):
            return
        self.eng[e].wait_ge(self.semobj[name], val)
        self.waited[e][name] = val
        self.ninstr += 1

    def _deps(self, r, w):
        toks = {}

        def add(t):
            if t is None:
                return
            if toks.get(t[0], 0) < t[1]:
                toks[t[0]] = t[1]
        for k in r:
            add(self.lastw.get(k))
        for k in w:
            add(self.lastw.get(k))
            for n, v in self.readers.get(k, {}).items():
                add((n, v))
        return list(toks.items())

    def _register(self, tok, r, w):
        for k in w:
            self.lastw[k] = tok
            self.readers[k] = {}
        for k in r:
            d = self.readers.setdefault(k, {})
            if d.get(tok[0], 0) < tok[1]:
                d[tok[0]] = tok[1]

    def op(self, e, fn, r=(), w=()):
        psk = [k for k in r if isinstance(k, tuple) and k[0] == 'ps']
        toks = dict(self._deps(r, w))
        me = 'c_' + e
        for k in psk:
            for n, v in self.readers.get(k, {}).items():
                if n != me and toks.get(n, 0) < v:
                    toks[n] = v
        for t in toks.items():
            self._wait(e, t)
        ins = fn(self.eng[e])
        self.ccnt[e] += 1
        ins.then_inc(self.csem[e], 1)
        tok = ('c_' + e, self.ccnt[e])
        self._register(tok, r, w)
        self.ninstr += 1
        return tok

    def dma(self, q, out, in_, r=(), w=(), **kw):
        slot = self.dnext[q] % self.NS
        self.dnext[q] += 1
        name = 'd_%s%d' % (q, slot)
        self._wait(q, (name, self.dval[q][slot]))
        for t in self._deps(r, w):
            self._wait(q, t)
        ins = self.eng[q].dma_start(out=out, in_=in_, **kw)
        ins.then_inc(self.dsem[q][slot], 16)
        self.dval[q][slot] += 16
        tok = (name, self.dval[q][slot])
        self._register(tok, r, w)
        self.ninstr += 1
        return tok

    def idma(self, fn, r=(), w=()):
        q = 'pool'
        slot = self.dnext[q] % self.NS
        self.dnext[q] += 1
        name = 'd_%s%d' % (q, slot)
        self._wait(q, (name, self.dval[q][slot]))
        for t in self._deps(r, w):
            self._wait(q, t)
        ins = fn(self.eng[q])
        ins.then_inc(self.dsem[q][slot], 16)
        self.dval[q][slot] += 16
        tok = (name, self.dval[q][slot])
        self._register(tok, r, w)
        self.ninstr += 1
        return tok

    def barrier(self):
        toks = [('c_' + e, self.ccnt[e]) for e in self.ccnt]
        for q in self.dsem:
            for i in range(self.NS):
                toks.append(('d_%s%d' % (q, i), self.dval[q][i]))
        for e in self.eng:
            for t in toks:
                self._wait(e, t)
        self.lastw = {}
        self.readers = {}


class Ctx:
    pass


_NMC = [0]


def _nm(name):
    _NMC[0] += 1
    return '%s_%d' % (name, _NMC[0])


def _bc_mid(ap2d, n):
    return ap2d.unsqueeze(1).broadcast_to([ap2d.shape[0], n, ap2d.shape[1]])


def _bc_last(ap2d, n):
    return ap2d.unsqueeze(2).broadcast_to([ap2d.shape[0], ap2d.shape[1], n])


def _pcol(v, nch):
    return np.ascontiguousarray(np.asarray(v, np.float32).reshape(nch, 128).T)


def prep_layer(inp, l):
    f = lambda k: np.asarray(inp[k], np.float32)
    o = {}
    o['w_in'] = np.ascontiguousarray(f('w_in')[l])
    o['mix_g'] = _pcol(f('mix_norm_g')[l], 8)
    o['ffn_g'] = _pcol(f('ffn_norm_g')[l], 8)
    cw = f('lru_conv_w')[l]
    o['lru_cw'] = np.ascontiguousarray(cw.reshape(4, 4, 128).transpose(2, 1, 0))
    o['lru_cb'] = _pcol(f('lru_conv_b')[l], 4)
    cwd = np.zeros((128, 4, 4, 128), np.float32)
    for ct_ in range(4):
        for j_ in range(4):
            cwd[np.arange(128), ct_, j_, np.arange(128)] = cw[j_, ct_ * 128:(ct_ + 1) * 128]
    o['lru_cwd'] = cwd
    for nm in ['a', 'x']:
        w = f('lru_w_' + nm)[l]
        bd = np.zeros((128, 2, 4, 128), np.float32)
        for di in range(2):
            for ct in range(4):
                bd[0:64, di, ct, 0:64] = w[di, 2 * ct]
                bd[64:128, di, ct, 64:128] = w[di, 2 * ct + 1]
        o['lru_w' + nm] = bd
        b = f('lru_b_' + nm)[l]
        o['lru_b' + nm] = np.ascontiguousarray(b.reshape(2, 4, 128).transpose(2, 0, 1))
    o['lru_lam'] = np.ascontiguousarray(f('lru_lam')[l].reshape(2, 4, 128).transpose(2, 0, 1))
    prep_gla(inp, l, o)
    prep_s5(inp, l, o)
    prep_rest(inp, l, o)
    return o


def build(T, nlayers, layer_shapes, dbg=()):
    nc = bass.Bass("TRN2", target_bir_lowering=False)
    kb = KB(nc)
    NTB = T // 512
    NT = T // 128
    c = Ctx()
    c.nc, c.kb, c.T, c.NTB, c.NT = nc, kb, T, NTB, NT

    def dram_in(name, shape, dt=F32):
        return nc.dram_tensor(name, list(shape), dt, kind="ExternalInput").ap()

    def dram_out(name, shape, dt=F32):
        return nc.dram_tensor(name, list(shape), dt, kind="ExternalOutput").ap()

    def dram_tmp(name, shape, dt=F32):
        return nc.dram_tensor(name, list(shape), dt, kind="Internal").ap()

    c.xT = dram_in('xT', [D, T])
    c.consts = dram_in('consts', [128, NCONST, 128])
    c.iota = dram_in('iota', [128, 513])
    c.fin_g = dram_in('fin_g', [128, 8])
    c.lw = []
    for l in range(nlayers):
        c.lw.append({k: dram_in('l%d_%s' % (l, k), shp) for k, shp in layer_shapes.items()})
    c.outT = dram_out('outT', [D, T])
    c.hT = dram_tmp('hT', [D, T])
    c.pmixT = dram_tmp('pmixT', [768, T])
    c.gT = dram_tmp('gT', [3072, T], BF16)
    c.ptm = dram_tmp('ptm', [T, 1056])
    c.alrT = dram_tmp('alrT', [32, T], BF16)
    c.yT = dram_tmp('yT', [1024, T], BF16)
    c.ffn_tm = dram_tmp('ffn_tm', [T, D])
    c.hn_tm = dram_tmp('hn_tm', [T, D], BF16)
    c.dbg = {}
    c.lvl = 99
    for name, shp in dbg:
        if name.startswith('lvl'):
            c.lvl = int(name[3:])
            continue
        c.dbg[name] = dram_out('dbg_' + name, shp)

    c.ps = [nc.psum_tensor('ps%d' % i, [128, 512], F32).__enter__() for i in range(8)]

    c.ones_bf = nc.sbuf_tensor(_nm('ones_bf'), [128, 128], BF16).__enter__()
    kb.op('dve', lambda e: e.memset(c.ones_bf[:], 1.0), w=['ones_bf'])

    load_consts(c)
    for l in range(nlayers):
        stage_norm_proj(c, l, src=(c.xT if l == 0 else c.hT), add_tm=(l > 0))
        kb.barrier()
        if 'skip_lru' not in c.dbg:
            stage_lru(c, l)
        kb.barrier()
        if 'skip_gla' not in c.dbg:
            stage_gla(c, l)
        kb.barrier()
        if 'skip_s5' not in c.dbg:
            stage_s5(c, l)
        kb.barrier()
        if 'stop_mixers' in c.dbg:
            continue
        CAPT = (2 * T // NE) // 128
        with ExitStack() as esl:
            seli = esl.enter_context(nc.sbuf_tensor(_nm('f_seli'), [128, NE, CAPT], I32))
            selg = esl.enter_context(nc.sbuf_tensor(_nm('f_selg'), [128, NE, CAPT], F32))
            with nc.sbuf_tensor(_nm('hnT'), [128, KC, T], BF16) as hnT:
                stage_merge(c, l, hnT)
                kb.barrier()
                if 'skip_ffn' not in c.dbg:
                    stage_ffn(c, l, hnT, seli, selg, 0)
                kb.barrier()
            if 'skip_ffn' not in c.dbg:
                stage_ffn(c, l, None, seli, selg, 1)
            kb.barrier()
    if 'stop_mixers' not in c.dbg:
        stage_final(c)
    kb.barrier()
    return nc, c


def stage_norm_proj(c, l, src, add_tm=False):
    nc, kb, T, NTB, NT = c.nc, c.kb, c.T, c.NTB, c.NT
    W = c.lw[l]
    with ExitStack() as es:
        xn = es.enter_context(nc.sbuf_tensor(_nm('xn'), [128, KC, T], BF16))
        hblk = es.enter_context(nc.sbuf_tensor(_nm('hblk'), [128, 2, KC, 512], F32))
        sq = es.enter_context(nc.sbuf_tensor(_nm('sq'), [128, 2, KC, 512], BF16))
        rstd = es.enter_context(nc.sbuf_tensor(_nm('rstd'), [128, 2, 512], F32))
        g_mix = es.enter_context(nc.sbuf_tensor(_nm('g_mix'), [128, KC], F32))
        wA = es.enter_context(nc.sbuf_tensor(_nm('wA'), [128, KC, 768], BF16))
        wB = es.enter_context(nc.sbuf_tensor(_nm('wB'), [128, KC, 1056], BF16))
        wG = es.enter_context(nc.sbuf_tensor(_nm('wG'), [128, 2, KC, 512], BF16))
        stf = es.enter_context(nc.sbuf_tensor(_nm('stf'), [128, 4, 512], F32))
        stg = es.enter_context(nc.sbuf_tensor(_nm('stg'), [128, 4, 512], BF16))
        sttm = es.enter_context(nc.sbuf_tensor(_nm('sttm'), [128, 2, 1056], F32))
        stalr = es.enter_context(nc.sbuf_tensor(_nm('stalr'), [32, T], BF16))
        ftile = es.enter_context(nc.sbuf_tensor(_nm('ftile'), [128, 2, 1024], F32))
        kb.dma('sp', g_mix[:], W['mix_g'][:, :], w=['g_mix'])
        w_in_v = W['w_in'].rearrange("(k p) c -> p k c", p=128)
        kb.dma('pool', wA[:], w_in_v[:, :, 0:768], w=['wA'])
        kb.dma('pool', wB[:], w_in_v[:, :, 768:1824], w=['wB'])
        srcv = src.rearrange("(k p) t -> p k t", p=128)
        for tb in range(NTB):
            b = tb % 2
            ts = slice(tb * 512, (tb + 1) * 512)
            kb.dma('sp', hblk[:, b], srcv[:, :, ts], w=[('hblk', b)])
            if add_tm:
                emit_add_tm(c, hblk[:, b], ('hblk', b), tb, ftile, (2, 3))
                kb.dma('sp', srcv[:, :, ts], hblk[:, b], r=[('hblk', b)], w=[('hTw', tb)])
            kb.op('act', lambda e: e.activation(out=sq[:, b], in_=hblk[:, b], func=AF.Square),
                  r=[('hblk', b)], w=[('sq', b)])
            pb = c.ps[b]
            for k in range(KC):
                kb.op('pe', lambda e, k=k: e.matmul(pb[:], lhsT=c.ones_bf[:], rhs=sq[:, b, k, :],
                                                    start=(k == 0), stop=(k == KC - 1)),
                      r=[('sq', b), 'ones_bf'], w=[('ps', b)])
            kb.op('act', lambda e: e.activation(out=rstd[:, b], in_=pb[:], func=AF.Sqrt,
                                                scale=1.0 / D, bias=EPS),
                  r=[('ps', b)], w=[('rstd', b)])
            kb.op('dve', lambda e: e.reciprocal(out=rstd[:, b], in_=rstd[:, b]),
                  r=[('rstd', b)], w=[('rstd', b)])
            for k in range(KC):
                kb.op('dve', lambda e, k=k: e.scalar_tensor_tensor(
                    out=xn[:, k, ts], in0=hblk[:, b, k, :], scalar=g_mix[:, k:k + 1], in1=rstd[:, b],
                    op0=ALU.mult, op1=ALU.mult),
                    r=[('hblk', b), ('rstd', b), 'g_mix'], w=[('xn', tb)])
        nps = 0
        nst = 0
        for cc in range(6):
            for tb in range(NTB):
                ts = slice(tb * 512, (tb + 1) * 512)
                pi = 2 + (nps % 4)
                nps += 1
                sb = nst % 4
                nst += 1
                pb = c.ps[pi]
                for k in range(KC):
                    kb.op('pe', lambda e, k=k, pb=pb: e.matmul(
                        pb[:], lhsT=wA[:, k, cc * 128:(cc + 1) * 128], rhs=xn[:, k, ts],
                        start=(k == 0), stop=(k == KC - 1)),
                        r=['wA', ('xn', tb)], w=[('ps', pi)])
                if tb % 2 == 0:
                    kb.op('act', lambda e, pb=pb: e.copy(out=stf[:, sb, :], in_=pb[:]),
                          r=[('ps', pi)], w=[('stf', sb)])
                else:
                    kb.op('dve', lambda e, pb=pb: e.tensor_copy(out=stf[:, sb, :], in_=pb[:]),
                          r=[('ps', pi)], w=[('stf', sb)])
                kb.dma('sp', c.pmixT[cc * 128:(cc + 1) * 128, ts], stf[:, sb, :], r=[('stf', sb)], w=[('pmixT', cc, tb)])
        for tb in range(NTB):
            ts = slice(tb * 512, (tb + 1) * 512)
            pi = 2 + (nps % 4)
            nps += 1
            pb = c.ps[pi]
            for k in range(KC):
                kb.op('pe', lambda e, k=k, pb=pb: e.matmul(
                    pb[0:32, :], lhsT=wB[:, k, 1024:1056], rhs=xn[:, k, ts],
                    start=(k == 0), stop=(k == KC - 1)),
                    r=['wB', ('xn', tb)], w=[('ps', pi)])
            kb.op('act', lambda e, pb=pb: e.copy(out=stalr[:, ts], in_=pb[0:32, :]),
                  r=[('ps', pi)], w=[('stalr', tb)])
        kb.dma('sp', c.alrT[:, :], stalr[:, :], r=[('stalr', tb) for tb in range(NTB)], w=['alrT'])
        for tt in range(NT):
            sb = tt % 2
            tb = tt // 4
            tsl = slice(tt * 128, (tt + 1) * 128)
            for gi, (c0, c1) in enumerate([(0, 512), (512, 1024), (1024, 1056)]):
                pi = 2 + (nps % 4)
                nps += 1
                pb = c.ps[pi]
                for k in range(KC):
                    kb.op('pe', lambda e, k=k, pb=pb: e.matmul(
                        pb[:, 0:c1 - c0], lhsT=xn[:, k, tsl], rhs=wB[:, k, c0:c1],
                        start=(k == 0), stop=(k == KC - 1)),
                        r=['wB', ('xn', tb)], w=[('ps', pi)])
                if gi == 0:
                    kb.op('act', lambda e, pb=pb: e.copy(out=sttm[:, sb, c0:c1], in_=pb[:, 0:c1 - c0]),
                          r=[('ps', pi)], w=[('sttm', sb, gi)])
                else:
                    kb.op('dve', lambda e, pb=pb: e.tensor_copy(out=sttm[:, sb, c0:c1], in_=pb[:, 0:c1 - c0]),
                          r=[('ps', pi)], w=[('sttm', sb, gi)])
            kb.dma('sp', c.ptm[tsl, :], sttm[:, sb, :], r=[('sttm', sb, gi) for gi in range(3)],
                   w=[('ptm', tt)])
        for cg in range(6):
            wb_ = cg % 2
            kb.dma('pool', wG[:, wb_], w_in_v[:, :, 1824 + cg * 512:1824 + (cg + 1) * 512], w=[('wG', wb_)])
            for c4 in range(4):
                cc = cg * 4 + c4
                for tb in range(NTB):
                    ts = slice(tb * 512, (tb + 1) * 512)
                    pi = 2 + (nps % 4)
                    nps += 1
                    pb = c.ps[pi]
                    for k in range(KC):
                        kb.op('pe', lambda e, k=k, pb=pb: e.matmul(
                            pb[:], lhsT=wG[:, wb_, k, c4 * 128:(c4 + 1) * 128], rhs=xn[:, k, ts],
                            start=(k == 0), stop=(k == KC - 1)),
                            r=[('wG', wb_), ('xn', tb)], w=[('ps', pi)])
                    sb = nst % 4
                    nst += 1
                    kb.op('act', lambda e, pb=pb: e.activation(out=stg[:, sb, :], in_=pb[:], func=AF.Sigmoid),
                          r=[('ps', pi)], w=[('stg', sb)])
                    kb.dma('sp', c.gT[cc * 128:(cc + 1) * 128, ts], stg[:, sb, :], r=[('stg', sb)], w=[('gT', cc, tb)])
        if 'pmixT' in c.dbg:
            kb.barrier()
            kb.dma('sp', c.dbg['pmixT'][:, :], c.pmixT[:, :], w=['dbg_pmixT'])
        kb.barrier()


def stage_lru(c, l):
    nc, kb, T, NTB, NT = c.nc, c.kb, c.T, c.NTB, c.NT
    W = c.lw[l]
    with ExitStack() as es:
        xbp = es.enter_context(nc.sbuf_tensor(_nm('xbpb'), [128, T + 4], BF16))
        l_cwd = es.enter_context(nc.sbuf_tensor(_nm('l_cwd'), [128, 4, 4, 128], BF16))
        xc = es.enter_context(nc.sbuf_tensor(_nm('xc'), [128, T], F32))
        xcb = es.enter_context(nc.sbuf_tensor(_nm('xcb'), [128, T], BF16))
        bA = es.enter_context(nc.sbuf_tensor(_nm('bA'), [128, T], F32))
        bB = es.enter_context(nc.sbuf_tensor(_nm('bB'), [128, T], F32))
        bC = es.enter_context(nc.sbuf_tensor(_nm('bC'), [128, T], F32))
        hf = es.enter_context(nc.sbuf_tensor(_nm('hf'), [128, T], F32))
        ybf = es.enter_context(nc.sbuf_tensor(_nm('ybf'), [128, 2, T], BF16))
        l_cw = es.enter_context(nc.sbuf_tensor(_nm('l_cw'), [128, 4, 4], F32))
        l_cb = es.enter_context(nc.sbuf_tensor(_nm('l_cb'), [128, 4], F32))
        l_wa = es.enter_context(nc.sbuf_tensor(_nm('l_wa'), [128, 2, 4, 128], BF16))
        l_wx = es.enter_context(nc.sbuf_tensor(_nm('l_wx'), [128, 2, 4, 128], BF16))
        l_ba = es.enter_context(nc.sbuf_tensor(_nm('l_ba'), [128, 8], F32))
        l_bx = es.enter_context(nc.sbuf_tensor(_nm('l_bx'), [128, 8], F32))
        l_lam = es.enter_context(nc.sbuf_tensor(_nm('l_lam'), [128, 8], F32))
        l_c = es.enter_context(nc.sbuf_tensor(_nm('l_c'), [128, 8], F32))
        l_c2 = es.enter_context(nc.sbuf_tensor(_nm('l_c2'), [128, 8], F32))
        l_t = es.enter_context(nc.sbuf_tensor(_nm('l_t'), [128, 6, 8], F32))
        kb.dma('sp', l_cw[:], W['lru_cw'][:, :, :], w=['l_cw'])
        kb.dma('pool', l_cwd[:], W['lru_cwd'][:, :, :, :], w=['l_cwd'])
        kb.dma('sp', l_cb[:], W['lru_cb'][:, :], w=['l_cb'])
        kb.dma('pool', l_wa[:], W['lru_wa'][:, :, :, :], w=['l_wa'])
        kb.dma('pool', l_wx[:], W['lru_wx'][:, :, :, :], w=['l_wx'])
        kb.dma('sp', l_ba[:], W['lru_ba'].rearrange("p a b -> p (a b)"), w=['l_ba'])
        kb.dma('sp', l_bx[:], W['lru_bx'].rearrange("p a b -> p (a b)"), w=['l_bx'])
        kb.dma('sp', l_lam[:], W['lru_lam'].rearrange("p a b -> p (a b)"), w=['l_lam'])
        P = 'l_par'
        tz, tw, tw2, tacc, tm, tabs = [l_t[:, i, :] for i in range(6)]
        dv = lambda fn, r=(P,), w=(P,): kb.op('dve', fn, r=list(r) + ['l_lam'], w=list(w))
        dv(lambda e: e.scalar_tensor_tensor(out=tabs, in0=l_lam[:], scalar=-1.0, in1=l_lam[:], op0=ALU.mult, op1=ALU.max))
        kb.op('act', lambda e: e.activation(out=tz, in_=tabs, func=AF.Exp, scale=-1.0), r=[P], w=[P])
        dv(lambda e: e.tensor_scalar(out=tw, in0=tz, scalar1=2.0, scalar2=None, op0=ALU.add))
        dv(lambda e: e.reciprocal(out=tw, in_=tw))
        dv(lambda e: e.tensor_tensor(out=tw, in0=tw, in1=tz, op=ALU.mult))
        dv(lambda e: e.tensor_tensor(out=tw2, in0=tw, in1=tw, op=ALU.mult))
        dv(lambda e: e.memset(tacc, 1.0 / 13.0))
        for kk in [11, 9, 7, 5, 3, 1]:
            dv(lambda e: e.tensor_tensor(out=tacc, in0=tacc, in1=tw2, op=ALU.mult))
            dv(lambda e, kk=kk: e.tensor_scalar(out=tacc, in0=tacc, scalar1=1.0 / kk, scalar2=None, op0=ALU.add))
        dv(lambda e: e.tensor_tensor(out=tacc, in0=tacc, in1=tw, op=ALU.mult))
        dv(lambda e: e.tensor_scalar(out=tm, in0=l_lam[:], scalar1=-1.0, scalar2=0.0, op0=ALU.mult, op1=ALU.max))
        dv(lambda e: e.scalar_tensor_tensor(out=tacc, in0=tacc, scalar=2.0, in1=tm, op0=ALU.mult, op1=ALU.add))
        dv(lambda e: e.tensor_scalar(out=l_c[:], in0=tacc, scalar1=-8.0, scalar2=None, op0=ALU.mult), w=[P, 'l_c'])
        dv(lambda e: e.tensor_scalar(out=l_c2[:], in0=tacc, scalar1=-16.0, scalar2=None, op0=ALU.mult), w=[P, 'l_c'])
        kb.op('dve', lambda e: e.memset(xbp[:, 0:2], 0.0), w=['xbp_pad'])
        kb.op('dve', lambda e: e.memset(xbp[:, T + 2:T + 4], 0.0), w=['xbp_pad'])
        nps = 0
        for ct in range(4):
            CW_ = min(2048, T)
            for c0 in range(0, T, CW_):
                kb.dma('pool', xbp[:, 2 + c0:2 + c0 + CW_], c.pmixT[256 + ct * 128:256 + (ct + 1) * 128, c0:c0 + CW_], w=[('xbp', c0)])
            for tb in range(NTB):
                ts = slice(tb * 512, (tb + 1) * 512)
                bk = 4 + tb % 2
                for j in range(4):
                    kb.op('pe', lambda e, j=j, bk=bk: e.matmul(c.ps[bk][:], lhsT=l_cwd[:, ct, j, :], rhs=xbp[:, tb * 512 + j:tb * 512 + j + 512],
                                                             start=(j == 0), stop=(j == 3)),
                          r=[('xbp', c0) for c0 in range(0, T, CW_)] + ['xbp_pad', 'l_cwd'], w=[('ps', bk)])
                kb.op('act', lambda e, bk=bk: e.activation(out=xc[:, ts], in_=c.ps[bk][:], func=AF.Identity, bias=l_cb[:, ct:ct + 1], scale=1.0),
                      r=[('ps', bk), 'l_cb'], w=['xc'])
                kb.op('dve', lambda e, bk=bk: e.tensor_scalar(out=xcb[:, ts], in0=c.ps[bk][:], scalar1=l_cb[:, ct:ct + 1], scalar2=None, op0=ALU.add),
                      r=[('ps', bk), 'l_cb'], w=['xcb'])
            for di in range(2):
                for (wt, bt, dst, key) in [(l_wa, l_ba, bA, 'bA'), (l_wx, l_bx, bB, 'bB')]:
                    for tb in range(NTB):
                        ts = slice(tb * 512, (tb + 1) * 512)
                        pi = nps % 4
                        nps += 1
                        pb = c.ps[pi]
                        kb.op('pe', lambda e, pb=pb, wt=wt: e.matmul(pb[:], lhsT=wt[:, di, ct, :], rhs=xcb[:, ts],
                                                                     start=True, stop=True),
                              r=['xcb', 'l_wa', 'l_wx'], w=[('ps', pi)])
                        kb.op('act', lambda e, pb=pb, bt=bt, dst=dst: e.activation(
                            out=dst[:, ts], in_=pb[:], func=AF.Sigmoid,
                            bias=bt[:, di * 4 + ct:di * 4 + ct + 1]),
                            r=[('ps', pi), 'l_ba', 'l_bx'], w=[key])
                cs = l_c[:, di * 4 + ct:di * 4 + ct + 1]
                c2s = l_c2[:, di * 4 + ct:di * 4 + ct + 1]
                kb.op('act', lambda e: e.activation(out=bC[:], in_=bA[:], func=AF.Exp, scale=cs),
                      r=['bA', 'l_c'], w=['bC'])
                kb.op('act', lambda e: e.activation(out=bA[:], in_=bA[:], func=AF.Exp, scale=c2s),
                      r=['bA', 'l_c'], w=['bA'])
                kb.op('act', lambda e: e.activation(out=bA[:], in_=bA[:], func=AF.Sqrt, scale=-1.0, bias=1.0),
                      r=['bA'], w=['bA'])
                kb.op('dve', lambda e: e.tensor_tensor(out=bB[:], in0=bB[:], in1=bA[:], op=ALU.mult),
                      r=['bA', 'bB'], w=['bB'])
                kb.op('dve', lambda e: e.tensor_tensor(out=bB[:], in0=bB[:], in1=xc[:], op=ALU.mult),
                      r=['xc', 'bB'], w=['bB'])
                if di == 0:
                    kb.op('dve', lambda e: e.tensor_tensor_scan(out=hf[:], data0=bC[:], data1=bB[:], initial=0.0,
                                                                op0=ALU.mult, op1=ALU.add),
                          r=['bC', 'bB'], w=['hf'])
                else:
                    rev = lambda t: bass.AP(t[:].tensor, t[:, T - 1:T].offset, [[t[:].ap[0][0], 128], [-1, T]])
                    kb.op('dve', lambda e: e.tensor_tensor_scan(out=rev(bA), data0=rev(bC), data1=rev(bB),
                                                                initial=0.0, op0=ALU.mult, op1=ALU.add),
                          r=['bC', 'bB', 'bA'], w=['bA'])
                    sb = ct % 2
                    kb.op('dve', lambda e: e.tensor_tensor(out=ybf[:, sb, :], in0=hf[:], in1=bA[:], op=ALU.add),
                          r=['hf', 'bA'], w=[('ybf', sb)])
                    kb.dma('sp', c.yT[256 + ct * 128:256 + (ct + 1) * 128, :], ybf[:, sb, :],
                           r=[('ybf', sb)], w=[('yT', 2 + ct)])
        if 'ylru' in c.dbg:
            kb.barrier()
            with nc.sbuf_tensor(_nm('dbgt'), [128, T], F32) as dbgt:
                for ct in range(4):
                    kb.dma('pool', dbgt[:], c.yT[256 + ct * 128:256 + (ct + 1) * 128, :], w=['dbgt'])
                    kb.dma('sp', c.dbg['ylru'][ct * 128:(ct + 1) * 128, :], dbgt[:], r=['dbgt'], w=[('dbgo', ct)])
                kb.barrier()
        kb.barrier()


LAYER_SHAPES = None


def layer_shapes_from(prep):
    return {k: v.shape for k, v in prep.items()}


NCONST = 10


def make_consts():
    s = np.arange(128)[:, None]
    t = np.arange(128)[None, :]
    cst = np.zeros((128, NCONST, 128), np.float32)
    cst[:, 0] = (s == t)
    cst[:, 1] = (s <= t) * (-1.0 / 16)
    cst[:, 2] = (s >= t) * (-1.0 / 16)
    cst[:, 3] = -1.0 / 16
    cst[:, 4] = (s <= t)
    cst[:, 5] = (s > t)
    cst[:, 6] = (s < t)
    cst[:, 7] = 1.0
    cst[:, 8] = (s <= t)
    cst[:, 9] = (s >= t)
    return cst


def prep_gla(inp, l, o):
    f = lambda k: np.asarray(inp[k], np.float32)
    wal = np.zeros((33, 512), np.float32)
    wal[0:16, 0:256] = f('gla_w_alpha')[l, 0]
    wal[16:32, 256:512] = f('gla_w_alpha')[l, 1]
    wal[32, 0:256] = f('gla_b_alpha')[l, 0]
    wal[32, 256:512] = f('gla_b_alpha')[l, 1]
    o['gla_wal'] = wal
    o['gla_ng'] = np.ascontiguousarray(np.broadcast_to(f('gla_norm_g')[l][None, :], (128, 64)))


def load_consts(c):
    nc, kb = c.nc, c.kb
    c.cst = nc.sbuf_tensor(_nm('cst'), [128, NCONST, 128], F32).__enter__()
    c.cstb = nc.sbuf_tensor(_nm('cstb'), [128, NCONST, 128], BF16).__enter__()
    kb.dma('sp', c.cst[:], c.consts[:, :, :], w=['cst'])
    kb.dma('pool', c.cstb[:], c.consts[:, :, :], w=['cst'])


def stage_gla(c, l):
    nc, kb, T, NTB, NT = c.nc, c.kb, c.T, c.NTB, c.NT
    W = c.lw[l]
    cst, cstb = c.cst, c.cstb
    ident_b = cstb[:, 0, :]
    with ExitStack() as es:
        A = lambda name, shp, dt: es.enter_context(nc.sbuf_tensor(_nm(name), shp, dt))
        ptile = A('g_pt', [128, 2, 1056], F32)
        alrx = A('g_alrx', [64, T], BF16)
        wal = A('g_wal', [64, 512], BF16)
        spt = A('g_sp', [128, 512], F32)
        E1 = A('g_E1', [128, 512], F32)
        E2 = A('g_E2', [128, 512], F32)
        decrow = A('g_decrow', [128, 512], F32)
        qd = A('g_qd', [128, 2, 256], BF16)
        kd = A('g_kd', [128, 2, 256], BF16)
        kk = A('g_kk', [128, 2, 256], BF16)
        vb = A('g_vb', [128, 256], BF16)
        qdT = A('g_qdT', [128, 4, T], BF16)
        kdT = A('g_kdT', [128, 4, 128], BF16)
        scm = A('g_scm', [128, 2, 4, 128], BF16)
        kv = A('g_kv', [128, NT, 4, 64], F32)
        dec = A('g_dec', [128, NT, 4], F32)
        oacc = A('g_oacc', [128, NT, 256], F32)
        sog = A('g_sog', [128, NT, 256], BF16)
        S = A('g_S', [128, 4, 64], F32)
        Sb = A('g_Sb', [128, 4, 64], BF16)
        ng = A('g_ng', [128, 64], F32)
        ygT = A('g_ygT', [128, 2, T], BF16)
        ss = A('g_ss', [128, NT * 4], F32)
        ps = c.ps
        kb.op('dve', lambda e: e.memset(alrx[32:64, :], 1.0), w=['alrx1'])
        kb.dma('sp', alrx[0:32, :], c.alrT[:, :], w=['alrx'])
        kb.op('dve', lambda e: e.memset(wal[:], 0.0), w=['wal'])
        kb.dma('pool', wal[0:33, :], W['gla_wal'][:, :], r=[], w=['wal'])
        kb.dma('sp', ng[:], W['gla_ng'][:, :], w=['ng'])
        for n in range(NT):
            b = n % 2
            nsl = slice(n * 128, (n + 1) * 128)
            kb.dma('sp', ptile[:, b, :], c.ptm[nsl, :], w=[('pt', b)])
            q_ap = ptile[:, b, 0:256]
            k_ap = ptile[:, b, 256:512]
            v_ap = ptile[:, b, 512:768]
            og_ap = ptile[:, b, 768:1024]
            kb.op('pe', lambda e: e.matmul(ps[0][:], lhsT=alrx[0:33, nsl], rhs=wal[0:33, :], start=True, stop=True),
                  r=['alrx', 'alrx1', 'wal'], w=[('ps', 0)])
            kb.op('act', lambda e: e.activation(out=spt[:], in_=ps[0][:], func=AF.Exp, scale=-1.0),
                  r=[('ps', 0)], w=['spt'])
            kb.op('act', lambda e: e.activation(out=spt[:], in_=spt[:], func=AF.Ln, bias=1.0),
                  r=['spt'], w=['spt'])
            if 'gla_stop0' in c.dbg:
                continue
            kb.op('pe', lambda e: e.matmul(ps[1][:, 0:256], lhsT=cst[:, 1, :], rhs=spt[:, 0:256], start=True, stop=True),
                  r=['spt', 'cst'], w=[('ps', 1)])
            kb.op('pe', lambda e: e.matmul(ps[1][:, 256:512], lhsT=cst[:, 2, :], rhs=spt[:, 256:512], start=True, stop=True),
                  r=['spt', 'cst'], w=[('ps', 1)])
            kb.op('pe', lambda e: e.matmul(ps[2][:], lhsT=cst[:, 3, :], rhs=spt[:], start=True, stop=True),
                  r=['spt', 'cst'], w=[('ps', 2)])
            if c.lvl < 1:
                continue
            for i4 in range(4):
                kb.op('pe', lambda e, i4=i4: e.matmul(ps[6][:, 256 + 2 * i4:258 + 2 * i4], lhsT=spt[:, i4 * 128:(i4 + 1) * 128],
                                                     rhs=cst[:, 3, 0:2], start=True, stop=True),
                      r=['spt', 'cst'], w=[('ps', 6)])
            kb.op('act', lambda e: e.activation(out=dec[:, n, :], in_=ps[6][:, 256:264:2], func=AF.Exp),
                  r=[('ps', 6)], w=['dec'])
            if c.lvl < 2:
                continue
            kb.op('act', lambda e: e.activation(out=E1[:], in_=ps[1][:], func=AF.Exp), r=[('ps', 1)], w=['E1'])
            kb.op('act', lambda e: e.activation(out=E2[:], in_=ps[1][:], func=AF.Exp, scale=-1.0), r=[('ps', 1)], w=['E2'])
            kb.op('act', lambda e: e.activation(out=decrow[:], in_=ps[2][:], func=AF.Exp), r=[('ps', 2)], w=['decrow'])
            if c.lvl < 3:
                continue
            v2 = lambda ap: ap.rearrange("p (a b) -> p a b", a=2)
            kb.op('dve', lambda e: e.scalar_tensor_tensor(out=qd[:], in0=_bc_mid(q_ap, 2), scalar=0.125, in1=v2(E1[:]),
                                                          op0=ALU.mult, op1=ALU.mult),
                  r=[('pt', b), 'E1'], w=['qd'])
            kb.op('dve', lambda e: e.tensor_tensor(out=kd[:], in0=_bc_mid(k_ap, 2), in1=v2(E2[:]), op=ALU.mult),
                  r=[('pt', b), 'E2'], w=['kd'])
            kb.op('dve', lambda e: e.tensor_tensor(out=E2[:], in0=E2[:], in1=decrow[:], op=ALU.mult),
                  r=['E2', 'decrow'], w=['E2'])
            kb.op('dve', lambda e: e.tensor_tensor(out=kk[:], in0=_bc_mid(k_ap, 2), in1=v2(E2[:]), op=ALU.mult),
                  r=[('pt', b), 'E2'], w=['kk'])
            if c.lvl < 4:
                continue
            kb.op('act', lambda e: e.copy(out=vb[:], in_=v_ap), r=[('pt', b)], w=['vb'])
            kb.op('act', lambda e: e.activation(out=decrow[:, 0:256], in_=og_ap, func=AF.Sigmoid), r=[('pt', b), 'E2'], w=['decrow'])
            kb.op('dve', lambda e: e.tensor_tensor(out=sog[:, n, :], in0=og_ap, in1=decrow[:, 0:256], op=ALU.mult), r=[('pt', b), 'decrow'], w=['sog'])
            if c.lvl < 5:
                continue
            psE = ps[3][:].bitcast(BF16)
            for i8 in range(8):
                src = (qd if i8 < 4 else kd)
                di, ct = (i8 % 4) // 2, i8 % 2
                kb.op('pe', lambda e, src=src, di=di, ct=ct, i8=i8: e.transpose(
                    out=psE[:, i8 * 128:(i8 + 1) * 128], in_=src[:, di, ct * 128:(ct + 1) * 128], identity=ident_b),
                    r=['qd', 'kd', 'cst'], w=[('ps', 3)])
            if 'noevac' in c.dbg:
                continue
            kb.op('act', lambda e: e.copy(out=qdT[:, :, nsl], in_=psE[:, 0:512].rearrange("p (a b) -> p a b", a=4)),
                  r=[('ps', 3)], w=[('qdT', n)])
            if 'evac_act_only' in c.dbg:
                continue
            kb.op('act', lambda e: e.copy(out=kdT[:], in_=psE[:, 512:1024].rearrange("p (a b) -> p a b", a=4)),
                  r=[('ps', 3)], w=['kdT'])
            if c.lvl < 6:
                continue
            for di in range(2):
                for h in range(4):
                    ct, r0 = h // 2, (h % 2) * 64
                    cb = di * 2 + h // 2
                    kb.op('pe', lambda e, di=di, h=h, ct=ct, r0=r0, cb=cb: e.matmul(
                        ps[4 + h % 2][:, cb * 128:(cb + 1) * 128], lhsT=kdT[r0:r0 + 64, di * 2 + ct, :],
                        rhs=qdT[r0:r0 + 64, di * 2 + ct, nsl], start=True, stop=True),
                        r=['kdT', ('qdT', n)], w=[('ps', 4 + h % 2)])
            mask4 = cst[:, 4:6, :].unsqueeze(2).broadcast_to([128, 2, 2, 128])
            for rg in range(2):
                kb.op('dve', lambda e, rg=rg: e.tensor_tensor(
                    out=scm[:, :, rg::2, :], in0=ps[4 + rg][:].rearrange("p (a b c) -> p a b c", a=2, b=2),
                    in1=mask4, op=ALU.mult),
                    r=[('ps', 4 + rg), 'cst'], w=[('scm', 0), ('scm', 1)])
            if c.lvl < 7:
                continue
            for h in range(4):
                for di in range(2):
                    kb.op('pe', lambda e, di=di, h=h: e.matmul(
                        ps[6][:, h * 64:(h + 1) * 64], lhsT=scm[:, di, h, :], rhs=vb[:, h * 64:(h + 1) * 64],
                        start=(di == 0), stop=(di == 1)),
                        r=[('scm', 0), ('scm', 1), 'vb'], w=[('ps', 6)])
            kb.op('act', lambda e: e.copy(out=oacc[:, n, :], in_=ps[6][:, 0:256]), r=[('ps', 6)], w=[('oacc', n)])
            if c.lvl < 8:
                continue
            for di in range(2):
                for hp in range(2):
                    i4 = di * 2 + hp
                    kb.op('pe', lambda e, di=di, hp=hp, i4=i4: e.matmul(
                        ps[7][:, i4 * 128:(i4 + 1) * 128], lhsT=kk[:, di, hp * 128:(hp + 1) * 128],
                        rhs=vb[:, hp * 128:(hp + 1) * 128], start=True, stop=True),
                        r=['kk', 'vb'], w=[('ps', 7)])
            p7 = ps[7][:].rearrange("p (a b) -> p a b", a=4)
            kb.op('act', lambda e: e.copy(out=kv[0:64, n, :, :], in_=p7[0:64, :, 0:64]), r=[('ps', 7)], w=[('kv', n, 0)])
            kb.op('dve', lambda e: e.tensor_copy(out=kv[64:128, n, :, :], in_=p7[64:128, :, 64:128]),
                  r=[('ps', 7)], w=[('kv', n, 1)])
        if 'gla_stop1' in c.dbg:
            kb.barrier()
            return
        kb.op('dve', lambda e: e.memset(S[:], 0.0), w=['S0', 'S1'])
        kb.op('dve', lambda e: e.memset(Sb[:], 0.0), w=['Sb0', 'Sb1'])
        for idx in range(NT):
            for di in range(2):
                n = idx if di == 0 else NT - 1 - idx
                nsl = slice(n * 128, (n + 1) * 128)
                if idx > 0:
                    for h in range(4):
                        hp, r0 = h // 2, (h % 2) * 64
                        bk = di * 2 + h % 2
                        kb.op('pe', lambda e, h=h, hp=hp, r0=r0, bk=bk: e.matmul(
                            ps[bk][:, hp * 64:(hp + 1) * 64], lhsT=qdT[r0:r0 + 64, di * 2 + hp, nsl],
                            rhs=Sb[r0:r0 + 64, di * 2 + hp, :], start=True, stop=True),
                            r=[('qdT', n), 'Sb%d' % di], w=[('ps', bk)])
                    for rg in range(2):
                        bk = di * 2 + rg
                        ov = oacc[:, n, :].rearrange("p (a b c) -> p a b c", a=2, b=2)[:, :, rg, :]
                        kb.op('dve', lambda e, bk=bk, ov=ov: e.tensor_tensor(
                            out=ov, in0=ov, in1=ps[bk][:, 0:128].rearrange("p (a c) -> p a c", a=2), op=ALU.add),
                            r=[('ps', bk), ('oacc', n)], w=[('oacc', n)])
                if idx < NT - 1:
                    for hp in range(2):
                        i4 = di * 2 + hp
                        kb.op('dve', lambda e, i4=i4: e.scalar_tensor_tensor(
                            out=S[:, i4, :], in0=S[:, i4, :], scalar=dec[:, n, i4:i4 + 1], in1=kv[:, n, i4, :],
                            op0=ALU.mult, op1=ALU.add),
                            r=['S%d' % di, 'dec', ('kv', n, 0), ('kv', n, 1)], w=['S%d' % di])
                    kb.op('act', lambda e: e.copy(out=Sb[:, di * 2:di * 2 + 2, :], in_=S[:, di * 2:di * 2 + 2, :]),
                          r=['S%d' % di], w=['Sb%d' % di])
        allo = [('oacc', n) for n in range(NT)]
        sqv = kv[:].rearrange("p n a b -> p (n a b)")
        oflat = oacc[:].rearrange("p n c -> p (n c)")
        kb.op('act', lambda e: e.activation(out=sqv, in_=oflat, func=AF.Square), r=allo + [('kv', n, j) for n in range(NT) for j in range(2)],
              w=['sqv'])
        kb.op('dve', lambda e: e.tensor_reduce(out=ss[:], in_=sqv.rearrange("p (a b) -> p a b", b=64), axis=AX.X, op=ALU.add),
              r=['sqv'], w=['ss'])
        kb.op('act', lambda e: e.activation(out=ss[:], in_=ss[:], func=AF.Sqrt, scale=1.0 / 64, bias=EPS), r=['ss'], w=['ss'])
        kb.op('dve', lambda e: e.reciprocal(out=ss[:], in_=ss[:]), r=['ss'], w=['ss'])
        o3 = oflat.rearrange("p (a b) -> p a b", b=64)
        kb.op('dve', lambda e: e.tensor_tensor(out=o3, in0=o3, in1=_bc_last(ss[:], 64), op=ALU.mult), r=allo + ['ss'], w=allo)
        kb.op('dve', lambda e: e.tensor_tensor(out=o3, in0=o3, in1=_bc_mid(ng[:], NT * 4), op=ALU.mult), r=allo + ['ng'], w=allo)
        sflat = sog[:].rearrange("p n c -> p (n c)")
        kb.op('dve', lambda e: e.tensor_tensor(out=sflat, in0=oflat, in1=sflat, op=ALU.mult), r=allo + ['sog'], w=['sog'])
        for n4 in range(NT // 4):
            pb = ps[2 + n4 % 2]
            pbb = pb[:].bitcast(BF16)
            for j in range(4):
                n = n4 * 4 + j
                for ct in range(2):
                    kb.op('pe', lambda e, n=n, ct=ct, j=j: e.transpose(
                        out=pbb[:, (ct * 4 + j) * 128:(ct * 4 + j + 1) * 128], in_=sog[:, n, ct * 128:(ct + 1) * 128],
                        identity=ident_b), r=['sog', 'cst'], w=[('ps', 2 + n4 % 2)])
            kb.op('act',
                  (lambda e: e.copy(out=ygT[:, :, n4 * 512:(n4 + 1) * 512], in_=pbb.rearrange("p (a b) -> p a b", a=2))),
                  r=[('ps', 2 + n4 % 2)], w=[('ygT', n4)])
        for ct in range(2):
            kb.dma('sp', c.yT[768 + ct * 128:768 + (ct + 1) * 128, :], ygT[:, ct, :],
                   r=[('ygT', n4) for n4 in range(NT // 4)], w=[('yT', 6 + ct)])
        if 'ygla' in c.dbg:
            kb.barrier()
            with nc.sbuf_tensor(_nm('dbgt2'), [128, T], F32) as dbgt:
                for ct in range(2):
                    kb.dma('pool', dbgt[:], c.yT[768 + ct * 128:768 + (ct + 1) * 128, :], w=['dbgt'])
                    kb.dma('sp', c.dbg['ygla'][ct * 128:(ct + 1) * 128, :], dbgt[:], r=['dbgt'], w=[('dbgo', ct)])
                kb.barrier()
        kb.barrier()


TWO_PI = 2.0 * math.pi
CW1 = 6.28125
CW2 = TWO_PI - CW1


def prep_s5(inp, l, o):
    f = lambda k: np.asarray(inp[k], np.float32)
    lr, li, ldt = f('s5_lam_re')[l], f('s5_lam_im')[l], f('s5_log_dt')[l]
    par = np.zeros((2, 3, 1024), np.float32)
    par[:, 0] = lr.reshape(2, 1024)
    par[:, 1] = li.reshape(2, 1024)
    par[:, 2] = np.repeat(ldt, 64, axis=1)
    o['s5_par_tm'] = np.ascontiguousarray(np.broadcast_to(par[None], (128, 2, 3, 1024)))
    o['s5_par_sm'] = np.ascontiguousarray(par.reshape(2, 3, 8, 128).transpose(3, 0, 1, 2))
    bre, bim = f('s5_b_re')[l], f('s5_b_im')[l]
    bT = np.zeros((128, 2, 2, 2, 512), np.float32)
    cre, cim = f('s5_c_re')[l], f('s5_c_im')[l]
    cT = np.zeros((128, 2, 8, 2, 128), np.float32)
    for di in range(2):
        for g in range(16):
            k, gl = g // 8, g % 8
            bT[gl * 16:(gl + 1) * 16, di, k, 0, gl * 64:(gl + 1) * 64] = bre[di, g].T
            bT[gl * 16:(gl + 1) * 16, di, k, 1, gl * 64:(gl + 1) * 64] = bim[di, g].T
            st, g2 = g // 2, g % 2
            cT[g2 * 64:(g2 + 1) * 64, di, st, 0, gl * 16:(gl + 1) * 16] = cre[di, g].T
            cT[g2 * 64:(g2 + 1) * 64, di, st, 1, gl * 16:(gl + 1) * 16] = cim[di, g].T
    o['s5_bT'] = bT
    o['s5_cT'] = cT
    o['s5_d'] = _pcol(f('s5_d')[l], 2)
    o['s5_wglu'] = np.ascontiguousarray(f('s5_w_glu')[l].reshape(2, 128, 256).transpose(1, 0, 2))
    kc = np.zeros((128, 2 + 256), np.float32)
    s = np.arange(128, dtype=np.float32)
    kc[:, 0] = s + 1
    kc[:, 1] = 128 - s
    kc[:, 2:130] = (s + 1)[None, :]
    kc[:, 130:258] = (128 - s)[None, :]
    o['s5_kc'] = kc


def _trig(kb, ang, n_t, ni_t, cos_t, sin_t, key):
    D_ = lambda fn, r, w: kb.op('dve', fn, r=r, w=w)
    D_(lambda e: e.tensor_scalar(out=n_t, in0=ang, scalar1=1.0 / TWO_PI, scalar2=None, op0=ALU.mult), [key], [key + 'n'])
    D_(lambda e: e.tensor_copy(out=ni_t, in_=n_t), [key + 'n'], [key + 'ni'])
    D_(lambda e: e.tensor_copy(out=n_t, in_=ni_t), [key + 'ni'], [key + 'n'])
    for (dst, shift) in [(sin_t, 0.0), (cos_t, math.pi / 2)]:
        D_(lambda e: e.scalar_tensor_tensor(out=dst, in0=n_t, scalar=-CW1, in1=ang, op0=ALU.mult, op1=ALU.add),
           [key, key + 'n'], [key + 'o'])
        D_(lambda e: e.scalar_tensor_tensor(out=dst, in0=n_t, scalar=-CW2, in1=dst, op0=ALU.mult, op1=ALU.add),
           [key + 'n', key + 'o'], [key + 'o'])
        if shift != 0.0:
            D_(lambda e: e.tensor_scalar(out=dst, in0=dst, scalar1=shift, scalar2=None, op0=ALU.add), [key + 'o'], [key + 'o'])
        for _ in range(2):
            D_(lambda e: e.tensor_scalar(out=ni_t.bitcast(F32), in0=dst, scalar1=math.pi, scalar2=-TWO_PI, op0=ALU.is_gt, op1=ALU.mult),
               [key + 'o'], [key + 'ni'])
            D_(lambda e: e.tensor_tensor(out=dst, in0=dst, in1=ni_t.bitcast(F32), op=ALU.add), [key + 'o', key + 'ni'], [key + 'o'])
        for _ in range(2):
            D_(lambda e: e.tensor_scalar(out=ni_t.bitcast(F32), in0=dst, scalar1=-math.pi, scalar2=TWO_PI, op0=ALU.is_lt, op1=ALU.mult),
               [key + 'o'], [key + 'ni'])
            D_(lambda e: e.tensor_tensor(out=dst, in0=dst, in1=ni_t.bitcast(F32), op=ALU.add), [key + 'o', key + 'ni'], [key + 'o'])
        D_(lambda e: e.tensor_scalar(out=dst, in0=dst, scalar1=math.pi, scalar2=-math.pi, op0=ALU.min, op1=ALU.max), [key + 'o'], [key + 'o'])
        kb.op('act', lambda e: e.activation(out=dst, in_=dst, func=AF.Sin), r=[key + 'o'], w=[key + 'o'])


def stage_s5(c, l):
    nc, kb, T, NTB, NT = c.nc, c.kb, c.T, c.NTB, c.NT
    W = c.lw[l]
    cst, cstb = c.cst, c.cstb
    ps = c.ps
    with ExitStack() as es:
        A = lambda name, shp, dt: es.enter_context(nc.sbuf_tensor(_nm(name), shp, dt))
        yacc = A('s_yacc', [128, 2, T], F32)
        uTb = A('s_uTb', [128, 2, T], BF16)
        Nre = A('s_Nre', [128, 2, 1024], F32)
        Nim = A('s_Nim', [128, 2, 1024], F32)
        Tre = A('s_Tre', [128, 2, 8, 128], F32)
        Tim = A('s_Tim', [128, 2, 8, 128], F32)
        BbT = A('s_BbT', [128, 2, 2, 2, 512], BF16)
        CT = A('s_CT', [128, 2, 8, 2, 128], BF16)
        sd = A('s_d', [128, 2], F32)
        kc = A('s_kc', [128, 258], F32)
        nkc = A('s_nkc', [128, 2], F32)
        psm = A('s_psm', [128, 2, 3, 8], F32)
        x1sm = A('s_x1sm', [128, 2, 8], F32)
        thsm = A('s_thsm', [128, 2, 8], F32)
        dtsm = A('s_dtsm', [128, 2, 8], F32)
        kb.dma('sp', sd[:], W['s5_d'][:, :], w=['sd'])
        kb.dma('sp', kc[:], W['s5_kc'][:, :], w=['kc'])
        kb.dma('sp', psm[:], W['s5_par_sm'][:, :, :, :], w=['psm'])
        kb.dma('pool', CT[:], W['s5_cT'][:, :, :, :, :], w=['CT'])
        kb.op('dve', lambda e: e.tensor_scalar(out=CT[:, :, :, 1, :], in0=CT[:, :, :, 1, :], scalar1=-1.0, scalar2=None, op0=ALU.mult),
              r=['CT'], w=['CT'])
        kb.op('dve', lambda e: e.tensor_scalar(out=nkc[:], in0=kc[:, 0:2], scalar1=-1.0, scalar2=None, op0=ALU.mult), r=['kc'], w=['nkc'])
        for k in range(2):
            kb.dma('sp', yacc[:, k, :], c.pmixT[k * 128:(k + 1) * 128, :], w=[('yacc', k)])
            kb.op('act', lambda e, k=k: e.copy(out=uTb[:, k, :], in_=yacc[:, k, :]), r=[('yacc', k)], w=[('uTb', k)])
            kb.op('dve', lambda e, k=k: e.tensor_scalar(out=yacc[:, k, :], in0=yacc[:, k, :], scalar1=sd[:, k:k + 1], scalar2=None,
                                                        op0=ALU.mult), r=[('yacc', k), 'sd', ('uTb', k)], w=[('yacc', k)])
        with ExitStack() as es2:
            B_ = lambda name, shp, dt: es2.enter_context(nc.sbuf_tensor(_nm(name), shp, dt))
            ptm = B_('s_ptm', [128, 3, 1024], F32)
            x1 = B_('s_x1', [128, 1024], F32)
            th = B_('s_th', [128, 1024], F32)
            t_n = B_('s_tn', [128, 2048], F32)
            t_ni = B_('s_tni', [128, 2048], I32)
            t_c = B_('s_tc', [128, 2048], F32)
            t_s = B_('s_ts', [128, 2048], F32)
            t_a = B_('s_ta', [128, 2048], F32)
            kR = B_('s_kR', [128, 1024], F32)
            kI = B_('s_kI', [128, 1024], F32)
            BT = B_('s_BT', [128, 2, 2, 512], F32)
            tb1 = B_('s_tb1', [128, 1024], F32)
            tb2 = B_('s_tb2', [128, 1024], F32)
            Dv = lambda fn, r, w: kb.op('dve', fn, r=r, w=w)
            for di in range(2):
                kb.dma('sp', ptm[:], W['s5_par_tm'][:, di, :, :], w=['ptm'])
                kb.dma('sp', BT[:], W['s5_bT'][:, di, :, :, :], w=['BT'])
                lr_, li_, ldt_ = ptm[:, 0, :], ptm[:, 1, :], ptm[:, 2, :]
                Dv(lambda e: e.tensor_scalar(out=lr_, in0=lr_, scalar1=-1e-4, scalar2=None, op0=ALU.min), ['ptm'], ['ptm'])
                kb.op('act', lambda e: e.activation(out=ldt_, in_=ldt_, func=AF.Exp), r=['ptm'], w=['ptm'])
                Dv(lambda e: e.tensor_tensor(out=x1[:], in0=lr_, in1=ldt_, op=ALU.mult), ['ptm'], ['x1'])
                Dv(lambda e: e.tensor_tensor(out=th[:], in0=li_, in1=ldt_, op=ALU.mult), ['ptm'], ['th'])
                Dv(lambda e: e.tensor_copy(out=t_a[:, 0:1024], in_=th[:]), ['th'], ['tg'])
                _trig(kb, t_a[:, 0:1024], t_n[:, 0:1024], t_ni[:, 0:1024], t_c[:, 0:1024], t_s[:, 0:1024], 'tg')
                kb.op('act', lambda e: e.activation(out=tb1[:], in_=x1[:], func=AF.Exp), r=['x1'], w=['tb1'])
                aR, aI = t_c[:, 0:1024], t_s[:, 0:1024]
                Dv(lambda e: e.tensor_tensor(out=aR, in0=aR, in1=tb1[:], op=ALU.mult), ['tgo', 'tb1'], ['tgo'])
                Dv(lambda e: e.tensor_tensor(out=aI, in0=aI, in1=tb1[:], op=ALU.mult), ['tgo', 'tb1'], ['tgo'])
                Dv(lambda e: e.tensor_tensor(out=tb1[:], in0=lr_, in1=lr_, op=ALU.mult), ['ptm', 'tgo'], ['tb1'])
                Dv(lambda e: e.tensor_tensor(out=tb2[:], in0=li_, in1=li_, op=ALU.mult), ['ptm'], ['tb2'])
                Dv(lambda e: e.tensor_tensor(out=tb1[:], in0=tb1[:], in1=tb2[:], op=ALU.add), ['tb1', 'tb2'], ['tb1'])
                Dv(lambda e: e.reciprocal(out=tb1[:], in_=tb1[:]), ['tb1'], ['tb1'])
                Dv(lambda e: e.tensor_scalar(out=aR, in0=aR, scalar1=-1.0, scalar2=None, op0=ALU.add), ['tgo'], ['tgo'])
                Dv(lambda e: e.tensor_tensor(out=kR[:], in0=aR, in1=lr_, op=ALU.mult), ['tgo', 'ptm'], ['kR'])
                Dv(lambda e: e.tensor_tensor(out=tb2[:], in0=aI, in1=li_, op=ALU.mult), ['tgo', 'ptm'], ['tb2'])
                Dv(lambda e: e.tensor_tensor(out=kR[:], in0=kR[:], in1=tb2[:], op=ALU.add), ['kR', 'tb2'], ['kR'])
                Dv(lambda e: e.tensor_tensor(out=kR[:], in0=kR[:], in1=tb1[:], op=ALU.mult), ['kR', 'tb1'], ['kR'])
                Dv(lambda e: e.tensor_tensor(out=kI[:], in0=aI, in1=lr_, op=ALU.mult), ['tgo', 'ptm'], ['kI'])
                Dv(lambda e: e.tensor_tensor(out=tb2[:], in0=aR, in1=li_, op=ALU.mult), ['tgo', 'ptm', 'kR'], ['tb2'])
                Dv(lambda e: e.tensor_tensor(out=kI[:], in0=kI[:], in1=tb2[:], op=ALU.subtract), ['kI', 'tb2'], ['kI'])
                Dv(lambda e: e.tensor_tensor(out=kI[:], in0=kI[:], in1=tb1[:], op=ALU.mult), ['kI', 'tb1'], ['kI'])
                kRv = kR[:].rearrange("p (k s) -> p k s", k=2)
                kIv = kI[:].rearrange("p (k s) -> p k s", k=2)
                t1v = tb1[:].rearrange("p (k s) -> p k s", k=2)
                t2v = tb2[:].rearrange("p (k s) -> p k s", k=2)
                Dv(lambda e: e.tensor_tensor(out=t1v, in0=kRv, in1=BT[:, :, 0, :], op=ALU.mult), ['kR', 'BT', 'kI'], ['tb1'])
                Dv(lambda e: e.tensor_tensor(out=t2v, in0=kIv, in1=BT[:, :, 1, :], op=ALU.mult), ['kI', 'BT'], ['tb2'])
                Dv(lambda e: e.tensor_tensor(out=BbT[:, di, :, 0, :], in0=t1v, in1=t2v, op=ALU.subtract), ['tb1', 'tb2'], ['BbT'])
                Dv(lambda e: e.tensor_tensor(out=t1v, in0=kRv, in1=BT[:, :, 1, :], op=ALU.mult), ['kR', 'BT', 'BbT'], ['tb1'])
                Dv(lambda e: e.tensor_tensor(out=t2v, in0=kIv, in1=BT[:, :, 0, :], op=ALU.mult), ['kI', 'BT', 'BbT'], ['tb2'])
                Dv(lambda e: e.tensor_tensor(out=BbT[:, di, :, 1, :], in0=t1v, in1=t2v, op=ALU.add), ['tb1', 'tb2'], ['BbT'])
                Dv(lambda e: e.tensor_scalar(out=t_a[:, 0:1024], in0=th[:], scalar1=kc[:, di:di + 1], scalar2=None, op0=ALU.mult),
                   ['th', 'kc', 'tgo', 'tg'], ['tg'])
                _trig(kb, t_a[:, 0:1024], t_n[:, 0:1024], t_ni[:, 0:1024], t_c[:, 0:1024], t_s[:, 0:1024], 'tg')
                kb.op('act', lambda e: e.activation(out=tb1[:], in_=x1[:], func=AF.Exp, scale=nkc[:, di:di + 1]),
                      r=['x1', 'nkc', 'BbT'], w=['tb1'])
                Dv(lambda e: e.tensor_tensor(out=Nre[:, di, :], in0=tb1[:], in1=t_c[:, 0:1024], op=ALU.mult), ['tb1', 'tgo'], ['Nre'])
                Dv(lambda e: e.scalar_tensor_tensor(out=Nim[:, di, :], in0=tb1[:], scalar=-1.0, in1=t_s[:, 0:1024],
                                                    op0=ALU.mult, op1=ALU.mult), ['tb1', 'tgo'], ['Nim'])
            Dv(lambda e: e.tensor_scalar(out=psm[:, :, 0, :], in0=psm[:, :, 0, :], scalar1=-1e-4, scalar2=None, op0=ALU.min), ['psm'], ['psm'])
            kb.op('act', lambda e: e.activation(out=dtsm[:], in_=psm[:, :, 2, :], func=AF.Exp), r=['psm'], w=['dtsm'])
            Dv(lambda e: e.tensor_tensor(out=x1sm[:], in0=psm[:, :, 0, :], in1=dtsm[:], op=ALU.mult), ['psm', 'dtsm'], ['x1sm'])
            Dv(lambda e: e.tensor_tensor(out=thsm[:], in0=psm[:, :, 1, :], in1=dtsm[:], op=ALU.mult), ['psm', 'dtsm'], ['thsm'])
            magT = tb1[:].rearrange("p (a b) -> p a b", a=8)
            for di in range(2):
                krow = kc[:, 2 + di * 128:2 + (di + 1) * 128]
                for st in range(8):
                    Dv(lambda e, st=st: e.tensor_scalar(out=t_a[:, (di * 8 + st) * 128:(di * 8 + st + 1) * 128], in0=krow,
                                                        scalar1=thsm[:, di, st:st + 1], scalar2=None, op0=ALU.mult),
                       ['kc', 'thsm', 'tg', 'tgo'], ['tg'])
            _trig(kb, t_a[:], t_n[:], t_ni[:], t_c[:], t_s[:], 'tg')
            for di in range(2):
                krow = kc[:, 2 + di * 128:2 + (di + 1) * 128]
                for st in range(8):
                    kb.op('act', lambda e, st=st: e.activation(out=magT[:, st, :], in_=krow, func=AF.Exp, scale=x1sm[:, di, st:st + 1]),
                          r=['kc', 'x1sm', 'Nre', 'Nim'], w=['tb1'])
                Dv(lambda e: e.tensor_tensor(out=Tre[:, di], in0=magT, in1=t_c[:, di * 1024:(di + 1) * 1024].rearrange("p (a b) -> p a b", a=8),
                                             op=ALU.mult), ['tb1', 'tgo'], ['Tre'])
                Dv(lambda e: e.tensor_tensor(out=Tim[:, di], in0=magT, in1=t_s[:, di * 1024:(di + 1) * 1024].rearrange("p (a b) -> p a b", a=8),
                                             op=ALU.mult), ['tb1', 'tgo'], ['Tim'])
            kb.barrier()
        with ExitStack() as es3:
            B_ = lambda name, shp, dt: es3.enter_context(nc.sbuf_tensor(_nm(name), shp, dt))
            tt = B_('s_tt', [128, 2, 4, 512], F32)
            Wc = B_('s_W', [128, 2, 2, 2, 2, 512], BF16)
            mm = B_('s_mm', [128, 2, 4, 4, 128], F32)
            xs = B_('s_xs', [128, 2, 2, 8, 128], F32)
            xb = B_('s_xb', [128, 2, 2, 8, 128], BF16)

            def chunk_of(idx, di):
                return idx if di == 0 else NT - 1 - idx

            def step_A(idx, di, k):
                n = chunk_of(idx, di)
                nsl = slice(n * 128, (n + 1) * 128)
                base = di * 4
                par = idx % 2
                ksl = slice(k * 512, (k + 1) * 512)
                for ri in range(2):
                    kb.op('pe', lambda e, ri=ri: e.matmul(ps[base + ri][:], lhsT=uTb[:, k, nsl], rhs=BbT[:, di, k, ri, :],
                                                          start=True, stop=True),
                          r=[('uTb', k), 'BbT'], w=[('ps', base + ri)])
                pr, pi_ = ps[base], ps[base + 1]
                for slot, (pp, tab, bkk, tkey) in enumerate([(pr, Nre, base, 'Nre'), (pi_, Nim, base + 1, 'Nim'),
                                                            (pi_, Nre, base + 1, 'Nre'), (pr, Nim, base, 'Nim')]):
                    kb.op('dve', lambda e, slot=slot, pp=pp, tab=tab: e.tensor_tensor(
                        out=tt[:, di, slot, :], in0=pp[:], in1=tab[:, di, ksl], op=ALU.mult),
                        r=[('ps', bkk), tkey], w=[('tt', di, slot)])
                kb.op('pool', lambda e: e.tensor_tensor(out=Wc[:, par, di, k, 0, :], in0=tt[:, di, 0, :], in1=tt[:, di, 1, :], op=ALU.subtract),
                      r=[('tt', di, 0), ('tt', di, 1)], w=[('W', par, di, k, 0)])
                kb.op('pool', lambda e: e.tensor_tensor(out=Wc[:, par, di, k, 1, :], in0=tt[:, di, 2, :], in1=tt[:, di, 3, :], op=ALU.add),
                      r=[('tt', di, 2), ('tt', di, 3)], w=[('W', par, di, k, 1)])

            def step_B(idx, di, k):
                base = di * 4
                par = idx % 2
                tri = cstb[:, 8 + di, :]
                ccol = 127 if di == 0 else 0
                for ri in range(2):
                    for s4 in range(4):
                        kb.op('pe', lambda e, ri=ri, s4=s4: e.matmul(
                            ps[base + 2 + ri][:, s4 * 128:(s4 + 1) * 128], lhsT=Wc[:, par, di, k, ri, s4 * 128:(s4 + 1) * 128], rhs=tri,
                            start=True, stop=True), r=[('W', par, di, k, ri), 'cst'], w=[('ps', base + 2 + ri)])
                for s4 in range(4):
                    st = k * 4 + s4
                    pre = ps[base + 2][:, s4 * 128:(s4 + 1) * 128]
                    pim = ps[base + 3][:, s4 * 128:(s4 + 1) * 128]
                    if idx == 0:
                        cr, ci = 0.0, 0.0
                    else:
                        cr = xs[:, di, 0, st, ccol:ccol + 1]
                        ci = xs[:, di, 1, st, ccol:ccol + 1]
                    rk = [('ps', base + 2), ('ps', base + 3), 'Tre', 'Tim', ('xs', di, k)]
                    for slot, (pp, cc_, tab) in enumerate([(pre, cr, Tre), (pim, ci, Tim), (pim, ci, Tre), (pre, cr, Tim)]):
                        kb.op('dve', lambda e, slot=slot, pp=pp, cc_=cc_, tab=tab, s4=s4, st=st: e.scalar_tensor_tensor(
                            out=mm[:, di, slot, s4, :], in0=pp, scalar=cc_, in1=tab[:, di, st, :], op0=ALU.add, op1=ALU.mult),
                            r=rk, w=[('mm', di, slot, s4)])
                kb.op('pool', lambda e: e.tensor_tensor(out=xs[:, di, 0, k * 4:(k + 1) * 4, :], in0=mm[:, di, 0], in1=mm[:, di, 1], op=ALU.subtract),
                      r=[('mm', di, 0, s4) for s4 in range(4)] + [('mm', di, 1, s4) for s4 in range(4)], w=[('xs', di, k)])
                kb.op('pool', lambda e: e.tensor_tensor(out=xs[:, di, 1, k * 4:(k + 1) * 4, :], in0=mm[:, di, 2], in1=mm[:, di, 3], op=ALU.add),
                      r=[('mm', di, 2, s4) for s4 in range(4)] + [('mm', di, 3, s4) for s4 in range(4)], w=[('xs', di, k)])
                kb.op('act', lambda e: e.copy(out=xb[:, di, :, k * 4:(k + 1) * 4, :], in_=xs[:, di, :, k * 4:(k + 1) * 4, :]),
                      r=[('xs', di, k)], w=[('xb', di, k)])

            def step_C(idx, di):
                n = chunk_of(idx, di)
                nsl = slice(n * 128, (n + 1) * 128)
                base = di * 4
                for k in range(2):
                    cnt = 0
                    for st in range(k * 4, k * 4 + 4):
                        for ri in range(2):
                            kb.op('pe', lambda e, k=k, st=st, ri=ri, cnt=cnt: e.matmul(
                                ps[base][:, k * 128:(k + 1) * 128], lhsT=CT[:, di, st, ri, :], rhs=xb[:, di, ri, st, :],
                                start=(cnt == 0), stop=(cnt == 7)), r=['CT', ('xb', di, k)], w=[('ps', base)])
                            cnt += 1
                kb.op('dve', lambda e: e.tensor_tensor(out=yacc[:, :, nsl], in0=yacc[:, :, nsl],
                                                       in1=ps[base][:, 0:256].rearrange("p (a b) -> p a b", a=2), op=ALU.add),
                      r=[('ps', base), ('yacc', n)], w=[('yacc', n)])

            for k in range(2):
                for di in range(2):
                    step_A(0, di, k)
            for idx in range(NT):
                if idx + 1 < NT:
                    for k in range(2):
                        for di in range(2):
                            step_A(idx + 1, di, k)
                for k in range(2):
                    for di in range(2):
                        step_B(idx, di, k)
                for di in range(2):
                    step_C(idx, di)
            kb.barrier()
        with ExitStack() as es3:
            B_ = lambda name, shp, dt: es3.enter_context(nc.sbuf_tensor(_nm(name), shp, dt))
            z = B_('s_z', [128, 2, T], F32)
            yst = B_('s_yst', [128, 2, 512], BF16)
            yf = yacc[:].rearrange("p k t -> p (k t)")
            zf = z[:].rearrange("p k t -> p (k t)")
            GC = 0.7978845608028654
            kb.op('act', lambda e: e.activation(out=zf, in_=yf, func=AF.Square), r=[('yacc', 0), ('yacc', 1)] + [('yacc', n) for n in range(NT)], w=['z'])
            kb.op('dve', lambda e: e.tensor_scalar(out=zf, in0=zf, scalar1=0.044715, scalar2=1.0, op0=ALU.mult, op1=ALU.add), r=['z'], w=['z'])
            kb.op('dve', lambda e: e.tensor_tensor(out=zf, in0=zf, in1=yf, op=ALU.mult), r=['z', ('yacc', 0), ('yacc', 1)], w=['z'])
            kb.op('act', lambda e: e.activation(out=zf, in_=zf, func=AF.Sigmoid, scale=2.0 * GC), r=['z'], w=['z'])
            kb.op('dve', lambda e: e.tensor_tensor(out=zf, in0=zf, in1=yf, op=ALU.mult), r=['z', ('yacc', 0), ('yacc', 1)], w=['z'])
            kb.op('act', lambda e: e.copy(out=uTb[:].rearrange("p k t -> p (k t)"), in_=zf), r=['z'], w=[('uTb', 0), ('uTb', 1)])
            wgl = B_('s_wgl', [128, 2, 256], BF16)
            kb.dma('pool', wgl[:], W['s5_wglu'][:, :, :], w=['wgl'])
            nps = 0
            for ko in range(2):
                for tb in range(NTB):
                    ts = slice(tb * 512, (tb + 1) * 512)
                    bk = 1 + nps % 4
                    nps += 1
                    for ki in range(2):
                        kb.op('pe', lambda e, ki=ki, bk=bk: e.matmul(ps[bk][:], lhsT=wgl[:, ki, ko * 128:(ko + 1) * 128], rhs=uTb[:, ki, ts],
                                                              start=(ki == 0), stop=(ki == 1)),
                              r=['wgl', ('uTb', 0), ('uTb', 1)], w=[('ps', bk)])
                    kb.op('act', lambda e, bk=bk: e.activation(out=yacc[:, ko, ts], in_=ps[bk][:], func=AF.Sigmoid),
                          r=[('ps', bk)], w=[('yg', ko, tb)])
                    yb_ = nps % 2
                    kb.op('dve', lambda e, yb_=yb_: e.tensor_tensor(out=yst[:, yb_, :], in0=yacc[:, ko, ts], in1=z[:, ko, ts],
                                                           op=ALU.mult), r=[('yg', ko, tb), 'z'], w=[('yst', yb_)])
                    kb.dma('sp', c.yT[ko * 128:(ko + 1) * 128, ts], yst[:, yb_, :], r=[('yst', yb_)], w=[('yT', ko, tb)])
        if 'ys5' in c.dbg:
            kb.barrier()
            with nc.sbuf_tensor(_nm('dbgt3'), [128, T], F32) as dbgt:
                for ct in range(2):
                    kb.dma('pool', dbgt[:], c.yT[ct * 128:(ct + 1) * 128, :], w=['dbgt'])
                    kb.dma('sp', c.dbg['ys5'][ct * 128:(ct + 1) * 128, :], dbgt[:], r=['dbgt'], w=[('dbgo', ct)])
                kb.barrier()
        kb.barrier()


def prep_rest(inp, l, o):
    f = lambda k: np.asarray(inp[k], np.float32)
    o['w_up'] = np.ascontiguousarray(np.concatenate([f('w_up_s5')[l], f('w_up_lru')[l], f('w_up_gla')[l]], axis=0))
    o['w_mix'] = np.ascontiguousarray(f('w_mix_out')[l])
    o['w_router'] = np.ascontiguousarray(f('w_router')[l].reshape(8, 128, 16).transpose(1, 0, 2))
    o['w_eg'] = np.ascontiguousarray(f('w_exp_gate')[l])
    o['w_eu'] = np.ascontiguousarray(f('w_exp_up')[l])
    o['w_ed'] = np.ascontiguousarray(f('w_exp_down')[l])


def make_iota():
    io = np.zeros((128, 513), np.float32)
    io[:, 0] = np.arange(128)
    io[:, 1:] = np.arange(512)[None, :]
    return io


def emit_rmsnorm(c, hb, hkey, gcol, gkey, out_of_k, outkeys, bank, sq, sqkey, rstd, rkey):
    kb = c.kb
    kb.op('act', lambda e: e.activation(out=sq, in_=hb, func=AF.Square), r=[hkey], w=[sqkey])
    pb = c.ps[bank]
    for k in range(KC):
        kb.op('pe', lambda e, k=k: e.matmul(pb[:], lhsT=c.ones_bf[:], rhs=sq[:, k, :], start=(k == 0), stop=(k == KC - 1)),
              r=[sqkey, 'ones_bf'], w=[('ps', bank)])
    kb.op('act', lambda e: e.activation(out=rstd, in_=pb[:], func=AF.Sqrt, scale=1.0 / D, bias=EPS), r=[('ps', bank)], w=[rkey])
    kb.op('dve', lambda e: e.reciprocal(out=rstd, in_=rstd), r=[rkey], w=[rkey])
    for k in range(KC):
        kb.op('dve', lambda e, k=k: e.scalar_tensor_tensor(out=out_of_k(k), in0=hb[:, k, :], scalar=gcol[:, k:k + 1], in1=rstd,
                                                           op0=ALU.mult, op1=ALU.mult),
              r=[hkey, rkey, gkey], w=outkeys)


def emit_add_tm(c, hb, hkey, tb, ftile, banks):
    kb = c.kb
    ident = c.cst[:, 0, :]
    for t4 in range(4):
        tt = tb * 4 + t4
        fb = t4 % 2
        kb.dma('sp', ftile[:, fb, :], c.ffn_tm[tt * 128:(tt + 1) * 128, :], w=[('ftile', fb)])
        for half in range(2):
            bk = banks[half]
            for k4 in range(4):
                k = half * 4 + k4
                kb.op('pe', lambda e, k=k, k4=k4, bk=bk: e.transpose(out=c.ps[bk][:, k4 * 128:(k4 + 1) * 128],
                                                                    in_=ftile[:, fb, k * 128:(k + 1) * 128], identity=ident),
                      r=[('ftile', fb), 'cst'], w=[('ps', bk)])
            hv = hb[:, half * 4:(half + 1) * 4, t4 * 128:(t4 + 1) * 128]
            kb.op('dve', lambda e, hv=hv, bk=bk: e.tensor_tensor(out=hv, in0=hv, in1=c.ps[bk][:].rearrange("p (a b) -> p a b", a=4), op=ALU.add),
                  r=[('ps', bk), hkey], w=[hkey])


def stage_merge(c, l, hnT):
    nc, kb, T, NTB, NT = c.nc, c.kb, c.T, c.NTB, c.NT
    W = c.lw[l]
    ps = c.ps
    res_src = c.xT if l == 0 else c.hT
    with ExitStack() as es:
        A = lambda name, shp, dt: es.enter_context(nc.sbuf_tensor(_nm(name), shp, dt))
        wup = A('m_wup', [128, 8, 1024], BF16)
        wmix = A('m_wmix', [128, 8, 1024], BF16)
        yblk = A('m_yblk', [128, 2, 8, 512], BF16)
        goc = A('m_goc', [128, 2, 3, 512], BF16)
        mrg = A('m_mrg', [128, 8, 512], BF16)
        hblk = A('m_hblk', [128, 2, 8, 512], F32)
        t1 = A('m_t1', [128, 2, 3, 512], F32)
        sq = A('m_sq', [128, 8, 512], BF16)
        rstd = A('m_rstd', [128, 512], F32)
        gff = A('m_gff', [128, 8], F32)
        kb.dma('pool', wup[:], W['w_up'].rearrange("(k p) d -> p k d", p=128), w=['wup'])
        kb.dma('pool', wmix[:], W['w_mix'].rearrange("(k p) d -> p k d", p=128), w=['wmix'])
        kb.dma('sp', gff[:], W['ffn_g'][:, :], w=['gff'])
        yTv = c.yT.rearrange("(k p) t -> p k t", p=128)
        gTv = c.gT.rearrange("(b o p) t -> p b o t", p=128, b=3)
        resv = res_src.rearrange("(k p) t -> p k t", p=128)
        hTv = c.hT.rearrange("(k p) t -> p k t", p=128)
        branch_k = [(0, 2), (2, 6), (6, 8)]
        ng = 0
        for tb in range(NTB):
            b = tb % 2
            ts = slice(tb * 512, (tb + 1) * 512)
            kb.dma('sp', yblk[:, b], yTv[:, :, ts], w=[('yblk', b)])
            kb.dma('sp', hblk[:, b], resv[:, :, ts], w=[('hblk', b)])
            for oc in range(8):
                gb = ng % 2
                ng += 1
                kb.dma('sp', goc[:, gb], gTv[:, :, oc, ts], w=[('goc', gb)])
                for br in range(3):
                    bk = gb * 3 + br
                    k0, k1 = branch_k[br]
                    for k in range(k0, k1):
                        kb.op('pe', lambda e, k=k, bk=bk, k0=k0, k1=k1: e.matmul(
                            ps[bk][:], lhsT=wup[:, k, oc * 128:(oc + 1) * 128], rhs=yblk[:, b, k, :],
                            start=(k == k0), stop=(k == k1 - 1)), r=['wup', ('yblk', b)], w=[('ps', bk)])
                tb_ = oc % 2
                kb.op('dve', lambda e: e.tensor_tensor(out=t1[:, tb_, 0, :], in0=ps[gb * 3][:], in1=goc[:, gb, 0, :], op=ALU.mult),
                      r=[('ps', gb * 3), ('goc', gb)], w=[('t1', tb_, 0)])
                kb.op('dve', lambda e: e.tensor_tensor(out=t1[:, tb_, 1, :], in0=ps[gb * 3 + 1][:], in1=goc[:, gb, 1, :], op=ALU.mult),
                      r=[('ps', gb * 3 + 1), ('goc', gb)], w=[('t1', tb_, 1)])
                kb.op('dve', lambda e: e.tensor_tensor(out=t1[:, tb_, 2, :], in0=ps[gb * 3 + 2][:], in1=goc[:, gb, 2, :], op=ALU.mult),
                      r=[('ps', gb * 3 + 2), ('goc', gb)], w=[('t1', tb_, 2)])
                kb.op('pool', lambda e: e.tensor_tensor(out=t1[:, tb_, 0, :], in0=t1[:, tb_, 0, :], in1=t1[:, tb_, 1, :], op=ALU.add),
                      r=[('t1', tb_, 0), ('t1', tb_, 1)], w=[('t1', tb_, 0)])
                kb.op('pool', lambda e: e.tensor_tensor(out=mrg[:, oc, :], in0=t1[:, tb_, 0, :], in1=t1[:, tb_, 2, :], op=ALU.add),
                      r=[('t1', tb_, 0), ('t1', tb_, 2)], w=[('mrg', oc)])
            for oc in range(8):
                bk = 6 + oc % 2
                for k in range(8):
                    kb.op('pe', lambda e, k=k, bk=bk: e.matmul(ps[bk][:], lhsT=wmix[:, k, oc * 128:(oc + 1) * 128], rhs=mrg[:, k, :],
                                                          start=(k == 0), stop=(k == 7)),
                          r=['wmix'] + [('mrg', kk) for kk in range(8)], w=[('ps', bk)])
                kb.op('dve', lambda e, bk=bk: e.tensor_tensor(out=hblk[:, b, oc, :], in0=hblk[:, b, oc, :], in1=ps[bk][:], op=ALU.add),
                      r=[('ps', bk), ('hblk', b)], w=[('hblk', b)])
            kb.dma('sp', hTv[:, :, ts], hblk[:, b], r=[('hblk', b)], w=[('hT', tb)])
            emit_rmsnorm(c, hblk[:, b], ('hblk', b), gff, 'gff', lambda k: hnT[:, k, ts], [('hnT', tb)], 6, sq[:], 'sq', rstd[:], 'rstd')
        if 'hmid' in c.dbg:
            kb.barrier()
            with nc.sbuf_tensor(_nm('dbgt4'), [128, T], F32) as dbgt:
                for ct in range(8):
                    kb.dma('sp', dbgt[:], c.hT[ct * 128:(ct + 1) * 128, :], w=['dbgt'])
                    kb.dma('sp', c.dbg['hmid'][ct * 128:(ct + 1) * 128, :], dbgt[:], r=['dbgt'], w=[('dbgo', ct)])
                kb.barrier()
        kb.barrier()


def stage_ffn(c, l, hnT, seli, selg, phase):
    nc, kb, T, NTB, NT = c.nc, c.kb, c.T, c.NTB, c.NT
    W = c.lw[l]
    ps = c.ps
    cst, cstb = c.cst, c.cstb
    CAP = 2 * T // NE
    CT_ = CAP // 128
    NJ = NT * NE
    with ExitStack() as es:
        A = lambda name, shp, dt: es.enter_context(nc.sbuf_tensor(_nm(name), shp, dt))
        for _once in ([0] if phase == 0 else []):
          with ExitStack() as es2:
            B_ = lambda name, shp, dt: es2.enter_context(nc.sbuf_tensor(_nm(name), shp, dt))
            wr = B_('f_wr', [128, 8, 16], BF16)
            iota = B_('f_iota', [128, 513], F32)
            probs = B_('f_probs', [128, NT, NE], F32)
            mask = B_('f_mask', [128, NT, NE], F32)
            maskb = B_('f_maskb', [128, NT, NE], BF16)
            gw = B_('f_gw', [128, NT, NE], F32)
            pos = B_('f_pos', [128, NT, NE], F32)
            base = B_('f_base', [128, NT, NE], F32)
            csum = B_('f_csum', [128, NT, NE], F32)
            cmpb = B_('f_cmpb', [128, NT, NE], BF16)
            red = B_('f_red', [128, NT], F32)
            lo = B_('f_lo', [128, NE], F32)
            mid = B_('f_mid', [128, NE], F32)
            cnt = B_('f_cnt', [128, NE], F32)
            R = B_('f_R', [128, NT, NE, 4], BF16)
            ghi_f = B_('f_ghif', [128, NT, NE], F32)
            OH = B_('f_OH', [128, 2, CAP], BF16)
            selsb = B_('f_selsb', [128, NE, CT_, 4], F32)
            stg = B_('f_stg', [128, 2, 1024], BF16)
            zt = B_('f_zt', [128, 1024], F32)
            kb.dma('pool', wr[:], W['w_router'][:, :, :], w=['wr'])
            kb.dma('sp', iota[:], c.iota[:, :], w=['iota'])
            kb.op('dve', lambda e: e.memset(zt[:], 0.0), w=['zt'])
            for tt in range(NT):
                kb.dma('sp', c.ffn_tm[tt * 128:(tt + 1) * 128, :], zt[:], r=['zt'], w=[('ffn_tm', tt)])
            for tt in range(NT):
                sb = tt % 2
                psE = ps[sb][:].bitcast(BF16)
                for k in range(8):
                    kb.op('pe', lambda e, k=k, psE=psE: e.transpose(out=psE[:, k * 128:(k + 1) * 128], in_=hnT[:, k, tt * 128:(tt + 1) * 128],
                                                                   identity=cstb[:, 0, :]),
                          r=[('hnT', tt // 4), 'cst'], w=[('ps', sb)])
                kb.op('act', lambda e, psE=psE: e.copy(out=stg[:, sb, :], in_=psE), r=[('ps', sb)], w=[('stg', sb)])
                kb.dma('sp', c.hn_tm[tt * 128:(tt + 1) * 128, :], stg[:, sb, :], r=[('stg', sb)], w=[('hn_tm', tt)])
            for tt in range(NT):
                for k in range(8):
                    kb.op('pe', lambda e, k=k: e.matmul(ps[2][:, tt * NE:(tt + 1) * NE], lhsT=hnT[:, k, tt * 128:(tt + 1) * 128], rhs=wr[:, k, :],
                                                        start=(k == 0), stop=(k == 7)), r=[('hnT', tt // 4), 'wr'], w=[('ps', 2)])
            lg = ps[2][:, 0:NJ].rearrange("p (j e) -> p j e", e=NE)
            Dv = lambda fn, r, w: kb.op('dve', fn, r=r, w=w)
            Dv(lambda e: e.tensor_reduce(out=red[:], in_=lg, axis=AX.X, op=ALU.max), [('ps', 2)], ['red'])
            Dv(lambda e: e.tensor_tensor(out=probs[:], in0=lg, in1=_bc_last(red[:], NE), op=ALU.subtract), [('ps', 2), 'red'], ['probs'])
            kb.op('act', lambda e: e.activation(out=probs[:], in_=probs[:], func=AF.Exp), r=['probs'], w=['probs'])
            Dv(lambda e: e.tensor_reduce(out=red[:], in_=probs[:], axis=AX.X, op=ALU.add), ['probs'], ['red'])
            Dv(lambda e: e.reciprocal(out=red[:], in_=red[:]), ['red'], ['red'])
            Dv(lambda e: e.tensor_tensor(out=probs[:], in0=probs[:], in1=_bc_last(red[:], NE), op=ALU.mult), ['probs', 'red'], ['probs'])
            Dv(lambda e: e.memset(lo[:], 0.0), [], ['lo'])
            pflat = probs[:].rearrange("p j e -> p (j e)")
            for it in range(1, 33):
                wstep = 2.0 ** (-it)
                Dv(lambda e: e.tensor_scalar(out=mid[:], in0=lo[:], scalar1=wstep, scalar2=None, op0=ALU.add), ['lo'], ['mid'])
                Dv(lambda e: e.tensor_tensor(out=cmpb[:], in0=probs[:], in1=_bc_mid(mid[:], NT), op=ALU.is_ge), ['probs', 'mid'], ['cmpb'])
                kb.op('pe', lambda e: e.matmul(ps[3][:, 0:NJ], lhsT=c.ones_bf[:], rhs=cmpb[:].rearrange("p j e -> p (j e)"), start=True, stop=True),
                      r=['cmpb', 'ones_bf'], w=[('ps', 3)])
                Dv(lambda e: e.tensor_reduce(out=cnt[:], in_=ps[3][:, 0:NJ].rearrange("p (j e) -> p e j", e=NE), axis=AX.X, op=ALU.add),
                   [('ps', 3)], ['cnt'])
                Dv(lambda e: e.tensor_scalar(out=cnt[:], in0=cnt[:], scalar1=float(CAP) - 0.5, scalar2=wstep, op0=ALU.is_ge, op1=ALU.mult),
                   ['cnt'], ['cnt'])
                Dv(lambda e: e.tensor_tensor(out=lo[:], in0=lo[:], in1=cnt[:], op=ALU.add), ['lo', 'cnt'], ['lo'])
            Dv(lambda e: e.tensor_tensor(out=mask[:], in0=probs[:], in1=_bc_mid(lo[:], NT), op=ALU.is_ge), ['probs', 'lo'], ['mask'])
            Dv(lambda e: e.tensor_copy(out=maskb[:], in_=mask[:]), ['mask'], ['maskb'])
            Dv(lambda e: e.tensor_tensor(out=gw[:], in0=probs[:], in1=mask[:], op=ALU.mult), ['probs', 'mask'], ['gw'])
            mbf = maskb[:].rearrange("p j e -> p (j e)")
            kb.op('pe', lambda e: e.matmul(ps[3][:, 0:NJ], lhsT=c.ones_bf[:], rhs=mbf, start=True, stop=True), r=['maskb', 'ones_bf'], w=[('ps', 3)])
            Dv(lambda e: e.tensor_copy(out=csum[:].rearrange("p j e -> p (j e)"), in_=ps[3][:, 0:NJ]), [('ps', 3)], ['csum'])
            Dv(lambda e: e.memset(base[:, 0, :], 0.0), [], ['base'])
            for j in range(1, NT):
                Dv(lambda e, j=j: e.tensor_tensor(out=base[:, j, :], in0=base[:, j - 1, :], in1=csum[:, j - 1, :], op=ALU.add), ['base', 'csum'], ['base'])
            for j in range(NT):
                kb.op('pe', lambda e, j=j: e.matmul(ps[3][:, j * NE:(j + 1) * NE], lhsT=cstb[:, 6, :], rhs=maskb[:, j, :], start=True, stop=True),
                      r=['maskb', 'cst'], w=[('ps', 3)])
            Dv(lambda e: e.tensor_tensor(out=pos[:].rearrange("p j e -> p (j e)"), in0=base[:].rearrange("p j e -> p (j e)"), in1=ps[3][:, 0:NJ],
                                         op=ALU.add), [('ps', 3), 'base'], ['pos'])
            pidx = bass.AP(iota[:].tensor, iota[:, 0:1].offset, [[iota[:].ap[0][0], 128], [0, NT], [0, NE]])
            Dv(lambda e: e.tensor_copy(out=R[:, :, :, 0], in_=pidx), ['iota'], ['R'])
            for j in range(NT):
                Dv(lambda e, j=j: e.memset(R[:, j, :, 1], float(j)), [], ['R'])
            Dv(lambda e: e.tensor_copy(out=R[:, :, :, 2], in_=gw[:]), ['gw'], ['R'])
            Dv(lambda e: e.tensor_copy(out=ghi_f[:], in_=R[:, :, :, 2]), ['R'], ['ghi_f'])
            Dv(lambda e: e.tensor_tensor(out=R[:, :, :, 3], in0=gw[:], in1=ghi_f[:], op=ALU.subtract), ['gw', 'ghi_f'], ['R'])
            noh = 0
            for e_ in range(NE):
                for j in range(NT):
                    ob = noh % 2
                    noh += 1
                    Dv(lambda e, j=j, ob=ob: e.tensor_scalar(out=OH[:, ob, :], in0=iota[:, 1:1 + CAP], scalar1=pos[:, j, e_:e_ + 1],
                                                             scalar2=mask[:, j, e_:e_ + 1], op0=ALU.is_equal, op1=ALU.mult),
                       ['iota', 'pos', 'mask'], [('OH', ob)])
                    for ct in range(CT_):
                        bk = 4 + ct
                        kb.op('pe', lambda e, j=j, ob=ob, ct=ct, bk=bk: e.matmul(ps[bk][:, 0:4], lhsT=OH[:, ob, ct * 128:(ct + 1) * 128],
                                                                              rhs=R[:, j, e_, :], start=(j == 0), stop=(j == NT - 1)),
                              r=[('OH', ob), 'R'], w=[('ps', bk)])
                for ct in range(CT_):
                    kb.op('act', lambda e, ct=ct: e.copy(out=selsb[:, e_, ct, :], in_=ps[4 + ct][:, 0:4]), r=[('ps', 4 + ct)], w=['selsb'])
            Dv(lambda e: e.scalar_tensor_tensor(out=selg[:], in0=selsb[:, :, :, 1], scalar=128.0, in1=selsb[:, :, :, 0], op0=ALU.mult, op1=ALU.add),
               ['selsb'], ['selg'])
            Dv(lambda e: e.tensor_copy(out=seli[:], in_=selg[:]), ['selg'], ['seli'])
            Dv(lambda e: e.tensor_tensor(out=selg[:], in0=selsb[:, :, :, 2], in1=selsb[:, :, :, 3], op=ALU.add), ['selsb', 'seli'], ['selg'])
            kb.barrier()
        for _once in ([0] if phase == 1 else []):
          with ExitStack() as es3:
            B_ = lambda name, shp, dt: es3.enter_context(nc.sbuf_tensor(_nm(name), shp, dt))
            xe = B_('f_xe', [128, 2, CT_, 1024], BF16)
            xeT = B_('f_xeT', [128, 8, CAP], BF16)
            wg = B_('f_wg', [128, 3, 8, 512], BF16)
            wu = B_('f_wu', [128, 3, 8, 512], BF16)
            wd = B_('f_wd', [128, 2, 16, 1024], BF16)
            hidT = B_('f_hidT', [128, 16, CAP], BF16)
            sg = B_('f_sg', [128, 2, CAP], F32)
            ye = B_('f_ye', [128, CT_, 1024], F32)
            deferred = []
            nwb = 0
            nab = 0
            nye = 0
            def emit_gather(ee):
                for ct in range(CT_):
                    kb.idma(lambda g, ct=ct: g.indirect_dma_start(
                        out=xe[:, ee % 2, ct, :], out_offset=None, in_=c.hn_tm[:, :],
                        in_offset=bass.IndirectOffsetOnAxis(ap=seli[:, ee, ct:ct + 1], axis=0)),
                        r=['seli'] + [('hn_tm', tt) for tt in range(NT)], w=[('xe', ee % 2, ct)])
            def emit_xeT(ee):
                for ct in range(CT_):
                    bk = 6 + ct % 2
                    psE = ps[bk][:].bitcast(BF16)
                    for k in range(8):
                        kb.op('pe', lambda e, k=k, ct=ct, psE=psE: e.transpose(out=psE[:, k * 128:(k + 1) * 128],
                                                                               in_=xe[:, ee % 2, ct, k * 128:(k + 1) * 128], identity=cstb[:, 0, :]),
                              r=[('xe', ee % 2, ct), 'cst'], w=[('ps', bk)])
                    kb.op('act', lambda e, ct=ct, psE=psE: e.copy(out=xeT[:, :, ct * 128:(ct + 1) * 128], in_=psE.rearrange("p (a b) -> p a b", a=8)),
                          r=[('ps', bk)], w=['xeT'])
            emit_gather(0)
            emit_xeT(0)
            for e_ in range(NE):
                if e_ + 1 < NE:
                    emit_gather(e_ + 1)
                for fg in range(4):
                    wb_ = nwb % 3
                    nwb += 1
                    fs = slice(fg * 512, (fg + 1) * 512)
                    kb.dma('pool', wg[:, wb_], W['w_eg'][e_].rearrange("(k p) f -> p k f", p=128)[:, :, fs], w=[('wg', wb_)])
                    kb.dma('pool', wu[:, wb_], W['w_eu'][e_].rearrange("(k p) f -> p k f", p=128)[:, :, fs], w=[('wu', wb_)])
                    kb.dma('pool', wd[:, e_ % 2, fg * 4:(fg + 1) * 4, :], W['w_ed'][e_][fs, :].rearrange("(c p) d -> p c d", p=128), w=[('wd', e_ % 2, fg)])
                    if fg == 1:
                        for fn_ in deferred:
                            fn_()
                        deferred = []
                    for fc in range(4):
                        fch = fg * 4 + fc
                        ab = nab % 2
                        nab += 1
                        pa, pb_ = ps[ab * 2], ps[ab * 2 + 1]
                        for (wt, pp, wkey, bk) in [(wg, pa, 'wg', ab * 2), (wu, pb_, 'wu', ab * 2 + 1)]:
                            for k in range(8):
                                kb.op('pe', lambda e, k=k, wt=wt, pp=pp: e.matmul(pp[:, 0:CAP], lhsT=wt[:, wb_, k, fc * 128:(fc + 1) * 128],
                                                                              rhs=xeT[:, k, :], start=(k == 0), stop=(k == 7)),
                                      r=[(wkey, wb_), 'xeT'], w=[('ps', bk)])
                        kb.op('act', lambda e, pa=pa: e.activation(out=sg[:, ab, :], in_=pa[:, 0:CAP], func=AF.Sigmoid), r=[('ps', ab * 2)], w=[('sg', ab)])
                        kb.op('dve', lambda e, pa=pa: e.tensor_tensor(out=sg[:, ab, :], in0=pa[:, 0:CAP], in1=sg[:, ab, :], op=ALU.mult),
                              r=[('ps', ab * 2), ('sg', ab)], w=[('sg', ab)])
                        kb.op('dve', lambda e, pb_=pb_: e.tensor_tensor(out=hidT[:, fch, :], in0=pb_[:, 0:CAP], in1=sg[:, ab, :], op=ALU.mult),
                              r=[('ps', ab * 2 + 1), ('sg', ab)], w=[('hidT', fch)])
                if e_ + 1 < NE:
                    emit_xeT(e_ + 1)
                for ct in range(CT_):
                    yb = ct
                    for half in range(2):
                        bk = 4 + half
                        for fch in range(16):
                            kb.op('pe', lambda e, fch=fch, half=half, bk=bk, ct=ct: e.matmul(
                                ps[bk][:], lhsT=hidT[:, fch, ct * 128:(ct + 1) * 128], rhs=wd[:, e_ % 2, fch, half * 512:(half + 1) * 512],
                                start=(fch == 0), stop=(fch == 15)), r=[('hidT', fch), ('wd', e_ % 2, fch // 4)], w=[('ps', bk)])
                        kb.op('dve', lambda e, half=half, bk=bk, ct=ct: e.tensor_scalar(
                            out=ye[:, yb, half * 512:(half + 1) * 512], in0=ps[bk][:], scalar1=selg[:, e_, ct:ct + 1], scalar2=None, op0=ALU.mult),
                            r=[('ps', bk), 'selg'], w=[('ye', yb, half)])

                    def scat(ct=ct, yb=yb, e_=e_):
                        kb.idma(lambda g: g.indirect_dma_start(
                            out=c.ffn_tm[:, :], out_offset=bass.IndirectOffsetOnAxis(ap=seli[:, e_, ct:ct + 1], axis=0),
                            in_=ye[:, yb, :], in_offset=None, compute_op=ALU.add),
                            r=['seli', ('ye', yb, 0), ('ye', yb, 1)] + [('scat', e_ - 1, c2) for c2 in range(CT_)], w=[('scat', e_, ct)])
                    deferred.append(scat)
            for fn_ in deferred:
                fn_()
            kb.barrier()
        kb.barrier()


def stage_final(c):
    nc, kb, T, NTB, NT = c.nc, c.kb, c.T, c.NTB, c.NT
    with ExitStack() as es:
        A = lambda name, shp, dt: es.enter_context(nc.sbuf_tensor(_nm(name), shp, dt))
        hblk = A('z_hblk', [128, 2, 8, 512], F32)
        oblk = A('z_oblk', [128, 2, 8, 512], F32)
        ftile = A('z_ftile', [128, 2, 1024], F32)
        sq = A('z_sq', [128, 8, 512], BF16)
        rstd = A('z_rstd', [128, 512], F32)
        gfin = A('z_gfin', [128, 8], F32)
        kb.dma('sp', gfin[:], c.fin_g[:, :], w=['gfin'])
        hTv = c.hT.rearrange("(k p) t -> p k t", p=128)
        oTv = c.outT.rearrange("(k p) t -> p k t", p=128)
        for tb in range(NTB):
            b = tb % 2
            ts = slice(tb * 512, (tb + 1) * 512)
            kb.dma('sp', hblk[:, b], hTv[:, :, ts], w=[('hblk', b)])
            if 'skip_ffn' not in c.dbg:
                emit_add_tm(c, hblk[:, b], ('hblk', b), tb, ftile, (0, 1))
            emit_rmsnorm(c, hblk[:, b], ('hblk', b), gfin, 'gfin', lambda k: oblk[:, b, k, :], [('oblk', b)], 2, sq[:], 'sq', rstd[:], 'rstd')
            kb.dma('sp', oTv[:, :, ts], oblk[:, b], r=[('oblk', b)], w=[('outT', tb)])
        kb.barrier()


_CACHE = {}


def kernel(**inputs):
    x = np.asarray(inputs['x'], np.float32)
    B, T, _ = x.shape
    L = np.asarray(inputs['w_in']).shape[0]
    preps = [prep_layer(inputs, l) for l in range(L)]
    shapes = layer_shapes_from(preps[0])
    key = (T, L)
    if key not in _CACHE:
        _CACHE[key] = build(T, L, shapes)
    nc, c = _CACHE[key]
    common = {'consts': make_consts(), 'iota': make_iota(), 'fin_g': _pcol(np.asarray(inputs['final_norm_g'], np.float32), 8)}
    for l in range(L):
        for k, v in preps[l].items():
            common['l%d_%s' % (l, k)] = v
    in_maps = []
    for b in range(B):
        m = dict(common)
        m['xT'] = np.ascontiguousarray(x[b].T)
        in_maps.append(m)
    res = run_bass_kernel_spmd(nc, in_maps, core_ids=list(range(B)))
    out = np.stack([np.ascontiguousarray(res.results[b]['outT'].T) for b in range(B)], axis=0)
    return out.astype(np.float32)
```
